# Optimizing a Trainium2 kernel written in Bass

```python
import jax, jax.numpy as jnp
from jax import lax
import numpy as np

D_MODEL = 1024
BATCH = 2
SEQ = 16384
DEPTH = 2

N_MIXERS = 2
N_HEADS = 16
HEAD_DIM = 64
ATTN_WIDTH = N_HEADS * HEAD_DIM
NSA_KV_GROUPS = 4
NSA_Q_PER_GROUP = N_HEADS // NSA_KV_GROUPS
CMP_BLOCK = 32
CMP_STRIDE = 16
CMP_HIDDEN = 2 * HEAD_DIM
SLC_BLOCK = 64
SLC_TOPK = 16
WINDOW = 512
FORCE_SCORE = 1.0e4
NSA_IN_COLS = ATTN_WIDTH + 6 * NSA_KV_GROUPS * HEAD_DIM + 3 * N_HEADS
FOX_IN_COLS = 3 * ATTN_WIDTH + N_HEADS
FOX_F_BIAS_INIT = 3.0
Q_BLOCK = 128
ROPE_THETA = 10000.0
PEER_HEADS = 8
PEER_N_KEYS = 128
PEER_N_EXPERTS = PEER_N_KEYS * PEER_N_KEYS
PEER_QUERY_DIM = 256
PEER_TOPK = 16
PEER_TOKEN_CHUNK = 128
RMS_EPS = 1e-6
NEG_INF = -1e30

kernel_name = 'hybrid_nsa_fox_peer'


def rms_norm(x, g):
    xf = x.astype(jnp.float32)
    y = xf * lax.rsqrt(jnp.mean(xf * xf, axis=-1, keepdims=True) + RMS_EPS)
    return (y * g.astype(jnp.float32)).astype(x.dtype)


def rope(x):
    seq = x.shape[1]
    half = HEAD_DIM // 2
    inv_freq = ROPE_THETA ** (-jnp.arange(half, dtype=jnp.float32) / half)
    ang = jnp.arange(seq, dtype=jnp.float32)[:, None] * inv_freq[None, :]
    cos = jnp.cos(ang)[None, :, None, :]
    sin = jnp.sin(ang)[None, :, None, :]
    x1 = x[..., :half].astype(jnp.float32)
    x2 = x[..., half:].astype(jnp.float32)
    return jnp.concatenate([x1 * cos - x2 * sin, x2 * cos + x1 * sin], axis=-1).astype(x.dtype)


def masked_softmax(s, mask):
    s = jnp.where(mask, s.astype(jnp.float32), NEG_INF)
    m = jnp.max(s, axis=-1, keepdims=True)
    p = jnp.exp(s - m) * mask
    return p / jnp.maximum(jnp.sum(p, axis=-1, keepdims=True), 1e-30)


def compress_blocks(tok, pe, w1, w2):
    seq = tok.shape[1]
    n_cmp = (seq - CMP_BLOCK) // CMP_STRIDE + 1
    idx = np.arange(n_cmp)[:, None] * CMP_STRIDE + np.arange(CMP_BLOCK)[None, :]
    blk = tok[:, idx] + jnp.transpose(pe, (1, 0, 2))[None, None]
    hid = jax.nn.gelu(jnp.einsum('bclgd,gldh->bgch', blk, w1))
    return jnp.einsum('bgch,ghd->bgcd', hid, w2)


def cmp_to_slc_matrix(n_cmp, n_slc):
    i = np.arange(n_cmp)[:, None]
    j = np.arange(n_slc)[None, :]
    lo = np.maximum(i * CMP_STRIDE, j * SLC_BLOCK)
    hi = np.minimum(i * CMP_STRIDE + CMP_BLOCK, (j + 1) * SLC_BLOCK)
    return jnp.asarray(np.maximum(hi - lo, 0).astype(np.float32) / CMP_BLOCK)


def nsa_mixer(x, w_in, pe_k, w1_k, w2_k, pe_v, w1_v, w2_v, w_out):
    B, S, _ = x.shape
    G, R, hd = NSA_KV_GROUPS, NSA_Q_PER_GROUP, HEAD_DIM
    kvd = G * hd
    proj = x @ w_in
    q = rope(proj[..., :ATTN_WIDTH].reshape(B, S, N_HEADS, hd))
    kv = proj[..., ATTN_WIDTH:ATTN_WIDTH + 6 * kvd].reshape(B, S, 6, G, hd)
    gates = jax.nn.sigmoid(proj[..., ATTN_WIDTH + 6 * kvd:].astype(jnp.float32))
    gates = gates.reshape(B, S, 3, G, R).transpose(0, 2, 3, 4, 1).astype(x.dtype)
    k_cmp = compress_blocks(rope(kv[:, :, 0]), pe_k, w1_k, w2_k)
    v_cmp = compress_blocks(kv[:, :, 1], pe_v, w1_v, w2_v)
    k_slc, v_slc = rope(kv[:, :, 2]), kv[:, :, 3]
    k_win, v_win = rope(kv[:, :, 4]), kv[:, :, 5]
    n_cmp = k_cmp.shape[2]
    n_slc = S // SLC_BLOCK
    n_sel = min(SLC_TOPK, n_slc)
    cmp_end = jnp.arange(n_cmp) * CMP_STRIDE + CMP_BLOCK - 1
    slc_map = cmp_to_slc_matrix(n_cmp, n_slc)
    q = q.reshape(B, S, G, R, hd).transpose(0, 2, 3, 1, 4) * (hd ** -0.5)
    k_blk = k_slc.reshape(B, n_slc, SLC_BLOCK, G, hd).transpose(0, 3, 1, 2, 4)
    v_blk = v_slc.reshape(B, n_slc, SLC_BLOCK, G, hd).transpose(0, 3, 1, 2, 4)
    pad = ((0, 0), (0, 0), (WINDOW, 0), (0, 0))
    k_win_p = jnp.pad(k_win.transpose(0, 2, 1, 3), pad)
    v_win_p = jnp.pad(v_win.transpose(0, 2, 1, 3), pad)
    bi = jnp.arange(B)[:, None, None, None]
    gi = jnp.arange(G)[None, :, None, None]
    blk_ids = jnp.arange(n_slc)
    win_off = jnp.arange(WINDOW + Q_BLOCK)

    def one_block(b_idx):
        t0 = b_idx * Q_BLOCK
        qb = lax.dynamic_slice_in_dim(q, t0, Q_BLOCK, axis=3)
        gb = lax.dynamic_slice_in_dim(gates, t0, Q_BLOCK, axis=4)
        t = t0 + jnp.arange(Q_BLOCK)
        s_c = jnp.einsum('bgrqd,bgcd->bgrqc', qb, k_cmp)
        p_c = masked_softmax(s_c, cmp_end[None, :] <= t[:, None])
        o_c = jnp.einsum('bgrqc,bgcd->bgrqd', p_c.astype(v_cmp.dtype), v_cmp)
        imp = jnp.einsum('bgrqc,cj->bgqj', p_c, slc_map)
        cur = t // SLC_BLOCK
        forced = (blk_ids[None, :] == 0) | (blk_ids[None, :] == cur[:, None]) | (blk_ids[None, :] == cur[:, None] - 1)
        causal_blk = blk_ids[None, :] <= cur[:, None]
        imp = jnp.where(forced, FORCE_SCORE, jnp.where(causal_blk, imp, -1.0))
        _, sel = lax.top_k(imp, n_sel)
        ks = k_blk[bi, gi, sel]
        vs = v_blk[bi, gi, sel]
        s_s = jnp.einsum('bgrqd,bgqknd->bgrqkn', qb, ks)
        pos_s = sel[..., None] * SLC_BLOCK + jnp.arange(SLC_BLOCK)
        m_s = (pos_s <= t[None, None, :, None, None]).reshape(B, G, 1, Q_BLOCK, n_sel * SLC_BLOCK)
        p_s = masked_softmax(s_s.reshape(B, G, R, Q_BLOCK, n_sel * SLC_BLOCK), m_s)
        p_s = p_s.reshape(B, G, R, Q_BLOCK, n_sel, SLC_BLOCK)
        o_s = jnp.einsum('bgrqkn,bgqknd->bgrqd', p_s.astype(vs.dtype), vs)
        kw = lax.dynamic_slice_in_dim(k_win_p, t0, WINDOW + Q_BLOCK, axis=2)
        vw = lax.dynamic_slice_in_dim(v_win_p, t0, WINDOW + Q_BLOCK, axis=2)
        pos_w = t0 - WINDOW + win_off
        m_w = (pos_w[None, :] >= 0) & (pos_w[None, :] <= t[:, None]) & (pos_w[None, :] > t[:, None] - WINDOW)
        p_w = masked_softmax(jnp.einsum('bgrqd,bgkd->bgrqk', qb, kw), m_w)
        o_w = jnp.einsum('bgrqk,bgkd->bgrqd', p_w.astype(vw.dtype), vw)
        return gb[:, 0][..., None] * o_c + gb[:, 1][..., None] * o_s + gb[:, 2][..., None] * o_w

    o = lax.map(one_block, jnp.arange(S // Q_BLOCK))
    o = o.transpose(1, 0, 4, 2, 3, 5).reshape(B, S, ATTN_WIDTH)
    return o @ w_out


def fox_mixer(x, w_in, f_bias, w_out):
    B, S, _ = x.shape
    hd = HEAD_DIM
    proj = x @ w_in
    q, k, v = [proj[..., i * ATTN_WIDTH:(i + 1) * ATTN_WIDTH].reshape(B, S, N_HEADS, hd).transpose(0, 2, 1, 3) for i in range(3)]
    q = q * (hd ** -0.5)
    log_f = jax.nn.log_sigmoid(proj[..., 3 * ATTN_WIDTH:].astype(jnp.float32) + f_bias.astype(jnp.float32))
    c = jnp.cumsum(log_f, axis=1).transpose(0, 2, 1)
    pos = jnp.arange(S)

    def one_block(b_idx):
        t0 = b_idx * Q_BLOCK
        qb = lax.dynamic_slice_in_dim(q, t0, Q_BLOCK, axis=2)
        cb = lax.dynamic_slice_in_dim(c, t0, Q_BLOCK, axis=2)
        t = t0 + jnp.arange(Q_BLOCK)
        s = jnp.einsum('bhqd,bhkd->bhqk', qb, k).astype(jnp.float32) + cb[..., None] - c[:, :, None, :]
        p = masked_softmax(s, pos[None, :] <= t[:, None])
        return jnp.einsum('bhqk,bhkd->bhqd', p.astype(v.dtype), v)

    o = lax.map(one_block, jnp.arange(S // Q_BLOCK))
    o = o.transpose(1, 0, 3, 2, 4).reshape(B, S, ATTN_WIDTH)
    return o @ w_out


def peer_ffn(x, w_q, sub_keys, u, v):
    B, S, D = x.shape
    hq = PEER_QUERY_DIM // 2
    q = (x @ w_q).reshape(B, S, PEER_HEADS, 2, hq)
    s_half = jnp.einsum('bshpc,hpkc->bshpk', q, sub_keys).astype(jnp.float32)
    top_s, top_i = lax.top_k(s_half, PEER_TOPK)
    cand_s = top_s[..., 0, :, None] + top_s[..., 1, None, :]
    cand_i = top_i[..., 0, :, None] * PEER_N_KEYS + top_i[..., 1, None, :]
    n_cand = PEER_TOPK * PEER_TOPK
    best_s, best_pos = lax.top_k(cand_s.reshape(B, S, PEER_HEADS, n_cand), PEER_TOPK)
    experts = jnp.take_along_axis(cand_i.reshape(B, S, PEER_HEADS, n_cand), best_pos, axis=-1)
    gate = jax.nn.softmax(best_s, axis=-1)
    n_chunks = (B * S) // PEER_TOKEN_CHUNK
    n_sel = PEER_HEADS * PEER_TOPK
    xs = (x.reshape(n_chunks, PEER_TOKEN_CHUNK, D),
          experts.reshape(n_chunks, PEER_TOKEN_CHUNK, n_sel),
          gate.reshape(n_chunks, PEER_TOKEN_CHUNK, n_sel))

    def one_chunk(args):
        xc, ec, gc = args
        h = jax.nn.gelu(jnp.einsum('ted,td->te', u[ec], xc))
        return jnp.einsum('te,ted->td', gc.astype(h.dtype) * h, v[ec])

    return lax.map(one_chunk, xs).reshape(B, S, D)


def setup_inputs(seed: int = 0) -> dict:
    key = jax.random.key(seed)
    ks = jax.random.split(key, 25)
    f32 = jnp.float32
    G = NSA_KV_GROUPS

    def nrm(k, shape, scale):
        return jax.random.normal(k, shape, f32) * scale

    def gain(k):
        return 1.0 + 0.02 * jax.random.normal(k, (D_MODEL,), f32)

    cmp_w1_shape = (G, CMP_BLOCK, HEAD_DIM, CMP_HIDDEN)
    cmp_w1_scale = (CMP_BLOCK * HEAD_DIM) ** -0.5
    keys_shape = (PEER_HEADS, 2, PEER_N_KEYS, PEER_QUERY_DIM // 2)
    return {
        'x': nrm(ks[0], (BATCH, SEQ, D_MODEL), 1.0),
        'l0_attn_norm': gain(ks[1]),
        'l0_w_in': nrm(ks[2], (D_MODEL, NSA_IN_COLS), D_MODEL ** -0.5),
        'l0_cmp_pe_k': nrm(ks[3], (G, CMP_BLOCK, HEAD_DIM), 0.02),
        'l0_cmp_w1_k': nrm(ks[4], cmp_w1_shape, cmp_w1_scale),
        'l0_cmp_w2_k': nrm(ks[5], (G, CMP_HIDDEN, HEAD_DIM), CMP_HIDDEN ** -0.5),
        'l0_cmp_pe_v': nrm(ks[6], (G, CMP_BLOCK, HEAD_DIM), 0.02),
        'l0_cmp_w1_v': nrm(ks[7], cmp_w1_shape, cmp_w1_scale),
        'l0_cmp_w2_v': nrm(ks[8], (G, CMP_HIDDEN, HEAD_DIM), CMP_HIDDEN ** -0.5),
        'l0_w_out': nrm(ks[9], (ATTN_WIDTH, D_MODEL), ATTN_WIDTH ** -0.5),
        'l0_ffn_norm': gain(ks[10]),
        'l0_peer_wq': nrm(ks[11], (D_MODEL, PEER_HEADS * PEER_QUERY_DIM), D_MODEL ** -0.5),
        'l0_peer_keys': nrm(ks[12], keys_shape, (PEER_QUERY_DIM // 2) ** -0.5),
        'l0_peer_u': nrm(ks[13], (PEER_N_EXPERTS, D_MODEL), D_MODEL ** -0.5),
        'l0_peer_v': nrm(ks[14], (PEER_N_EXPERTS, D_MODEL), D_MODEL ** -0.5),
        'l1_attn_norm': gain(ks[15]),
        'l1_w_in': nrm(ks[16], (D_MODEL, FOX_IN_COLS), D_MODEL ** -0.5),
        'l1_f_bias': FOX_F_BIAS_INIT + nrm(ks[17], (N_HEADS,), 0.1),
        'l1_w_out': nrm(ks[18], (ATTN_WIDTH, D_MODEL), ATTN_WIDTH ** -0.5),
        'l1_ffn_norm': gain(ks[19]),
        'l1_peer_wq': nrm(ks[20], (D_MODEL, PEER_HEADS * PEER_QUERY_DIM), D_MODEL ** -0.5),
        'l1_peer_keys': nrm(ks[21], keys_shape, (PEER_QUERY_DIM // 2) ** -0.5),
        'l1_peer_u': nrm(ks[22], (PEER_N_EXPERTS, D_MODEL), D_MODEL ** -0.5),
        'l1_peer_v': nrm(ks[23], (PEER_N_EXPERTS, D_MODEL), D_MODEL ** -0.5),
        'final_norm': gain(ks[24]),
    }


def reference(x, l0_attn_norm, l0_w_in, l0_cmp_pe_k, l0_cmp_w1_k, l0_cmp_w2_k, l0_cmp_pe_v, l0_cmp_w1_v, l0_cmp_w2_v, l0_w_out,
              l0_ffn_norm, l0_peer_wq, l0_peer_keys, l0_peer_u, l0_peer_v,
              l1_attn_norm, l1_w_in, l1_f_bias, l1_w_out,
              l1_ffn_norm, l1_peer_wq, l1_peer_keys, l1_peer_u, l1_peer_v,
              final_norm):
    mixers = (nsa_mixer, fox_mixer)
    attn_norms = (l0_attn_norm, l1_attn_norm)
    mixer_args = ((l0_w_in, l0_cmp_pe_k, l0_cmp_w1_k, l0_cmp_w2_k, l0_cmp_pe_v, l0_cmp_w1_v, l0_cmp_w2_v, l0_w_out),
                  (l1_w_in, l1_f_bias, l1_w_out))
    ffn_norms = (l0_ffn_norm, l1_ffn_norm)
    peer_args = ((l0_peer_wq, l0_peer_keys, l0_peer_u, l0_peer_v),
                 (l1_peer_wq, l1_peer_keys, l1_peer_u, l1_peer_v))
    h = x
    for i in range(DEPTH):
        h = h + mixers[i % N_MIXERS](rms_norm(h, attn_norms[i]), *mixer_args[i])
        h = h + peer_ffn(rms_norm(h, ffn_norms[i]), *peer_args[i])
    return rms_norm(h, final_norm)
```

```python
import contextlib
import numpy as np
import concourse.bass as bass
import concourse.mybir as mybir
from concourse.bass_utils import run_bass_kernel_spmd

F32 = mybir.dt.float32
BF16 = mybir.dt.bfloat16
AF = mybir.ActivationFunctionType
ALU = mybir.AluOpType
AX = mybir.AxisListType

N_DMA_SEMS = 8


class Prog:
    COMPUTE = ("pe", "dve", "act", "pool")
    QUEUES = ("sp", "act", "pool")

    def __init__(self, nc):
        self.nc = nc
        self.ops = {e: [] for e in ("pe", "dve", "act", "pool", "sp")}
        self.cnt = {}
        self.last_w = {}
        self.readers = {}
        self.waited = {e: {} for e in self.ops}
        self.dma_n = {q: 0 for q in self.QUEUES}
        self.semkeys = []
        for e in self.COMPUTE:
            self._mk(("c", e))
        for q in self.QUEUES:
            for i in range(N_DMA_SEMS):
                self._mk(("d", q, i))
        self._mk(("cc",))
        self.sems = {}

    def _mk(self, k):
        self.cnt[k] = 0
        self.semkeys.append(k)

    def _need(self, eng, tok, waits):
        if tok is None:
            return
        k, v = tok
        if k == ("c", eng) and eng == "pe":
            return
        if self.waited[eng].get(k, 0) >= v:
            return
        self.waited[eng][k] = v
        waits.append((k, v))

    def _deps(self, eng, reads, writes):
        waits = []
        for b in reads:
            self._need(eng, self.last_w.get(b), waits)
        for b in writes:
            self._need(eng, self.last_w.get(b), waits)
            for t in self.readers.get(b, {}).items():
                if t[0] == ("c", eng):
                    continue
                self._need(eng, t, waits)
        return waits

    def _commit(self, tok, reads, writes):
        for b in reads:
            self.readers.setdefault(b, {})[tok[0]] = tok[1]
        for b in writes:
            self.last_w[b] = tok
            self.readers[b] = {}

    def op(self, eng, fn, reads=(), writes=()):
        waits = self._deps(eng, reads, writes)
        k = ("c", eng)
        self.cnt[k] += 1
        tok = (k, self.cnt[k])
        self._commit(tok, reads, writes)
        self.ops[eng].append((waits, fn, (k, 1)))
        return tok

    def dma(self, q, fn, reads=(), writes=()):
        waits = self._deps(q, reads, writes)
        n = self.dma_n[q]
        self.dma_n[q] += 1
        k = ("d", q, n % N_DMA_SEMS)
        self._need(q, (k, self.cnt[k]) if self.cnt[k] else None, waits)
        self.cnt[k] += 16
        tok = (k, self.cnt[k])
        self._commit(tok, reads, writes)
        self.ops[q].append((waits, fn, (k, 16)))
        return tok

    def cc(self, fn, reads=(), writes=()):
        waits = self._deps("pool", reads, writes)
        k = ("cc",)
        self._need("pool", (k, self.cnt[k]) if self.cnt[k] else None, waits)
        self.cnt[k] += 1
        tok = (k, self.cnt[k])
        self._commit(tok, reads, writes)
        self.ops["pool"].append((waits, fn, (k, 1)))
        return tok

    def I(self, eng, method, r=(), w=(), **kw):
        return self.op(eng, lambda e: getattr(e, method)(**kw), reads=r, writes=w)

    def MM(self, out, lhsT, rhs, start=True, stop=True, r=(), w=()):
        return self.op("pe", lambda e: e.matmul(out, lhsT=lhsT, rhs=rhs, start=start, stop=stop), reads=r, writes=w)

    def D(self, q, out, in_, r=(), w=(), **kw):
        return self.dma(q, lambda e: e.dma_start(out=out, in_=in_, **kw), reads=r, writes=w)

    def barrier(self):
        for eng in self.ops:
            waits = []
            for k in self.semkeys:
                if self.cnt[k]:
                    self._need(eng, (k, self.cnt[k]), waits)
            self.ops[eng].append((waits, None, None))
        self.last_w = {}
        self.readers = {}

    def wait_all(self, eng, bufs):
        waits = []
        for b in bufs:
            self._need(eng, self.last_w.get(b), waits)
        self.ops[eng].append((waits, None, None))

    def alloc_sems(self, st):
        for k in self.semkeys:
            self.sems[k] = st.enter_context(self.nc.semaphore("s_" + "_".join(map(str, k))))

    def emit(self):
        nc = self.nc
        import contextlib
        with contextlib.ExitStack() as st:
            if not self.sems:
                self.alloc_sems(st)
            block = st.enter_context(nc.Block())
            engobj = {"pe": "tensor", "dve": "vector", "act": "scalar", "pool": "gpsimd", "sp": "sync"}

            def mk(ename):
                ops = self.ops[ename]

                def body(eng):
                    for waits, fn, inc in ops:
                        for (k, v) in waits:
                            eng.wait_ge(self.sems[k], v)
                        if fn is not None:
                            ins = fn(eng)
                            ins.then_inc(self.sems[inc[0]], inc[1])
                return body

            for ename, attr in engobj.items():
                if self.ops[ename]:
                    getattr(block, attr)(mk(ename))
        self.ops = {e: [] for e in self.ops}


EPS = 1e-6


def consts_a():
    c = {}
    q = np.arange(128)[:, None]; rel = np.arange(512)[None, :] - 256
    cur = (q >= 64).astype(np.int64)
    M = (rel <= cur - 2).astype(np.float32)
    A = np.where(rel == cur, 10000.0, np.where(rel == cur - 1, 10001.0, np.where(rel > cur, -1.0, 0.0))).astype(np.float32)
    c["pats"] = np.stack([M, A], 1)
    ci = np.arange(128)[:, None]; qi = np.arange(128)[None, :]
    D = (16 * ci - qi).astype(np.float32)
    Dz = D.copy(); Dz[0, :] = 1e9
    c["D16"] = np.stack([D, Dz], 1)
    c["tril"] = np.stack([(ci <= qi), (ci > qi)], 1).astype(np.float32)
    c["Sel"] = np.broadcast_to(np.eye(12, dtype=np.float32)[:, :, None], (12, 12, 64)).copy()
    cc = np.arange(1024)[:, None] - 1; jj = np.arange(256)[None, :]
    lo = np.maximum(cc * 16, jj * 64); hi = np.minimum(cc * 16 + 32, (jj + 1) * 64)
    m = np.maximum(hi - lo, 0).astype(np.float32) / 32.0
    m[0, :] = 0.0
    c["slcm"] = np.ascontiguousarray(m.reshape(8, 128, 256).transpose(1, 0, 2))
    return c


def epat_table(S):
    n = np.arange(S)[None, :]; r = np.arange(64)[:, None]
    return (30000.0 * (((n // 64) % 64) == r)).astype(np.float32)


def rope_tables(S):
    half = 32
    inv = (10000.0 ** (-np.arange(half, dtype=np.float32) / half)).astype(np.float32)
    ang = (np.arange(S, dtype=np.float32)[None, :] * inv[:, None]).astype(np.float32)
    cos = np.cos(ang).astype(np.float32); sin = np.sin(ang).astype(np.float32)
    cosf = np.concatenate([cos, cos], 0); sinf = np.concatenate([-sin, sin], 0)
    rk = np.stack([cosf, sinf], 1)
    return np.ascontiguousarray(rk * 0.125), np.ascontiguousarray(rk)


def host_inputs_a(xb, gattn, w_in, pe_k, w1_k, w2_k, pe_v, w1_v, w2_v, g, consts, ropeq, ropek):
    def sw(w):
        w = w.reshape(w.shape[0], -1, 64)
        return np.concatenate([w[..., 32:], w[..., :32]], -1).reshape(w.shape[0], -1)
    kv = lambda i: w_in[:, 1024 + i * 256 + g * 64:1024 + i * 256 + (g + 1) * 64]
    wq = w_in[:, g * 256:(g + 1) * 256]
    d = dict(consts)
    d["xT"] = np.ascontiguousarray(xb.T)
    d["gattn"] = gattn
    d["wqa"] = np.ascontiguousarray(np.concatenate([wq, sw(wq)], 1))
    d["wka"] = np.ascontiguousarray(np.concatenate([kv(0), kv(1), sw(kv(0)), kv(2), sw(kv(2)), kv(4), sw(kv(4))], 1))
    d["wtok"] = np.ascontiguousarray(np.concatenate([kv(3), kv(5)], 1))
    gc = [2560 + br * 16 + g * 4 + r for br in range(3) for r in range(4)]
    d["wg"] = np.ascontiguousarray(w_in[:, gc])
    d["w1s"] = np.ascontiguousarray(np.concatenate([w1_k[g].transpose(1, 0, 2), w1_v[g].transpose(1, 0, 2)], 0))
    d["peT"] = np.ascontiguousarray(np.concatenate([pe_k[g].T, pe_v[g].T], 0))
    d["w2s"] = np.ascontiguousarray(np.stack([w2_k[g], w2_v[g]], 1))
    d["ropeq"] = ropeq; d["ropek"] = ropek
    d["Epat"] = epat_table(xb.shape[0])
    return d


def declare_a(nc, S, pfx="", fused=False):
    d = {}

    def t(name, shape, kind="ExternalInput", dt=F32):
        d[name] = nc.dram_tensor(pfx + name, shape, dt, kind=kind).ap()

    t("xT", [1024, S]); t("gattn", [1024]); t("wqa", [1024, 512]); t("wka", [1024, 448]); t("wtok", [1024, 128]); t("wg", [1024, 12])
    t("w1s", [128, 32, 128]); t("peT", [128, 32]); t("w2s", [128, 2, 64]); t("ropeq", [64, 2, S]); t("ropek", [64, 2, S])
    t("pats", [128, 2, 512]); t("D16", [128, 2, 128]); t("tril", [128, 2, 128]); t("Epat", [64, S]); t("Sel", [12, 12, 64])
    t("slcm", [128, 8, 256])
    t("oT", [256, S], kind="Internal" if fused else "ExternalOutput")
    return d


def emit_a(nc, P, S, d, oT_dst=None):
    if oT_dst is None:
        oT_dst = lambda t0, n: d["oT"][:, t0:t0 + n]
    NBLK = S // 512
    NCH = S // 128
    with contextlib.ExitStack() as st:
        sb = lambda n, s, dt=F32: st.enter_context(nc.sbuf_tensor("a_" + n, s, dt))
        ps = lambda n, s, dt=F32: st.enter_context(nc.psum_tensor("a_" + n, s, dt))
        KsT = sb("KsT", [128, S], BF16); Vs = sb("Vs", [128, NCH, 65], BF16)
        KwT = sb("KwT", [64, 8, 128], BF16); Vw = sb("Vw", [128, 8, 65], BF16)
        KcT = sb("KcT", [64, 1024], BF16); Vc = sb("Vc", [128, 8, 64], BF16)
        slcm = sb("slcm", [128, 8, 256], BF16)
        Qaug = [sb(f"Qaug{i}", [128, 4, 512], BF16) for i in range(2)]
        pats = sb("pats", [128, 2, 512]); D16 = sb("D16", [128, 2, 128]); tril = sb("tril", [128, 2, 128], BF16)
        Sel = sb("Sel", [12, 12, 64])
        wqa = sb("wqa", [128, 8, 512], BF16); wka = sb("wka", [128, 8, 448], BF16); wtok = sb("wtok", [128, 8, 128], BF16)
        wg = sb("wg", [128, 8, 12], BF16)
        w1s = sb("w1s", [128, 32, 128], BF16); peT = sb("peT", [128, 32], BF16); w2s = sb("w2s", [128, 2, 64], BF16)
        hb = sb("hb", [128, 2]); g = sb("g", [128, 8])
        ones_b = sb("ones_b", [128, 128], BF16); ones_f = sb("ones_f", [128, 128]); identf = sb("identf", [128, 128])
        xT = sb("xT", [128, 8, 512]); sq = sb("sq", [128, 8, 512], BF16); rstd = sb("rstd", [128, 512]); xnb = sb("xnb", [128, 8, 512], BF16)
        rq = sb("rq", [64, 2, 512]); rk = sb("rk", [64, 2, 512])
        t1 = [sb(f"t1_{i}", [64, 512]) for i in range(2)]; t2 = [sb(f"t2_{i}", [64, 512]) for i in range(2)]
        Qd = sb("Qd", [64, 4, 4, 128], BF16)
        CV = sb("CV", [128, 528], BF16); hidk = sb("hidk", [128, 32], BF16); hvp = sb("hvp", [128, 128], BF16)
        gT = sb("gT", [12, 512])
        PTc = sb("PTc", [128, 8, 512], BF16); PT = [sb(f"PT{i}", [128, 512], BF16) for i in range(3)]
        zc = sb("zc", [1, 512]); impS = sb("impS", [128, 256]); scr = sb("scr", [128, 256]); m8 = sb("m8", [128, 16])
        NT = sb("NT", [128, 256])
        zrow = sb("zrow", [65, 512]); gbs = sb("gbs", [64, 512]); acc = [sb(f"acc{i}", [64, 512]) for i in range(2)]
        tmp = sb("tmp", [64, 512])
        pp = [ps(f"pp{i}", [128, 512]) for i in range(2)]
        ST = [ps(f"ST{i}", [128, 512]) for i in range(2)]
        OA = [ps(f"OA{i}", [65, 512]) for i in range(2)]
        IMP = ps("IMP", [128, 512]); AUX = ps("AUX", [128, 512])

        for nm, tl in (("wqa", wqa), ("wka", wka), ("wtok", wtok), ("wg", wg)):
            P.D("pool", out=tl[:], in_=d[nm].rearrange("(m p) c -> p m c", p=128), w=[nm])
        for nm, tl in (("w1s", w1s), ("peT", peT), ("w2s", w2s), ("slcm", slcm), ("tril", tril)):
            P.D("pool", out=tl[:], in_=d[nm], w=[nm])
        for nm, tl in (("pats", pats), ("D16", D16), ("Sel", Sel)):
            P.D("sp", out=tl[:], in_=d[nm], w=[nm])
        P.D("pool", out=KsT[64:128, :], in_=d["Epat"], w=["KsE"])
        P.D("sp", out=g[:], in_=d["gattn"].rearrange("(m p) -> p m", p=128), w=["g"], allow_slow_non_contiguous=True)
        P.I("pool", "memset", w=["ones_b"], ap=ones_b[:], constant=1.0)
        P.I("pool", "memset", w=["ones_f"], ap=ones_f[:], constant=1.0)
        P.I("pool", "memset", w=["identf"], ap=identf[:], constant=1.0)
        P.I("pool", "affine_select", r=["identf"], w=["identf"], out=identf[:], in_=identf[:], pattern=[[-1, 128]],
            compare_op=ALU.is_equal, fill=0.0, base=0, channel_multiplier=1)
        P.I("pool", "memset", w=["Vs"], ap=Vs[:, :, 64:65], constant=1.0)
        P.I("pool", "memset", w=["Vw"], ap=Vw[:, :, 64:65], constant=1.0)
        P.I("pool", "memset", w=["KcT"], ap=KcT[:], constant=0.0)
        P.I("pool", "memset", w=["Vc"], ap=Vc[:], constant=0.0)
        P.I("pool", "memset", w=["CVk", "CVv"], ap=CV[:], constant=0.0)
        P.I("pool", "memset", w=["hvp"], ap=hvp[:], constant=0.0)
        for kvi in range(2):
            rows = slice(kvi * 64, kvi * 64 + 64)
            for l in range(32):
                P.MM(pp[0][:, kvi:kvi + 1], w1s[rows, l, :], peT[rows, l:l + 1], start=(l == 0), stop=(l == 31), r=["w1s", "peT"], w=["pp0"])
        P.I("dve", "tensor_copy", r=["pp0"], w=["hb"], out=hb[:], in_=pp[0][:, 0:2])

        cnt = {"pp": 0, "t": 0, "pt": 0, "oa": 0, "acc": 0}

        def proj(c0, M):
            i = cnt["pp"] % 2; cnt["pp"] += 1
            return pp[i], f"pp{i}"

        for blk in range(NBLK):
            tsl = slice(blk * 512, (blk + 1) * 512)
            P.D("sp", out=xT[:], in_=d["xT"].rearrange("(m p) t -> p m t", p=128)[:, :, tsl], w=["xT"])
            P.D("sp", out=rq[:], in_=d["ropeq"][:, :, tsl], w=["rq"])
            P.D("sp", out=rk[:], in_=d["ropek"][:, :, tsl], w=["rk"])
            P.I("act", "activation", r=["xT"], w=["sq"], out=sq[:], in_=xT[:], func=AF.Square)
            for m in range(8):
                P.MM(AUX[:], ones_b[:], sq[:, m, :], start=(m == 0), stop=(m == 7), r=["ones_b", "sq"], w=["AUX"])
            P.I("act", "activation", r=["AUX"], w=["rstd"], out=rstd[:], in_=AUX[:], func=AF.Sqrt, scale=1.0 / 1024, bias=EPS)
            P.I("dve", "reciprocal", r=["rstd"], w=["rstd"], out=rstd[:], in_=rstd[:])
            for m in range(8):
                P.I("dve", "scalar_tensor_tensor", r=["xT", "g", "rstd"], w=["xnb"], out=xnb[:, m, :], in0=xT[:, m, :],
                    scalar=g[:, m:m + 1], in1=rstd[:], op0=ALU.mult, op1=ALU.mult)

            def fmproj(wt, wk_, c0, M):
                p_, pk = proj(c0, M)
                for m in range(8):
                    P.MM(p_[0:M, :], wt[:, m, c0:c0 + M], xnb[:, m, :], start=(m == 0), stop=(m == 7), r=[wk_, "xnb"], w=[pk])
                return p_, pk

            def rope(wt, wk_, ca, cb, tab, tabk, out_ap, outk):
                pa, pak = fmproj(wt, wk_, ca, 64)
                i = cnt["t"] % 2; cnt["t"] += 1
                P.I("dve", "tensor_tensor", r=[pak, tabk], w=[f"t1_{i}"], out=t1[i][:], in0=pa[0:64, :], in1=tab[:, 0, :], op=ALU.mult)
                pb, pbk = fmproj(wt, wk_, cb, 64)
                P.I("dve", "tensor_tensor", r=[pbk, tabk], w=[f"t2_{i}"], out=t2[i][:], in0=pb[0:64, :], in1=tab[:, 1, :], op=ALU.mult)
                a_, b_ = t1[i][:], t2[i][:]
                if len(out_ap.shape) == 3:
                    a_ = a_.rearrange("p (a q) -> p a q", a=4); b_ = b_.rearrange("p (a q) -> p a q", a=4)
                P.I("pool", "tensor_tensor", r=[f"t1_{i}", f"t2_{i}"], w=[outk], out=out_ap, in0=a_, in1=b_, op=ALU.add)

            for r in range(4):
                rope(wqa, "wqa", r * 64, 256 + r * 64, rq, "rq", Qd[:, :, r, :], "Qd")
            rope(wka, "wka", 192, 256, rk, "rk", KsT[0:64, tsl], "KsT")
            rope(wka, "wka", 320, 384, rk, "rk", KwT[:, (blk % 2) * 4:(blk % 2) * 4 + 4, :], "KwT")
            pa, pak = fmproj(wka, "wka", 0, 128)
            i = cnt["t"] % 2; cnt["t"] += 1
            P.I("dve", "tensor_tensor", r=[pak, "rk"], w=[f"t1_{i}"], out=t1[i][:], in0=pa[0:64, :], in1=rk[:, 0, :], op=ALU.mult)
            P.I("act", "copy", r=[pak], w=["CVv"], out=CV[64:128, 16:528], in_=pa[64:128, :])
            pb, pbk = fmproj(wka, "wka", 128, 64)
            P.I("dve", "tensor_tensor", r=[pbk, "rk"], w=[f"t2_{i}"], out=t2[i][:], in0=pb[0:64, :], in1=rk[:, 1, :], op=ALU.mult)
            P.I("pool", "tensor_tensor", r=[f"t1_{i}", f"t2_{i}"], w=["CVk"], out=CV[0:64, 16:528], in0=t1[i][:], in1=t2[i][:], op=ALU.add)
            pgt, pgk = fmproj(wg, "wg", 0, 12)
            P.I("act", "activation", r=[pgk], w=["gT"], out=gT[:], in_=pgt[0:12, :], func=AF.Sigmoid)
            for t4 in range(4):
                ch = blk * 4 + t4
                i = cnt["pp"] % 2; cnt["pp"] += 1
                for m in range(8):
                    P.MM(pp[i][:, 0:128], xnb[:, m, t4 * 128:(t4 + 1) * 128], wtok[:, m, :], start=(m == 0), stop=(m == 7),
                         r=["wtok", "xnb"], w=[f"pp{i}"])
                P.I("act", "copy", r=[f"pp{i}"], w=["Vs"], out=Vs[:, ch, 0:64], in_=pp[i][:, 0:64])
                P.I("act", "copy", r=[f"pp{i}"], w=["Vw"], out=Vw[:, ch % 8, 0:64], in_=pp[i][:, 64:128])
            CVv = CV[:].rearrange("p (c s) -> p c s", s=16)
            i = cnt["pp"] % 2; cnt["pp"] += 1
            for l in range(32):
                P.MM(pp[i][:, 0:32], w1s[0:64, l, :], CVv[0:64, l // 16:l // 16 + 32, l % 16], start=(l == 0), stop=(l == 31),
                     r=["w1s", "CVk"], w=[f"pp{i}"])
            P.I("act", "activation", r=[f"pp{i}", "hb"], w=["hidk"], out=hidk[:], in_=pp[i][:, 0:32], func=AF.Gelu_apprx_tanh, bias=hb[:, 0:1])
            i2 = cnt["pp"] % 2; cnt["pp"] += 1
            P.MM(pp[i2][0:64, 0:32], w2s[:, 0, :], hidk[:], r=["w2s", "hidk"], w=[f"pp{i2}"])
            P.I("dve", "tensor_copy", r=[f"pp{i2}"], w=["KcT"], out=KcT[:, 32 * blk:32 * blk + 32], in_=pp[i2][0:64, 0:32])
            i = cnt["pp"] % 2; cnt["pp"] += 1
            for l in range(32):
                P.MM(pp[i][:, 0:32], w1s[64:128, l, :], CVv[64:128, l // 16:l // 16 + 32, l % 16], start=(l == 0), stop=(l == 31),
                     r=["w1s", "CVv"], w=[f"pp{i}"])
            off = (32 * blk) % 128
            P.I("act", "activation", r=[f"pp{i}", "hb"], w=["hvp"], out=hvp[:, off:off + 32], in_=pp[i][:, 0:32], func=AF.Gelu_apprx_tanh, bias=hb[:, 1:2])
            i2 = cnt["pp"] % 2; cnt["pp"] += 1
            P.MM(pp[i2][:, 0:64], hvp[:], w2s[:, 1, :], r=["w2s", "hvp"], w=[f"pp{i2}"])
            P.I("dve", "tensor_copy", r=[f"pp{i2}"], w=["Vc"], out=Vc[off:off + 32, (32 * blk) // 128, :], in_=pp[i2][off:off + 32, 0:64])
            P.I("act", "copy", r=["CVk", "CVv"], w=["CVk", "CVv"], out=CV[:, 0:16], in_=CV[:, 512:528])

            for qi in range(4):
                QB = blk * 4 + qi
                t0 = 128 * QB
                Qb = Qd[:, qi, :, :].rearrange("p r q -> p (r q)")

                def combine(br, ot, otk, normalize):
                    ai = cnt["acc"] % 2
                    for r in range(4):
                        P.MM(AUX[0:64, r * 128:(r + 1) * 128], Sel[:, br * 4 + r, :], gT[:, qi * 128:(qi + 1) * 128], r=["Sel", "gT"], w=["AUX"])
                    P.I("act", "copy", r=["AUX"], w=["gbs"], out=gbs[:], in_=AUX[0:64, :])
                    if normalize:
                        P.I("dve", "tensor_scalar", r=[otk], w=["zrow"], out=zrow[64:65, :], in0=ot[64:65, :], scalar1=1e-30, scalar2=None, op0=ALU.max)
                        P.I("dve", "reciprocal", r=["zrow"], w=["zrow"], out=zrow[64:65, :], in_=zrow[64:65, :])
                        P.MM(AUX[0:64, :], ones_f[64:65, 0:64], zrow[64:65, :], r=["ones_f", "zrow"], w=["AUX"])
                        P.I("dve", "tensor_tensor", r=["gbs", "AUX"], w=["gbs"], out=gbs[:], in0=gbs[:], in1=AUX[0:64, :], op=ALU.mult)
                    if br == 0:
                        P.I("dve", "tensor_tensor", r=[otk, "gbs"], w=[f"acc{ai}"], out=acc[ai][:], in0=ot[0:64, :], in1=gbs[:], op=ALU.mult)
                    else:
                        P.I("dve", "tensor_tensor", r=[otk, "gbs"], w=["tmp"], out=tmp[:], in0=ot[0:64, :], in1=gbs[:], op=ALU.mult)
                        P.I("pool", "tensor_tensor", r=["tmp", f"acc{ai}"], w=[f"acc{ai}"], out=acc[ai][:], in0=acc[ai][:], in1=tmp[:], op=ALU.add)

                jmax = (t0 + 112) // 2048
                nj = jmax + 1
                for j in range(nj):
                    si = cnt["pt"] % 2; cnt["pt"] += 1
                    P.MM(ST[si][:], KcT[:, j * 128:(j + 1) * 128], Qb, r=["KcT", "Qd"], w=[f"ST{si}"])
                    P.I("act", "activation", r=[f"ST{si}"], w=[f"PTc{j}"], out=PTc[:, j, :], in_=ST[si][:], func=AF.Exp)
                    delta = t0 - 2048 * j - 15
                    if j == 0 or delta < 2032:
                        P.I("dve", "scalar_tensor_tensor", r=["D16", f"PTc{j}"], w=[f"PTc{j}"], out=PTc[:, j, :].rearrange("p (r q) -> p r q", r=4),
                            in0=D16[:, 1 if j == 0 else 0, :].unsqueeze(1).to_broadcast([128, 4, 128]), scalar=float(delta),
                            in1=PTc[:, j, :].rearrange("p (r q) -> p r q", r=4), op0=ALU.is_le, op1=ALU.mult)
                    P.MM(AUX[0:1, :], ones_b[:, 0:1], PTc[:, j, :], start=(j == 0), stop=(j == jmax), r=["ones_b", f"PTc{j}"], w=["AUX"])
                P.I("dve", "tensor_scalar", r=["AUX"], w=["zc"], out=zc[:], in0=AUX[0:1, :], scalar1=1e-30, scalar2=None, op0=ALU.max)
                P.I("dve", "reciprocal", r=["zc"], w=["zc"], out=zc[:], in_=zc[:])
                P.MM(AUX[:], ones_f[0:1, :], zc[0:1, :], r=["ones_f", "zc"], w=["AUX"])
                pk_all = [f"PTc{j}" for j in range(nj)]
                P.I("dve", "tensor_tensor", r=pk_all + ["AUX"], w=pk_all, out=PTc[:, 0:nj, :], in0=PTc[:, 0:nj, :],
                    in1=AUX[:].unsqueeze(1).to_broadcast([128, nj, 512]), op=ALU.mult)
                oi = cnt["oa"] % 2; cnt["oa"] += 1
                for j in range(nj):
                    P.MM(OA[oi][0:64, :], Vc[:, j, :], PTc[:, j, :], start=(j == 0), stop=(j == jmax), r=["Vc", f"PTc{j}"], w=[f"OA{oi}"])
                for j in range(nj):
                    for r in range(4):
                        P.MM(IMP[:, 0:256], PTc[:, j, r * 128:(r + 1) * 128], slcm[:, j, :], start=(j == 0 and r == 0),
                             stop=(j == jmax and r == 3), r=["slcm", f"PTc{j}"], w=["IMP"])
                combine(0, OA[oi], f"OA{oi}", False)
                jb = 2 * QB
                P.I("dve", "tensor_tensor", r=["IMP", "pats"], w=["impS"], out=impS[:], in0=IMP[:, 0:256], in1=pats[:, 0, 256 - jb:512 - jb], op=ALU.mult)
                P.I("dve", "tensor_tensor", r=["impS", "pats"], w=["impS"], out=impS[:], in0=impS[:], in1=pats[:, 1, 256 - jb:512 - jb], op=ALU.add)
                P.I("dve", "memset", r=["impS"], w=["impS"], ap=impS[:, 0:1], constant=10002.0)
                P.I("dve", "max", r=["impS"], w=["m8"], out=m8[:, 0:8], in_=impS[:])
                P.I("dve", "match_replace", r=["impS", "m8"], w=["scr"], out=scr[:], in_to_replace=m8[:, 0:8], in_values=impS[:], imm_value=-2.0)
                P.I("dve", "max", r=["scr"], w=["m8"], out=m8[:, 8:16], in_=scr[:])
                P.I("dve", "tensor_scalar", r=["impS", "m8"], w=["NT"], out=NT[:], in0=impS[:], scalar1=m8[:, 15:16], scalar2=1.0,
                    op0=ALU.is_ge, op1=ALU.subtract)
                for jt in range(2):
                    P.op("pe", (lambda jt: (lambda e: e.transpose(out=IMP[:, jt * 128:(jt + 1) * 128], in_=NT[:, jt * 128:(jt + 1) * 128], identity=identf[:])))(jt),
                         reads=["NT", "identf"], writes=["IMP"])
                qa = QB % 2
                ng = QB // 32 + 1
                P.I("pool", "tensor_copy", r=["Qd"], w=[f"Qaug{qa}q"], out=Qaug[qa][0:64, 0:ng, :],
                    in_=Qb.unsqueeze(1).to_broadcast([64, ng, 512]))
                for g_ in range(ng):
                    half = g_ % 2
                    P.I("act", "copy", r=["IMP"], w=[f"Qaug{qa}m"], out=Qaug[qa][64:128, g_, :].rearrange("p (r q) -> p r q", r=4),
                        in_=IMP[64 * half:64 * half + 64, (g_ // 2) * 128:(g_ // 2 + 1) * 128].unsqueeze(1).to_broadcast([64, 4, 128]))

                for br in (1, 2):
                    kcs = list(range(0, QB + 1)) if br == 1 else list(range(max(0, QB - 4), QB + 1))
                    oi = cnt["oa"] % 2; cnt["oa"] += 1
                    ot = OA[oi]; otk = f"OA{oi}"
                    base = cnt["pt"]

                    def qk(n, br=br, kcs=kcs, base=base, qa=qa):
                        kc = kcs[n]; si = (base + n) % 2
                        if br == 1:
                            P.MM(ST[si][:], KsT[:, kc * 128:(kc + 1) * 128], Qaug[qa][:, kc // 32, :],
                                 r=["KsT", "KsE", f"Qaug{qa}q", f"Qaug{qa}m"], w=[f"ST{si}"])
                        else:
                            P.MM(ST[si][:], KwT[:, kc % 8, :], Qb, r=["KwT", "Qd"], w=[f"ST{si}"])

                    qk(0)
                    for n, kc in enumerate(kcs):
                        if n + 1 < len(kcs):
                            qk(n + 1)
                        si = (base + n) % 2
                        pi = cnt["pt"] % 3; cnt["pt"] += 1
                        pt = PT[pi]; ptk = f"PT{pi}"
                        P.I("act", "activation", r=[f"ST{si}"], w=[ptk], out=pt[:], in_=ST[si][:], func=AF.Exp)
                        if kc == QB:
                            P.I("dve", "tensor_tensor", r=[ptk, "tril"], w=[ptk], out=pt[:].rearrange("p (r q) -> p r q", r=4),
                                in0=pt[:].rearrange("p (r q) -> p r q", r=4), in1=tril[:, 0, :].unsqueeze(1).to_broadcast([128, 4, 128]), op=ALU.mult)
                        if br == 2 and kc == QB - 4:
                            P.I("dve", "tensor_tensor", r=[ptk, "tril"], w=[ptk], out=pt[:].rearrange("p (r q) -> p r q", r=4),
                                in0=pt[:].rearrange("p (r q) -> p r q", r=4), in1=tril[:, 1, :].unsqueeze(1).to_broadcast([128, 4, 128]), op=ALU.mult)
                        vv = Vs[:, kc, :] if br == 1 else Vw[:, kc % 8, :]
                        P.MM(ot[:], vv, pt[:], start=(n == 0), stop=(n == len(kcs) - 1), r=["Vs" if br == 1 else "Vw", ptk], w=[otk])
                    combine(br, ot, otk, True)
                ai = cnt["acc"] % 2; cnt["acc"] += 1
                P.D("sp", out=oT_dst(t0, 128).rearrange("(r x) q -> x r q", x=64), in_=acc[ai][:].rearrange("p (r q) -> p r q", r=4),
                    r=[f"acc{ai}"], w=[f"oT{QB}"])
        P.wait_all("sp", [f"oT{q}" for q in range(NBLK * 4)])
        P.barrier()
        P.emit()


EPS = 1e-6
NEG = -1.0e30


def declare_b(nc, T, pfx=""):
    d = {}

    def inp(name, shape, dt=F32):
        d[name] = nc.dram_tensor(pfx + name, shape, dt, kind="ExternalInput").ap()

    def outp(name, shape, dt=F32):
        d[name] = nc.dram_tensor(pfx + name, shape, dt, kind="ExternalOutput").ap()

    def scr(name, shape, dt=F32):
        d[name] = nc.dram_tensor(pfx + name, shape, dt, kind="Internal").ap()

    inp("hT", [1024, T]); inp("oT", [1024, T]); inp("wout", [1024, 1024]); inp("gffn", [1024])
    inp("wq", [1024, 2048]); inp("keysT", [16, 128, 128]); inp("uT", [1024, 16384]); inp("v", [16384, 1024])
    inp("gnext", [1024])
    outp("hT_out", [1024, T]); outp("nT_out", [1024, T])
    scr("h2T", [1024, T]); scr("hnbf", [1024, T], BF16); scr("qTd", [128, T // 128, 16, 128])
    scr("GT", [T // 128, 128, 16384], BF16); scr("uTbf", [1024, 16384], BF16); scr("vbf", [16384, 1024], BF16)
    return d


def emit_b(nc, P, T, d, cast_weights=True, pfx="", oT_blk=None, nT_dst=None):
    NB = T // 512
    NT = T // 128
    NB2 = T // 256
    fm = lambda ap: ap.rearrange("(m p) t -> p m t", p=128)

    if cast_weights:
        for i in range(8):
            P.D("pool", out=d["uTbf"][i * 128:(i + 1) * 128, :], in_=d["uT"][i * 128:(i + 1) * 128, :], w=[f"uTbf{i}"])
        for i in range(8):
            P.D("pool", out=d["vbf"][i * 2048:(i + 1) * 2048, :], in_=d["v"][i * 2048:(i + 1) * 2048, :], w=[f"vbf{i}"])

    with contextlib.ExitStack() as st:
        sb = lambda n, s, dt=F32: st.enter_context(nc.sbuf_tensor(pfx + "p0_" + n, s, dt))
        ps = lambda n, s, dt=F32: st.enter_context(nc.psum_tensor(pfx + "p0_" + n, s, dt))
        wout = sb("wout", [128, 8, 1024], BF16)
        g = sb("g", [128, 8]); ones = sb("ones", [128, 128])
        hTt = [sb(f"hTt{i}", [128, 8, 512]) for i in range(2)]
        oTt = [sb(f"oTt{i}", [128, 8, 512], BF16) for i in range(2)]
        h2 = sb("h2", [128, 8, 512]); hnb = sb("hnb", [128, 8, 512], BF16)
        rstd = sb("rstd", [128, 512])
        wqt = [sb(f"wqt{i}", [128, 8, 512]) for i in range(2)]
        qs = [sb(f"qs{i}", [128, 4, 512]) for i in range(2)]
        pp = [ps(f"pp{i}", [128, 512]) for i in range(2)]
        ss = ps("ss", [128, 512])

        P.D("pool", out=wout[:], in_=d["wout"].rearrange("(k p) c -> p k c", p=128), w=["wout"])
        P.D("sp", out=g[:], in_=d["gffn"].rearrange("(m p) -> p m", p=128), w=["g"], allow_slow_non_contiguous=True)
        P.I("pool", "memset", w=["ones"], ap=ones[:], constant=1.0)
        nmm = 0
        nwq = 0
        if oT_blk is not None:
            P.dma("sp", lambda e: e.dma_start(out=d["oTq"].rearrange("r (c t) -> c r t", t=min(1024, T)), in_=oT_blk()), writes=["oTq"])
        for b in range(NB):
            tsl = slice(b * 512, (b + 1) * 512)
            ht, ot = hTt[b % 2], oTt[b % 2]
            hk, ok = f"hTt{b % 2}", f"oTt{b % 2}"
            P.D("sp", out=ht[:], in_=fm(d["hT"])[:, :, tsl], w=[hk])
            if oT_blk is None:
                P.D("pool", out=ot[:], in_=fm(d["oT"])[:, :, tsl], w=[ok])
            else:
                P.D("pool", out=ot[:], in_=fm(d["oTq"])[:, :, tsl], r=["oTq"], w=[ok])
            for m in range(8):
                p_ = pp[nmm % 2]; pk = f"pp{nmm % 2}"; nmm += 1
                for k in range(8):
                    P.MM(p_[:], wout[:, k, m * 128:(m + 1) * 128], ot[:, k, :], start=(k == 0), stop=(k == 7),
                         r=["wout", ok], w=[pk])
                P.I("dve", "tensor_tensor", r=[pk, hk], w=["h2"], out=h2[:, m, :], in0=p_[:], in1=ht[:, m, :], op=ALU.add)
            P.D("sp", out=fm(d["h2T"])[:, :, tsl], in_=h2[:], r=["h2"], w=[f"h2T{b}"])
            P.I("act", "activation", r=["h2"], w=[hk], out=ht[:], in_=h2[:], func=AF.Square)
            for m in range(8):
                P.MM(ss[:], ones[:], ht[:, m, :], start=(m == 0), stop=(m == 7), r=["ones", hk], w=["ss"])
            P.I("act", "activation", r=["ss"], w=["rstd"], out=rstd[:], in_=ss[:], func=AF.Sqrt, scale=1.0 / 1024, bias=EPS)
            P.I("dve", "reciprocal", r=["rstd"], w=["rstd"], out=rstd[:], in_=rstd[:])
            for m in range(8):
                P.I("dve", "scalar_tensor_tensor", r=["h2", "g", "rstd"], w=["h2"], out=h2[:, m, :], in0=h2[:, m, :],
                    scalar=g[:, m:m + 1], in1=rstd[:], op0=ALU.mult, op1=ALU.mult)
            P.I("pool", "tensor_copy", r=["h2"], w=["hnb"], out=hnb[:], in_=h2[:])
            P.D("sp", out=fm(d["hnbf"])[:, :, tsl], in_=hnb[:], r=["hnb"], w=[f"hnbf{b}"])
            for jg in range(4):
                wt = wqt[nwq % 2]; wk = f"wqt{nwq % 2}"; q_ = qs[nwq % 2]; qk = f"qs{nwq % 2}"; nwq += 1
                P.D("sp", out=wt[:], in_=d["wq"].rearrange("(m p) c -> p m c", p=128)[:, :, jg * 512:(jg + 1) * 512], w=[wk])
                for jj in range(4):
                    p_ = pp[nmm % 2]; pk = f"pp{nmm % 2}"; nmm += 1
                    for m in range(8):
                        P.MM(p_[:], wt[:, m, jj * 128:(jj + 1) * 128], h2[:, m, :], start=(m == 0), stop=(m == 7),
                             r=[wk, "h2"], w=[pk])
                    P.I("act", "copy", r=[pk], w=[qk], out=q_[:, jj, :], in_=p_[:])
                for t4 in range(4):
                    P.D("sp", out=d["qTd"][:, b * 4 + t4, jg * 4:(jg + 1) * 4, :], in_=q_[:, :, t4 * 128:(t4 + 1) * 128],
                        r=[qk], w=[f"qTd{b}_{jg}_{t4}"])
        P.barrier()
        P.emit()

    with contextlib.ExitStack() as st:
        sb = lambda n, s, dt=F32: st.enter_context(nc.sbuf_tensor(pfx + "p1_" + n, s, dt))
        ps = lambda n, s, dt=F32: st.enter_context(nc.psum_tensor(pfx + "p1_" + n, s, dt))
        keys = sb("keys", [128, 16, 128]); ident = sb("ident", [128, 128], BF16); identf = sb("identf", [128, 128])
        qt = [sb(f"qt{i}", [128, 16, 128]) for i in range(2)]
        S12 = [sb(f"S12{i}", [128, 16, 128]) for i in range(2)]
        scr_ = sb("scr", [128, 256]); TS = sb("TS", [128, 16, 16]); cand = sb("cand", [128, 8, 256]); BS = sb("BS", [128, 8, 16])
        ex = sb("ex", [128, 8, 16]); Z = sb("Z", [128, 8]); lnZ = sb("lnZ", [128, 8]); bias = sb("bias", [128, 8])
        SUM = [sb(f"SUM{i}", [128, 8, 128]) for i in range(6)]
        E = [sb(f"E{i}", [128, 1024], BF16) for i in range(3)]
        GH = [[sb(f"GH{i}_{h}", [128, 1024], BF16) for h in range(8)] for i in range(2)]
        GTp = [sb(f"GTp{i}", [128, 8, 128], BF16) for i in range(4)]
        sc = ps("sc", [128, 16, 128])
        acc = [ps(f"acc{i}", [128, 4, 128]) for i in range(2)]

        P.D("sp", out=keys[:], in_=d["keysT"].rearrange("j c k -> c j k"), w=["keys"])
        P.I("pool", "memset", w=["identf"], ap=identf[:], constant=1.0)
        P.I("pool", "affine_select", r=["identf"], w=["identf"], out=identf[:], in_=identf[:], pattern=[[-1, 128]],
            compare_op=ALU.is_equal, fill=0.0, base=0, channel_multiplier=1)
        P.I("pool", "tensor_copy", r=["identf"], w=["ident"], out=ident[:], in_=identf[:])
        nsum = 0; nacc = 0; ngtp = 0; ngh = 0
        for tt in range(NT):
            q_ = qt[tt % 2]; qk = f"qt{tt % 2}"; S = S12[tt % 2]; Sk = f"S12{tt % 2}"
            P.D("sp", out=q_[:], in_=d["qTd"][:, tt, :, :], w=[qk])
            for j in range(16):
                P.MM(sc[:, j, :], q_[:, j, :], keys[:, j, :], r=[qk, "keys"], w=["sc"])
            P.I("act", "copy", r=["sc"], w=[Sk + "a"], out=S[:, 0:8, :], in_=sc[:, 0:8, :])
            P.I("dve", "tensor_copy", r=["sc"], w=[Sk + "b"], out=S[:, 8:16, :], in_=sc[:, 8:16, :])
            Sr = [Sk + "a", Sk + "b"]
            for j in range(16):
                P.I("dve", "max", r=Sr, w=["TS"], out=TS[:, j, 0:8], in_=S[:, j, :])
                P.I("dve", "match_replace", r=Sr + ["TS"], w=["scr"], out=scr_[:, 0:128], in_to_replace=TS[:, j, 0:8],
                    in_values=S[:, j, :], imm_value=NEG)
                P.I("dve", "max", r=["scr"], w=["TS"], out=TS[:, j, 8:16], in_=scr_[:, 0:128])
            TS4 = TS[:].rearrange("p (h two) a -> p h two a", two=2)
            P.I("dve", "tensor_tensor", r=["TS"], w=["cand"], out=cand[:].rearrange("p h (a b) -> p h a b", b=16),
                in0=TS4[:, :, 0, :].unsqueeze(3).to_broadcast([128, 8, 16, 16]),
                in1=TS4[:, :, 1, :].unsqueeze(2).to_broadcast([128, 8, 16, 16]), op=ALU.add)
            for h in range(8):
                P.I("dve", "max", r=["cand"], w=["BS"], out=BS[:, h, 0:8], in_=cand[:, h, :])
                P.I("dve", "match_replace", r=["cand", "BS"], w=["scr"], out=scr_[:, 0:256], in_to_replace=BS[:, h, 0:8],
                    in_values=cand[:, h, :], imm_value=NEG)
                P.I("dve", "max", r=["scr"], w=["BS"], out=BS[:, h, 8:16], in_=scr_[:, 0:256])
            P.I("dve", "tensor_tensor", r=["BS"], w=["ex"], out=ex[:], in0=BS[:], in1=BS[:, :, 0:1].to_broadcast([128, 8, 16]),
                op=ALU.subtract)
            P.I("act", "activation", r=["ex"], w=["ex"], out=ex[:], in_=ex[:], func=AF.Exp)
            P.I("dve", "reduce_sum", r=["ex"], w=["Z"], out=Z[:], in_=ex[:], axis=AX.X)
            P.I("act", "activation", r=["Z"], w=["lnZ"], out=lnZ[:], in_=Z[:], func=AF.Ln)
            P.I("dve", "scalar_tensor_tensor", r=["BS", "lnZ"], w=["bias"], out=bias[:], in0=BS[:, :, 0], scalar=-1.0,
                in1=lnZ[:], op0=ALU.mult, op1=ALU.subtract)
            def add_op(it):
                sx_, h_ = it // 8, it % 8
                su = SUM[it % 6]; sk = f"SUM{it % 6}"
                if False:
                    for a in range(8):
                        P.I("act", "activation", r=Sr, w=[sk], out=su[:, a, :], in_=S[:, 2 * h_ + 1, :], func=AF.Identity,
                            bias=S[:, 2 * h_, sx_ * 8 + a:sx_ * 8 + a + 1], scale=1.0)
                else:
                    P.I("dve", "tensor_tensor", r=Sr, w=[sk], out=su[:],
                        in0=S[:, 2 * h_, sx_ * 8:(sx_ + 1) * 8].unsqueeze(2).to_broadcast([128, 8, 128]),
                        in1=S[:, 2 * h_ + 1, :].unsqueeze(1).to_broadcast([128, 8, 128]), op=ALU.add)

            LOOK = 4
            deferred = []
            for it0 in range(LOOK):
                add_op(it0)
            for sx in range(16):
                ghs = GH[ngh % 2]; gk = f"GH{ngh % 2}_"; ngh += 1
                for h in range(8):
                    it = sx * 8 + h
                    if it + LOOK < 128:
                        add_op(it + LOOK)
                    if h == 4 and deferred:
                        deferred.pop(0)()
                    su = SUM[it % 6]; sk = f"SUM{it % 6}"; e_ = E[it % 3]; ek = f"E{it % 3}"
                    P.I("act", "activation", r=[sk, "bias"], w=[ek], out=e_[:], in_=su[:].rearrange("p a b -> p (a b)"),
                        func=AF.Exp, bias=bias[:, h:h + 1])
                    P.I("dve", "scalar_tensor_tensor", r=[sk, ek, "BS"], w=[gk + str(h)], out=ghs[h][:],
                        in0=su[:].rearrange("p a b -> p (a b)"), scalar=BS[:, h, 15:16], in1=e_[:], op0=ALU.is_ge, op1=ALU.mult)
                def flush(sx=sx, tt=tt, ghs=ghs, gk=gk):
                    nonlocal nacc, ngtp
                    gp = GTp[ngtp % 4]; gpk = f"GTp{ngtp % 4}"; ngtp += 1
                    for c4 in range(2):
                        a_ = acc[nacc % 2]; ak = f"acc{nacc % 2}"; nacc += 1
                        for ci in range(4):
                            c = c4 * 4 + ci
                            for h in range(8):
                                P.MM(a_[:, ci, :], ghs[h][:, c * 128:(c + 1) * 128], ident[:], start=(h == 0), stop=(h == 7),
                                     r=[gk + str(h), "ident"], w=[ak])
                        P.I("act", "copy", r=[ak], w=[gpk], out=gp[:, c4 * 4:(c4 + 1) * 4, :], in_=a_[:])
                    P.D("sp", out=d["GT"][tt, :, sx * 1024:(sx + 1) * 1024], in_=gp[:].rearrange("p c t -> p (c t)"), r=[gpk],
                        w=[f"GT{tt}_{sx}"])
                deferred.append(flush)
            while deferred:
                deferred.pop(0)()
        P.barrier()
        P.emit()

    with contextlib.ExitStack() as st:
        sb = lambda n, s, dt=F32: st.enter_context(nc.sbuf_tensor(pfx + "p2_" + n, s, dt))
        ps = lambda n, s, dt=F32: st.enter_context(nc.psum_tensor(pfx + "p2_" + n, s, dt))
        U = [sb(f"U{i}", [128, 8, 1024], BF16) for i in range(2)]
        V = [sb(f"V{i}", [128, 8, 1024], BF16) for i in range(2)]
        Gg = [sb(f"Gg{i}", [128, 2, 8, 128], BF16) for i in range(2)]
        hn2 = [sb(f"hn2{i}", [128, 8, 256], BF16) for i in range(2)]
        ge = [sb(f"ge{i}", [128, 2, 256], BF16) for i in range(2)]
        gh2 = [sb(f"gh2{i}", [128, 2, 256], BF16) for i in range(2)]
        h2b = sb("h2b", [128, 8, 256]); h3 = sb("h3", [128, 8, 256]); sq2 = sb("sq2", [128, 8, 256])
        rstd2 = sb("rstd2", [128, 256]); nrm = sb("nrm", [128, 8, 256]); gn = sb("gn", [128, 8]); ones2 = sb("ones2", [128, 128])
        Y = ps("Y", [128, 8, 256])
        Hp = [ps(f"Hp{i}", [128, 2, 256]) for i in range(2)]
        ss2 = ps("ss2", [128, 256])
        P.D("sp", out=gn[:], in_=d["gnext"].rearrange("(m p) -> p m", p=128), w=["gn"], allow_slow_non_contiguous=True)
        P.I("pool", "memset", w=["ones2"], ap=ones2[:], constant=1.0)
        zl = sb("zl", [128, 128], BF16); zr = sb("zr", [128, 512], BF16)
        P.I("pool", "memset", w=["zl"], ap=zl[:], constant=0.0)
        P.I("pool", "memset", w=["zr"], ap=zr[:], constant=0.0)
        Yb = Y[:].rearrange("p (b two) t -> p b (two t)", two=2)
        nw = 0; npair = 0
        for blk in range(NB2):
            tsl = slice(blk * 256, (blk + 1) * 256)
            hb = hn2[blk % 2]; hbk = f"hn2{blk % 2}"
            P.D("sp", out=hb[:], in_=fm(d["hnbf"])[:, :, tsl], w=[hbk])
            P.D("sp", out=h2b[:], in_=fm(d["h2T"])[:, :, tsl], w=["h2b"])
            for bk in range(4):
                P.MM(Yb[:, bk, :], zl[:], zr[:], start=True, stop=False, r=["zl", "zr"], w=["Y"])
            for eg in range(16):
                u_ = U[nw % 2]; uk = f"U{nw % 2}"; v_ = V[nw % 2]; vk = f"V{nw % 2}"; g_ = Gg[nw % 2]; gk = f"Gg{nw % 2}"; nw += 1
                P.D("sp", out=u_[:], in_=d["uTbf"].rearrange("(m p) e -> p m e", p=128)[:, :, eg * 1024:(eg + 1) * 1024], w=[uk])
                P.D("act", out=v_[:], in_=d["vbf"].rearrange("(c p) x -> p c x", p=128)[:, eg * 8:(eg + 1) * 8, :], w=[vk])
                for t2 in range(2):
                    P.D("sp", out=g_[:, t2, :, :], in_=d["GT"][blk * 2 + t2, :, eg * 1024:(eg + 1) * 1024].rearrange("p (c t) -> p c t", t=128),
                        w=[gk + str(t2)])
                for pr in range(4):
                    hp = Hp[npair % 2]; hpk = f"Hp{npair % 2}"; ge_ = ge[npair % 2]; gek = f"ge{npair % 2}"
                    gh_ = gh2[npair % 2]; ghk = f"gh2{npair % 2}"; npair += 1
                    for cc in range(2):
                        c = pr * 2 + cc
                        for m in range(8):
                            P.MM(hp[:, cc, :], u_[:, m, c * 128:(c + 1) * 128], hb[:, m, :], start=(m == 0), stop=(m == 7),
                                 r=[uk, hbk], w=[hpk])
                    P.I("act", "activation", r=[hpk], w=[gek], out=ge_[:], in_=hp[:], func=AF.Gelu_apprx_tanh)
                    P.I("dve", "tensor_tensor", r=[gek, gk + "0", gk + "1"], w=[ghk],
                        out=gh_[:].rearrange("p c (tt t) -> p c tt t", t=128), in0=ge_[:].rearrange("p c (tt t) -> p c tt t", t=128),
                        in1=g_[:, :, pr * 2:pr * 2 + 2, :].rearrange("p tt c t -> p c tt t"), op=ALU.mult)
                    for cc in range(2):
                        c = pr * 2 + cc
                        for m in range(8):
                            P.MM(Y[:, m, :], v_[:, c, m * 128:(m + 1) * 128], gh_[:, cc, :], start=False,
                                 stop=(eg == 15 and c == 7), r=[vk, ghk], w=["Y"])
            for m in range(8):
                P.I("dve", "tensor_tensor", r=["Y", "h2b"], w=["h3"], out=h3[:, m, :], in0=Y[:, m, :], in1=h2b[:, m, :], op=ALU.add)
            P.D("sp", out=fm(d["hT_out"])[:, :, tsl], in_=h3[:], r=["h3"], w=[f"hT_out{blk}"])
            P.I("act", "activation", r=["h3"], w=["sq2"], out=sq2[:], in_=h3[:], func=AF.Square)
            for m in range(8):
                P.MM(ss2[:], ones2[:], sq2[:, m, :], start=(m == 0), stop=(m == 7), r=["ones2", "sq2"], w=["ss2"])
            P.I("act", "activation", r=["ss2"], w=["rstd2"], out=rstd2[:], in_=ss2[:], func=AF.Sqrt, scale=1.0 / 1024, bias=EPS)
            P.I("dve", "reciprocal", r=["rstd2"], w=["rstd2"], out=rstd2[:], in_=rstd2[:])
            for m in range(8):
                P.I("dve", "scalar_tensor_tensor", r=["h3", "gn", "rstd2"], w=["nrm"], out=nrm[:, m, :], in0=h3[:, m, :],
                    scalar=gn[:, m:m + 1], in1=rstd2[:], op0=ALU.mult, op1=ALU.mult)
            P.D("sp", out=(fm(d["nT_out"])[:, :, tsl] if nT_dst is None else nT_dst(blk)), in_=nrm[:], r=["nrm"], w=[f"nT_out{blk}"])
        P.wait_all("sp", [f"nT_out{b}" for b in range(NB2)] + [f"hT_out{b}" for b in range(NB2)])
        P.barrier()
        P.emit()


def declare_c(nc, S, pfx="", fused=False):
    d = {}

    def t(name, shape, kind, dt=F32):
        d[name] = nc.dram_tensor(pfx + name, shape, dt, kind=kind).ap()

    if not fused:
        t("nT", [1024, S], "ExternalInput")
    t("wq", [1024, 256], "ExternalInput"); t("wk", [1024, 256], "ExternalInput"); t("wv", [1024, 256], "ExternalInput")
    t("wf", [1024, 4], "ExternalInput"); t("fb", [4], "ExternalInput")
    t("maskd", [128, 4, 512], "ExternalInput")
    t("oT", [256, S], "Internal" if fused else "ExternalOutput")
    return d


def emit_c(nc, P, S, d, nT_loads=None, oT_dst=None):
    if oT_dst is None:
        oT_dst = lambda t0, n: d["oT"][:, t0:t0 + n]
    NBLK = S // 512
    NCH = S // 128
    with contextlib.ExitStack() as st:
        sb = lambda n, s, dt=F32: st.enter_context(nc.sbuf_tensor("c_" + n, s, dt))
        ps = lambda n, s, dt=F32: st.enter_context(nc.psum_tensor("c_" + n, s, dt))
        KT = sb("KT", [128, 2, S], BF16)
        Vr = sb("Vr", [128, NCH, 4, 65], BF16)
        Cr = sb("Cr", [128, NCH, 4]); nbias = sb("nbias", [128, 4, NCH])
        nb = [sb(f"nb{i}", [128, 8, 512], BF16) for i in range(2)]
        wq = sb("wq", [128, 8, 256], BF16); wk = sb("wk", [128, 8, 256], BF16); wv = sb("wv", [128, 8, 256], BF16)
        wf = sb("wf", [128, 8, 4], BF16); fb = sb("fb", [128, 4])
        QT = [sb(f"QT{i}", [128, 512], BF16) for i in range(2)]
        PT = [sb(f"PT{i}", [128, 512], BF16) for i in range(3)]
        maskd = sb("maskd", [128, 4, 512], BF16)
        tri = sb("tri", [128, 128]); ones = sb("ones", [128, 128])
        lf = sb("lf", [128, 4]); tot = sb("tot", [128, 4]); totmid = sb("totmid", [128, 4])
        zrow = sb("zrow", [65, 512]); ocp = sb("ocp", [64, 512]); osb = [sb(f"osb{i}", [64, 512]) for i in range(2)]
        pp = [ps(f"pp{i}", [128, 512]) for i in range(2)]
        ST = [ps(f"ST{i}", [128, 512]) for i in range(2)]
        OT = [ps(f"OT{i}", [65, 512]) for i in range(2)]
        ZB = ps("ZB", [64, 512]); cs = ps("cs", [128, 8])

        for nm, tl in (("wq", wq), ("wk", wk), ("wv", wv)):
            P.D("pool", out=tl[:], in_=d[nm].rearrange("(m p) c -> p m c", p=128), w=[nm])
        P.D("pool", out=wf[:], in_=d["wf"].rearrange("(m p) c -> p m c", p=128), w=["wf"])
        P.D("sp", out=fb[:], in_=d["fb"].partition_broadcast(128), w=["fb"])
        P.D("pool", out=maskd[:], in_=d["maskd"], w=["maskd"])
        P.I("pool", "memset", w=["ones"], ap=ones[:], constant=1.0)
        P.I("pool", "memset", w=["tri"], ap=tri[:], constant=1.0)
        P.I("pool", "affine_select", r=["tri"], w=["tri"], out=tri[:], in_=tri[:], pattern=[[1, 128]],
            compare_op=ALU.is_ge, fill=0.0, base=0, channel_multiplier=-1)
        P.I("pool", "memset", w=["tot"], ap=tot[:], constant=0.0)
        P.I("pool", "memset", w=["Vr"], ap=Vr[:, :, :, 64:65], constant=1.0)
        npp = 0; nst = 0; npt = 0; nhead = 0
        for blk in range(NBLK):
            tsl = slice(blk * 512, (blk + 1) * 512)
            n_ = nb[blk % 2]; nk = f"nb{blk % 2}"
            if nT_loads is None:
                P.D("pool", out=n_[:], in_=d["nT"].rearrange("(m p) t -> p m t", p=128)[:, :, tsl], w=[nk])
            else:
                for csl, src in nT_loads(blk):
                    P.D("pool", out=n_[:, :, csl], in_=src, w=[nk])
            for pair in range(2):
                p_ = pp[npp % 2]; pk = f"pp{npp % 2}"; npp += 1
                for m in range(8):
                    P.MM(p_[:], wq[:, m, pair * 128:(pair + 1) * 128], n_[:, m, :], start=(m == 0), stop=(m == 7), r=["wq", nk], w=[pk])
                P.I("act", "mul", r=[pk], w=[f"QT{pair}"], out=QT[pair][:], in_=p_[:], mul=0.125)
                p_ = pp[npp % 2]; pk = f"pp{npp % 2}"; npp += 1
                for m in range(8):
                    P.MM(p_[:], wk[:, m, pair * 128:(pair + 1) * 128], n_[:, m, :], start=(m == 0), stop=(m == 7), r=["wk", nk], w=[pk])
                P.I("dve", "tensor_copy", r=[pk], w=["KT"], out=KT[:, pair, tsl], in_=p_[:])
            for t4 in range(4):
                ch = blk * 4 + t4
                p_ = pp[npp % 2]; pk = f"pp{npp % 2}"; npp += 1
                for m in range(8):
                    P.MM(p_[:, 0:256], n_[:, m, t4 * 128:(t4 + 1) * 128], wv[:, m, :], start=(m == 0), stop=(m == 7), r=["wv", nk], w=[pk])
                P.I("act", "copy", r=[pk], w=["Vr"], out=Vr[:, ch, :, 0:64], in_=p_[:, 0:256].rearrange("p (h x) -> p h x", x=64))
                for m in range(8):
                    P.MM(cs[:, 0:4], n_[:, m, t4 * 128:(t4 + 1) * 128], wf[:, m, :], start=(m == 0), stop=(m == 7), r=["wf", nk], w=["cs"])
                P.I("dve", "tensor_tensor", r=["cs", "fb"], w=["lf"], out=lf[:], in0=cs[:, 0:4], in1=fb[:], op=ALU.add)
                P.I("act", "activation", r=["lf"], w=["lf"], out=lf[:], in_=lf[:], func=AF.Exp, scale=-1.0)
                P.I("act", "activation", r=["lf"], w=["lf"], out=lf[:], in_=lf[:], func=AF.Ln, bias=1.0)
                P.I("dve", "tensor_scalar", r=["lf"], w=["lf"], out=lf[:], in0=lf[:], scalar1=-1.0, scalar2=None, op0=ALU.mult)
                P.MM(cs[:, 0:4], tri[:], lf[:], r=["tri", "lf"], w=["cs"])
                P.MM(cs[:, 4:8], ones[:], lf[:], r=["ones", "lf"], w=["cs"])
                P.I("dve", "tensor_tensor", r=["cs", "tot"], w=["Cr"], out=Cr[:, ch, :], in0=cs[:, 0:4], in1=tot[:], op=ALU.add)
                P.I("dve", "tensor_tensor", r=["cs", "tot"], w=["tot"], out=tot[:], in0=cs[:, 4:8], in1=tot[:], op=ALU.add)
                if t4 == 1:
                    P.I("dve", "tensor_copy", r=["tot"], w=["totmid"], out=totmid[:], in_=tot[:])
            nch = blk * 4 + 4
            for hl in range(4):
                P.I("dve", "tensor_scalar", r=["Cr", "totmid"], w=["nbias"], out=nbias[:, hl, 0:nch], in0=Cr[:, 0:nch, hl],
                    scalar1=totmid[:, hl:hl + 1], scalar2=-1.0, op0=ALU.subtract, op1=ALU.mult)
            pairs = [(hl, kc) for hl in range(4) for kc in range(nch)]

            def qk(i):
                nonlocal nst
                hl, kc = pairs[i]
                rows = slice((hl % 2) * 64, (hl % 2) * 64 + 64)
                s_ = ST[i % 2]
                P.MM(s_[:], KT[rows, hl // 2, kc * 128:(kc + 1) * 128], QT[hl // 2][rows, :], r=["KT", f"QT{hl // 2}"], w=[f"ST{i % 2}"])

            qk(0)
            for i, (hl, kc) in enumerate(pairs):
                if i + 1 < len(pairs):
                    qk(i + 1)
                s_ = ST[i % 2]; sk = f"ST{i % 2}"
                pt = PT[npt % 3]; ptk = f"PT{npt % 3}"; npt += 1
                P.I("act", "activation", r=[sk, "nbias"], w=[ptk], out=pt[:], in_=s_[:], func=AF.Exp, bias=nbias[:, hl, kc:kc + 1])
                if kc >= blk * 4:
                    P.I("dve", "tensor_tensor", r=[ptk, "maskd"], w=[ptk], out=pt[:], in0=pt[:], in1=maskd[:, kc - blk * 4, :], op=ALU.mult)
                if kc == 0:
                    ot = OT[nhead % 2]; otk = f"OT{nhead % 2}"; ob = osb[nhead % 2]; obk = f"osb{nhead % 2}"; nhead += 1
                P.MM(ot[:], Vr[:, kc, hl, :], pt[:], start=(kc == 0), stop=(kc == nch - 1), r=["Vr", ptk], w=[otk])
                if kc == nch - 1:
                    P.I("dve", "tensor_scalar", r=[otk], w=["zrow"], out=zrow[64:65, :], in0=ot[64:65, :], scalar1=1e-30, scalar2=None, op0=ALU.max)
                    P.I("dve", "reciprocal", r=["zrow"], w=["zrow"], out=zrow[64:65, :], in_=zrow[64:65, :])
                    P.MM(ZB[:], ones[64:65, 0:64], zrow[64:65, :], r=["ones", "zrow"], w=["ZB"])
                    P.I("act", "copy", r=[otk], w=["ocp"], out=ocp[:], in_=ot[0:64, :])
                    P.I("dve", "tensor_tensor", r=["ocp", "ZB"], w=[obk], out=ob[:], in0=ocp[:], in1=ZB[:], op=ALU.mult)
                    P.D("sp", out=oT_dst(blk * 512, 512)[hl * 64:(hl + 1) * 64, :], in_=ob[:], r=[obk], w=[f"oT{blk}_{hl}"])
        P.wait_all("sp", [f"oT{b}_{h}" for b in range(NBLK) for h in range(4)])
        P.barrier()
        P.emit()


S_FULL = 16384
T_CORE = 4096
G4 = [[0, 1, 2, 3], [4, 5, 6, 7]]
_PROG = {}
_B_IN = (("wout", [1024, 1024]), ("gffn", [1024]), ("wq", [1024, 2048]), ("keysT", [16, 128, 128]), ("uT", [1024, 16384]),
         ("v", [16384, 1024]), ("gnext", [1024]))


def _declare_b_fused(nc, T, pfx, hT_ap, out_kind):
    d = {}
    for name, shape in _B_IN:
        d[name] = nc.dram_tensor(pfx + name, shape, F32, kind="ExternalInput").ap()
    d["hT"] = hT_ap
    d["hT_out"] = nc.dram_tensor(pfx + "hT_out", [1024, T], F32, kind="Internal").ap()
    d["nT_out"] = nc.dram_tensor(pfx + "nT_out", [1024, T], F32, kind=out_kind).ap()
    scr = lambda name, shape, dt=F32: nc.dram_tensor(pfx + name, shape, dt, kind="Internal").ap()
    d["h2T"] = scr("h2T", [1024, T]); d["hnbf"] = scr("hnbf", [1024, T], BF16); d["qTd"] = scr("qTd", [128, T // 128, 16, 128])
    d["oTq"] = scr("oTq", [1024, T])
    d["GT"] = scr("GT", [T // 128, 128, 16384], BF16); d["uTbf"] = scr("uTbf", [1024, 16384], BF16); d["vbf"] = scr("vbf", [16384, 1024], BF16)
    return d


def _build_fused(S=S_FULL, T=T_CORE):
    if (S, T) in _PROG:
        return _PROG[(S, T)]
    nc = bass.Bass("TRN2", target_bir_lowering=False)
    with contextlib.ExitStack() as st:
        P = Prog(nc)
        P.alloc_sems(st)
        da = declare_a(nc, S, pfx="A_", fused=True)
        xTs = nc.dram_tensor("B0_xTs", [1024, T], F32, kind="ExternalInput").ap()
        db0 = _declare_b_fused(nc, T, "B0_", xTs, "Internal")
        dc = declare_c(nc, S, pfx="C_", fused=True)
        db1 = _declare_b_fused(nc, T, "B1_", db0["hT_out"], "ExternalOutput")
        CW = min(1024, T)
        NC1 = S // CW
        CW2 = 256
        NC2 = T // CW2
        dt_ = lambda name, shape: nc.dram_tensor(name, shape, F32, kind="Internal").ap()
        x1_in = dt_("x1_in", [NC1, 256, CW]); x1_out = dt_("x1_out", [NC1, 1024, CW])
        x2_in = dt_("x2_in", [NC2, 1024, CW2]); x2_out = dt_("x2_out", [NC2, 4096, CW2])
        x3_in = dt_("x3_in", [NC1, 256, CW]); x3_out = dt_("x3_out", [NC1, 1024, CW])

        def gather(src, dst, n, name):
            for j in range(n):
                P.cc((lambda j: (lambda e: e.collective_compute("AllGather", ALU.bypass, replica_groups=G4, ins=[src[j]], outs=[dst[j]])))(j),
                     writes=[name])
            P.barrier()

        def chunked_dst(buf):
            return lambda t0, n: buf[t0 // CW, :, (t0 % CW):(t0 % CW) + n]

        def quarter_src(buf):
            def f():
                q = nc.partition_id() % 4
                return buf[bass.ds(q * (T // CW), T // CW), :, :]
            return f

        bpr = T // 512

        def nT_loads(blk):
            rank, lb = blk // bpr, blk % bpr
            return [(slice(h * CW2, (h + 1) * CW2),
                     x2_out[lb * 2 + h, rank * 1024:(rank + 1) * 1024, :].rearrange("(m p) t -> p m t", p=128)) for h in range(2)]

        emit_a(nc, P, S, da, oT_dst=chunked_dst(x1_in))
        gather(x1_in, x1_out, NC1, "x1")
        emit_b(nc, P, T, db0, pfx="B0_", oT_blk=quarter_src(x1_out),
               nT_dst=lambda blk: x2_in[blk].rearrange("(m p) t -> p m t", p=128))
        gather(x2_in, x2_out, NC2, "x2")
        emit_c(nc, P, S, dc, nT_loads=nT_loads, oT_dst=chunked_dst(x3_in))
        gather(x3_in, x3_out, NC1, "x3")
        emit_b(nc, P, T, db1, pfx="B1_", oT_blk=quarter_src(x3_out))
    _PROG[(S, T)] = nc
    return nc


def _peer_inputs(pfx, wout, gffn, wq, keys, u, v, gnext):
    f32 = lambda a: np.ascontiguousarray(np.asarray(a, dtype=np.float32))
    return {pfx + "wout": f32(wout), pfx + "gffn": f32(gffn), pfx + "wq": f32(wq),
            pfx + "keysT": np.ascontiguousarray(f32(keys).transpose(0, 1, 3, 2).reshape(16, 128, 128)),
            pfx + "uT": np.ascontiguousarray(f32(u).T), pfx + "v": f32(v), pfx + "gnext": f32(gnext)}


def kernel(x, l0_attn_norm, l0_w_in, l0_cmp_pe_k, l0_cmp_w1_k, l0_cmp_w2_k, l0_cmp_pe_v, l0_cmp_w1_v, l0_cmp_w2_v, l0_w_out,
           l0_ffn_norm, l0_peer_wq, l0_peer_keys, l0_peer_u, l0_peer_v,
           l1_attn_norm, l1_w_in, l1_f_bias, l1_w_out,
           l1_ffn_norm, l1_peer_wq, l1_peer_keys, l1_peer_u, l1_peer_v,
           final_norm):
    f32 = lambda a: np.ascontiguousarray(np.asarray(a, dtype=np.float32))
    x = f32(x)
    B, S, D = x.shape
    T_CORE = S // 4
    nc = _build_fused(S, T_CORE)
    consts = consts_a(); ropeq, ropek = rope_tables(S)
    args0 = [f32(a) for a in (l0_attn_norm, l0_w_in, l0_cmp_pe_k, l0_cmp_w1_k, l0_cmp_w2_k, l0_cmp_pe_v, l0_cmp_w1_v, l0_cmp_w2_v)]
    pb0 = _peer_inputs("B0_", l0_w_out, l0_ffn_norm, l0_peer_wq, l0_peer_keys, l0_peer_u, l0_peer_v, l1_attn_norm)
    pb1 = _peer_inputs("B1_", l1_w_out, l1_ffn_norm, l1_peer_wq, l1_peer_keys, l1_peer_u, l1_peer_v, final_norm)
    w1 = f32(l1_w_in); fbias = f32(l1_f_bias)
    kk = np.arange(128)[:, None, None]; ii = np.arange(4)[None, :, None]; qq = np.arange(512)[None, None, :]
    maskd = (kk <= qq - 128 * ii).astype(np.float32)
    xT = [np.ascontiguousarray(x[b].T) for b in range(B)]
    maps = []
    for c in range(8):
        b, q = c // 4, c % 4
        m = {"A_" + k: v for k, v in host_inputs_a(x[b], *args0, q, consts, ropeq, ropek).items()}
        m["A_xT"] = xT[b]
        m["B0_xTs"] = np.ascontiguousarray(xT[b][:, q * T_CORE:(q + 1) * T_CORE])
        m.update(pb0); m.update(pb1)
        h0 = 4 * q
        m.update({"C_wq": np.ascontiguousarray(w1[:, h0 * 64:(h0 + 4) * 64]),
                  "C_wk": np.ascontiguousarray(w1[:, 1024 + h0 * 64:1024 + (h0 + 4) * 64]),
                  "C_wv": np.ascontiguousarray(w1[:, 2048 + h0 * 64:2048 + (h0 + 4) * 64]),
                  "C_wf": np.ascontiguousarray(w1[:, 3072 + h0:3072 + h0 + 4]), "C_fb": fbias[h0:h0 + 4].copy(), "C_maskd": maskd})
        maps.append(m)
    res = run_bass_kernel_spmd(nc, maps, core_ids=list(range(8))).results
    out = np.empty((B, S, D), np.float32)
    for c in range(8):
        b, q = c // 4, c % 4
        out[b, q * T_CORE:(q + 1) * T_CORE, :] = res[c]["B1_nT_out"].T
    return out
```

```python
import contextlib
import numpy as np
import concourse.bass as bass
import concourse.mybir as mybir
from concourse.bass_utils import run_bass_kernel_spmd

F32 = mybir.dt.float32
BF16 = mybir.dt.bfloat16
AF = mybir.ActivationFunctionType
ALU = mybir.AluOpType
AX = mybir.AxisListType

N_DMA_SEMS = 8


class Prog:
    COMPUTE = ("pe", "dve", "act", "pool")
    QUEUES = ("sp", "act", "pool")

    def __init__(self, nc):
        self.nc = nc
        self.ops = {e: [] for e in ("pe", "dve", "act", "pool", "sp")}
        self.cnt = {}
        self.last_w = {}
        self.readers = {}
        self.waited = {e: {} for e in self.ops}
        self.dma_n = {q: 0 for q in self.QUEUES}
        self.semkeys = []
        for e in self.COMPUTE:
            self._mk(("c", e))
        for q in self.QUEUES:
            for i in range(N_DMA_SEMS):
                self._mk(("d", q, i))
        self._mk(("cc",))
        self.sems = {}
        self.pending = {e: False for e in self.ops}
        self.lazy_pe_inc = False

    def _mk(self, k):
        self.cnt[k] = 0
        self.semkeys.append(k)

    def _need(self, eng, tok, waits):
        if tok is None:
            return
        k, v = tok
        if k == ("c", eng) and eng == "pe":
            return
        if self.waited[eng].get(k, 0) >= v:
            return
        self.waited[eng][k] = v
        waits.append((k, v))

    def _deps(self, eng, reads, writes):
        waits = []
        for b in reads:
            self._need(eng, self.last_w.get(b), waits)
        for b in writes:
            self._need(eng, self.last_w.get(b), waits)
            for t in self.readers.get(b, {}).items():
                if t[0] == ("c", eng):
                    continue
                self._need(eng, t, waits)
        return waits

    def _commit(self, tok, reads, writes):
        for b in reads:
            self.readers.setdefault(b, {})[tok[0]] = tok[1]
        for b in writes:
            self.last_w[b] = tok
            self.readers[b] = {}

    def op(self, eng, fn, reads=(), writes=(), inc=True):
        waits = self._deps(eng, reads, writes)
        k = ("c", eng)
        if inc:
            self.cnt[k] += 1
            tok = (k, self.cnt[k])
            self.pending[eng] = False
        else:
            tok = (k, self.cnt[k] + 1)
            self.pending[eng] = True
        self._commit(tok, reads, writes)
        self.ops[eng].append((waits, fn, (k, 1) if inc else None))
        return tok

    def dma(self, q, fn, reads=(), writes=()):
        waits = self._deps(q, reads, writes)
        n = self.dma_n[q]
        self.dma_n[q] += 1
        k = ("d", q, n % N_DMA_SEMS)
        self._need(q, (k, self.cnt[k]) if self.cnt[k] else None, waits)
        self.cnt[k] += 16
        tok = (k, self.cnt[k])
        self._commit(tok, reads, writes)
        self.ops[q].append((waits, fn, (k, 16)))
        return tok

    def cc(self, fn, reads=(), writes=()):
        waits = self._deps("pool", reads, writes)
        k = ("cc",)
        self._need("pool", (k, self.cnt[k]) if self.cnt[k] else None, waits)
        self.cnt[k] += 1
        tok = (k, self.cnt[k])
        self._commit(tok, reads, writes)
        self.ops["pool"].append((waits, fn, (k, 1)))
        return tok

    def I(self, eng, method, r=(), w=(), **kw):
        return self.op(eng, lambda e: getattr(e, method)(**kw), reads=r, writes=w)

    def MM(self, out, lhsT, rhs, start=True, stop=True, r=(), w=()):
        return self.op("pe", lambda e: e.matmul(out, lhsT=lhsT, rhs=rhs, start=start, stop=stop), reads=r, writes=w,
                       inc=(stop or not self.lazy_pe_inc))

    def D(self, q, out, in_, r=(), w=(), **kw):
        return self.dma(q, lambda e: e.dma_start(out=out, in_=in_, **kw), reads=r, writes=w)

    def barrier(self):
        assert not any(self.pending.values()), self.pending
        for eng in self.ops:
            waits = []
            for k in self.semkeys:
                if self.cnt[k]:
                    self._need(eng, (k, self.cnt[k]), waits)
            self.ops[eng].append((waits, None, None))
        self.last_w = {}
        self.readers = {}

    def wait_all(self, eng, bufs):
        waits = []
        for b in bufs:
            self._need(eng, self.last_w.get(b), waits)
        self.ops[eng].append((waits, None, None))

    def alloc_sems(self, st):
        for k in self.semkeys:
            self.sems[k] = st.enter_context(self.nc.semaphore("s_" + "_".join(map(str, k))))

    def emit(self):
        nc = self.nc
        import contextlib
        with contextlib.ExitStack() as st:
            if not self.sems:
                self.alloc_sems(st)
            block = st.enter_context(nc.Block())
            engobj = {"pe": "tensor", "dve": "vector", "act": "scalar", "pool": "gpsimd", "sp": "sync"}

            def mk(ename):
                ops = self.ops[ename]

                def body(eng):
                    for waits, fn, inc in ops:
                        for (k, v) in waits:
                            eng.wait_ge(self.sems[k], v)
                        if fn is not None:
                            ins = fn(eng)
                            if inc is not None:
                                ins.then_inc(self.sems[inc[0]], inc[1])
                return body

            for ename, attr in engobj.items():
                if self.ops[ename]:
                    getattr(block, attr)(mk(ename))
        self.ops = {e: [] for e in self.ops}


EPS = 1e-6


def consts_a():
    c = {}
    q = np.arange(128)[:, None]; rel = np.arange(512)[None, :] - 256
    cur = (q >= 64).astype(np.int64)
    M = (rel <= cur - 2).astype(np.float32)
    A = np.where(rel == cur, 10000.0, np.where(rel == cur - 1, 10001.0, np.where(rel > cur, -1.0, 0.0))).astype(np.float32)
    c["pats"] = np.stack([M, A], 1)
    ci = np.arange(128)[:, None]; qi = np.arange(128)[None, :]
    D = (16 * ci - qi).astype(np.float32)
    Dz = D.copy(); Dz[0, :] = 1e9
    c["D16"] = np.stack([D, Dz], 1)
    c["tril"] = np.stack([(ci <= qi), (ci > qi)], 1).astype(np.float32)
    c["Sel"] = np.broadcast_to(np.eye(12, dtype=np.float32)[:, :, None], (12, 12, 64)).copy()
    cc = np.arange(1024)[:, None] - 1; jj = np.arange(256)[None, :]
    lo = np.maximum(cc * 16, jj * 64); hi = np.minimum(cc * 16 + 32, (jj + 1) * 64)
    m = np.maximum(hi - lo, 0).astype(np.float32) / 32.0
    m[0, :] = 0.0
    c["slcm"] = np.ascontiguousarray(m.reshape(8, 128, 256).transpose(1, 0, 2))
    return c


def epat_table(S):
    n = np.arange(S)[None, :]; r = np.arange(64)[:, None]
    return (30000.0 * (((n // 64) % 64) == r)).astype(np.float32)


def rope_tables(S):
    half = 32
    inv = (10000.0 ** (-np.arange(half, dtype=np.float32) / half)).astype(np.float32)
    ang = (np.arange(S, dtype=np.float32)[None, :] * inv[:, None]).astype(np.float32)
    cos = np.cos(ang).astype(np.float32); sin = np.sin(ang).astype(np.float32)
    cosf = np.concatenate([cos, cos], 0); sinf = np.concatenate([-sin, sin], 0)
    rk = np.stack([cosf, sinf], 1)
    return np.ascontiguousarray(rk * 0.125), np.ascontiguousarray(rk)


def host_inputs_a(xb, gattn, w_in, pe_k, w1_k, w2_k, pe_v, w1_v, w2_v, g, consts, ropeq, ropek):
    def sw(w):
        w = w.reshape(w.shape[0], -1, 64)
        return np.concatenate([w[..., 32:], w[..., :32]], -1).reshape(w.shape[0], -1)
    kv = lambda i: w_in[:, 1024 + i * 256 + g * 64:1024 + i * 256 + (g + 1) * 64]
    wq = w_in[:, g * 256:(g + 1) * 256]
    d = dict(consts)
    d["xT"] = np.ascontiguousarray(xb.T)
    d["gattn"] = gattn
    d["wqa"] = np.ascontiguousarray(np.concatenate([wq, sw(wq)], 1))
    d["wka"] = np.ascontiguousarray(np.concatenate([kv(0), kv(1), sw(kv(0)), kv(2), sw(kv(2)), kv(4), sw(kv(4))], 1))
    d["wtok"] = np.ascontiguousarray(np.concatenate([kv(3), kv(5)], 1))
    gc = [2560 + br * 16 + g * 4 + r for br in range(3) for r in range(4)]
    d["wg"] = np.ascontiguousarray(w_in[:, gc])
    d["w1s"] = np.ascontiguousarray(np.concatenate([w1_k[g].transpose(1, 0, 2), w1_v[g].transpose(1, 0, 2)], 0))
    d["peT"] = np.ascontiguousarray(np.concatenate([pe_k[g].T, pe_v[g].T], 0))
    d["w2s"] = np.ascontiguousarray(np.stack([w2_k[g], w2_v[g]], 1))
    d["ropeq"] = ropeq; d["ropek"] = ropek
    d["Epat"] = epat_table(xb.shape[0])
    return d


def declare_a(nc, S, pfx="", fused=False):
    d = {}

    def t(name, shape, kind="ExternalInput", dt=F32):
        d[name] = nc.dram_tensor(pfx + name, shape, dt, kind=kind).ap()

    t("xT", [1024, S]); t("gattn", [1024]); t("wqa", [1024, 512]); t("wka", [1024, 448]); t("wtok", [1024, 128]); t("wg", [1024, 12])
    t("w1s", [128, 32, 128]); t("peT", [128, 32]); t("w2s", [128, 2, 64]); t("ropeq", [64, 2, S]); t("ropek", [64, 2, S])
    t("pats", [128, 2, 512]); t("D16", [128, 2, 128]); t("tril", [128, 2, 128]); t("Epat", [64, S]); t("Sel", [12, 12, 64])
    t("slcm", [128, 8, 256])
    t("oT", [256, S], kind="Internal" if fused else "ExternalOutput")
    return d


def emit_a(nc, P, S, d, oT_dst=None):
    if oT_dst is None:
        oT_dst = lambda t0, n: d["oT"][:, t0:t0 + n]
    NBLK = S // 512
    NCH = S // 128
    with contextlib.ExitStack() as st:
        sb = lambda n, s, dt=F32: st.enter_context(nc.sbuf_tensor("a_" + n, s, dt))
        ps = lambda n, s, dt=F32: st.enter_context(nc.psum_tensor("a_" + n, s, dt))
        KsT = sb("KsT", [128, S], BF16); Vs = sb("Vs", [128, NCH, 128], BF16)
        KwT = sb("KwT", [128, 8, 128], BF16); Vw = sb("Vw", [128, 8, 128], BF16)
        KcT = sb("KcT", [128, 1024], BF16); Vc = sb("Vc", [128, 8, 128], BF16)
        slcm = sb("slcm", [128, 8, 256], BF16)
        Qaug = [sb(f"Qaug{i}", [128, 4, 512], BF16) for i in range(2)]
        pats = sb("pats", [128, 2, 512]); D16 = sb("D16", [128, 2, 128]); tril = sb("tril", [128, 2, 128], BF16)
        Sel = sb("Sel", [12, 12, 64])
        wqa = sb("wqa", [128, 8, 512], BF16); wka = sb("wka", [128, 8, 448], BF16); wtok = sb("wtok", [128, 8, 128], BF16)
        wg = sb("wg", [128, 8, 12], BF16)
        w1s = sb("w1s", [128, 32, 128], BF16); peT = sb("peT", [128, 32], BF16); w2s = sb("w2s", [128, 2, 64], BF16)
        hb = sb("hb", [128, 2]); g = sb("g", [128, 8])
        ones_b = sb("ones_b", [128, 128], BF16); ones_f = sb("ones_f", [128, 128]); identf = sb("identf", [128, 128])
        xT = sb("xT", [128, 8, 512]); sq = sb("sq", [128, 8, 512], BF16); rstd = sb("rstd", [128, 512]); xnb = sb("xnb", [128, 8, 512], BF16)
        rq = sb("rq", [64, 2, 512]); rk = sb("rk", [64, 2, 512])
        t1 = [sb(f"t1_{i}", [64, 512]) for i in range(2)]; t2 = [sb(f"t2_{i}", [64, 512]) for i in range(2)]
        Qd = sb("Qd", [64, 4, 4, 128], BF16)
        CV = sb("CV", [128, 528], BF16); hidk = sb("hidk", [128, 32], BF16); hvp = sb("hvp", [128, 128], BF16)
        gT = sb("gT", [12, 512])
        PTc = sb("PTc", [128, 8, 512], BF16); PT = [sb(f"PT{i}", [128, 512], BF16) for i in range(3)]
        zc = sb("zc", [1, 512]); impS = sb("impS", [128, 256]); scr = sb("scr", [128, 256]); m8 = sb("m8", [128, 16])
        NT = sb("NT", [128, 256])
        zrow = sb("zrow", [65, 512]); gbs = sb("gbs", [64, 512]); acc = [sb(f"acc{i}", [64, 512]) for i in range(2)]
        tmp = sb("tmp", [64, 512])
        pp = [ps(f"pp{i}", [128, 512]) for i in range(2)]
        ST = [ps(f"ST{i}", [128, 512]) for i in range(2)]
        OA = [ps(f"OA{i}", [128, 512]) for i in range(2)]
        IMP = ps("IMP", [128, 512]); AUX = ps("AUX", [128, 512])

        for nm, tl in (("wqa", wqa), ("wka", wka), ("wtok", wtok), ("wg", wg)):
            P.D("pool", out=tl[:], in_=d[nm].rearrange("(m p) c -> p m c", p=128), w=[nm])
        for nm, tl in (("w1s", w1s), ("peT", peT), ("w2s", w2s), ("slcm", slcm), ("tril", tril)):
            P.D("pool", out=tl[:], in_=d[nm], w=[nm])
        for nm, tl in (("pats", pats), ("D16", D16), ("Sel", Sel)):
            P.D("sp", out=tl[:], in_=d[nm], w=[nm])
        P.D("pool", out=KsT[64:128, :], in_=d["Epat"], w=["KsE"])
        P.D("sp", out=g[:], in_=d["gattn"].rearrange("(m p) -> p m", p=128), w=["g"], allow_slow_non_contiguous=True)
        P.I("pool", "memset", w=["ones_b"], ap=ones_b[:], constant=1.0)
        P.I("pool", "memset", w=["ones_f"], ap=ones_f[:], constant=1.0)
        P.I("pool", "memset", w=["identf"], ap=identf[:], constant=1.0)
        P.I("pool", "affine_select", r=["identf"], w=["identf"], out=identf[:], in_=identf[:], pattern=[[-1, 128]],
            compare_op=ALU.is_equal, fill=0.0, base=0, channel_multiplier=1)
        P.I("pool", "memset", w=["Vs"], ap=Vs[:], constant=0.0)
        P.I("pool", "memset", r=["Vs"], w=["Vs"], ap=Vs[:, :, 64:65], constant=1.0)
        P.I("pool", "memset", w=["Vw"], ap=Vw[:], constant=0.0)
        P.I("pool", "memset", r=["Vw"], w=["Vw"], ap=Vw[:, :, 64:65], constant=1.0)
        P.I("pool", "memset", w=["KwT"], ap=KwT[:], constant=0.0)
        for i in range(2):
            P.I("pool", "memset", w=[f"Qaug{i}q", f"Qaug{i}m"], ap=Qaug[i][:], constant=0.0)
        P.I("pool", "memset", w=["KcT"], ap=KcT[:], constant=0.0)
        P.I("pool", "memset", w=["Vc"], ap=Vc[:], constant=0.0)
        P.I("pool", "memset", w=["CVk", "CVv"], ap=CV[:], constant=0.0)
        P.I("pool", "memset", w=["hvp"], ap=hvp[:], constant=0.0)
        for kvi in range(2):
            rows = slice(kvi * 64, kvi * 64 + 64)
            for l in range(32):
                P.MM(pp[0][:, kvi:kvi + 1], w1s[rows, l, :], peT[rows, l:l + 1], start=(l == 0), stop=(l == 31), r=["w1s", "peT"], w=["pp0"])
        P.I("dve", "tensor_copy", r=["pp0"], w=["hb"], out=hb[:], in_=pp[0][:, 0:2])

        cnt = {"pp": 0, "t": 0, "pt": 0, "oa": 0, "acc": 0}

        def proj(c0, M):
            i = cnt["pp"] % 2; cnt["pp"] += 1
            return pp[i], f"pp{i}"

        for blk in range(NBLK):
            tsl = slice(blk * 512, (blk + 1) * 512)
            P.D("sp", out=xT[:], in_=d["xT"].rearrange("(m p) t -> p m t", p=128)[:, :, tsl], w=["xT"])
            P.D("sp", out=rq[:], in_=d["ropeq"][:, :, tsl], w=["rq"])
            P.D("sp", out=rk[:], in_=d["ropek"][:, :, tsl], w=["rk"])
            P.I("act", "activation", r=["xT"], w=["sq"], out=sq[:], in_=xT[:], func=AF.Square)
            for m in range(8):
                P.MM(AUX[:], ones_b[:], sq[:, m, :], start=(m == 0), stop=(m == 7), r=["ones_b", "sq"], w=["AUX"])
            P.I("act", "activation", r=["AUX"], w=["rstd"], out=rstd[:], in_=AUX[:], func=AF.Sqrt, scale=1.0 / 1024, bias=EPS)
            P.I("dve", "reciprocal", r=["rstd"], w=["rstd"], out=rstd[:], in_=rstd[:])
            for m in range(8):
                P.I("dve", "scalar_tensor_tensor", r=["xT", "g", "rstd"], w=["xnb"], out=xnb[:, m, :], in0=xT[:, m, :],
                    scalar=g[:, m:m + 1], in1=rstd[:], op0=ALU.mult, op1=ALU.mult)

            def fmproj(wt, wk_, c0, M):
                p_, pk = proj(c0, M)
                for m in range(8):
                    P.MM(p_[0:M, :], wt[:, m, c0:c0 + M], xnb[:, m, :], start=(m == 0), stop=(m == 7), r=[wk_, "xnb"], w=[pk])
                return p_, pk

            def rope(wt, wk_, ca, cb, tab, tabk, out_ap, outk):
                pa, pak = fmproj(wt, wk_, ca, 64)
                i = cnt["t"] % 2; cnt["t"] += 1
                P.I("dve", "tensor_tensor", r=[pak, tabk], w=[f"t1_{i}"], out=t1[i][:], in0=pa[0:64, :], in1=tab[:, 0, :], op=ALU.mult)
                pb, pbk = fmproj(wt, wk_, cb, 64)
                P.I("dve", "tensor_tensor", r=[pbk, tabk], w=[f"t2_{i}"], out=t2[i][:], in0=pb[0:64, :], in1=tab[:, 1, :], op=ALU.mult)
                a_, b_ = t1[i][:], t2[i][:]
                if len(out_ap.shape) == 3:
                    a_ = a_.rearrange("p (a q) -> p a q", a=4); b_ = b_.rearrange("p (a q) -> p a q", a=4)
                P.I("pool", "tensor_tensor", r=[f"t1_{i}", f"t2_{i}"], w=[outk], out=out_ap, in0=a_, in1=b_, op=ALU.add)

            for r in range(4):
                rope(wqa, "wqa", r * 64, 256 + r * 64, rq, "rq", Qd[:, :, r, :], "Qd")
            rope(wka, "wka", 192, 256, rk, "rk", KsT[0:64, tsl], "KsT")
            rope(wka, "wka", 320, 384, rk, "rk", KwT[0:64, (blk % 2) * 4:(blk % 2) * 4 + 4, :], "KwT")
            pa, pak = fmproj(wka, "wka", 0, 128)
            i = cnt["t"] % 2; cnt["t"] += 1
            P.I("dve", "tensor_tensor", r=[pak, "rk"], w=[f"t1_{i}"], out=t1[i][:], in0=pa[0:64, :], in1=rk[:, 0, :], op=ALU.mult)
            P.I("act", "copy", r=[pak], w=["CVv"], out=CV[64:128, 16:528], in_=pa[64:128, :])
            pb, pbk = fmproj(wka, "wka", 128, 64)
            P.I("dve", "tensor_tensor", r=[pbk, "rk"], w=[f"t2_{i}"], out=t2[i][:], in0=pb[0:64, :], in1=rk[:, 1, :], op=ALU.mult)
            P.I("pool", "tensor_tensor", r=[f"t1_{i}", f"t2_{i}"], w=["CVk"], out=CV[0:64, 16:528], in0=t1[i][:], in1=t2[i][:], op=ALU.add)
            pgt, pgk = fmproj(wg, "wg", 0, 12)
            P.I("act", "activation", r=[pgk], w=["gT"], out=gT[:], in_=pgt[0:12, :], func=AF.Sigmoid)
            for t4 in range(4):
                ch = blk * 4 + t4
                i = cnt["pp"] % 2; cnt["pp"] += 1
                for m in range(8):
                    P.MM(pp[i][:, 0:128], xnb[:, m, t4 * 128:(t4 + 1) * 128], wtok[:, m, :], start=(m == 0), stop=(m == 7),
                         r=["wtok", "xnb"], w=[f"pp{i}"])
                P.I("act", "copy", r=[f"pp{i}"], w=["Vs"], out=Vs[:, ch, 0:64], in_=pp[i][:, 0:64])
                P.I("act", "copy", r=[f"pp{i}"], w=["Vw"], out=Vw[:, ch % 8, 0:64], in_=pp[i][:, 64:128])
            CVv = CV[:].rearrange("p (c s) -> p c s", s=16)
            i = cnt["pp"] % 2; cnt["pp"] += 1
            for l in range(32):
                P.MM(pp[i][:, 0:32], w1s[0:64, l, :], CVv[0:64, l // 16:l // 16 + 32, l % 16], start=(l == 0), stop=(l == 31),
                     r=["w1s", "CVk"], w=[f"pp{i}"])
            P.I("act", "activation", r=[f"pp{i}", "hb"], w=["hidk"], out=hidk[:], in_=pp[i][:, 0:32], func=AF.Gelu_apprx_tanh, bias=hb[:, 0:1])
            i2 = cnt["pp"] % 2; cnt["pp"] += 1
            P.MM(pp[i2][0:64, 0:32], w2s[:, 0, :], hidk[:], r=["w2s", "hidk"], w=[f"pp{i2}"])
            P.I("dve", "tensor_copy", r=[f"pp{i2}"], w=["KcT"], out=KcT[0:64, 32 * blk:32 * blk + 32], in_=pp[i2][0:64, 0:32])
            i = cnt["pp"] % 2; cnt["pp"] += 1
            for l in range(32):
                P.MM(pp[i][:, 0:32], w1s[64:128, l, :], CVv[64:128, l // 16:l // 16 + 32, l % 16], start=(l == 0), stop=(l == 31),
                     r=["w1s", "CVv"], w=[f"pp{i}"])
            off = (32 * blk) % 128
            P.I("act", "activation", r=[f"pp{i}", "hb"], w=["hvp"], out=hvp[:, off:off + 32], in_=pp[i][:, 0:32], func=AF.Gelu_apprx_tanh, bias=hb[:, 1:2])
            i2 = cnt["pp"] % 2; cnt["pp"] += 1
            P.MM(pp[i2][:, 0:64], hvp[:], w2s[:, 1, :], r=["w2s", "hvp"], w=[f"pp{i2}"])
            P.I("dve", "tensor_copy", r=[f"pp{i2}"], w=["Vc"], out=Vc[off:off + 32, (32 * blk) // 128, 0:64], in_=pp[i2][off:off + 32, 0:64])
            P.I("act", "copy", r=["CVk", "CVv"], w=["CVk", "CVv"], out=CV[:, 0:16], in_=CV[:, 512:528])

            for qi in range(4):
                QB = blk * 4 + qi
                t0 = 128 * QB
                Qb = Qd[:, qi, :, :].rearrange("p r q -> p (r q)")

                def combine(br, ot, otk, normalize):
                    ai = cnt["acc"] % 2
                    for r in range(4):
                        P.MM(AUX[0:64, r * 128:(r + 1) * 128], Sel[:, br * 4 + r, :], gT[:, qi * 128:(qi + 1) * 128], r=["Sel", "gT"], w=["AUX"])
                    P.I("act", "copy", r=["AUX"], w=["gbs"], out=gbs[:], in_=AUX[0:64, :])
                    if normalize:
                        P.I("dve", "tensor_scalar", r=[otk], w=["zrow"], out=zrow[64:65, :], in0=ot[64:65, :], scalar1=1e-30, scalar2=None, op0=ALU.max)
                        P.I("dve", "reciprocal", r=["zrow"], w=["zrow"], out=zrow[64:65, :], in_=zrow[64:65, :])
                        P.MM(AUX[0:64, :], ones_f[64:65, 0:64], zrow[64:65, :], r=["ones_f", "zrow"], w=["AUX"])
                        P.I("dve", "tensor_tensor", r=["gbs", "AUX"], w=["gbs"], out=gbs[:], in0=gbs[:], in1=AUX[0:64, :], op=ALU.mult)
                    if br == 0:
                        P.I("dve", "tensor_tensor", r=[otk, "gbs"], w=[f"acc{ai}"], out=acc[ai][:], in0=ot[0:64, :], in1=gbs[:], op=ALU.mult)
                    else:
                        P.I("dve", "tensor_tensor", r=[otk, "gbs"], w=["tmp"], out=tmp[:], in0=ot[0:64, :], in1=gbs[:], op=ALU.mult)
                        P.I("pool", "tensor_tensor", r=["tmp", f"acc{ai}"], w=[f"acc{ai}"], out=acc[ai][:], in0=acc[ai][:], in1=tmp[:], op=ALU.add)

                qa = QB % 2
                ng = QB // 32 + 1
                P.I("pool", "tensor_copy", r=["Qd"], w=[f"Qaug{qa}q"], out=Qaug[qa][0:64, 0:ng, :],
                    in_=Qb.unsqueeze(1).to_broadcast([64, ng, 512]))
                Qfull = Qaug[qa][:, 0, :]
                Qr = [f"Qaug{qa}q", f"Qaug{qa}m"]
                jmax = (t0 + 112) // 2048
                nj = jmax + 1
                for j in range(nj):
                    si = cnt["pt"] % 2; cnt["pt"] += 1
                    P.MM(ST[si][:], KcT[:, j * 128:(j + 1) * 128], Qfull, r=["KcT"] + Qr, w=[f"ST{si}"])
                    P.I("act", "activation", r=[f"ST{si}"], w=[f"PTc{j}"], out=PTc[:, j, :], in_=ST[si][:], func=AF.Exp)
                    delta = t0 - 2048 * j - 15
                    if j == 0 or delta < 2032:
                        P.I("dve", "scalar_tensor_tensor", r=["D16", f"PTc{j}"], w=[f"PTc{j}"], out=PTc[:, j, :].rearrange("p (r q) -> p r q", r=4),
                            in0=D16[:, 1 if j == 0 else 0, :].unsqueeze(1).to_broadcast([128, 4, 128]), scalar=float(delta),
                            in1=PTc[:, j, :].rearrange("p (r q) -> p r q", r=4), op0=ALU.is_le, op1=ALU.mult)
                    P.MM(AUX[0:1, :], ones_b[:, 0:1], PTc[:, j, :], start=(j == 0), stop=(j == jmax), r=["ones_b", f"PTc{j}"], w=["AUX"])
                P.I("dve", "tensor_scalar", r=["AUX"], w=["zc"], out=zc[:], in0=AUX[0:1, :], scalar1=1e-30, scalar2=None, op0=ALU.max)
                P.I("dve", "reciprocal", r=["zc"], w=["zc"], out=zc[:], in_=zc[:])
                P.MM(AUX[:], ones_f[0:1, :], zc[0:1, :], r=["ones_f", "zc"], w=["AUX"])
                pk_all = [f"PTc{j}" for j in range(nj)]
                P.I("dve", "tensor_tensor", r=pk_all + ["AUX"], w=pk_all, out=PTc[:, 0:nj, :], in0=PTc[:, 0:nj, :],
                    in1=AUX[:].unsqueeze(1).to_broadcast([128, nj, 512]), op=ALU.mult)
                oi = cnt["oa"] % 2; cnt["oa"] += 1
                for j in range(nj):
                    P.MM(OA[oi][:], Vc[:, j, :], PTc[:, j, :], start=(j == 0), stop=(j == jmax), r=["Vc", f"PTc{j}"], w=[f"OA{oi}"])
                for j in range(nj):
                    for r in range(4):
                        P.MM(IMP[:, 0:256], PTc[:, j, r * 128:(r + 1) * 128], slcm[:, j, :], start=(j == 0 and r == 0),
                             stop=(j == jmax and r == 3), r=["slcm", f"PTc{j}"], w=["IMP"])
                combine(0, OA[oi], f"OA{oi}", False)
                jb = 2 * QB
                P.I("dve", "tensor_tensor", r=["IMP", "pats"], w=["impS"], out=impS[:], in0=IMP[:, 0:256], in1=pats[:, 0, 256 - jb:512 - jb], op=ALU.mult)
                P.I("dve", "tensor_tensor", r=["impS", "pats"], w=["impS"], out=impS[:], in0=impS[:], in1=pats[:, 1, 256 - jb:512 - jb], op=ALU.add)
                P.I("dve", "memset", r=["impS"], w=["impS"], ap=impS[:, 0:1], constant=10002.0)
                P.I("dve", "max", r=["impS"], w=["m8"], out=m8[:, 0:8], in_=impS[:])
                P.I("dve", "match_replace", r=["impS", "m8"], w=["scr"], out=scr[:], in_to_replace=m8[:, 0:8], in_values=impS[:], imm_value=-2.0)
                P.I("dve", "max", r=["scr"], w=["m8"], out=m8[:, 8:16], in_=scr[:])
                P.I("dve", "tensor_scalar", r=["impS", "m8"], w=["NT"], out=NT[:], in0=impS[:], scalar1=m8[:, 15:16], scalar2=1.0,
                    op0=ALU.is_ge, op1=ALU.subtract)
                for jt in range(2):
                    P.op("pe", (lambda jt: (lambda e: e.transpose(out=IMP[:, jt * 128:(jt + 1) * 128], in_=NT[:, jt * 128:(jt + 1) * 128], identity=identf[:])))(jt),
                         reads=["NT", "identf"], writes=["IMP"])
                for g_ in range(ng):
                    half = g_ % 2
                    P.I("act", "copy", r=["IMP"], w=[f"Qaug{qa}m"], out=Qaug[qa][64:128, g_, :].rearrange("p (r q) -> p r q", r=4),
                        in_=IMP[64 * half:64 * half + 64, (g_ // 2) * 128:(g_ // 2 + 1) * 128].unsqueeze(1).to_broadcast([64, 4, 128]))

                for br in (1, 2):
                    kcs = list(range(0, QB + 1)) if br == 1 else list(range(max(0, QB - 4), QB + 1))
                    oi = cnt["oa"] % 2; cnt["oa"] += 1
                    ot = OA[oi]; otk = f"OA{oi}"
                    base = cnt["pt"]

                    def qk(n, br=br, kcs=kcs, base=base, qa=qa, Qfull=Qfull, Qr=Qr):
                        kc = kcs[n]; si = (base + n) % 2
                        if br == 1:
                            P.MM(ST[si][:], KsT[:, kc * 128:(kc + 1) * 128], Qaug[qa][:, kc // 32, :],
                                 r=["KsT", "KsE", f"Qaug{qa}q", f"Qaug{qa}m"], w=[f"ST{si}"])
                        else:
                            P.MM(ST[si][:], KwT[:, kc % 8, :], Qfull, r=["KwT"] + Qr, w=[f"ST{si}"])

                    qk(0)
                    for n, kc in enumerate(kcs):
                        if n + 1 < len(kcs):
                            qk(n + 1)
                        si = (base + n) % 2
                        pi = cnt["pt"] % 3; cnt["pt"] += 1
                        pt = PT[pi]; ptk = f"PT{pi}"
                        P.I("act", "activation", r=[f"ST{si}"], w=[ptk], out=pt[:], in_=ST[si][:], func=AF.Exp)
                        if kc == QB:
                            P.I("dve", "tensor_tensor", r=[ptk, "tril"], w=[ptk], out=pt[:].rearrange("p (r q) -> p r q", r=4),
                                in0=pt[:].rearrange("p (r q) -> p r q", r=4), in1=tril[:, 0, :].unsqueeze(1).to_broadcast([128, 4, 128]), op=ALU.mult)
                        if br == 2 and kc == QB - 4:
                            P.I("dve", "tensor_tensor", r=[ptk, "tril"], w=[ptk], out=pt[:].rearrange("p (r q) -> p r q", r=4),
                                in0=pt[:].rearrange("p (r q) -> p r q", r=4), in1=tril[:, 1, :].unsqueeze(1).to_broadcast([128, 4, 128]), op=ALU.mult)
                        vv = Vs[:, kc, :] if br == 1 else Vw[:, kc % 8, :]
                        P.MM(ot[:], vv, pt[:], start=(n == 0), stop=(n == len(kcs) - 1), r=["Vs" if br == 1 else "Vw", ptk], w=[otk])
                    combine(br, ot, otk, True)
                ai = cnt["acc"] % 2; cnt["acc"] += 1
                P.D("sp", out=oT_dst(t0, 128).rearrange("(r x) q -> x r q", x=64), in_=acc[ai][:].rearrange("p (r q) -> p r q", r=4),
                    r=[f"acc{ai}"], w=[f"oT{QB}"])
        P.wait_all("sp", [f"oT{q}" for q in range(NBLK * 4)])
        P.barrier()
        P.emit()


EPS = 1e-6
NEG = -1.0e30


def declare_b(nc, T, pfx=""):
    d = {}

    def inp(name, shape, dt=F32):
        d[name] = nc.dram_tensor(pfx + name, shape, dt, kind="ExternalInput").ap()

    def outp(name, shape, dt=F32):
        d[name] = nc.dram_tensor(pfx + name, shape, dt, kind="ExternalOutput").ap()

    def scr(name, shape, dt=F32):
        d[name] = nc.dram_tensor(pfx + name, shape, dt, kind="Internal").ap()

    inp("hT", [1024, T]); inp("oT", [1024, T]); inp("wout", [1024, 1024]); inp("gffn", [1024])
    inp("wq", [1024, 2048]); inp("keysT", [16, 128, 128]); inp("uT", [1024, 16384]); inp("v", [16384, 1024])
    inp("gnext", [1024])
    outp("hT_out", [1024, T]); outp("nT_out", [1024, T])
    scr("h2T", [1024, T]); scr("hnbf", [1024, T], BF16); scr("qTd", [128, T // 128, 16, 128])
    scr("GT", [T // 128, 128, 16384], BF16); scr("uTbf", [1024, 16384], BF16); scr("vbf", [16384, 1024], BF16)
    return d


def emit_b(nc, P, T, d, cast_weights=True, pfx="", oT_blk=None, nT_dst=None):
    NB = T // 512
    NT = T // 128
    NB2 = T // 256
    fm = lambda ap: ap.rearrange("(m p) t -> p m t", p=128)

    if cast_weights:
        for i in range(8):
            P.D("pool", out=d["uTbf"][i * 128:(i + 1) * 128, :], in_=d["uT"][i * 128:(i + 1) * 128, :], w=[f"uTbf{i}"])
        for i in range(8):
            P.D("pool", out=d["vbf"][i * 2048:(i + 1) * 2048, :], in_=d["v"][i * 2048:(i + 1) * 2048, :], w=[f"vbf{i}"])

    with contextlib.ExitStack() as st:
        sb = lambda n, s, dt=F32: st.enter_context(nc.sbuf_tensor(pfx + "p0_" + n, s, dt))
        ps = lambda n, s, dt=F32: st.enter_context(nc.psum_tensor(pfx + "p0_" + n, s, dt))
        wout = sb("wout", [128, 8, 1024], BF16)
        g = sb("g", [128, 8]); ones = sb("ones", [128, 128])
        hTt = [sb(f"hTt{i}", [128, 8, 512]) for i in range(2)]
        oTt = [sb(f"oTt{i}", [128, 8, 512], BF16) for i in range(2)]
        h2 = sb("h2", [128, 8, 512]); hnb = sb("hnb", [128, 8, 512], BF16)
        rstd = sb("rstd", [128, 512])
        wqt = [sb(f"wqt{i}", [128, 8, 512]) for i in range(2)]
        qs = [sb(f"qs{i}", [128, 4, 512]) for i in range(2)]
        pp = [ps(f"pp{i}", [128, 512]) for i in range(2)]
        ss = ps("ss", [128, 512])

        P.D("pool", out=wout[:], in_=d["wout"].rearrange("(k p) c -> p k c", p=128), w=["wout"])
        P.D("sp", out=g[:], in_=d["gffn"].rearrange("(m p) -> p m", p=128), w=["g"], allow_slow_non_contiguous=True)
        P.I("pool", "memset", w=["ones"], ap=ones[:], constant=1.0)
        nmm = 0
        nwq = 0
        if oT_blk is not None:
            P.dma("sp", lambda e: e.dma_start(out=d["oTq"].rearrange("r (c t) -> c r t", t=min(1024, T)), in_=oT_blk()), writes=["oTq"])
        for b in range(NB):
            tsl = slice(b * 512, (b + 1) * 512)
            ht, ot = hTt[b % 2], oTt[b % 2]
            hk, ok = f"hTt{b % 2}", f"oTt{b % 2}"
            P.D("sp", out=ht[:], in_=fm(d["hT"])[:, :, tsl], w=[hk])
            if oT_blk is None:
                P.D("pool", out=ot[:], in_=fm(d["oT"])[:, :, tsl], w=[ok])
            else:
                P.D("pool", out=ot[:], in_=fm(d["oTq"])[:, :, tsl], r=["oTq"], w=[ok])
            for m in range(8):
                p_ = pp[nmm % 2]; pk = f"pp{nmm % 2}"; nmm += 1
                for k in range(8):
                    P.MM(p_[:], wout[:, k, m * 128:(m + 1) * 128], ot[:, k, :], start=(k == 0), stop=(k == 7),
                         r=["wout", ok], w=[pk])
                P.I("dve", "tensor_tensor", r=[pk, hk], w=["h2"], out=h2[:, m, :], in0=p_[:], in1=ht[:, m, :], op=ALU.add)
            P.D("sp", out=fm(d["h2T"])[:, :, tsl], in_=h2[:], r=["h2"], w=[f"h2T{b}"])
            P.I("act", "activation", r=["h2"], w=[hk], out=ht[:], in_=h2[:], func=AF.Square)
            for m in range(8):
                P.MM(ss[:], ones[:], ht[:, m, :], start=(m == 0), stop=(m == 7), r=["ones", hk], w=["ss"])
            P.I("act", "activation", r=["ss"], w=["rstd"], out=rstd[:], in_=ss[:], func=AF.Sqrt, scale=1.0 / 1024, bias=EPS)
            P.I("dve", "reciprocal", r=["rstd"], w=["rstd"], out=rstd[:], in_=rstd[:])
            for m in range(8):
                P.I("dve", "scalar_tensor_tensor", r=["h2", "g", "rstd"], w=["h2"], out=h2[:, m, :], in0=h2[:, m, :],
                    scalar=g[:, m:m + 1], in1=rstd[:], op0=ALU.mult, op1=ALU.mult)
            P.I("pool", "tensor_copy", r=["h2"], w=["hnb"], out=hnb[:], in_=h2[:])
            P.D("sp", out=fm(d["hnbf"])[:, :, tsl], in_=hnb[:], r=["hnb"], w=[f"hnbf{b}"])
            for jg in range(4):
                wt = wqt[nwq % 2]; wk = f"wqt{nwq % 2}"; q_ = qs[nwq % 2]; qk = f"qs{nwq % 2}"; nwq += 1
                P.D("sp", out=wt[:], in_=d["wq"].rearrange("(m p) c -> p m c", p=128)[:, :, jg * 512:(jg + 1) * 512], w=[wk])
                for jj in range(4):
                    p_ = pp[nmm % 2]; pk = f"pp{nmm % 2}"; nmm += 1
                    for m in range(8):
                        P.MM(p_[:], wt[:, m, jj * 128:(jj + 1) * 128], h2[:, m, :], start=(m == 0), stop=(m == 7),
                             r=[wk, "h2"], w=[pk])
                    P.I("act", "copy", r=[pk], w=[qk], out=q_[:, jj, :], in_=p_[:])
                for t4 in range(4):
                    P.D("sp", out=d["qTd"][:, b * 4 + t4, jg * 4:(jg + 1) * 4, :], in_=q_[:, :, t4 * 128:(t4 + 1) * 128],
                        r=[qk], w=[f"qTd{b}_{jg}_{t4}"])
        P.barrier()
        P.emit()

    with contextlib.ExitStack() as st:
        sb = lambda n, s, dt=F32: st.enter_context(nc.sbuf_tensor(pfx + "p1_" + n, s, dt))
        ps = lambda n, s, dt=F32: st.enter_context(nc.psum_tensor(pfx + "p1_" + n, s, dt))
        keys = sb("keys", [128, 16, 128]); ident = sb("ident", [128, 128], BF16); identf = sb("identf", [128, 128])
        qt = [sb(f"qt{i}", [128, 16, 128]) for i in range(2)]
        S12 = [sb(f"S12{i}", [128, 16, 128]) for i in range(2)]
        scr_ = sb("scr", [128, 256]); TS = sb("TS", [128, 16, 16]); cand = sb("cand", [128, 8, 256]); BS = sb("BS", [128, 8, 16])
        ex = sb("ex", [128, 8, 16]); Z = sb("Z", [128, 8]); lnZ = sb("lnZ", [128, 8]); bias = sb("bias", [128, 8])
        SUM = [sb(f"SUM{i}", [128, 8, 128]) for i in range(6)]
        E = [sb(f"E{i}", [128, 1024], BF16) for i in range(3)]
        GH = [[sb(f"GH{i}_{h}", [128, 1024], BF16) for h in range(8)] for i in range(2)]
        GTp = [sb(f"GTp{i}", [128, 8, 128], BF16) for i in range(4)]
        sc = ps("sc", [128, 16, 128])
        acc = [ps(f"acc{i}", [128, 4, 128]) for i in range(2)]

        P.D("sp", out=keys[:], in_=d["keysT"].rearrange("j c k -> c j k"), w=["keys"])
        P.I("pool", "memset", w=["identf"], ap=identf[:], constant=1.0)
        P.I("pool", "affine_select", r=["identf"], w=["identf"], out=identf[:], in_=identf[:], pattern=[[-1, 128]],
            compare_op=ALU.is_equal, fill=0.0, base=0, channel_multiplier=1)
        P.I("pool", "tensor_copy", r=["identf"], w=["ident"], out=ident[:], in_=identf[:])
        nsum = 0; nacc = 0; ngtp = 0; ngh = 0
        for tt in range(NT):
            q_ = qt[tt % 2]; qk = f"qt{tt % 2}"; S = S12[tt % 2]; Sk = f"S12{tt % 2}"
            P.D("sp", out=q_[:], in_=d["qTd"][:, tt, :, :], w=[qk])
            for j in range(16):
                P.MM(sc[:, j, :], q_[:, j, :], keys[:, j, :], r=[qk, "keys"], w=["sc"])
            P.I("act", "copy", r=["sc"], w=[Sk + "a"], out=S[:, 0:8, :], in_=sc[:, 0:8, :])
            P.I("dve", "tensor_copy", r=["sc"], w=[Sk + "b"], out=S[:, 8:16, :], in_=sc[:, 8:16, :])
            Sr = [Sk + "a", Sk + "b"]
            for j in range(16):
                P.I("dve", "max", r=Sr, w=["TS"], out=TS[:, j, 0:8], in_=S[:, j, :])
                P.I("dve", "match_replace", r=Sr + ["TS"], w=["scr"], out=scr_[:, 0:128], in_to_replace=TS[:, j, 0:8],
                    in_values=S[:, j, :], imm_value=NEG)
                P.I("dve", "max", r=["scr"], w=["TS"], out=TS[:, j, 8:16], in_=scr_[:, 0:128])
            TS4 = TS[:].rearrange("p (h two) a -> p h two a", two=2)
            P.I("dve", "tensor_tensor", r=["TS"], w=["cand"], out=cand[:].rearrange("p h (a b) -> p h a b", b=16),
                in0=TS4[:, :, 0, :].unsqueeze(3).to_broadcast([128, 8, 16, 16]),
                in1=TS4[:, :, 1, :].unsqueeze(2).to_broadcast([128, 8, 16, 16]), op=ALU.add)
            for h in range(8):
                P.I("dve", "max", r=["cand"], w=["BS"], out=BS[:, h, 0:8], in_=cand[:, h, :])
                P.I("dve", "match_replace", r=["cand", "BS"], w=["scr"], out=scr_[:, 0:256], in_to_replace=BS[:, h, 0:8],
                    in_values=cand[:, h, :], imm_value=NEG)
                P.I("dve", "max", r=["scr"], w=["BS"], out=BS[:, h, 8:16], in_=scr_[:, 0:256])
            P.I("dve", "tensor_tensor", r=["BS"], w=["ex"], out=ex[:], in0=BS[:], in1=BS[:, :, 0:1].to_broadcast([128, 8, 16]),
                op=ALU.subtract)
            P.I("act", "activation", r=["ex"], w=["ex"], out=ex[:], in_=ex[:], func=AF.Exp)
            P.I("dve", "reduce_sum", r=["ex"], w=["Z"], out=Z[:], in_=ex[:], axis=AX.X)
            P.I("act", "activation", r=["Z"], w=["lnZ"], out=lnZ[:], in_=Z[:], func=AF.Ln)
            P.I("dve", "scalar_tensor_tensor", r=["BS", "lnZ"], w=["bias"], out=bias[:], in0=BS[:, :, 0], scalar=-1.0,
                in1=lnZ[:], op0=ALU.mult, op1=ALU.subtract)
            def add_op(it):
                sx_, h_ = it // 8, it % 8
                su = SUM[it % 6]; sk = f"SUM{it % 6}"
                if False:
                    for a in range(8):
                        P.I("act", "activation", r=Sr, w=[sk], out=su[:, a, :], in_=S[:, 2 * h_ + 1, :], func=AF.Identity,
                            bias=S[:, 2 * h_, sx_ * 8 + a:sx_ * 8 + a + 1], scale=1.0)
                else:
                    P.I("dve", "tensor_tensor", r=Sr, w=[sk], out=su[:],
                        in0=S[:, 2 * h_, sx_ * 8:(sx_ + 1) * 8].unsqueeze(2).to_broadcast([128, 8, 128]),
                        in1=S[:, 2 * h_ + 1, :].unsqueeze(1).to_broadcast([128, 8, 128]), op=ALU.add)

            LOOK = 4
            deferred = []
            for it0 in range(LOOK):
                add_op(it0)
            for sx in range(16):
                ghs = GH[ngh % 2]; gk = f"GH{ngh % 2}_"; ngh += 1
                for h in range(8):
                    it = sx * 8 + h
                    if it + LOOK < 128:
                        add_op(it + LOOK)
                    if h == 4 and deferred:
                        deferred.pop(0)()
                    su = SUM[it % 6]; sk = f"SUM{it % 6}"; e_ = E[it % 3]; ek = f"E{it % 3}"
                    P.I("act", "activation", r=[sk, "bias"], w=[ek], out=e_[:], in_=su[:].rearrange("p a b -> p (a b)"),
                        func=AF.Exp, bias=bias[:, h:h + 1])
                    P.I("dve", "scalar_tensor_tensor", r=[sk, ek, "BS"], w=[gk + str(h)], out=ghs[h][:],
                        in0=su[:].rearrange("p a b -> p (a b)"), scalar=BS[:, h, 15:16], in1=e_[:], op0=ALU.is_ge, op1=ALU.mult)
                def flush(sx=sx, tt=tt, ghs=ghs, gk=gk):
                    nonlocal nacc, ngtp
                    gp = GTp[ngtp % 4]; gpk = f"GTp{ngtp % 4}"; ngtp += 1
                    for c4 in range(2):
                        a_ = acc[nacc % 2]; ak = f"acc{nacc % 2}"; nacc += 1
                        for ci in range(4):
                            c = c4 * 4 + ci
                            for h in range(8):
                                P.MM(a_[:, ci, :], ghs[h][:, c * 128:(c + 1) * 128], ident[:], start=(h == 0), stop=(h == 7),
                                     r=[gk + str(h), "ident"], w=[ak])
                        P.I("act", "copy", r=[ak], w=[gpk], out=gp[:, c4 * 4:(c4 + 1) * 4, :], in_=a_[:])
                    P.D("sp", out=d["GT"][tt, :, sx * 1024:(sx + 1) * 1024], in_=gp[:].rearrange("p c t -> p (c t)"), r=[gpk],
                        w=[f"GT{tt}_{sx}"])
                deferred.append(flush)
            while deferred:
                deferred.pop(0)()
        P.barrier()
        P.emit()

    with contextlib.ExitStack() as st:
        sb = lambda n, s, dt=F32: st.enter_context(nc.sbuf_tensor(pfx + "p2_" + n, s, dt))
        ps = lambda n, s, dt=F32: st.enter_context(nc.psum_tensor(pfx + "p2_" + n, s, dt))
        U = [sb(f"U{i}", [128, 8, 1024], BF16) for i in range(2)]
        V = [sb(f"V{i}", [128, 8, 1024], BF16) for i in range(2)]
        Gg = [sb(f"Gg{i}", [128, 2, 8, 128], BF16) for i in range(2)]
        hn2 = [sb(f"hn2{i}", [128, 8, 256], BF16) for i in range(2)]
        ge = [sb(f"ge{i}", [128, 2, 256], BF16) for i in range(2)]
        gh2 = [sb(f"gh2{i}", [128, 2, 256], BF16) for i in range(2)]
        h2b = sb("h2b", [128, 8, 256]); h3 = sb("h3", [128, 8, 256]); sq2 = sb("sq2", [128, 8, 256])
        rstd2 = sb("rstd2", [128, 256]); nrm = sb("nrm", [128, 8, 256]); gn = sb("gn", [128, 8]); ones2 = sb("ones2", [128, 128])
        Y = ps("Y", [128, 8, 256])
        Hp = [ps(f"Hp{i}", [128, 2, 256]) for i in range(2)]
        ss2 = ps("ss2", [128, 256])
        P.D("sp", out=gn[:], in_=d["gnext"].rearrange("(m p) -> p m", p=128), w=["gn"], allow_slow_non_contiguous=True)
        P.I("pool", "memset", w=["ones2"], ap=ones2[:], constant=1.0)
        zl = sb("zl", [128, 128], BF16); zr = sb("zr", [128, 512], BF16)
        P.I("pool", "memset", w=["zl"], ap=zl[:], constant=0.0)
        P.I("pool", "memset", w=["zr"], ap=zr[:], constant=0.0)
        Yb = Y[:].rearrange("p (b two) t -> p b (two t)", two=2)
        nw = 0; npair = 0
        for blk in range(NB2):
            tsl = slice(blk * 256, (blk + 1) * 256)
            hb = hn2[blk % 2]; hbk = f"hn2{blk % 2}"
            P.D("sp", out=hb[:], in_=fm(d["hnbf"])[:, :, tsl], w=[hbk])
            P.D("sp", out=h2b[:], in_=fm(d["h2T"])[:, :, tsl], w=["h2b"])
            for bk in range(4):
                P.MM(Yb[:, bk, :], zl[:], zr[:], start=True, stop=False, r=["zl", "zr"], w=["Y"])
            for eg in range(16):
                u_ = U[nw % 2]; uk = f"U{nw % 2}"; v_ = V[nw % 2]; vk = f"V{nw % 2}"; g_ = Gg[nw % 2]; gk = f"Gg{nw % 2}"; nw += 1
                P.D("sp", out=u_[:], in_=d["uTbf"].rearrange("(m p) e -> p m e", p=128)[:, :, eg * 1024:(eg + 1) * 1024], w=[uk])
                P.D("act", out=v_[:], in_=d["vbf"].rearrange("(c p) x -> p c x", p=128)[:, eg * 8:(eg + 1) * 8, :], w=[vk])
                for t2 in range(2):
                    P.D("sp", out=g_[:, t2, :, :], in_=d["GT"][blk * 2 + t2, :, eg * 1024:(eg + 1) * 1024].rearrange("p (c t) -> p c t", t=128),
                        w=[gk + str(t2)])
                for pr in range(4):
                    hp = Hp[npair % 2]; hpk = f"Hp{npair % 2}"; ge_ = ge[npair % 2]; gek = f"ge{npair % 2}"
                    gh_ = gh2[npair % 2]; ghk = f"gh2{npair % 2}"; npair += 1
                    for cc in range(2):
                        c = pr * 2 + cc
                        for m in range(8):
                            P.MM(hp[:, cc, :], u_[:, m, c * 128:(c + 1) * 128], hb[:, m, :], start=(m == 0), stop=(m == 7),
                                 r=[uk, hbk], w=[hpk])
                    P.I("act", "activation", r=[hpk], w=[gek], out=ge_[:], in_=hp[:], func=AF.Gelu_apprx_tanh)
                    P.I("dve", "tensor_tensor", r=[gek, gk + "0", gk + "1"], w=[ghk],
                        out=gh_[:].rearrange("p c (tt t) -> p c tt t", t=128), in0=ge_[:].rearrange("p c (tt t) -> p c tt t", t=128),
                        in1=g_[:, :, pr * 2:pr * 2 + 2, :].rearrange("p tt c t -> p c tt t"), op=ALU.mult)
                    for cc in range(2):
                        c = pr * 2 + cc
                        for m in range(8):
                            P.MM(Y[:, m, :], v_[:, c, m * 128:(m + 1) * 128], gh_[:, cc, :], start=False,
                                 stop=(eg == 15 and c == 7), r=[vk, ghk], w=["Y"])
            for m in range(8):
                P.I("dve", "tensor_tensor", r=["Y", "h2b"], w=["h3"], out=h3[:, m, :], in0=Y[:, m, :], in1=h2b[:, m, :], op=ALU.add)
            P.D("sp", out=fm(d["hT_out"])[:, :, tsl], in_=h3[:], r=["h3"], w=[f"hT_out{blk}"])
            P.I("act", "activation", r=["h3"], w=["sq2"], out=sq2[:], in_=h3[:], func=AF.Square)
            for m in range(8):
                P.MM(ss2[:], ones2[:], sq2[:, m, :], start=(m == 0), stop=(m == 7), r=["ones2", "sq2"], w=["ss2"])
            P.I("act", "activation", r=["ss2"], w=["rstd2"], out=rstd2[:], in_=ss2[:], func=AF.Sqrt, scale=1.0 / 1024, bias=EPS)
            P.I("dve", "reciprocal", r=["rstd2"], w=["rstd2"], out=rstd2[:], in_=rstd2[:])
            for m in range(8):
                P.I("dve", "scalar_tensor_tensor", r=["h3", "gn", "rstd2"], w=["nrm"], out=nrm[:, m, :], in0=h3[:, m, :],
                    scalar=gn[:, m:m + 1], in1=rstd2[:], op0=ALU.mult, op1=ALU.mult)
            P.D("sp", out=(fm(d["nT_out"])[:, :, tsl] if nT_dst is None else nT_dst(blk)), in_=nrm[:], r=["nrm"], w=[f"nT_out{blk}"])
        P.wait_all("sp", [f"nT_out{b}" for b in range(NB2)] + [f"hT_out{b}" for b in range(NB2)])
        P.barrier()
        P.emit()


def declare_c(nc, S, pfx="", fused=False):
    d = {}

    def t(name, shape, kind, dt=F32):
        d[name] = nc.dram_tensor(pfx + name, shape, dt, kind=kind).ap()

    if not fused:
        t("nT", [1024, S], "ExternalInput")
    t("wq", [1024, 256], "ExternalInput"); t("wk", [1024, 256], "ExternalInput"); t("wv", [1024, 256], "ExternalInput")
    t("wf", [1024, 4], "ExternalInput"); t("fb", [4], "ExternalInput")
    t("maskd", [128, 4, 512], "ExternalInput")
    t("oT", [256, S], "Internal" if fused else "ExternalOutput")
    return d


def emit_c(nc, P, S, d, nT_loads=None, oT_dst=None):
    if oT_dst is None:
        oT_dst = lambda t0, n: d["oT"][:, t0:t0 + n]
    NBLK = S // 512
    NCH = S // 128
    with contextlib.ExitStack() as st:
        sb = lambda n, s, dt=F32: st.enter_context(nc.sbuf_tensor("c_" + n, s, dt))
        ps = lambda n, s, dt=F32: st.enter_context(nc.psum_tensor("c_" + n, s, dt))
        KT = sb("KT", [128, 2, S], BF16)
        Vr = sb("Vr", [128, NCH, 4 * 65 + 63], BF16)
        Cr = sb("Cr", [128, NCH, 4]); nbias = sb("nbias", [128, 4, NCH])
        nb = [sb(f"nb{i}", [128, 8, 512], BF16) for i in range(2)]
        wq = sb("wq", [128, 8, 256], BF16); wk = sb("wk", [128, 8, 256], BF16); wv = sb("wv", [128, 8, 256], BF16)
        wf = sb("wf", [128, 8, 4], BF16); fb = sb("fb", [128, 4])
        QT = [sb(f"QT{i}", [128, 512], BF16) for i in range(4)]
        PT = [sb(f"PT{i}", [128, 512], BF16) for i in range(3)]
        maskd = sb("maskd", [128, 4, 512], BF16)
        tri = sb("tri", [128, 128]); ones = sb("ones", [128, 128])
        lf = sb("lf", [128, 4]); tot = sb("tot", [128, 4]); totmid = sb("totmid", [128, 4])
        zrow = sb("zrow", [65, 512]); ocp = sb("ocp", [64, 512]); osb = [sb(f"osb{i}", [64, 512]) for i in range(2)]
        pp = [ps(f"pp{i}", [128, 512]) for i in range(2)]
        ST = [ps(f"ST{i}", [128, 512]) for i in range(2)]
        OT = [ps(f"OT{i}", [128, 512]) for i in range(2)]
        ZB = ps("ZB", [64, 512]); cs = ps("cs", [128, 8])

        for nm, tl in (("wq", wq), ("wk", wk), ("wv", wv)):
            P.D("pool", out=tl[:], in_=d[nm].rearrange("(m p) c -> p m c", p=128), w=[nm])
        P.D("pool", out=wf[:], in_=d["wf"].rearrange("(m p) c -> p m c", p=128), w=["wf"])
        P.D("sp", out=fb[:], in_=d["fb"].partition_broadcast(128), w=["fb"])
        P.D("pool", out=maskd[:], in_=d["maskd"], w=["maskd"])
        P.I("pool", "memset", w=["ones"], ap=ones[:], constant=1.0)
        P.I("pool", "memset", w=["tri"], ap=tri[:], constant=1.0)
        P.I("pool", "affine_select", r=["tri"], w=["tri"], out=tri[:], in_=tri[:], pattern=[[1, 128]],
            compare_op=ALU.is_ge, fill=0.0, base=0, channel_multiplier=-1)
        P.I("pool", "memset", w=["tot"], ap=tot[:], constant=0.0)
        P.I("pool", "memset", w=["Vr"], ap=Vr[:], constant=0.0)
        P.I("pool", "memset", r=["Vr"], w=["Vr"], ap=Vr[:, :, 0:260].rearrange("p c (h x) -> p c h x", x=65)[:, :, :, 64:65], constant=1.0)
        for i in range(4):
            P.I("pool", "memset", w=[f"QT{i}"], ap=QT[i][:], constant=0.0)
        npp = 0; nst = 0; npt = 0; nhead = 0
        for blk in range(NBLK):
            tsl = slice(blk * 512, (blk + 1) * 512)
            n_ = nb[blk % 2]; nk = f"nb{blk % 2}"
            if nT_loads is None:
                P.D("pool", out=n_[:], in_=d["nT"].rearrange("(m p) t -> p m t", p=128)[:, :, tsl], w=[nk])
            else:
                for csl, src in nT_loads(blk):
                    P.D("pool", out=n_[:, :, csl], in_=src, w=[nk])
            for pair in range(2):
                p_ = pp[npp % 2]; pk = f"pp{npp % 2}"; npp += 1
                for m in range(8):
                    P.MM(p_[:], wq[:, m, pair * 128:(pair + 1) * 128], n_[:, m, :], start=(m == 0), stop=(m == 7), r=["wq", nk], w=[pk])
                for hh in range(2):
                    rs = slice(hh * 64, hh * 64 + 64)
                    P.I("act", "mul", r=[pk], w=[f"QT{2 * pair + hh}"], out=QT[2 * pair + hh][rs, :], in_=p_[rs, :], mul=0.125)
                p_ = pp[npp % 2]; pk = f"pp{npp % 2}"; npp += 1
                for m in range(8):
                    P.MM(p_[:], wk[:, m, pair * 128:(pair + 1) * 128], n_[:, m, :], start=(m == 0), stop=(m == 7), r=["wk", nk], w=[pk])
                P.I("dve", "tensor_copy", r=[pk], w=["KT"], out=KT[:, pair, tsl], in_=p_[:])
            for t4 in range(4):
                ch = blk * 4 + t4
                p_ = pp[npp % 2]; pk = f"pp{npp % 2}"; npp += 1
                for m in range(8):
                    P.MM(p_[:, 0:256], n_[:, m, t4 * 128:(t4 + 1) * 128], wv[:, m, :], start=(m == 0), stop=(m == 7), r=["wv", nk], w=[pk])
                P.I("act", "copy", r=[pk], w=["Vr"], out=Vr[:, ch, 0:260].rearrange("p (h x) -> p h x", x=65)[:, :, 0:64], in_=p_[:, 0:256].rearrange("p (h x) -> p h x", x=64))
                for m in range(8):
                    P.MM(cs[:, 0:4], n_[:, m, t4 * 128:(t4 + 1) * 128], wf[:, m, :], start=(m == 0), stop=(m == 7), r=["wf", nk], w=["cs"])
                P.I("dve", "tensor_tensor", r=["cs", "fb"], w=["lf"], out=lf[:], in0=cs[:, 0:4], in1=fb[:], op=ALU.add)
                P.I("act", "activation", r=["lf"], w=["lf"], out=lf[:], in_=lf[:], func=AF.Exp, scale=-1.0)
                P.I("act", "activation", r=["lf"], w=["lf"], out=lf[:], in_=lf[:], func=AF.Ln, bias=1.0)
                P.I("dve", "tensor_scalar", r=["lf"], w=["lf"], out=lf[:], in0=lf[:], scalar1=-1.0, scalar2=None, op0=ALU.mult)
                P.MM(cs[:, 0:4], tri[:], lf[:], r=["tri", "lf"], w=["cs"])
                P.MM(cs[:, 4:8], ones[:], lf[:], r=["ones", "lf"], w=["cs"])
                P.I("dve", "tensor_tensor", r=["cs", "tot"], w=["Cr"], out=Cr[:, ch, :], in0=cs[:, 0:4], in1=tot[:], op=ALU.add)
                P.I("dve", "tensor_tensor", r=["cs", "tot"], w=["tot"], out=tot[:], in0=cs[:, 4:8], in1=tot[:], op=ALU.add)
                if t4 == 1:
                    P.I("dve", "tensor_copy", r=["tot"], w=["totmid"], out=totmid[:], in_=tot[:])
            nch = blk * 4 + 4
            for hl in range(4):
                P.I("dve", "tensor_scalar", r=["Cr", "totmid"], w=["nbias"], out=nbias[:, hl, 0:nch], in0=Cr[:, 0:nch, hl],
                    scalar1=totmid[:, hl:hl + 1], scalar2=-1.0, op0=ALU.subtract, op1=ALU.mult)
            pairs = [(hl, kc) for hl in range(4) for kc in range(nch)]

            def qk(i):
                nonlocal nst
                hl, kc = pairs[i]
                s_ = ST[i % 2]
                P.MM(s_[:], KT[:, hl // 2, kc * 128:(kc + 1) * 128], QT[hl][:], r=["KT", f"QT{hl}"], w=[f"ST{i % 2}"])

            qk(0)
            for i, (hl, kc) in enumerate(pairs):
                if i + 1 < len(pairs):
                    qk(i + 1)
                s_ = ST[i % 2]; sk = f"ST{i % 2}"
                pt = PT[npt % 3]; ptk = f"PT{npt % 3}"; npt += 1
                P.I("act", "activation", r=[sk, "nbias"], w=[ptk], out=pt[:], in_=s_[:], func=AF.Exp, bias=nbias[:, hl, kc:kc + 1])
                if kc >= blk * 4:
                    P.I("dve", "tensor_tensor", r=[ptk, "maskd"], w=[ptk], out=pt[:], in0=pt[:], in1=maskd[:, kc - blk * 4, :], op=ALU.mult)
                if kc == 0:
                    ot = OT[nhead % 2]; otk = f"OT{nhead % 2}"; ob = osb[nhead % 2]; obk = f"osb{nhead % 2}"; nhead += 1
                P.MM(ot[:], Vr[:, kc, hl * 65:hl * 65 + 128], pt[:], start=(kc == 0), stop=(kc == nch - 1), r=["Vr", ptk], w=[otk])
                if kc == nch - 1:
                    P.I("dve", "tensor_scalar", r=[otk], w=["zrow"], out=zrow[64:65, :], in0=ot[64:65, :], scalar1=1e-30, scalar2=None, op0=ALU.max)
                    P.I("dve", "reciprocal", r=["zrow"], w=["zrow"], out=zrow[64:65, :], in_=zrow[64:65, :])
                    P.MM(ZB[:], ones[64:65, 0:64], zrow[64:65, :], r=["ones", "zrow"], w=["ZB"])
                    P.I("act", "copy", r=[otk], w=["ocp"], out=ocp[:], in_=ot[0:64, :])
                    P.I("dve", "tensor_tensor", r=["ocp", "ZB"], w=[obk], out=ob[:], in0=ocp[:], in1=ZB[:], op=ALU.mult)
                    P.D("sp", out=oT_dst(blk * 512, 512)[hl * 64:(hl + 1) * 64, :], in_=ob[:], r=[obk], w=[f"oT{blk}_{hl}"])
        P.wait_all("sp", [f"oT{b}_{h}" for b in range(NBLK) for h in range(4)])
        P.barrier()
        P.emit()


S_FULL = 16384
T_CORE = 4096
G4 = [[0, 1, 2, 3], [4, 5, 6, 7]]
_PROG = {}
_B_IN = (("wout", [1024, 1024]), ("gffn", [1024]), ("wq", [1024, 2048]), ("keysT", [16, 128, 128]), ("uT", [1024, 16384]),
         ("v", [16384, 1024]), ("gnext", [1024]))


def _declare_b_fused(nc, T, pfx, hT_ap, out_kind):
    d = {}
    for name, shape in _B_IN:
        d[name] = nc.dram_tensor(pfx + name, shape, F32, kind="ExternalInput").ap()
    d["hT"] = hT_ap
    d["hT_out"] = nc.dram_tensor(pfx + "hT_out", [1024, T], F32, kind="Internal").ap()
    d["nT_out"] = nc.dram_tensor(pfx + "nT_out", [1024, T], F32, kind=out_kind).ap()
    scr = lambda name, shape, dt=F32: nc.dram_tensor(pfx + name, shape, dt, kind="Internal").ap()
    d["h2T"] = scr("h2T", [1024, T]); d["hnbf"] = scr("hnbf", [1024, T], BF16); d["qTd"] = scr("qTd", [128, T // 128, 16, 128])
    d["oTq"] = scr("oTq", [1024, T])
    d["GT"] = scr("GT", [T // 128, 128, 16384], BF16); d["uTbf"] = scr("uTbf", [1024, 16384], BF16); d["vbf"] = scr("vbf", [16384, 1024], BF16)
    return d


def _build_fused(S=S_FULL, T=T_CORE):
    if (S, T) in _PROG:
        return _PROG[(S, T)]
    nc = bass.Bass("TRN2", target_bir_lowering=False)
    with contextlib.ExitStack() as st:
        P = Prog(nc)
        P.alloc_sems(st)
        da = declare_a(nc, S, pfx="A_", fused=True)
        xTs = nc.dram_tensor("B0_xTs", [1024, T], F32, kind="ExternalInput").ap()
        db0 = _declare_b_fused(nc, T, "B0_", xTs, "Internal")
        dc = declare_c(nc, S, pfx="C_", fused=True)
        db1 = _declare_b_fused(nc, T, "B1_", db0["hT_out"], "ExternalOutput")
        CW = min(1024, T)
        NC1 = S // CW
        CW2 = 256
        NC2 = T // CW2
        dt_ = lambda name, shape: nc.dram_tensor(name, shape, F32, kind="Internal").ap()
        x1_in = dt_("x1_in", [NC1, 256, CW]); x1_out = dt_("x1_out", [NC1, 1024, CW])
        x2_in = dt_("x2_in", [NC2, 1024, CW2]); x2_out = dt_("x2_out", [NC2, 4096, CW2])
        x3_in = dt_("x3_in", [NC1, 256, CW]); x3_out = dt_("x3_out", [NC1, 1024, CW])

        def gather(src, dst, n, name):
            for j in range(n):
                P.cc((lambda j: (lambda e: e.collective_compute("AllGather", ALU.bypass, replica_groups=G4, ins=[src[j]], outs=[dst[j]])))(j),
                     writes=[name])
            P.barrier()

        def chunked_dst(buf):
            return lambda t0, n: buf[t0 // CW, :, (t0 % CW):(t0 % CW) + n]

        def quarter_src(buf):
            def f():
                q = nc.partition_id() % 4
                return buf[bass.ds(q * (T // CW), T // CW), :, :]
            return f

        bpr = T // 512

        def nT_loads(blk):
            rank, lb = blk // bpr, blk % bpr
            return [(slice(h * CW2, (h + 1) * CW2),
                     x2_out[lb * 2 + h, rank * 1024:(rank + 1) * 1024, :].rearrange("(m p) t -> p m t", p=128)) for h in range(2)]

        emit_a(nc, P, S, da, oT_dst=chunked_dst(x1_in))
        gather(x1_in, x1_out, NC1, "x1")
        emit_b(nc, P, T, db0, pfx="B0_", oT_blk=quarter_src(x1_out),
               nT_dst=lambda blk: x2_in[blk].rearrange("(m p) t -> p m t", p=128))
        gather(x2_in, x2_out, NC2, "x2")
        emit_c(nc, P, S, dc, nT_loads=nT_loads, oT_dst=chunked_dst(x3_in))
        gather(x3_in, x3_out, NC1, "x3")
        emit_b(nc, P, T, db1, pfx="B1_", oT_blk=quarter_src(x3_out))
    _PROG[(S, T)] = nc
    return nc


def _peer_inputs(pfx, wout, gffn, wq, keys, u, v, gnext):
    f32 = lambda a: np.ascontiguousarray(np.asarray(a, dtype=np.float32))
    return {pfx + "wout": f32(wout), pfx + "gffn": f32(gffn), pfx + "wq": f32(wq),
            pfx + "keysT": np.ascontiguousarray(f32(keys).transpose(0, 1, 3, 2).reshape(16, 128, 128)),
            pfx + "uT": np.ascontiguousarray(f32(u).T), pfx + "v": f32(v), pfx + "gnext": f32(gnext)}


def kernel(x, l0_attn_norm, l0_w_in, l0_cmp_pe_k, l0_cmp_w1_k, l0_cmp_w2_k, l0_cmp_pe_v, l0_cmp_w1_v, l0_cmp_w2_v, l0_w_out,
           l0_ffn_norm, l0_peer_wq, l0_peer_keys, l0_peer_u, l0_peer_v,
           l1_attn_norm, l1_w_in, l1_f_bias, l1_w_out,
           l1_ffn_norm, l1_peer_wq, l1_peer_keys, l1_peer_u, l1_peer_v,
           final_norm):
    f32 = lambda a: np.ascontiguousarray(np.asarray(a, dtype=np.float32))
    x = f32(x)
    B, S, D = x.shape
    T_CORE = S // 4
    nc = _build_fused(S, T_CORE)
    consts = consts_a(); ropeq, ropek = rope_tables(S)
    args0 = [f32(a) for a in (l0_attn_norm, l0_w_in, l0_cmp_pe_k, l0_cmp_w1_k, l0_cmp_w2_k, l0_cmp_pe_v, l0_cmp_w1_v, l0_cmp_w2_v)]
    pb0 = _peer_inputs("B0_", l0_w_out, l0_ffn_norm, l0_peer_wq, l0_peer_keys, l0_peer_u, l0_peer_v, l1_attn_norm)
    pb1 = _peer_inputs("B1_", l1_w_out, l1_ffn_norm, l1_peer_wq, l1_peer_keys, l1_peer_u, l1_peer_v, final_norm)
    w1 = f32(l1_w_in); fbias = f32(l1_f_bias)
    kk = np.arange(128)[:, None, None]; ii = np.arange(4)[None, :, None]; qq = np.arange(512)[None, None, :]
    maskd = (kk <= qq - 128 * ii).astype(np.float32)
    xT = [np.ascontiguousarray(x[b].T) for b in range(B)]
    maps = []
    for c in range(8):
        b, q = c // 4, c % 4
        m = {"A_" + k: v for k, v in host_inputs_a(x[b], *args0, q, consts, ropeq, ropek).items()}
        m["A_xT"] = xT[b]
        m["B0_xTs"] = np.ascontiguousarray(xT[b][:, q * T_CORE:(q + 1) * T_CORE])
        m.update(pb0); m.update(pb1)
        h0 = 4 * q
        m.update({"C_wq": np.ascontiguousarray(w1[:, h0 * 64:(h0 + 4) * 64]),
                  "C_wk": np.ascontiguousarray(w1[:, 1024 + h0 * 64:1024 + (h0 + 4) * 64]),
                  "C_wv": np.ascontiguousarray(w1[:, 2048 + h0 * 64:2048 + (h0 + 4) * 64]),
                  "C_wf": np.ascontiguousarray(w1[:, 3072 + h0:3072 + h0 + 4]), "C_fb": fbias[h0:h0 + 4].copy(), "C_maskd": maskd})
        maps.append(m)
    res = run_bass_kernel_spmd(nc, maps, core_ids=list(range(8))).results
    out = np.empty((B, S, D), np.float32)
    for c in range(8):
        b, q = c // 4, c % 4
        out[b, q * T_CORE:(q + 1) * T_CORE, :] = res[c]["B1_nT_out"].T
    return out
```

```python
import contextlib
import numpy as np
import concourse.bass as bass
import concourse.mybir as mybir
from concourse.bass_utils import run_bass_kernel_spmd

F32 = mybir.dt.float32
BF16 = mybir.dt.bfloat16
AF = mybir.ActivationFunctionType
ALU = mybir.AluOpType
AX = mybir.AxisListType

N_DMA_SEMS = 8


class Prog:
    COMPUTE = ("pe", "dve", "act", "pool")
    QUEUES = ("sp", "act", "pool")

    def __init__(self, nc):
        self.nc = nc
        self.ops = {e: [] for e in ("pe", "dve", "act", "pool", "sp")}
        self.cnt = {}
        self.last_w = {}
        self.readers = {}
        self.waited = {e: {} for e in self.ops}
        self.dma_n = {q: 0 for q in self.QUEUES}
        self.semkeys = []
        for e in self.COMPUTE:
            self._mk(("c", e))
        for q in self.QUEUES:
            for i in range(N_DMA_SEMS):
                self._mk(("d", q, i))
        self._mk(("cc",))
        self.sems = {}
        self.pending = {e: False for e in self.ops}
        self.lazy_pe_inc = False

    def _mk(self, k):
        self.cnt[k] = 0
        self.semkeys.append(k)

    def _need(self, eng, tok, waits):
        if tok is None:
            return
        k, v = tok
        if k == ("c", eng) and eng == "pe":
            return
        if self.waited[eng].get(k, 0) >= v:
            return
        self.waited[eng][k] = v
        waits.append((k, v))

    def _deps(self, eng, reads, writes):
        waits = []
        for b in reads:
            self._need(eng, self.last_w.get(b), waits)
        for b in writes:
            self._need(eng, self.last_w.get(b), waits)
            for t in self.readers.get(b, {}).items():
                if t[0] == ("c", eng):
                    continue
                self._need(eng, t, waits)
        return waits

    def _commit(self, tok, reads, writes):
        for b in reads:
            self.readers.setdefault(b, {})[tok[0]] = tok[1]
        for b in writes:
            self.last_w[b] = tok
            self.readers[b] = {}

    def op(self, eng, fn, reads=(), writes=(), inc=True):
        waits = self._deps(eng, reads, writes)
        k = ("c", eng)
        if inc:
            self.cnt[k] += 1
            tok = (k, self.cnt[k])
            self.pending[eng] = False
        else:
            tok = (k, self.cnt[k] + 1)
            self.pending[eng] = True
        self._commit(tok, reads, writes)
        self.ops[eng].append((waits, fn, (k, 1) if inc else None))
        return tok

    def dma(self, q, fn, reads=(), writes=()):
        waits = self._deps(q, reads, writes)
        n = self.dma_n[q]
        self.dma_n[q] += 1
        k = ("d", q, n % N_DMA_SEMS)
        self._need(q, (k, self.cnt[k]) if self.cnt[k] else None, waits)
        self.cnt[k] += 16
        tok = (k, self.cnt[k])
        self._commit(tok, reads, writes)
        self.ops[q].append((waits, fn, (k, 16)))
        return tok

    def cc(self, fn, reads=(), writes=()):
        waits = self._deps("pool", reads, writes)
        k = ("cc",)
        self._need("pool", (k, self.cnt[k]) if self.cnt[k] else None, waits)
        self.cnt[k] += 1
        tok = (k, self.cnt[k])
        self._commit(tok, reads, writes)
        self.ops["pool"].append((waits, fn, (k, 1)))
        return tok

    def I(self, eng, method, r=(), w=(), **kw):
        return self.op(eng, lambda e: getattr(e, method)(**kw), reads=r, writes=w)

    def MM(self, out, lhsT, rhs, start=True, stop=True, r=(), w=()):
        return self.op("pe", lambda e: e.matmul(out, lhsT=lhsT, rhs=rhs, start=start, stop=stop), reads=r, writes=w,
                       inc=(stop or not self.lazy_pe_inc))

    def D(self, q, out, in_, r=(), w=(), **kw):
        return self.dma(q, lambda e: e.dma_start(out=out, in_=in_, **kw), reads=r, writes=w)

    def barrier(self):
        assert not any(self.pending.values()), self.pending
        for eng in self.ops:
            waits = []
            for k in self.semkeys:
                if self.cnt[k]:
                    self._need(eng, (k, self.cnt[k]), waits)
            self.ops[eng].append((waits, None, None))
        self.last_w = {}
        self.readers = {}

    def wait_all(self, eng, bufs):
        waits = []
        for b in bufs:
            self._need(eng, self.last_w.get(b), waits)
        self.ops[eng].append((waits, None, None))

    def alloc_sems(self, st):
        for k in self.semkeys:
            self.sems[k] = st.enter_context(self.nc.semaphore("s_" + "_".join(map(str, k))))

    def emit(self):
        nc = self.nc
        import contextlib
        with contextlib.ExitStack() as st:
            if not self.sems:
                self.alloc_sems(st)
            block = st.enter_context(nc.Block())
            engobj = {"pe": "tensor", "dve": "vector", "act": "scalar", "pool": "gpsimd", "sp": "sync"}

            def mk(ename):
                ops = self.ops[ename]

                def body(eng):
                    for waits, fn, inc in ops:
                        for (k, v) in waits:
                            eng.wait_ge(self.sems[k], v)
                        if fn is not None:
                            ins = fn(eng)
                            if inc is not None:
                                ins.then_inc(self.sems[inc[0]], inc[1])
                return body

            for ename, attr in engobj.items():
                if self.ops[ename]:
                    getattr(block, attr)(mk(ename))
        self.ops = {e: [] for e in self.ops}


EPS = 1e-6


def consts_a():
    c = {}
    q = np.arange(128)[:, None]; rel = np.arange(512)[None, :] - 256
    cur = (q >= 64).astype(np.int64)
    M = (rel <= cur - 2).astype(np.float32)
    A = np.where(rel == cur, 10000.0, np.where(rel == cur - 1, 10001.0, np.where(rel > cur, -1.0, 0.0))).astype(np.float32)
    c["pats"] = np.stack([M, A], 1)
    ci = np.arange(128)[:, None]; qi = np.arange(128)[None, :]
    D = (16 * ci - qi).astype(np.float32)
    Dz = D.copy(); Dz[0, :] = 1e9
    c["D16"] = np.stack([D, Dz], 1)
    c["tril"] = np.stack([(ci <= qi), (ci > qi)], 1).astype(np.float32)
    c["Sel"] = np.broadcast_to(np.eye(12, dtype=np.float32)[:, :, None], (12, 12, 64)).copy()
    cc = np.arange(1024)[:, None] - 1; jj = np.arange(256)[None, :]
    lo = np.maximum(cc * 16, jj * 64); hi = np.minimum(cc * 16 + 32, (jj + 1) * 64)
    m = np.maximum(hi - lo, 0).astype(np.float32) / 32.0
    m[0, :] = 0.0
    c["slcm"] = np.ascontiguousarray(m.reshape(8, 128, 256).transpose(1, 0, 2))
    return c


def epat_table(S):
    n = np.arange(S)[None, :]; r = np.arange(64)[:, None]
    return (30000.0 * (((n // 64) % 64) == r)).astype(np.float32)


def rope_tables(S):
    half = 32
    inv = (10000.0 ** (-np.arange(half, dtype=np.float32) / half)).astype(np.float32)
    ang = (np.arange(S, dtype=np.float32)[None, :] * inv[:, None]).astype(np.float32)
    cos = np.cos(ang).astype(np.float32); sin = np.sin(ang).astype(np.float32)
    cosf = np.concatenate([cos, cos], 0); sinf = np.concatenate([-sin, sin], 0)
    rk = np.stack([cosf, sinf], 1)
    return np.ascontiguousarray(rk * 0.125), np.ascontiguousarray(rk)


def host_inputs_a(xb, gattn, w_in, pe_k, w1_k, w2_k, pe_v, w1_v, w2_v, g, consts, ropeq, ropek):
    def sw(w):
        w = w.reshape(w.shape[0], -1, 64)
        return np.concatenate([w[..., 32:], w[..., :32]], -1).reshape(w.shape[0], -1)
    kv = lambda i: w_in[:, 1024 + i * 256 + g * 64:1024 + i * 256 + (g + 1) * 64]
    wq = w_in[:, g * 256:(g + 1) * 256]
    d = dict(consts)
    d["xT"] = np.ascontiguousarray(xb.T)
    d["gattn"] = gattn
    d["wqa"] = np.ascontiguousarray(np.concatenate([wq, sw(wq)], 1))
    d["wka"] = np.ascontiguousarray(np.concatenate([kv(0), kv(1), sw(kv(0)), kv(2), sw(kv(2)), kv(4), sw(kv(4))], 1))
    d["wtok"] = np.ascontiguousarray(np.concatenate([kv(3), kv(5)], 1))
    gc = [2560 + br * 16 + g * 4 + r for br in range(3) for r in range(4)]
    d["wg"] = np.ascontiguousarray(w_in[:, gc])
    d["w1s"] = np.ascontiguousarray(np.concatenate([w1_k[g].transpose(1, 0, 2), w1_v[g].transpose(1, 0, 2)], 0))
    d["peT"] = np.ascontiguousarray(np.concatenate([pe_k[g].T, pe_v[g].T], 0))
    d["w2s"] = np.ascontiguousarray(np.stack([w2_k[g], w2_v[g]], 1))
    d["ropeq"] = ropeq; d["ropek"] = ropek
    d["Epat"] = epat_table(xb.shape[0])
    return d


def declare_a(nc, S, pfx="", fused=False):
    d = {}

    def t(name, shape, kind="ExternalInput", dt=F32):
        d[name] = nc.dram_tensor(pfx + name, shape, dt, kind=kind).ap()

    t("xT", [1024, S]); t("gattn", [1024]); t("wqa", [1024, 512]); t("wka", [1024, 448]); t("wtok", [1024, 128]); t("wg", [1024, 12])
    t("w1s", [128, 32, 128]); t("peT", [128, 32]); t("w2s", [128, 2, 64]); t("ropeq", [64, 2, S]); t("ropek", [64, 2, S])
    t("pats", [128, 2, 512]); t("D16", [128, 2, 128]); t("tril", [128, 2, 128]); t("Epat", [64, S]); t("Sel", [12, 12, 64])
    t("slcm", [128, 8, 256])
    t("oT", [256, S], kind="Internal" if fused else "ExternalOutput")
    return d


def emit_a(nc, P, S, d, oT_dst=None):
    if oT_dst is None:
        oT_dst = lambda t0, n: d["oT"][:, t0:t0 + n]
    NBLK = S // 512
    NCH = S // 128
    with contextlib.ExitStack() as st:
        sb = lambda n, s, dt=F32: st.enter_context(nc.sbuf_tensor("a_" + n, s, dt))
        ps = lambda n, s, dt=F32: st.enter_context(nc.psum_tensor("a_" + n, s, dt))
        KsT = sb("KsT", [128, S], BF16); Vs = sb("Vs", [128, NCH, 128], BF16)
        KwT = sb("KwT", [128, 8, 128], BF16); Vw = sb("Vw", [128, 8, 128], BF16)
        KcT = sb("KcT", [128, 1024], BF16); Vc = sb("Vc", [128, 8, 128], BF16)
        slcm = sb("slcm", [128, 8, 256], BF16)
        Qaug = [sb(f"Qaug{i}", [128, 4, 512], BF16) for i in range(2)]
        pats = sb("pats", [128, 2, 512]); D16 = sb("D16", [128, 2, 128]); tril = sb("tril", [128, 2, 128], BF16)
        Sel = sb("Sel", [12, 12, 64])
        wqa = sb("wqa", [128, 8, 512], BF16); wka = sb("wka", [128, 8, 448], BF16); wtok = sb("wtok", [128, 8, 128], BF16)
        wg = sb("wg", [128, 8, 12], BF16)
        w1s = sb("w1s", [128, 32, 128], BF16); peT = sb("peT", [128, 32], BF16); w2s = sb("w2s", [128, 2, 64], BF16)
        hb = sb("hb", [128, 2]); g = sb("g", [128, 8])
        ones_b = sb("ones_b", [128, 128], BF16); ones_f = sb("ones_f", [128, 128]); identf = sb("identf", [128, 128])
        xT = sb("xT", [128, 8, 512]); sq = sb("sq", [128, 8, 512], BF16); rstd = sb("rstd", [128, 512]); xnb = sb("xnb", [128, 8, 512], BF16)
        rq = sb("rq", [64, 2, 512]); rk = sb("rk", [64, 2, 512])
        t1 = [sb(f"t1_{i}", [64, 512]) for i in range(2)]; t2 = [sb(f"t2_{i}", [64, 512]) for i in range(2)]
        Qd = sb("Qd", [64, 4, 4, 128], BF16)
        CV = sb("CV", [128, 528], BF16); hidk = sb("hidk", [128, 32], BF16); hvp = sb("hvp", [128, 128], BF16)
        gT = sb("gT", [12, 512])
        PTc = sb("PTc", [128, 8, 512], BF16); PT = [sb(f"PT{i}", [128, 512], BF16) for i in range(4)]
        zc = sb("zc", [1, 512]); impS = sb("impS", [128, 256]); scr = sb("scr", [128, 256]); m8 = sb("m8", [128, 16])
        NT = sb("NT", [128, 256])
        zrow = sb("zrow", [65, 512]); gbs = sb("gbs", [64, 512]); acc = [sb(f"acc{i}", [64, 512]) for i in range(2)]
        tmp = sb("tmp", [64, 512])
        pp0 = ps("pp0", [128, 512])
        ST = [ps(f"ST{i}", [128, 512]) for i in range(3)]
        OA = [ps(f"OA{i}", [128, 512]) for i in range(2)]
        IMP = ps("IMP", [128, 512]); AUX = ps("AUX", [128, 512])
        pp = [pp0, IMP]; ppk = ["pp0", "IMP"]

        for nm, tl in (("wqa", wqa), ("wka", wka), ("wtok", wtok), ("wg", wg)):
            P.D("pool", out=tl[:], in_=d[nm].rearrange("(m p) c -> p m c", p=128), w=[nm])
        for nm, tl in (("w1s", w1s), ("peT", peT), ("w2s", w2s), ("slcm", slcm), ("tril", tril)):
            P.D("pool", out=tl[:], in_=d[nm], w=[nm])
        for nm, tl in (("pats", pats), ("D16", D16), ("Sel", Sel)):
            P.D("sp", out=tl[:], in_=d[nm], w=[nm])
        P.D("pool", out=KsT[64:128, :], in_=d["Epat"], w=["KsE"])
        P.D("sp", out=g[:], in_=d["gattn"].rearrange("(m p) -> p m", p=128), w=["g"], allow_slow_non_contiguous=True)
        P.I("pool", "memset", w=["ones_b"], ap=ones_b[:], constant=1.0)
        P.I("pool", "memset", w=["ones_f"], ap=ones_f[:], constant=1.0)
        P.I("pool", "memset", w=["identf"], ap=identf[:], constant=1.0)
        P.I("pool", "affine_select", r=["identf"], w=["identf"], out=identf[:], in_=identf[:], pattern=[[-1, 128]],
            compare_op=ALU.is_equal, fill=0.0, base=0, channel_multiplier=1)
        P.I("pool", "memset", w=["Vs"], ap=Vs[:], constant=0.0)
        P.I("pool", "memset", r=["Vs"], w=["Vs"], ap=Vs[:, :, 64:65], constant=1.0)
        P.I("pool", "memset", w=["Vw"], ap=Vw[:], constant=0.0)
        P.I("pool", "memset", r=["Vw"], w=["Vw"], ap=Vw[:, :, 64:65], constant=1.0)
        P.I("pool", "memset", w=["KwT"], ap=KwT[:], constant=0.0)
        for i in range(2):
            P.I("pool", "memset", w=[f"Qaug{i}q", f"Qaug{i}m"], ap=Qaug[i][:], constant=0.0)
        P.I("pool", "memset", w=["KcT"], ap=KcT[:], constant=0.0)
        P.I("pool", "memset", w=["Vc"], ap=Vc[:], constant=0.0)
        P.I("pool", "memset", w=["CVk", "CVv"], ap=CV[:], constant=0.0)
        P.I("pool", "memset", w=["hvp"], ap=hvp[:], constant=0.0)
        for kvi in range(2):
            rows = slice(kvi * 64, kvi * 64 + 64)
            for l in range(32):
                P.MM(pp[0][:, kvi:kvi + 1], w1s[rows, l, :], peT[rows, l:l + 1], start=(l == 0), stop=(l == 31), r=["w1s", "peT"], w=["pp0"])
        P.I("dve", "tensor_copy", r=["pp0"], w=["hb"], out=hb[:], in_=pp[0][:, 0:2])

        cnt = {"pp": 0, "t": 0, "pt": 0, "oa": 0, "acc": 0}

        def proj(c0, M):
            i = cnt["pp"] % 2; cnt["pp"] += 1
            return pp[i], ppk[i]

        for blk in range(NBLK):
            tsl = slice(blk * 512, (blk + 1) * 512)
            P.D("sp", out=xT[:], in_=d["xT"].rearrange("(m p) t -> p m t", p=128)[:, :, tsl], w=["xT"])
            P.D("sp", out=rq[:], in_=d["ropeq"][:, :, tsl], w=["rq"])
            P.D("sp", out=rk[:], in_=d["ropek"][:, :, tsl], w=["rk"])
            P.I("act", "activation", r=["xT"], w=["sq"], out=sq[:], in_=xT[:], func=AF.Square)
            for m in range(8):
                P.MM(AUX[:], ones_b[:], sq[:, m, :], start=(m == 0), stop=(m == 7), r=["ones_b", "sq"], w=["AUX"])
            P.I("act", "activation", r=["AUX"], w=["rstd"], out=rstd[:], in_=AUX[:], func=AF.Sqrt, scale=1.0 / 1024, bias=EPS)
            P.I("dve", "reciprocal", r=["rstd"], w=["rstd"], out=rstd[:], in_=rstd[:])
            for m in range(8):
                P.I("dve", "scalar_tensor_tensor", r=["xT", "g", "rstd"], w=["xnb"], out=xnb[:, m, :], in0=xT[:, m, :],
                    scalar=g[:, m:m + 1], in1=rstd[:], op0=ALU.mult, op1=ALU.mult)

            def fmproj(wt, wk_, c0, M):
                p_, pk = proj(c0, M)
                for m in range(8):
                    P.MM(p_[0:M, :], wt[:, m, c0:c0 + M], xnb[:, m, :], start=(m == 0), stop=(m == 7), r=[wk_, "xnb"], w=[pk])
                return p_, pk

            def rope(wt, wk_, ca, cb, tab, tabk, out_ap, outk):
                pa, pak = fmproj(wt, wk_, ca, 64)
                i = cnt["t"] % 2; cnt["t"] += 1
                P.I("dve", "tensor_tensor", r=[pak, tabk], w=[f"t1_{i}"], out=t1[i][:], in0=pa[0:64, :], in1=tab[:, 0, :], op=ALU.mult)
                pb, pbk = fmproj(wt, wk_, cb, 64)
                P.I("dve", "tensor_tensor", r=[pbk, tabk], w=[f"t2_{i}"], out=t2[i][:], in0=pb[0:64, :], in1=tab[:, 1, :], op=ALU.mult)
                a_, b_ = t1[i][:], t2[i][:]
                if len(out_ap.shape) == 3:
                    a_ = a_.rearrange("p (a q) -> p a q", a=4); b_ = b_.rearrange("p (a q) -> p a q", a=4)
                P.I("pool", "tensor_tensor", r=[f"t1_{i}", f"t2_{i}"], w=[outk], out=out_ap, in0=a_, in1=b_, op=ALU.add)

            for r in range(4):
                rope(wqa, "wqa", r * 64, 256 + r * 64, rq, "rq", Qd[:, :, r, :], "Qd")
            rope(wka, "wka", 192, 256, rk, "rk", KsT[0:64, tsl], "KsT")
            rope(wka, "wka", 320, 384, rk, "rk", KwT[0:64, (blk % 2) * 4:(blk % 2) * 4 + 4, :], "KwT")
            pa, pak = fmproj(wka, "wka", 0, 128)
            i = cnt["t"] % 2; cnt["t"] += 1
            P.I("dve", "tensor_tensor", r=[pak, "rk"], w=[f"t1_{i}"], out=t1[i][:], in0=pa[0:64, :], in1=rk[:, 0, :], op=ALU.mult)
            P.I("act", "copy", r=[pak], w=["CVv"], out=CV[64:128, 16:528], in_=pa[64:128, :])
            pb, pbk = fmproj(wka, "wka", 128, 64)
            P.I("dve", "tensor_tensor", r=[pbk, "rk"], w=[f"t2_{i}"], out=t2[i][:], in0=pb[0:64, :], in1=rk[:, 1, :], op=ALU.mult)
            P.I("pool", "tensor_tensor", r=[f"t1_{i}", f"t2_{i}"], w=["CVk"], out=CV[0:64, 16:528], in0=t1[i][:], in1=t2[i][:], op=ALU.add)
            pgt, pgk = fmproj(wg, "wg", 0, 12)
            P.I("act", "activation", r=[pgk], w=["gT"], out=gT[:], in_=pgt[0:12, :], func=AF.Sigmoid)
            for t4 in range(4):
                ch = blk * 4 + t4
                i = cnt["pp"] % 2; cnt["pp"] += 1
                for m in range(8):
                    P.MM(pp[i][:, 0:128], xnb[:, m, t4 * 128:(t4 + 1) * 128], wtok[:, m, :], start=(m == 0), stop=(m == 7),
                         r=["wtok", "xnb"], w=[ppk[i]])
                P.I("act", "copy", r=[ppk[i]], w=["Vs"], out=Vs[:, ch, 0:64], in_=pp[i][:, 0:64])
                P.I("act", "copy", r=[ppk[i]], w=["Vw"], out=Vw[:, ch % 8, 0:64], in_=pp[i][:, 64:128])
            CVv = CV[:].rearrange("p (c s) -> p c s", s=16)
            i = cnt["pp"] % 2; cnt["pp"] += 1
            for l in range(32):
                P.MM(pp[i][:, 0:32], w1s[0:64, l, :], CVv[0:64, l // 16:l // 16 + 32, l % 16], start=(l == 0), stop=(l == 31),
                     r=["w1s", "CVk"], w=[ppk[i]])
            P.I("act", "activation", r=[ppk[i], "hb"], w=["hidk"], out=hidk[:], in_=pp[i][:, 0:32], func=AF.Gelu_apprx_tanh, bias=hb[:, 0:1])
            i2 = cnt["pp"] % 2; cnt["pp"] += 1
            P.MM(pp[i2][0:64, 0:32], w2s[:, 0, :], hidk[:], r=["w2s", "hidk"], w=[ppk[i2]])
            P.I("dve", "tensor_copy", r=[ppk[i2]], w=["KcT"], out=KcT[0:64, 32 * blk:32 * blk + 32], in_=pp[i2][0:64, 0:32])
            i = cnt["pp"] % 2; cnt["pp"] += 1
            for l in range(32):
                P.MM(pp[i][:, 0:32], w1s[64:128, l, :], CVv[64:128, l // 16:l // 16 + 32, l % 16], start=(l == 0), stop=(l == 31),
                     r=["w1s", "CVv"], w=[ppk[i]])
            off = (32 * blk) % 128
            P.I("act", "activation", r=[ppk[i], "hb"], w=["hvp"], out=hvp[:, off:off + 32], in_=pp[i][:, 0:32], func=AF.Gelu_apprx_tanh, bias=hb[:, 1:2])
            i2 = cnt["pp"] % 2; cnt["pp"] += 1
            P.MM(pp[i2][:, 0:64], hvp[:], w2s[:, 1, :], r=["w2s", "hvp"], w=[ppk[i2]])
            P.I("dve", "tensor_copy", r=[ppk[i2]], w=["Vc"], out=Vc[off:off + 32, (32 * blk) // 128, 0:64], in_=pp[i2][off:off + 32, 0:64])
            P.I("act", "copy", r=["CVk", "CVv"], w=["CVk", "CVv"], out=CV[:, 0:16], in_=CV[:, 512:528])

            for qi in range(4):
                QB = blk * 4 + qi
                t0 = 128 * QB
                Qb = Qd[:, qi, :, :].rearrange("p r q -> p (r q)")

                def combine(br, ot, otk, normalize):
                    ai = cnt["acc"] % 2
                    for r in range(4):
                        P.MM(AUX[0:64, r * 128:(r + 1) * 128], Sel[:, br * 4 + r, :], gT[:, qi * 128:(qi + 1) * 128], r=["Sel", "gT"], w=["AUX"])
                    P.I("act", "copy", r=["AUX"], w=["gbs"], out=gbs[:], in_=AUX[0:64, :])
                    if normalize:
                        P.I("dve", "tensor_scalar", r=[otk], w=["zrow"], out=zrow[64:65, :], in0=ot[64:65, :], scalar1=1e-30, scalar2=None, op0=ALU.max)
                        P.I("dve", "reciprocal", r=["zrow"], w=["zrow"], out=zrow[64:65, :], in_=zrow[64:65, :])
                        P.MM(AUX[0:64, :], ones_f[64:65, 0:64], zrow[64:65, :], r=["ones_f", "zrow"], w=["AUX"])
                        P.I("dve", "tensor_tensor", r=["gbs", "AUX"], w=["gbs"], out=gbs[:], in0=gbs[:], in1=AUX[0:64, :], op=ALU.mult)
                    if br == 0:
                        P.I("dve", "tensor_tensor", r=[otk, "gbs"], w=[f"acc{ai}"], out=acc[ai][:], in0=ot[0:64, :], in1=gbs[:], op=ALU.mult)
                    else:
                        P.I("dve", "tensor_tensor", r=[otk, "gbs"], w=["tmp"], out=tmp[:], in0=ot[0:64, :], in1=gbs[:], op=ALU.mult)
                        P.I("pool", "tensor_tensor", r=["tmp", f"acc{ai}"], w=[f"acc{ai}"], out=acc[ai][:], in0=acc[ai][:], in1=tmp[:], op=ALU.add)

                qa = QB % 2
                ng = QB // 32 + 1
                P.I("pool", "tensor_copy", r=["Qd"], w=[f"Qaug{qa}q"], out=Qaug[qa][0:64, 0:ng, :],
                    in_=Qb.unsqueeze(1).to_broadcast([64, ng, 512]))
                Qfull = Qaug[qa][:, 0, :]
                Qr = [f"Qaug{qa}q", f"Qaug{qa}m"]
                jmax = (t0 + 112) // 2048
                nj = jmax + 1
                for j in range(nj):
                    si = cnt["pt"] % 3; cnt["pt"] += 1
                    P.MM(ST[si][:], KcT[:, j * 128:(j + 1) * 128], Qfull, r=["KcT"] + Qr, w=[f"ST{si}"])
                    P.I("act", "activation", r=[f"ST{si}"], w=[f"PTc{j}"], out=PTc[:, j, :], in_=ST[si][:], func=AF.Exp)
                    delta = t0 - 2048 * j - 15
                    if j == 0 or delta < 2032:
                        P.I("dve", "scalar_tensor_tensor", r=["D16", f"PTc{j}"], w=[f"PTc{j}"], out=PTc[:, j, :].rearrange("p (r q) -> p r q", r=4),
                            in0=D16[:, 1 if j == 0 else 0, :].unsqueeze(1).to_broadcast([128, 4, 128]), scalar=float(delta),
                            in1=PTc[:, j, :].rearrange("p (r q) -> p r q", r=4), op0=ALU.is_le, op1=ALU.mult)
                    P.MM(AUX[0:1, :], ones_b[:, 0:1], PTc[:, j, :], start=(j == 0), stop=(j == jmax), r=["ones_b", f"PTc{j}"], w=["AUX"])
                P.I("dve", "tensor_scalar", r=["AUX"], w=["zc"], out=zc[:], in0=AUX[0:1, :], scalar1=1e-30, scalar2=None, op0=ALU.max)
                P.I("dve", "reciprocal", r=["zc"], w=["zc"], out=zc[:], in_=zc[:])
                P.MM(AUX[:], ones_f[0:1, :], zc[0:1, :], r=["ones_f", "zc"], w=["AUX"])
                pk_all = [f"PTc{j}" for j in range(nj)]
                P.I("dve", "tensor_tensor", r=pk_all + ["AUX"], w=pk_all, out=PTc[:, 0:nj, :], in0=PTc[:, 0:nj, :],
                    in1=AUX[:].unsqueeze(1).to_broadcast([128, nj, 512]), op=ALU.mult)
                oi = cnt["oa"] % 2; cnt["oa"] += 1
                for j in range(nj):
                    P.MM(OA[oi][:], Vc[:, j, :], PTc[:, j, :], start=(j == 0), stop=(j == jmax), r=["Vc", f"PTc{j}"], w=[f"OA{oi}"])
                for j in range(nj):
                    for r in range(4):
                        P.MM(IMP[:, 0:256], PTc[:, j, r * 128:(r + 1) * 128], slcm[:, j, :], start=(j == 0 and r == 0),
                             stop=(j == jmax and r == 3), r=["slcm", f"PTc{j}"], w=["IMP"])
                combine(0, OA[oi], f"OA{oi}", False)
                jb = 2 * QB
                P.I("dve", "tensor_tensor", r=["IMP", "pats"], w=["impS"], out=impS[:], in0=IMP[:, 0:256], in1=pats[:, 0, 256 - jb:512 - jb], op=ALU.mult)
                P.I("dve", "tensor_tensor", r=["impS", "pats"], w=["impS"], out=impS[:], in0=impS[:], in1=pats[:, 1, 256 - jb:512 - jb], op=ALU.add)
                P.I("dve", "memset", r=["impS"], w=["impS"], ap=impS[:, 0:1], constant=10002.0)
                P.I("dve", "max", r=["impS"], w=["m8"], out=m8[:, 0:8], in_=impS[:])
                P.I("dve", "match_replace", r=["impS", "m8"], w=["scr"], out=scr[:], in_to_replace=m8[:, 0:8], in_values=impS[:], imm_value=-2.0)
                P.I("dve", "max", r=["scr"], w=["m8"], out=m8[:, 8:16], in_=scr[:])
                P.I("dve", "tensor_scalar", r=["impS", "m8"], w=["NT"], out=NT[:], in0=impS[:], scalar1=m8[:, 15:16], scalar2=1.0,
                    op0=ALU.is_ge, op1=ALU.subtract)
                for jt in range(2):
                    P.op("pe", (lambda jt: (lambda e: e.transpose(out=IMP[:, jt * 128:(jt + 1) * 128], in_=NT[:, jt * 128:(jt + 1) * 128], identity=identf[:])))(jt),
                         reads=["NT", "identf"], writes=["IMP"])
                for g_ in range(ng):
                    half = g_ % 2
                    P.I("act", "copy", r=["IMP"], w=[f"Qaug{qa}m"], out=Qaug[qa][64:128, g_, :].rearrange("p (r q) -> p r q", r=4),
                        in_=IMP[64 * half:64 * half + 64, (g_ // 2) * 128:(g_ // 2 + 1) * 128].unsqueeze(1).to_broadcast([64, 4, 128]))

                for br in (1, 2):
                    kcs = list(range(0, QB + 1)) if br == 1 else list(range(max(0, QB - 4), QB + 1))
                    oi = cnt["oa"] % 2; cnt["oa"] += 1
                    ot = OA[oi]; otk = f"OA{oi}"
                    base = cnt["pt"]

                    def qk(n, br=br, kcs=kcs, base=base, qa=qa, Qfull=Qfull, Qr=Qr):
                        kc = kcs[n]; si = (base + n) % 3
                        if br == 1:
                            P.MM(ST[si][:], KsT[:, kc * 128:(kc + 1) * 128], Qaug[qa][:, kc // 32, :],
                                 r=["KsT", "KsE", f"Qaug{qa}q", f"Qaug{qa}m"], w=[f"ST{si}"])
                        else:
                            P.MM(ST[si][:], KwT[:, kc % 8, :], Qfull, r=["KwT"] + Qr, w=[f"ST{si}"])

                    qk(0)
                    if len(kcs) > 1:
                        qk(1)
                    for n, kc in enumerate(kcs):
                        if n + 2 < len(kcs):
                            qk(n + 2)
                        si = (base + n) % 3
                        pi = cnt["pt"] % 4; cnt["pt"] += 1
                        pt = PT[pi]; ptk = f"PT{pi}"
                        P.I("act", "activation", r=[f"ST{si}"], w=[ptk], out=pt[:], in_=ST[si][:], func=AF.Exp)
                        if kc == QB:
                            P.I("dve", "tensor_tensor", r=[ptk, "tril"], w=[ptk], out=pt[:].rearrange("p (r q) -> p r q", r=4),
                                in0=pt[:].rearrange("p (r q) -> p r q", r=4), in1=tril[:, 0, :].unsqueeze(1).to_broadcast([128, 4, 128]), op=ALU.mult)
                        if br == 2 and kc == QB - 4:
                            P.I("dve", "tensor_tensor", r=[ptk, "tril"], w=[ptk], out=pt[:].rearrange("p (r q) -> p r q", r=4),
                                in0=pt[:].rearrange("p (r q) -> p r q", r=4), in1=tril[:, 1, :].unsqueeze(1).to_broadcast([128, 4, 128]), op=ALU.mult)
                        vv = Vs[:, kc, :] if br == 1 else Vw[:, kc % 8, :]
                        P.MM(ot[:], vv, pt[:], start=(n == 0), stop=(n == len(kcs) - 1), r=["Vs" if br == 1 else "Vw", ptk], w=[otk])
                    combine(br, ot, otk, True)
                ai = cnt["acc"] % 2; cnt["acc"] += 1
                P.D("sp", out=oT_dst(t0, 128).rearrange("(r x) q -> x r q", x=64), in_=acc[ai][:].rearrange("p (r q) -> p r q", r=4),
                    r=[f"acc{ai}"], w=[f"oT{QB}"])
        P.wait_all("sp", [f"oT{q}" for q in range(NBLK * 4)])
        P.barrier()
        P.emit()


EPS = 1e-6
NEG = -1.0e30


def declare_b(nc, T, pfx=""):
    d = {}

    def inp(name, shape, dt=F32):
        d[name] = nc.dram_tensor(pfx + name, shape, dt, kind="ExternalInput").ap()

    def outp(name, shape, dt=F32):
        d[name] = nc.dram_tensor(pfx + name, shape, dt, kind="ExternalOutput").ap()

    def scr(name, shape, dt=F32):
        d[name] = nc.dram_tensor(pfx + name, shape, dt, kind="Internal").ap()

    inp("hT", [1024, T]); inp("oT", [1024, T]); inp("wout", [1024, 1024]); inp("gffn", [1024])
    inp("wq", [1024, 2048]); inp("keysT", [16, 128, 128]); inp("uT", [1024, 16384]); inp("v", [16384, 1024])
    inp("gnext", [1024])
    outp("hT_out", [1024, T]); outp("nT_out", [1024, T])
    scr("h2T", [1024, T]); scr("hnbf", [1024, T], BF16); scr("qTd", [128, T // 128, 16, 128])
    scr("GT", [T // 128, 128, 16384], BF16); scr("uTbf", [1024, 16384], BF16); scr("vbf", [16384, 1024], BF16)
    return d


def emit_b(nc, P, T, d, cast_weights=True, pfx="", oT_blk=None, nT_dst=None):
    NB = T // 512
    NT = T // 128
    NB2 = T // 256
    fm = lambda ap: ap.rearrange("(m p) t -> p m t", p=128)

    if cast_weights:
        for i in range(8):
            P.D("pool", out=d["uTbf"][i * 128:(i + 1) * 128, :], in_=d["uT"][i * 128:(i + 1) * 128, :], w=[f"uTbf{i}"])
        for i in range(8):
            P.D("pool", out=d["vbf"][i * 2048:(i + 1) * 2048, :], in_=d["v"][i * 2048:(i + 1) * 2048, :], w=[f"vbf{i}"])

    with contextlib.ExitStack() as st:
        sb = lambda n, s, dt=F32: st.enter_context(nc.sbuf_tensor(pfx + "p0_" + n, s, dt))
        ps = lambda n, s, dt=F32: st.enter_context(nc.psum_tensor(pfx + "p0_" + n, s, dt))
        wout = sb("wout", [128, 8, 1024], BF16)
        g = sb("g", [128, 8]); ones = sb("ones", [128, 128])
        hTt = [sb(f"hTt{i}", [128, 8, 512]) for i in range(2)]
        oTt = [sb(f"oTt{i}", [128, 8, 512], BF16) for i in range(2)]
        h2 = sb("h2", [128, 8, 512]); hnb = sb("hnb", [128, 8, 512], BF16)
        rstd = sb("rstd", [128, 512])
        wqt = [sb(f"wqt{i}", [128, 8, 512]) for i in range(2)]
        qs = [sb(f"qs{i}", [128, 4, 512]) for i in range(2)]
        pp = [ps(f"pp{i}", [128, 512]) for i in range(2)]
        ss = ps("ss", [128, 512])

        P.D("pool", out=wout[:], in_=d["wout"].rearrange("(k p) c -> p k c", p=128), w=["wout"])
        P.D("sp", out=g[:], in_=d["gffn"].rearrange("(m p) -> p m", p=128), w=["g"], allow_slow_non_contiguous=True)
        P.I("pool", "memset", w=["ones"], ap=ones[:], constant=1.0)
        nmm = 0
        nwq = 0
        if oT_blk is not None:
            P.dma("sp", lambda e: e.dma_start(out=d["oTq"].rearrange("r (c t) -> c r t", t=min(1024, T)), in_=oT_blk()), writes=["oTq"])
        for b in range(NB):
            tsl = slice(b * 512, (b + 1) * 512)
            ht, ot = hTt[b % 2], oTt[b % 2]
            hk, ok = f"hTt{b % 2}", f"oTt{b % 2}"
            P.D("sp", out=ht[:], in_=fm(d["hT"])[:, :, tsl], w=[hk])
            if oT_blk is None:
                P.D("pool", out=ot[:], in_=fm(d["oT"])[:, :, tsl], w=[ok])
            else:
                P.D("pool", out=ot[:], in_=fm(d["oTq"])[:, :, tsl], r=["oTq"], w=[ok])
            for m in range(8):
                p_ = pp[nmm % 2]; pk = f"pp{nmm % 2}"; nmm += 1
                for k in range(8):
                    P.MM(p_[:], wout[:, k, m * 128:(m + 1) * 128], ot[:, k, :], start=(k == 0), stop=(k == 7),
                         r=["wout", ok], w=[pk])
                P.I("dve", "tensor_tensor", r=[pk, hk], w=["h2"], out=h2[:, m, :], in0=p_[:], in1=ht[:, m, :], op=ALU.add)
            P.D("sp", out=fm(d["h2T"])[:, :, tsl], in_=h2[:], r=["h2"], w=[f"h2T{b}"])
            P.I("act", "activation", r=["h2"], w=[hk], out=ht[:], in_=h2[:], func=AF.Square)
            for m in range(8):
                P.MM(ss[:], ones[:], ht[:, m, :], start=(m == 0), stop=(m == 7), r=["ones", hk], w=["ss"])
            P.I("act", "activation", r=["ss"], w=["rstd"], out=rstd[:], in_=ss[:], func=AF.Sqrt, scale=1.0 / 1024, bias=EPS)
            P.I("dve", "reciprocal", r=["rstd"], w=["rstd"], out=rstd[:], in_=rstd[:])
            for m in range(8):
                P.I("dve", "scalar_tensor_tensor", r=["h2", "g", "rstd"], w=["h2"], out=h2[:, m, :], in0=h2[:, m, :],
                    scalar=g[:, m:m + 1], in1=rstd[:], op0=ALU.mult, op1=ALU.mult)
            P.I("pool", "tensor_copy", r=["h2"], w=["hnb"], out=hnb[:], in_=h2[:])
            P.D("sp", out=fm(d["hnbf"])[:, :, tsl], in_=hnb[:], r=["hnb"], w=[f"hnbf{b}"])
            for jg in range(4):
                wt = wqt[nwq % 2]; wk = f"wqt{nwq % 2}"; q_ = qs[nwq % 2]; qk = f"qs{nwq % 2}"; nwq += 1
                P.D("sp", out=wt[:], in_=d["wq"].rearrange("(m p) c -> p m c", p=128)[:, :, jg * 512:(jg + 1) * 512], w=[wk])
                for jj in range(4):
                    p_ = pp[nmm % 2]; pk = f"pp{nmm % 2}"; nmm += 1
                    for m in range(8):
                        P.MM(p_[:], wt[:, m, jj * 128:(jj + 1) * 128], h2[:, m, :], start=(m == 0), stop=(m == 7),
                             r=[wk, "h2"], w=[pk])
                    P.I("act", "copy", r=[pk], w=[qk], out=q_[:, jj, :], in_=p_[:])
                for t4 in range(4):
                    P.D("sp", out=d["qTd"][:, b * 4 + t4, jg * 4:(jg + 1) * 4, :], in_=q_[:, :, t4 * 128:(t4 + 1) * 128],
                        r=[qk], w=[f"qTd{b}_{jg}_{t4}"])
        P.barrier()
        P.emit()

    with contextlib.ExitStack() as st:
        sb = lambda n, s, dt=F32: st.enter_context(nc.sbuf_tensor(pfx + "p1_" + n, s, dt))
        ps = lambda n, s, dt=F32: st.enter_context(nc.psum_tensor(pfx + "p1_" + n, s, dt))
        keys = sb("keys", [128, 16, 128]); ident = sb("ident", [128, 128], BF16); identf = sb("identf", [128, 128])
        qt = [sb(f"qt{i}", [128, 16, 128]) for i in range(2)]
        S12 = [sb(f"S12{i}", [128, 16, 128]) for i in range(2)]
        scr_ = sb("scr", [128, 256]); TS = sb("TS", [128, 16, 16]); cand = sb("cand", [128, 8, 256]); BS = sb("BS", [128, 8, 16])
        ex = sb("ex", [128, 8, 16]); Z = sb("Z", [128, 8]); lnZ = sb("lnZ", [128, 8]); bias = sb("bias", [128, 8])
        SUM = [sb(f"SUM{i}", [128, 8, 128]) for i in range(6)]
        E = [sb(f"E{i}", [128, 1024], BF16) for i in range(3)]
        GH = [[sb(f"GH{i}_{h}", [128, 1024], BF16) for h in range(8)] for i in range(2)]
        GTp = [sb(f"GTp{i}", [128, 8, 128], BF16) for i in range(4)]
        sc = ps("sc", [128, 16, 128])
        acc = [ps(f"acc{i}", [128, 4, 128]) for i in range(2)]

        P.D("sp", out=keys[:], in_=d["keysT"].rearrange("j c k -> c j k"), w=["keys"])
        P.I("pool", "memset", w=["identf"], ap=identf[:], constant=1.0)
        P.I("pool", "affine_select", r=["identf"], w=["identf"], out=identf[:], in_=identf[:], pattern=[[-1, 128]],
            compare_op=ALU.is_equal, fill=0.0, base=0, channel_multiplier=1)
        P.I("pool", "tensor_copy", r=["identf"], w=["ident"], out=ident[:], in_=identf[:])
        nsum = 0; nacc = 0; ngtp = 0; ngh = 0
        for tt in range(NT):
            q_ = qt[tt % 2]; qk = f"qt{tt % 2}"; S = S12[tt % 2]; Sk = f"S12{tt % 2}"
            P.D("sp", out=q_[:], in_=d["qTd"][:, tt, :, :], w=[qk])
            for j in range(16):
                P.MM(sc[:, j, :], q_[:, j, :], keys[:, j, :], r=[qk, "keys"], w=["sc"])
            P.I("act", "copy", r=["sc"], w=[Sk + "a"], out=S[:, 0:8, :], in_=sc[:, 0:8, :])
            P.I("dve", "tensor_copy", r=["sc"], w=[Sk + "b"], out=S[:, 8:16, :], in_=sc[:, 8:16, :])
            Sr = [Sk + "a", Sk + "b"]
            for j in range(16):
                P.I("dve", "max", r=Sr, w=["TS"], out=TS[:, j, 0:8], in_=S[:, j, :])
                P.I("dve", "match_replace", r=Sr + ["TS"], w=["scr"], out=scr_[:, 0:128], in_to_replace=TS[:, j, 0:8],
                    in_values=S[:, j, :], imm_value=NEG)
                P.I("dve", "max", r=["scr"], w=["TS"], out=TS[:, j, 8:16], in_=scr_[:, 0:128])
            TS4 = TS[:].rearrange("p (h two) a -> p h two a", two=2)
            P.I("dve", "tensor_tensor", r=["TS"], w=["cand"], out=cand[:].rearrange("p h (a b) -> p h a b", b=16),
                in0=TS4[:, :, 0, :].unsqueeze(3).to_broadcast([128, 8, 16, 16]),
                in1=TS4[:, :, 1, :].unsqueeze(2).to_broadcast([128, 8, 16, 16]), op=ALU.add)
            for h in range(8):
                P.I("dve", "max", r=["cand"], w=["BS"], out=BS[:, h, 0:8], in_=cand[:, h, :])
                P.I("dve", "match_replace", r=["cand", "BS"], w=["scr"], out=scr_[:, 0:256], in_to_replace=BS[:, h, 0:8],
                    in_values=cand[:, h, :], imm_value=NEG)
                P.I("dve", "max", r=["scr"], w=["BS"], out=BS[:, h, 8:16], in_=scr_[:, 0:256])
            P.I("dve", "tensor_tensor", r=["BS"], w=["ex"], out=ex[:], in0=BS[:], in1=BS[:, :, 0:1].to_broadcast([128, 8, 16]),
                op=ALU.subtract)
            P.I("act", "activation", r=["ex"], w=["ex"], out=ex[:], in_=ex[:], func=AF.Exp)
            P.I("dve", "reduce_sum", r=["ex"], w=["Z"], out=Z[:], in_=ex[:], axis=AX.X)
            P.I("act", "activation", r=["Z"], w=["lnZ"], out=lnZ[:], in_=Z[:], func=AF.Ln)
            P.I("dve", "scalar_tensor_tensor", r=["BS", "lnZ"], w=["bias"], out=bias[:], in0=BS[:, :, 0], scalar=-1.0,
                in1=lnZ[:], op0=ALU.mult, op1=ALU.subtract)
            def add_op(it):
                sx_, h_ = it // 8, it % 8
                su = SUM[it % 6]; sk = f"SUM{it % 6}"
                if False:
                    for a in range(8):
                        P.I("act", "activation", r=Sr, w=[sk], out=su[:, a, :], in_=S[:, 2 * h_ + 1, :], func=AF.Identity,
                            bias=S[:, 2 * h_, sx_ * 8 + a:sx_ * 8 + a + 1], scale=1.0)
                else:
                    P.I("dve", "tensor_tensor", r=Sr, w=[sk], out=su[:],
                        in0=S[:, 2 * h_, sx_ * 8:(sx_ + 1) * 8].unsqueeze(2).to_broadcast([128, 8, 128]),
                        in1=S[:, 2 * h_ + 1, :].unsqueeze(1).to_broadcast([128, 8, 128]), op=ALU.add)

            LOOK = 4
            deferred = []
            for it0 in range(LOOK):
                add_op(it0)
            for sx in range(16):
                ghs = GH[ngh % 2]; gk = f"GH{ngh % 2}_"; ngh += 1
                for h in range(8):
                    it = sx * 8 + h
                    if it + LOOK < 128:
                        add_op(it + LOOK)
                    if h == 4 and deferred:
                        deferred.pop(0)()
                    su = SUM[it % 6]; sk = f"SUM{it % 6}"; e_ = E[it % 3]; ek = f"E{it % 3}"
                    P.I("act", "activation", r=[sk, "bias"], w=[ek], out=e_[:], in_=su[:].rearrange("p a b -> p (a b)"),
                        func=AF.Exp, bias=bias[:, h:h + 1])
                    P.I("dve", "scalar_tensor_tensor", r=[sk, ek, "BS"], w=[gk + str(h)], out=ghs[h][:],
                        in0=su[:].rearrange("p a b -> p (a b)"), scalar=BS[:, h, 15:16], in1=e_[:], op0=ALU.is_ge, op1=ALU.mult)
                def flush(sx=sx, tt=tt, ghs=ghs, gk=gk):
                    nonlocal nacc, ngtp
                    gp = GTp[ngtp % 4]; gpk = f"GTp{ngtp % 4}"; ngtp += 1
                    for c4 in range(2):
                        a_ = acc[nacc % 2]; ak = f"acc{nacc % 2}"; nacc += 1
                        for ci in range(4):
                            c = c4 * 4 + ci
                            for h in range(8):
                                P.MM(a_[:, ci, :], ghs[h][:, c * 128:(c + 1) * 128], ident[:], start=(h == 0), stop=(h == 7),
                                     r=[gk + str(h), "ident"], w=[ak])
                        P.I("act", "copy", r=[ak], w=[gpk], out=gp[:, c4 * 4:(c4 + 1) * 4, :], in_=a_[:])
                    P.D("sp", out=d["GT"][tt, :, sx * 1024:(sx + 1) * 1024], in_=gp[:].rearrange("p c t -> p (c t)"), r=[gpk],
                        w=[f"GT{tt}_{sx}"])
                deferred.append(flush)
            while deferred:
                deferred.pop(0)()
        P.barrier()
        P.emit()

    with contextlib.ExitStack() as st:
        sb = lambda n, s, dt=F32: st.enter_context(nc.sbuf_tensor(pfx + "p2_" + n, s, dt))
        ps = lambda n, s, dt=F32: st.enter_context(nc.psum_tensor(pfx + "p2_" + n, s, dt))
        U = [sb(f"U{i}", [128, 8, 1024], BF16) for i in range(2)]
        V = [sb(f"V{i}", [128, 8, 1024], BF16) for i in range(2)]
        Gg = [sb(f"Gg{i}", [128, 2, 8, 128], BF16) for i in range(2)]
        hn2 = [sb(f"hn2{i}", [128, 8, 256], BF16) for i in range(2)]
        ge = [sb(f"ge{i}", [128, 2, 256], BF16) for i in range(2)]
        gh2 = [sb(f"gh2{i}", [128, 2, 256], BF16) for i in range(2)]
        h2b = sb("h2b", [128, 8, 256]); h3 = sb("h3", [128, 8, 256]); sq2 = sb("sq2", [128, 8, 256])
        rstd2 = sb("rstd2", [128, 256]); nrm = sb("nrm", [128, 8, 256]); gn = sb("gn", [128, 8]); ones2 = sb("ones2", [128, 128])
        Y = ps("Y", [128, 8, 256])
        Hp = [ps(f"Hp{i}", [128, 2, 256]) for i in range(2)]
        ss2 = ps("ss2", [128, 256])
        P.D("sp", out=gn[:], in_=d["gnext"].rearrange("(m p) -> p m", p=128), w=["gn"], allow_slow_non_contiguous=True)
        P.I("pool", "memset", w=["ones2"], ap=ones2[:], constant=1.0)
        zl = sb("zl", [128, 128], BF16); zr = sb("zr", [128, 512], BF16)
        P.I("pool", "memset", w=["zl"], ap=zl[:], constant=0.0)
        P.I("pool", "memset", w=["zr"], ap=zr[:], constant=0.0)
        Yb = Y[:].rearrange("p (b two) t -> p b (two t)", two=2)
        nw = 0; npair = 0
        for blk in range(NB2):
            tsl = slice(blk * 256, (blk + 1) * 256)
            hb = hn2[blk % 2]; hbk = f"hn2{blk % 2}"
            P.D("sp", out=hb[:], in_=fm(d["hnbf"])[:, :, tsl], w=[hbk])
            P.D("sp", out=h2b[:], in_=fm(d["h2T"])[:, :, tsl], w=["h2b"])
            for bk in range(4):
                P.MM(Yb[:, bk, :], zl[:], zr[:], start=True, stop=False, r=["zl", "zr"], w=["Y"])
            for eg in range(16):
                u_ = U[nw % 2]; uk = f"U{nw % 2}"; v_ = V[nw % 2]; vk = f"V{nw % 2}"; g_ = Gg[nw % 2]; gk = f"Gg{nw % 2}"; nw += 1
                P.D("sp", out=u_[:], in_=d["uTbf"].rearrange("(m p) e -> p m e", p=128)[:, :, eg * 1024:(eg + 1) * 1024], w=[uk])
                P.D("act", out=v_[:], in_=d["vbf"].rearrange("(c p) x -> p c x", p=128)[:, eg * 8:(eg + 1) * 8, :], w=[vk])
                for t2 in range(2):
                    P.D("sp", out=g_[:, t2, :, :], in_=d["GT"][blk * 2 + t2, :, eg * 1024:(eg + 1) * 1024].rearrange("p (c t) -> p c t", t=128),
                        w=[gk + str(t2)])
                for pr in range(4):
                    hp = Hp[npair % 2]; hpk = f"Hp{npair % 2}"; ge_ = ge[npair % 2]; gek = f"ge{npair % 2}"
                    gh_ = gh2[npair % 2]; ghk = f"gh2{npair % 2}"; npair += 1
                    for cc in range(2):
                        c = pr * 2 + cc
                        for m in range(8):
                            P.MM(hp[:, cc, :], u_[:, m, c * 128:(c + 1) * 128], hb[:, m, :], start=(m == 0), stop=(m == 7),
                                 r=[uk, hbk], w=[hpk])
                    P.I("act", "activation", r=[hpk], w=[gek], out=ge_[:], in_=hp[:], func=AF.Gelu_apprx_tanh)
                    P.I("dve", "tensor_tensor", r=[gek, gk + "0", gk + "1"], w=[ghk],
                        out=gh_[:].rearrange("p c (tt t) -> p c tt t", t=128), in0=ge_[:].rearrange("p c (tt t) -> p c tt t", t=128),
                        in1=g_[:, :, pr * 2:pr * 2 + 2, :].rearrange("p tt c t -> p c tt t"), op=ALU.mult)
                    for cc in range(2):
                        c = pr * 2 + cc
                        for m in range(8):
                            P.MM(Y[:, m, :], v_[:, c, m * 128:(m + 1) * 128], gh_[:, cc, :], start=False,
                                 stop=(eg == 15 and c == 7), r=[vk, ghk], w=["Y"])
            for m in range(8):
                P.I("dve", "tensor_tensor", r=["Y", "h2b"], w=["h3"], out=h3[:, m, :], in0=Y[:, m, :], in1=h2b[:, m, :], op=ALU.add)
            P.D("sp", out=fm(d["hT_out"])[:, :, tsl], in_=h3[:], r=["h3"], w=[f"hT_out{blk}"])
            P.I("act", "activation", r=["h3"], w=["sq2"], out=sq2[:], in_=h3[:], func=AF.Square)
            for m in range(8):
                P.MM(ss2[:], ones2[:], sq2[:, m, :], start=(m == 0), stop=(m == 7), r=["ones2", "sq2"], w=["ss2"])
            P.I("act", "activation", r=["ss2"], w=["rstd2"], out=rstd2[:], in_=ss2[:], func=AF.Sqrt, scale=1.0 / 1024, bias=EPS)
            P.I("dve", "reciprocal", r=["rstd2"], w=["rstd2"], out=rstd2[:], in_=rstd2[:])
            for m in range(8):
                P.I("dve", "scalar_tensor_tensor", r=["h3", "gn", "rstd2"], w=["nrm"], out=nrm[:, m, :], in0=h3[:, m, :],
                    scalar=gn[:, m:m + 1], in1=rstd2[:], op0=ALU.mult, op1=ALU.mult)
            P.D("sp", out=(fm(d["nT_out"])[:, :, tsl] if nT_dst is None else nT_dst(blk)), in_=nrm[:], r=["nrm"], w=[f"nT_out{blk}"])
        P.wait_all("sp", [f"nT_out{b}" for b in range(NB2)] + [f"hT_out{b}" for b in range(NB2)])
        P.barrier()
        P.emit()


def declare_c(nc, S, pfx="", fused=False):
    d = {}

    def t(name, shape, kind, dt=F32):
        d[name] = nc.dram_tensor(pfx + name, shape, dt, kind=kind).ap()

    if not fused:
        t("nT", [1024, S], "ExternalInput")
    t("wq", [1024, 256], "ExternalInput"); t("wk", [1024, 256], "ExternalInput"); t("wv", [1024, 256], "ExternalInput")
    t("wf", [1024, 4], "ExternalInput"); t("fb", [4], "ExternalInput")
    t("maskd", [128, 4, 512], "ExternalInput")
    t("oT", [256, S], "Internal" if fused else "ExternalOutput")
    return d


def emit_c(nc, P, S, d, nT_loads=None, oT_dst=None):
    if oT_dst is None:
        oT_dst = lambda t0, n: d["oT"][:, t0:t0 + n]
    NBLK = S // 512
    NCH = S // 128
    with contextlib.ExitStack() as st:
        sb = lambda n, s, dt=F32: st.enter_context(nc.sbuf_tensor("c_" + n, s, dt))
        ps = lambda n, s, dt=F32: st.enter_context(nc.psum_tensor("c_" + n, s, dt))
        KT = sb("KT", [128, 2, S], BF16)
        Vr = sb("Vr", [128, NCH, 4 * 65 + 63], BF16)
        Cr = sb("Cr", [128, NCH, 4]); nbias = sb("nbias", [128, 4, NCH])
        nb = [sb(f"nb{i}", [128, 8, 512], BF16) for i in range(2)]
        wq = sb("wq", [128, 8, 256], BF16); wk = sb("wk", [128, 8, 256], BF16); wv = sb("wv", [128, 8, 256], BF16)
        wf = sb("wf", [128, 8, 4], BF16); fb = sb("fb", [128, 4])
        QT = [sb(f"QT{i}", [128, 512], BF16) for i in range(4)]
        PT = [sb(f"PT{i}", [128, 512], BF16) for i in range(4)]
        maskd = sb("maskd", [128, 4, 512], BF16)
        tri = sb("tri", [128, 128]); ones = sb("ones", [128, 128])
        lf = sb("lf", [128, 4]); tot = sb("tot", [128, 4]); totmid = sb("totmid", [128, 4])
        zrow = sb("zrow", [65, 512]); ocp = sb("ocp", [64, 512]); osb = [sb(f"osb{i}", [64, 512]) for i in range(2)]
        pp = [ps(f"pp{i}", [128, 512]) for i in range(2)]
        ST = [ps(f"ST{i}", [128, 512]) for i in range(3)]
        OT = [ps(f"OT{i}", [128, 512]) for i in range(2)]
        MISC = ps("MISC", [128, 512]); ZB = MISC[0:64, :]; cs = MISC[:, 0:8]

        for nm, tl in (("wq", wq), ("wk", wk), ("wv", wv)):
            P.D("pool", out=tl[:], in_=d[nm].rearrange("(m p) c -> p m c", p=128), w=[nm])
        P.D("pool", out=wf[:], in_=d["wf"].rearrange("(m p) c -> p m c", p=128), w=["wf"])
        P.D("sp", out=fb[:], in_=d["fb"].partition_broadcast(128), w=["fb"])
        P.D("pool", out=maskd[:], in_=d["maskd"], w=["maskd"])
        P.I("pool", "memset", w=["ones"], ap=ones[:], constant=1.0)
        P.I("pool", "memset", w=["tri"], ap=tri[:], constant=1.0)
        P.I("pool", "affine_select", r=["tri"], w=["tri"], out=tri[:], in_=tri[:], pattern=[[1, 128]],
            compare_op=ALU.is_ge, fill=0.0, base=0, channel_multiplier=-1)
        P.I("pool", "memset", w=["tot"], ap=tot[:], constant=0.0)
        P.I("pool", "memset", w=["Vr"], ap=Vr[:], constant=0.0)
        P.I("pool", "memset", r=["Vr"], w=["Vr"], ap=Vr[:, :, 0:260].rearrange("p c (h x) -> p c h x", x=65)[:, :, :, 64:65], constant=1.0)
        for i in range(4):
            P.I("pool", "memset", w=[f"QT{i}"], ap=QT[i][:], constant=0.0)
        npp = 0; nst = 0; npt = 0; nhead = 0
        for blk in range(NBLK):
            tsl = slice(blk * 512, (blk + 1) * 512)
            n_ = nb[blk % 2]; nk = f"nb{blk % 2}"
            if nT_loads is None:
                P.D("pool", out=n_[:], in_=d["nT"].rearrange("(m p) t -> p m t", p=128)[:, :, tsl], w=[nk])
            else:
                for csl, src in nT_loads(blk):
                    P.D("pool", out=n_[:, :, csl], in_=src, w=[nk])
            for pair in range(2):
                p_ = pp[npp % 2]; pk = f"pp{npp % 2}"; npp += 1
                for m in range(8):
                    P.MM(p_[:], wq[:, m, pair * 128:(pair + 1) * 128], n_[:, m, :], start=(m == 0), stop=(m == 7), r=["wq", nk], w=[pk])
                for hh in range(2):
                    rs = slice(hh * 64, hh * 64 + 64)
                    P.I("act", "mul", r=[pk], w=[f"QT{2 * pair + hh}"], out=QT[2 * pair + hh][rs, :], in_=p_[rs, :], mul=0.125)
                p_ = pp[npp % 2]; pk = f"pp{npp % 2}"; npp += 1
                for m in range(8):
                    P.MM(p_[:], wk[:, m, pair * 128:(pair + 1) * 128], n_[:, m, :], start=(m == 0), stop=(m == 7), r=["wk", nk], w=[pk])
                P.I("dve", "tensor_copy", r=[pk], w=["KT"], out=KT[:, pair, tsl], in_=p_[:])
            for t4 in range(4):
                ch = blk * 4 + t4
                p_ = pp[npp % 2]; pk = f"pp{npp % 2}"; npp += 1
                for m in range(8):
                    P.MM(p_[:, 0:256], n_[:, m, t4 * 128:(t4 + 1) * 128], wv[:, m, :], start=(m == 0), stop=(m == 7), r=["wv", nk], w=[pk])
                P.I("act", "copy", r=[pk], w=["Vr"], out=Vr[:, ch, 0:260].rearrange("p (h x) -> p h x", x=65)[:, :, 0:64], in_=p_[:, 0:256].rearrange("p (h x) -> p h x", x=64))
                for m in range(8):
                    P.MM(cs[:, 0:4], n_[:, m, t4 * 128:(t4 + 1) * 128], wf[:, m, :], start=(m == 0), stop=(m == 7), r=["wf", nk], w=["MISC"])
                P.I("dve", "tensor_tensor", r=["MISC", "fb"], w=["lf"], out=lf[:], in0=cs[:, 0:4], in1=fb[:], op=ALU.add)
                P.I("act", "activation", r=["lf"], w=["lf"], out=lf[:], in_=lf[:], func=AF.Exp, scale=-1.0)
                P.I("act", "activation", r=["lf"], w=["lf"], out=lf[:], in_=lf[:], func=AF.Ln, bias=1.0)
                P.I("dve", "tensor_scalar", r=["lf"], w=["lf"], out=lf[:], in0=lf[:], scalar1=-1.0, scalar2=None, op0=ALU.mult)
                P.MM(cs[:, 0:4], tri[:], lf[:], r=["tri", "lf"], w=["MISC"])
                P.MM(cs[:, 4:8], ones[:], lf[:], r=["ones", "lf"], w=["MISC"])
                P.I("dve", "tensor_tensor", r=["MISC", "tot"], w=["Cr"], out=Cr[:, ch, :], in0=cs[:, 0:4], in1=tot[:], op=ALU.add)
                P.I("dve", "tensor_tensor", r=["MISC", "tot"], w=["tot"], out=tot[:], in0=cs[:, 4:8], in1=tot[:], op=ALU.add)
                if t4 == 1:
                    P.I("dve", "tensor_copy", r=["tot"], w=["totmid"], out=totmid[:], in_=tot[:])
            nch = blk * 4 + 4
            for hl in range(4):
                P.I("dve", "tensor_scalar", r=["Cr", "totmid"], w=["nbias"], out=nbias[:, hl, 0:nch], in0=Cr[:, 0:nch, hl],
                    scalar1=totmid[:, hl:hl + 1], scalar2=-1.0, op0=ALU.subtract, op1=ALU.mult)
            pairs = [(hl, kc) for hl in range(4) for kc in range(nch)]

            def qk(i):
                nonlocal nst
                hl, kc = pairs[i]
                s_ = ST[i % 3]
                P.MM(s_[:], KT[:, hl // 2, kc * 128:(kc + 1) * 128], QT[hl][:], r=["KT", f"QT{hl}"], w=[f"ST{i % 3}"])

            qk(0)
            if len(pairs) > 1:
                qk(1)
            for i, (hl, kc) in enumerate(pairs):
                if i + 2 < len(pairs):
                    qk(i + 2)
                s_ = ST[i % 3]; sk = f"ST{i % 3}"
                pt = PT[npt % 4]; ptk = f"PT{npt % 4}"; npt += 1
                P.I("act", "activation", r=[sk, "nbias"], w=[ptk], out=pt[:], in_=s_[:], func=AF.Exp, bias=nbias[:, hl, kc:kc + 1])
                if kc >= blk * 4:
                    P.I("dve", "tensor_tensor", r=[ptk, "maskd"], w=[ptk], out=pt[:], in0=pt[:], in1=maskd[:, kc - blk * 4, :], op=ALU.mult)
                if kc == 0:
                    ot = OT[nhead % 2]; otk = f"OT{nhead % 2}"; ob = osb[nhead % 2]; obk = f"osb{nhead % 2}"; nhead += 1
                P.MM(ot[:], Vr[:, kc, hl * 65:hl * 65 + 128], pt[:], start=(kc == 0), stop=(kc == nch - 1), r=["Vr", ptk], w=[otk])
                if kc == nch - 1:
                    P.I("dve", "tensor_scalar", r=[otk], w=["zrow"], out=zrow[64:65, :], in0=ot[64:65, :], scalar1=1e-30, scalar2=None, op0=ALU.max)
                    P.I("dve", "reciprocal", r=["zrow"], w=["zrow"], out=zrow[64:65, :], in_=zrow[64:65, :])
                    P.MM(ZB, ones[64:65, 0:64], zrow[64:65, :], r=["ones", "zrow"], w=["MISC"])
                    P.I("act", "copy", r=[otk], w=["ocp"], out=ocp[:], in_=ot[0:64, :])
                    P.I("dve", "tensor_tensor", r=["ocp", "MISC"], w=[obk], out=ob[:], in0=ocp[:], in1=ZB, op=ALU.mult)
                    P.D("sp", out=oT_dst(blk * 512, 512)[hl * 64:(hl + 1) * 64, :], in_=ob[:], r=[obk], w=[f"oT{blk}_{hl}"])
        P.wait_all("sp", [f"oT{b}_{h}" for b in range(NBLK) for h in range(4)])
        P.barrier()
        P.emit()


S_FULL = 16384
T_CORE = 4096
G4 = [[0, 1, 2, 3], [4, 5, 6, 7]]
_PROG = {}
_B_IN = (("wout", [1024, 1024]), ("gffn", [1024]), ("wq", [1024, 2048]), ("keysT", [16, 128, 128]), ("uT", [1024, 16384]),
         ("v", [16384, 1024]), ("gnext", [1024]))


def _declare_b_fused(nc, T, pfx, hT_ap, out_kind):
    d = {}
    for name, shape in _B_IN:
        d[name] = nc.dram_tensor(pfx + name, shape, F32, kind="ExternalInput").ap()
    d["hT"] = hT_ap
    d["hT_out"] = nc.dram_tensor(pfx + "hT_out", [1024, T], F32, kind="Internal").ap()
    d["nT_out"] = nc.dram_tensor(pfx + "nT_out", [1024, T], F32, kind=out_kind).ap()
    scr = lambda name, shape, dt=F32: nc.dram_tensor(pfx + name, shape, dt, kind="Internal").ap()
    d["h2T"] = scr("h2T", [1024, T]); d["hnbf"] = scr("hnbf", [1024, T], BF16); d["qTd"] = scr("qTd", [128, T // 128, 16, 128])
    d["oTq"] = scr("oTq", [1024, T])
    d["GT"] = scr("GT", [T // 128, 128, 16384], BF16); d["uTbf"] = scr("uTbf", [1024, 16384], BF16); d["vbf"] = scr("vbf", [16384, 1024], BF16)
    return d


def _build_fused(S=S_FULL, T=T_CORE):
    if (S, T) in _PROG:
        return _PROG[(S, T)]
    nc = bass.Bass("TRN2", target_bir_lowering=False)
    with contextlib.ExitStack() as st:
        P = Prog(nc)
        P.alloc_sems(st)
        da = declare_a(nc, S, pfx="A_", fused=True)
        xTs = nc.dram_tensor("B0_xTs", [1024, T], F32, kind="ExternalInput").ap()
        db0 = _declare_b_fused(nc, T, "B0_", xTs, "Internal")
        dc = declare_c(nc, S, pfx="C_", fused=True)
        db1 = _declare_b_fused(nc, T, "B1_", db0["hT_out"], "ExternalOutput")
        CW = min(1024, T)
        NC1 = S // CW
        CW2 = 256
        NC2 = T // CW2
        dt_ = lambda name, shape: nc.dram_tensor(name, shape, F32, kind="Internal").ap()
        x1_in = dt_("x1_in", [NC1, 256, CW]); x1_out = dt_("x1_out", [NC1, 1024, CW])
        x2_in = dt_("x2_in", [NC2, 1024, CW2]); x2_out = dt_("x2_out", [NC2, 4096, CW2])
        x3_in = dt_("x3_in", [NC1, 256, CW]); x3_out = dt_("x3_out", [NC1, 1024, CW])

        def gather(src, dst, n, name):
            for j in range(n):
                P.cc((lambda j: (lambda e: e.collective_compute("AllGather", ALU.bypass, replica_groups=G4, ins=[src[j]], outs=[dst[j]])))(j),
                     writes=[name])
            P.barrier()

        def chunked_dst(buf):
            return lambda t0, n: buf[t0 // CW, :, (t0 % CW):(t0 % CW) + n]

        def quarter_src(buf):
            def f():
                q = nc.partition_id() % 4
                return buf[bass.ds(q * (T // CW), T // CW), :, :]
            return f

        bpr = T // 512

        def nT_loads(blk):
            rank, lb = blk // bpr, blk % bpr
            return [(slice(h * CW2, (h + 1) * CW2),
                     x2_out[lb * 2 + h, rank * 1024:(rank + 1) * 1024, :].rearrange("(m p) t -> p m t", p=128)) for h in range(2)]

        emit_a(nc, P, S, da, oT_dst=chunked_dst(x1_in))
        gather(x1_in, x1_out, NC1, "x1")
        emit_b(nc, P, T, db0, pfx="B0_", oT_blk=quarter_src(x1_out),
               nT_dst=lambda blk: x2_in[blk].rearrange("(m p) t -> p m t", p=128))
        gather(x2_in, x2_out, NC2, "x2")
        emit_c(nc, P, S, dc, nT_loads=nT_loads, oT_dst=chunked_dst(x3_in))
        gather(x3_in, x3_out, NC1, "x3")
        emit_b(nc, P, T, db1, pfx="B1_", oT_blk=quarter_src(x3_out))
    _PROG[(S, T)] = nc
    return nc


def _peer_inputs(pfx, wout, gffn, wq, keys, u, v, gnext):
    f32 = lambda a: np.ascontiguousarray(np.asarray(a, dtype=np.float32))
    return {pfx + "wout": f32(wout), pfx + "gffn": f32(gffn), pfx + "wq": f32(wq),
            pfx + "keysT": np.ascontiguousarray(f32(keys).transpose(0, 1, 3, 2).reshape(16, 128, 128)),
            pfx + "uT": np.ascontiguousarray(f32(u).T), pfx + "v": f32(v), pfx + "gnext": f32(gnext)}


def kernel(x, l0_attn_norm, l0_w_in, l0_cmp_pe_k, l0_cmp_w1_k, l0_cmp_w2_k, l0_cmp_pe_v, l0_cmp_w1_v, l0_cmp_w2_v, l0_w_out,
           l0_ffn_norm, l0_peer_wq, l0_peer_keys, l0_peer_u, l0_peer_v,
           l1_attn_norm, l1_w_in, l1_f_bias, l1_w_out,
           l1_ffn_norm, l1_peer_wq, l1_peer_keys, l1_peer_u, l1_peer_v,
           final_norm):
    f32 = lambda a: np.ascontiguousarray(np.asarray(a, dtype=np.float32))
    x = f32(x)
    B, S, D = x.shape
    T_CORE = S // 4
    nc = _build_fused(S, T_CORE)
    consts = consts_a(); ropeq, ropek = rope_tables(S)
    args0 = [f32(a) for a in (l0_attn_norm, l0_w_in, l0_cmp_pe_k, l0_cmp_w1_k, l0_cmp_w2_k, l0_cmp_pe_v, l0_cmp_w1_v, l0_cmp_w2_v)]
    pb0 = _peer_inputs("B0_", l0_w_out, l0_ffn_norm, l0_peer_wq, l0_peer_keys, l0_peer_u, l0_peer_v, l1_attn_norm)
    pb1 = _peer_inputs("B1_", l1_w_out, l1_ffn_norm, l1_peer_wq, l1_peer_keys, l1_peer_u, l1_peer_v, final_norm)
    w1 = f32(l1_w_in); fbias = f32(l1_f_bias)
    kk = np.arange(128)[:, None, None]; ii = np.arange(4)[None, :, None]; qq = np.arange(512)[None, None, :]
    maskd = (kk <= qq - 128 * ii).astype(np.float32)
    xT = [np.ascontiguousarray(x[b].T) for b in range(B)]
    maps = []
    for c in range(8):
        b, q = c // 4, c % 4
        m = {"A_" + k: v for k, v in host_inputs_a(x[b], *args0, q, consts, ropeq, ropek).items()}
        m["A_xT"] = xT[b]
        m["B0_xTs"] = np.ascontiguousarray(xT[b][:, q * T_CORE:(q + 1) * T_CORE])
        m.update(pb0); m.update(pb1)
        h0 = 4 * q
        m.update({"C_wq": np.ascontiguousarray(w1[:, h0 * 64:(h0 + 4) * 64]),
                  "C_wk": np.ascontiguousarray(w1[:, 1024 + h0 * 64:1024 + (h0 + 4) * 64]),
                  "C_wv": np.ascontiguousarray(w1[:, 2048 + h0 * 64:2048 + (h0 + 4) * 64]),
                  "C_wf": np.ascontiguousarray(w1[:, 3072 + h0:3072 + h0 + 4]), "C_fb": fbias[h0:h0 + 4].copy(), "C_maskd": maskd})
        maps.append(m)
    res = run_bass_kernel_spmd(nc, maps, core_ids=list(range(8))).results
    out = np.empty((B, S, D), np.float32)
    for c in range(8):
        b, q = c // 4, c % 4
        out[b, q * T_CORE:(q + 1) * T_CORE, :] = res[c]["B1_nT_out"].T
    return out
```

```python
import contextlib
import numpy as np
import concourse.bass as bass
import concourse.mybir as mybir
from concourse.bass_utils import run_bass_kernel_spmd

F32 = mybir.dt.float32
BF16 = mybir.dt.bfloat16
AF = mybir.ActivationFunctionType
ALU = mybir.AluOpType
AX = mybir.AxisListType

N_DMA_SEMS = 8


class Prog:
    COMPUTE = ("pe", "dve", "act", "pool")
    QUEUES = ("sp", "act", "pool")

    def __init__(self, nc):
        self.nc = nc
        self.ops = {e: [] for e in ("pe", "dve", "act", "pool", "sp")}
        self.cnt = {}
        self.last_w = {}
        self.readers = {}
        self.waited = {e: {} for e in self.ops}
        self.dma_n = {q: 0 for q in self.QUEUES}
        self.semkeys = []
        for e in self.COMPUTE:
            self._mk(("c", e))
        for q in self.QUEUES:
            for i in range(N_DMA_SEMS):
                self._mk(("d", q, i))
        self._mk(("cc",))
        self.sems = {}
        self.pending = {e: False for e in self.ops}
        self.lazy_pe_inc = False

    def _mk(self, k):
        self.cnt[k] = 0
        self.semkeys.append(k)

    def _need(self, eng, tok, waits):
        if tok is None:
            return
        k, v = tok
        if k == ("c", eng) and eng == "pe":
            return
        if self.waited[eng].get(k, 0) >= v:
            return
        self.waited[eng][k] = v
        waits.append((k, v))

    def _deps(self, eng, reads, writes):
        waits = []
        for b in reads:
            self._need(eng, self.last_w.get(b), waits)
        for b in writes:
            self._need(eng, self.last_w.get(b), waits)
            for t in self.readers.get(b, {}).items():
                if t[0] == ("c", eng):
                    continue
                self._need(eng, t, waits)
        return waits

    def _commit(self, tok, reads, writes):
        for b in reads:
            self.readers.setdefault(b, {})[tok[0]] = tok[1]
        for b in writes:
            self.last_w[b] = tok
            self.readers[b] = {}

    def op(self, eng, fn, reads=(), writes=(), inc=True):
        waits = self._deps(eng, reads, writes)
        k = ("c", eng)
        if inc:
            self.cnt[k] += 1
            tok = (k, self.cnt[k])
            self.pending[eng] = False
        else:
            tok = (k, self.cnt[k] + 1)
            self.pending[eng] = True
        self._commit(tok, reads, writes)
        self.ops[eng].append((waits, fn, (k, 1) if inc else None))
        return tok

    def dma(self, q, fn, reads=(), writes=()):
        waits = self._deps(q, reads, writes)
        n = self.dma_n[q]
        self.dma_n[q] += 1
        k = ("d", q, n % N_DMA_SEMS)
        self._need(q, (k, self.cnt[k]) if self.cnt[k] else None, waits)
        self.cnt[k] += 16
        tok = (k, self.cnt[k])
        self._commit(tok, reads, writes)
        self.ops[q].append((waits, fn, (k, 16)))
        return tok

    def cc(self, fn, reads=(), writes=()):
        waits = self._deps("pool", reads, writes)
        k = ("cc",)
        self._need("pool", (k, self.cnt[k]) if self.cnt[k] else None, waits)
        self.cnt[k] += 1
        tok = (k, self.cnt[k])
        self._commit(tok, reads, writes)
        self.ops["pool"].append((waits, fn, (k, 1)))
        return tok

    def I(self, eng, method, r=(), w=(), **kw):
        return self.op(eng, lambda e: getattr(e, method)(**kw), reads=r, writes=w)

    def MM(self, out, lhsT, rhs, start=True, stop=True, r=(), w=()):
        return self.op("pe", lambda e: e.matmul(out, lhsT=lhsT, rhs=rhs, start=start, stop=stop), reads=r, writes=w,
                       inc=(stop or not self.lazy_pe_inc))

    def D(self, q, out, in_, r=(), w=(), **kw):
        return self.dma(q, lambda e: e.dma_start(out=out, in_=in_, **kw), reads=r, writes=w)

    def barrier(self):
        assert not any(self.pending.values()), self.pending
        for eng in self.ops:
            waits = []
            for k in self.semkeys:
                if self.cnt[k]:
                    self._need(eng, (k, self.cnt[k]), waits)
            self.ops[eng].append((waits, None, None))
        self.last_w = {}
        self.readers = {}

    def wait_all(self, eng, bufs):
        waits = []
        for b in bufs:
            self._need(eng, self.last_w.get(b), waits)
        self.ops[eng].append((waits, None, None))

    def alloc_sems(self, st):
        for k in self.semkeys:
            self.sems[k] = st.enter_context(self.nc.semaphore("s_" + "_".join(map(str, k))))

    def emit(self):
        nc = self.nc
        import contextlib
        with contextlib.ExitStack() as st:
            if not self.sems:
                self.alloc_sems(st)
            block = st.enter_context(nc.Block())
            engobj = {"pe": "tensor", "dve": "vector", "act": "scalar", "pool": "gpsimd", "sp": "sync"}

            def mk(ename):
                ops = self.ops[ename]

                def body(eng):
                    for waits, fn, inc in ops:
                        for (k, v) in waits:
                            eng.wait_ge(self.sems[k], v)
                        if fn is not None:
                            ins = fn(eng)
                            if inc is not None:
                                ins.then_inc(self.sems[inc[0]], inc[1])
                return body

            for ename, attr in engobj.items():
                if self.ops[ename]:
                    getattr(block, attr)(mk(ename))
        self.ops = {e: [] for e in self.ops}


EPS = 1e-6


def consts_a():
    c = {}
    q = np.arange(128)[:, None]; rel = np.arange(512)[None, :] - 256
    cur = (q >= 64).astype(np.int64)
    M = (rel <= cur - 2).astype(np.float32)
    A = np.where(rel == cur, 10000.0, np.where(rel == cur - 1, 10001.0, np.where(rel > cur, -1.0, 0.0))).astype(np.float32)
    c["pats"] = np.stack([M, A], 1)
    ci = np.arange(128)[:, None]; qi = np.arange(128)[None, :]
    D = (16 * ci - qi).astype(np.float32)
    Dz = D.copy(); Dz[0, :] = 1e9
    c["D16"] = np.stack([D, Dz], 1)
    c["tril"] = np.stack([(ci <= qi), (ci > qi)], 1).astype(np.float32)
    c["Sel"] = np.broadcast_to(np.eye(12, dtype=np.float32)[:, :, None], (12, 12, 64)).copy()
    cc = np.arange(1024)[:, None] - 1; jj = np.arange(256)[None, :]
    lo = np.maximum(cc * 16, jj * 64); hi = np.minimum(cc * 16 + 32, (jj + 1) * 64)
    m = np.maximum(hi - lo, 0).astype(np.float32) / 32.0
    m[0, :] = 0.0
    c["slcm"] = np.ascontiguousarray(m.reshape(8, 128, 256).transpose(1, 0, 2))
    return c


def epat_table(S):
    n = np.arange(S)[None, :]; r = np.arange(64)[:, None]
    return (30000.0 * (((n // 64) % 64) == r)).astype(np.float32)


def rope_tables(S):
    half = 32
    inv = (10000.0 ** (-np.arange(half, dtype=np.float32) / half)).astype(np.float32)
    ang = (np.arange(S, dtype=np.float32)[None, :] * inv[:, None]).astype(np.float32)
    cos = np.cos(ang).astype(np.float32); sin = np.sin(ang).astype(np.float32)
    cosf = np.concatenate([cos, cos], 0); sinf = np.concatenate([-sin, sin], 0)
    rk = np.stack([cosf, sinf], 1)
    return np.ascontiguousarray(rk * 0.125), np.ascontiguousarray(rk)


def host_inputs_a(xb, gattn, w_in, pe_k, w1_k, w2_k, pe_v, w1_v, w2_v, g, consts, ropeq, ropek):
    def sw(w):
        w = w.reshape(w.shape[0], -1, 64)
        return np.concatenate([w[..., 32:], w[..., :32]], -1).reshape(w.shape[0], -1)
    kv = lambda i: w_in[:, 1024 + i * 256 + g * 64:1024 + i * 256 + (g + 1) * 64]
    wq = w_in[:, g * 256:(g + 1) * 256]
    d = dict(consts)
    d["xT"] = np.ascontiguousarray(xb.T)
    d["gattn"] = gattn
    d["wqa"] = np.ascontiguousarray(np.concatenate([wq, sw(wq)], 1))
    d["wka"] = np.ascontiguousarray(np.concatenate([kv(0), kv(1), sw(kv(0)), kv(2), sw(kv(2)), kv(4), sw(kv(4))], 1))
    d["wtok"] = np.ascontiguousarray(np.concatenate([kv(3), kv(5)], 1))
    gc = [2560 + br * 16 + g * 4 + r for br in range(3) for r in range(4)]
    d["wg"] = np.ascontiguousarray(w_in[:, gc])
    d["w1s"] = np.ascontiguousarray(np.concatenate([w1_k[g].transpose(1, 0, 2), w1_v[g].transpose(1, 0, 2)], 0))
    d["peT"] = np.ascontiguousarray(np.concatenate([pe_k[g].T, pe_v[g].T], 0))
    d["w2s"] = np.ascontiguousarray(np.stack([w2_k[g], w2_v[g]], 1))
    d["ropeq"] = ropeq; d["ropek"] = ropek
    d["Epat"] = epat_table(xb.shape[0])
    return d


def declare_a(nc, S, pfx="", fused=False):
    d = {}

    def t(name, shape, kind="ExternalInput", dt=F32):
        d[name] = nc.dram_tensor(pfx + name, shape, dt, kind=kind).ap()

    t("xT", [1024, S]); t("gattn", [1024]); t("wqa", [1024, 512]); t("wka", [1024, 448]); t("wtok", [1024, 128]); t("wg", [1024, 12])
    t("w1s", [128, 32, 128]); t("peT", [128, 32]); t("w2s", [128, 2, 64]); t("ropeq", [64, 2, S]); t("ropek", [64, 2, S])
    t("pats", [128, 2, 512]); t("D16", [128, 2, 128]); t("tril", [128, 2, 128]); t("Epat", [64, S]); t("Sel", [12, 12, 64])
    t("slcm", [128, 8, 256])
    t("oT", [256, S], kind="Internal" if fused else "ExternalOutput")
    return d


def emit_a(nc, P, S, d, oT_dst=None):
    if oT_dst is None:
        oT_dst = lambda t0, n: d["oT"][:, t0:t0 + n]
    NBLK = S // 512
    NCH = S // 128
    with contextlib.ExitStack() as st:
        sb = lambda n, s, dt=F32: st.enter_context(nc.sbuf_tensor("a_" + n, s, dt))
        ps = lambda n, s, dt=F32: st.enter_context(nc.psum_tensor("a_" + n, s, dt))
        KsT = sb("KsT", [128, S], BF16); Vs = sb("Vs", [128, NCH, 128], BF16)
        KwT = sb("KwT", [128, 8, 128], BF16); Vw = sb("Vw", [128, 8, 128], BF16)
        KcT = sb("KcT", [128, 1024], BF16); Vc = sb("Vc", [128, 8, 128], BF16)
        slcm = sb("slcm", [128, 8, 256], BF16)
        Qaug = [sb(f"Qaug{i}", [128, 4, 512], BF16) for i in range(2)]
        pats = sb("pats", [128, 2, 512]); D16 = sb("D16", [128, 2, 128]); tril = sb("tril", [128, 2, 128], BF16)
        Sel = sb("Sel", [12, 12, 64])
        wqa = sb("wqa", [128, 8, 512], BF16); wka = sb("wka", [128, 8, 448], BF16); wtok = sb("wtok", [128, 8, 128], BF16)
        wg = sb("wg", [128, 8, 12], BF16)
        w1s = sb("w1s", [128, 32, 128], BF16); peT = sb("peT", [128, 32], BF16); w2s = sb("w2s", [128, 2, 64], BF16)
        hb = sb("hb", [128, 2]); g = sb("g", [128, 8])
        ones_b = sb("ones_b", [128, 128], BF16); ones_f = sb("ones_f", [128, 128]); identf = sb("identf", [128, 128])
        xT = sb("xT", [128, 8, 512]); sq = sb("sq", [128, 8, 512], BF16); rstd = sb("rstd", [128, 512]); xnb = sb("xnb", [128, 8, 512], BF16)
        rq = sb("rq", [64, 2, 512]); rk = sb("rk", [64, 2, 512])
        t1 = [sb(f"t1_{i}", [64, 512]) for i in range(2)]; t2 = [sb(f"t2_{i}", [64, 512]) for i in range(2)]
        Qd = sb("Qd", [64, 4, 4, 128], BF16)
        CV = sb("CV", [128, 528], BF16); hidk = sb("hidk", [128, 32], BF16); hvp = sb("hvp", [128, 128], BF16)
        gT = sb("gT", [12, 512])
        PTc = sb("PTc", [128, 8, 512], BF16); PT = [sb(f"PT{i}", [128, 512], BF16) for i in range(4)]
        zc = sb("zc", [1, 512]); impS = sb("impS", [128, 256]); scr = sb("scr", [128, 256]); m8 = sb("m8", [128, 16])
        NT = sb("NT", [128, 256])
        zrow = sb("zrow", [65, 512]); gbs = sb("gbs", [64, 512]); acc = [sb(f"acc{i}", [64, 512]) for i in range(2)]
        tmp = sb("tmp", [64, 512])
        pp0 = ps("pp0", [128, 512])
        ST = [ps(f"ST{i}", [128, 512]) for i in range(3)]
        OA = [ps(f"OA{i}", [128, 512]) for i in range(2)]
        IMP = ps("IMP", [128, 512]); AUX = ps("AUX", [128, 512])
        pp = [pp0, IMP]; ppk = ["pp0", "IMP"]

        for nm, tl in (("wqa", wqa), ("wka", wka), ("wtok", wtok), ("wg", wg)):
            P.D("pool", out=tl[:], in_=d[nm].rearrange("(m p) c -> p m c", p=128), w=[nm])
        for nm, tl in (("w1s", w1s), ("peT", peT), ("w2s", w2s), ("slcm", slcm), ("tril", tril)):
            P.D("pool", out=tl[:], in_=d[nm], w=[nm])
        for nm, tl in (("pats", pats), ("D16", D16), ("Sel", Sel)):
            P.D("sp", out=tl[:], in_=d[nm], w=[nm])
        P.D("pool", out=KsT[64:128, :], in_=d["Epat"], w=["KsE"])
        P.D("sp", out=g[:], in_=d["gattn"].rearrange("(m p) -> p m", p=128), w=["g"], allow_slow_non_contiguous=True)
        P.I("pool", "memset", w=["ones_b"], ap=ones_b[:], constant=1.0)
        P.I("pool", "memset", w=["ones_f"], ap=ones_f[:], constant=1.0)
        P.I("pool", "memset", w=["identf"], ap=identf[:], constant=1.0)
        P.I("pool", "affine_select", r=["identf"], w=["identf"], out=identf[:], in_=identf[:], pattern=[[-1, 128]],
            compare_op=ALU.is_equal, fill=0.0, base=0, channel_multiplier=1)
        P.I("pool", "memset", w=["Vs"], ap=Vs[:], constant=0.0)
        P.I("pool", "memset", r=["Vs"], w=["Vs"], ap=Vs[:, :, 64:65], constant=1.0)
        P.I("pool", "memset", w=["Vw"], ap=Vw[:], constant=0.0)
        P.I("pool", "memset", r=["Vw"], w=["Vw"], ap=Vw[:, :, 64:65], constant=1.0)
        P.I("pool", "memset", w=["KwT"], ap=KwT[:], constant=0.0)
        for i in range(2):
            P.I("pool", "memset", w=[f"Qaug{i}q", f"Qaug{i}m"], ap=Qaug[i][:], constant=0.0)
        P.I("pool", "memset", w=["KcT"], ap=KcT[:], constant=0.0)
        P.I("pool", "memset", w=["Vc"], ap=Vc[:], constant=0.0)
        P.I("pool", "memset", w=["CVk", "CVv"], ap=CV[:], constant=0.0)
        P.I("pool", "memset", w=["hvp"], ap=hvp[:], constant=0.0)
        for kvi in range(2):
            rows = slice(kvi * 64, kvi * 64 + 64)
            for l in range(32):
                P.MM(pp[0][:, kvi:kvi + 1], w1s[rows, l, :], peT[rows, l:l + 1], start=(l == 0), stop=(l == 31), r=["w1s", "peT"], w=["pp0"])
        P.I("dve", "tensor_copy", r=["pp0"], w=["hb"], out=hb[:], in_=pp[0][:, 0:2])

        cnt = {"pp": 0, "t": 0, "pt": 0, "oa": 0, "acc": 0}

        def proj(c0, M):
            i = cnt["pp"] % 2; cnt["pp"] += 1
            return pp[i], ppk[i]

        for blk in range(NBLK):
            tsl = slice(blk * 512, (blk + 1) * 512)
            P.D("sp", out=xT[:], in_=d["xT"].rearrange("(m p) t -> p m t", p=128)[:, :, tsl], w=["xT"])
            P.D("sp", out=rq[:], in_=d["ropeq"][:, :, tsl], w=["rq"])
            P.D("sp", out=rk[:], in_=d["ropek"][:, :, tsl], w=["rk"])
            P.I("act", "activation", r=["xT"], w=["sq"], out=sq[:], in_=xT[:], func=AF.Square)
            for m in range(8):
                P.MM(AUX[:], ones_b[:], sq[:, m, :], start=(m == 0), stop=(m == 7), r=["ones_b", "sq"], w=["AUX"])
            P.I("act", "activation", r=["AUX"], w=["rstd"], out=rstd[:], in_=AUX[:], func=AF.Sqrt, scale=1.0 / 1024, bias=EPS)
            P.I("dve", "reciprocal", r=["rstd"], w=["rstd"], out=rstd[:], in_=rstd[:])
            for m in range(8):
                P.I("dve", "scalar_tensor_tensor", r=["xT", "g", "rstd"], w=["xnb"], out=xnb[:, m, :], in0=xT[:, m, :],
                    scalar=g[:, m:m + 1], in1=rstd[:], op0=ALU.mult, op1=ALU.mult)

            def fmproj(wt, wk_, c0, M):
                p_, pk = proj(c0, M)
                for m in range(8):
                    P.MM(p_[0:M, :], wt[:, m, c0:c0 + M], xnb[:, m, :], start=(m == 0), stop=(m == 7), r=[wk_, "xnb"], w=[pk])
                return p_, pk

            def rope(wt, wk_, ca, cb, tab, tabk, out_ap, outk):
                pa, pak = fmproj(wt, wk_, ca, 64)
                i = cnt["t"] % 2; cnt["t"] += 1
                P.I("dve", "tensor_tensor", r=[pak, tabk], w=[f"t1_{i}"], out=t1[i][:], in0=pa[0:64, :], in1=tab[:, 0, :], op=ALU.mult)
                pb, pbk = fmproj(wt, wk_, cb, 64)
                P.I("dve", "tensor_tensor", r=[pbk, tabk], w=[f"t2_{i}"], out=t2[i][:], in0=pb[0:64, :], in1=tab[:, 1, :], op=ALU.mult)
                a_, b_ = t1[i][:], t2[i][:]
                if len(out_ap.shape) == 3:
                    a_ = a_.rearrange("p (a q) -> p a q", a=4); b_ = b_.rearrange("p (a q) -> p a q", a=4)
                P.I("pool", "tensor_tensor", r=[f"t1_{i}", f"t2_{i}"], w=[outk], out=out_ap, in0=a_, in1=b_, op=ALU.add)

            for r in range(4):
                rope(wqa, "wqa", r * 64, 256 + r * 64, rq, "rq", Qd[:, :, r, :], "Qd")
            rope(wka, "wka", 192, 256, rk, "rk", KsT[0:64, tsl], "KsT")
            rope(wka, "wka", 320, 384, rk, "rk", KwT[0:64, (blk % 2) * 4:(blk % 2) * 4 + 4, :], "KwT")
            pa, pak = fmproj(wka, "wka", 0, 128)
            i = cnt["t"] % 2; cnt["t"] += 1
            P.I("dve", "tensor_tensor", r=[pak, "rk"], w=[f"t1_{i}"], out=t1[i][:], in0=pa[0:64, :], in1=rk[:, 0, :], op=ALU.mult)
            P.I("act", "copy", r=[pak], w=["CVv"], out=CV[64:128, 16:528], in_=pa[64:128, :])
            pb, pbk = fmproj(wka, "wka", 128, 64)
            P.I("dve", "tensor_tensor", r=[pbk, "rk"], w=[f"t2_{i}"], out=t2[i][:], in0=pb[0:64, :], in1=rk[:, 1, :], op=ALU.mult)
            P.I("pool", "tensor_tensor", r=[f"t1_{i}", f"t2_{i}"], w=["CVk"], out=CV[0:64, 16:528], in0=t1[i][:], in1=t2[i][:], op=ALU.add)
            pgt, pgk = fmproj(wg, "wg", 0, 12)
            P.I("act", "activation", r=[pgk], w=["gT"], out=gT[:], in_=pgt[0:12, :], func=AF.Sigmoid)
            for t4 in range(4):
                ch = blk * 4 + t4
                i = cnt["pp"] % 2; cnt["pp"] += 1
                for m in range(8):
                    P.MM(pp[i][:, 0:128], xnb[:, m, t4 * 128:(t4 + 1) * 128], wtok[:, m, :], start=(m == 0), stop=(m == 7),
                         r=["wtok", "xnb"], w=[ppk[i]])
                P.I("act", "copy", r=[ppk[i]], w=["Vs"], out=Vs[:, ch, 0:64], in_=pp[i][:, 0:64])
                P.I("act", "copy", r=[ppk[i]], w=["Vw"], out=Vw[:, ch % 8, 0:64], in_=pp[i][:, 64:128])
            CVv = CV[:].rearrange("p (c s) -> p c s", s=16)
            i = cnt["pp"] % 2; cnt["pp"] += 1
            for l in range(32):
                P.MM(pp[i][:, 0:32], w1s[0:64, l, :], CVv[0:64, l // 16:l // 16 + 32, l % 16], start=(l == 0), stop=(l == 31),
                     r=["w1s", "CVk"], w=[ppk[i]])
            P.I("act", "activation", r=[ppk[i], "hb"], w=["hidk"], out=hidk[:], in_=pp[i][:, 0:32], func=AF.Gelu_apprx_tanh, bias=hb[:, 0:1])
            i2 = cnt["pp"] % 2; cnt["pp"] += 1
            P.MM(pp[i2][0:64, 0:32], w2s[:, 0, :], hidk[:], r=["w2s", "hidk"], w=[ppk[i2]])
            P.I("dve", "tensor_copy", r=[ppk[i2]], w=["KcT"], out=KcT[0:64, 32 * blk:32 * blk + 32], in_=pp[i2][0:64, 0:32])
            i = cnt["pp"] % 2; cnt["pp"] += 1
            for l in range(32):
                P.MM(pp[i][:, 0:32], w1s[64:128, l, :], CVv[64:128, l // 16:l // 16 + 32, l % 16], start=(l == 0), stop=(l == 31),
                     r=["w1s", "CVv"], w=[ppk[i]])
            off = (32 * blk) % 128
            P.I("act", "activation", r=[ppk[i], "hb"], w=["hvp"], out=hvp[:, off:off + 32], in_=pp[i][:, 0:32], func=AF.Gelu_apprx_tanh, bias=hb[:, 1:2])
            i2 = cnt["pp"] % 2; cnt["pp"] += 1
            P.MM(pp[i2][:, 0:64], hvp[:], w2s[:, 1, :], r=["w2s", "hvp"], w=[ppk[i2]])
            P.I("dve", "tensor_copy", r=[ppk[i2]], w=["Vc"], out=Vc[off:off + 32, (32 * blk) // 128, 0:64], in_=pp[i2][off:off + 32, 0:64])
            P.I("act", "copy", r=["CVk", "CVv"], w=["CVk", "CVv"], out=CV[:, 0:16], in_=CV[:, 512:528])

            for qi in range(4):
                QB = blk * 4 + qi
                t0 = 128 * QB
                Qb = Qd[:, qi, :, :].rearrange("p r q -> p (r q)")

                def combine(br, ot, otk, normalize):
                    ai = cnt["acc"] % 2
                    for r in range(4):
                        P.MM(AUX[0:64, r * 128:(r + 1) * 128], Sel[:, br * 4 + r, :], gT[:, qi * 128:(qi + 1) * 128], r=["Sel", "gT"], w=["AUX"])
                    P.I("act", "copy", r=["AUX"], w=["gbs"], out=gbs[:], in_=AUX[0:64, :])
                    if normalize:
                        P.I("dve", "tensor_scalar", r=[otk], w=["zrow"], out=zrow[64:65, :], in0=ot[64:65, :], scalar1=1e-30, scalar2=None, op0=ALU.max)
                        P.I("dve", "reciprocal", r=["zrow"], w=["zrow"], out=zrow[64:65, :], in_=zrow[64:65, :])
                        P.MM(AUX[0:64, :], ones_f[64:65, 0:64], zrow[64:65, :], r=["ones_f", "zrow"], w=["AUX"])
                        P.I("dve", "tensor_tensor", r=["gbs", "AUX"], w=["gbs"], out=gbs[:], in0=gbs[:], in1=AUX[0:64, :], op=ALU.mult)
                    if br == 0:
                        P.I("dve", "tensor_tensor", r=[otk, "gbs"], w=[f"acc{ai}"], out=acc[ai][:], in0=ot[0:64, :], in1=gbs[:], op=ALU.mult)
                    else:
                        P.I("dve", "tensor_tensor", r=[otk, "gbs"], w=["tmp"], out=tmp[:], in0=ot[0:64, :], in1=gbs[:], op=ALU.mult)
                        P.I("pool", "tensor_tensor", r=["tmp", f"acc{ai}"], w=[f"acc{ai}"], out=acc[ai][:], in0=acc[ai][:], in1=tmp[:], op=ALU.add)

                qa = QB % 2
                ng = QB // 32 + 1
                P.I("pool", "tensor_copy", r=["Qd"], w=[f"Qaug{qa}q"], out=Qaug[qa][0:64, 0:ng, :],
                    in_=Qb.unsqueeze(1).to_broadcast([64, ng, 512]))
                Qfull = Qaug[qa][:, 0, :]
                Qr = [f"Qaug{qa}q", f"Qaug{qa}m"]
                jmax = (t0 + 112) // 2048
                nj = jmax + 1
                for j in range(nj):
                    si = cnt["pt"] % 3; cnt["pt"] += 1
                    P.MM(ST[si][:], KcT[:, j * 128:(j + 1) * 128], Qfull, r=["KcT"] + Qr, w=[f"ST{si}"])
                    P.I("act", "activation", r=[f"ST{si}"], w=[f"PTc{j}"], out=PTc[:, j, :], in_=ST[si][:], func=AF.Exp)
                    delta = t0 - 2048 * j - 15
                    if j == 0 or delta < 2032:
                        P.I("dve", "scalar_tensor_tensor", r=["D16", f"PTc{j}"], w=[f"PTc{j}"], out=PTc[:, j, :].rearrange("p (r q) -> p r q", r=4),
                            in0=D16[:, 1 if j == 0 else 0, :].unsqueeze(1).to_broadcast([128, 4, 128]), scalar=float(delta),
                            in1=PTc[:, j, :].rearrange("p (r q) -> p r q", r=4), op0=ALU.is_le, op1=ALU.mult)
                    P.MM(AUX[0:1, :], ones_b[:, 0:1], PTc[:, j, :], start=(j == 0), stop=(j == jmax), r=["ones_b", f"PTc{j}"], w=["AUX"])
                P.I("dve", "tensor_scalar", r=["AUX"], w=["zc"], out=zc[:], in0=AUX[0:1, :], scalar1=1e-30, scalar2=None, op0=ALU.max)
                P.I("dve", "reciprocal", r=["zc"], w=["zc"], out=zc[:], in_=zc[:])
                P.MM(AUX[:], ones_f[0:1, :], zc[0:1, :], r=["ones_f", "zc"], w=["AUX"])
                pk_all = [f"PTc{j}" for j in range(nj)]
                P.I("dve", "tensor_tensor", r=pk_all + ["AUX"], w=pk_all, out=PTc[:, 0:nj, :], in0=PTc[:, 0:nj, :],
                    in1=AUX[:].unsqueeze(1).to_broadcast([128, nj, 512]), op=ALU.mult)
                oi = cnt["oa"] % 2; cnt["oa"] += 1
                for j in range(nj):
                    P.MM(OA[oi][:], Vc[:, j, :], PTc[:, j, :], start=(j == 0), stop=(j == jmax), r=["Vc", f"PTc{j}"], w=[f"OA{oi}"])
                for j in range(nj):
                    for r in range(4):
                        P.MM(IMP[:, 0:256], PTc[:, j, r * 128:(r + 1) * 128], slcm[:, j, :], start=(j == 0 and r == 0),
                             stop=(j == jmax and r == 3), r=["slcm", f"PTc{j}"], w=["IMP"])
                combine(0, OA[oi], f"OA{oi}", False)
                jb = 2 * QB
                P.I("dve", "tensor_tensor", r=["IMP", "pats"], w=["impS"], out=impS[:], in0=IMP[:, 0:256], in1=pats[:, 0, 256 - jb:512 - jb], op=ALU.mult)
                P.I("dve", "tensor_tensor", r=["impS", "pats"], w=["impS"], out=impS[:], in0=impS[:], in1=pats[:, 1, 256 - jb:512 - jb], op=ALU.add)
                P.I("dve", "memset", r=["impS"], w=["impS"], ap=impS[:, 0:1], constant=10002.0)
                P.I("dve", "max", r=["impS"], w=["m8"], out=m8[:, 0:8], in_=impS[:])
                P.I("dve", "match_replace", r=["impS", "m8"], w=["scr"], out=scr[:], in_to_replace=m8[:, 0:8], in_values=impS[:], imm_value=-2.0)
                P.I("dve", "max", r=["scr"], w=["m8"], out=m8[:, 8:16], in_=scr[:])
                P.I("dve", "tensor_scalar", r=["impS", "m8"], w=["NT"], out=NT[:], in0=impS[:], scalar1=m8[:, 15:16], scalar2=1.0,
                    op0=ALU.is_ge, op1=ALU.subtract)
                for jt in range(2):
                    P.op("pe", (lambda jt: (lambda e: e.transpose(out=IMP[:, jt * 128:(jt + 1) * 128], in_=NT[:, jt * 128:(jt + 1) * 128], identity=identf[:])))(jt),
                         reads=["NT", "identf"], writes=["IMP"])
                for g_ in range(ng):
                    half = g_ % 2
                    P.I("act", "copy", r=["IMP"], w=[f"Qaug{qa}m"], out=Qaug[qa][64:128, g_, :].rearrange("p (r q) -> p r q", r=4),
                        in_=IMP[64 * half:64 * half + 64, (g_ // 2) * 128:(g_ // 2 + 1) * 128].unsqueeze(1).to_broadcast([64, 4, 128]))

                for br in (1, 2):
                    kcs = list(range(0, QB + 1)) if br == 1 else list(range(max(0, QB - 4), QB + 1))
                    oi = cnt["oa"] % 2; cnt["oa"] += 1
                    ot = OA[oi]; otk = f"OA{oi}"
                    base = cnt["pt"]

                    def qk(n, br=br, kcs=kcs, base=base, qa=qa, Qfull=Qfull, Qr=Qr):
                        kc = kcs[n]; si = (base + n) % 3
                        if br == 1:
                            P.MM(ST[si][:], KsT[:, kc * 128:(kc + 1) * 128], Qaug[qa][:, kc // 32, :],
                                 r=["KsT", "KsE", f"Qaug{qa}q", f"Qaug{qa}m"], w=[f"ST{si}"])
                        else:
                            P.MM(ST[si][:], KwT[:, kc % 8, :], Qfull, r=["KwT"] + Qr, w=[f"ST{si}"])

                    qk(0)
                    if len(kcs) > 1:
                        qk(1)
                    for n, kc in enumerate(kcs):
                        if n + 2 < len(kcs):
                            qk(n + 2)
                        si = (base + n) % 3
                        pi = cnt["pt"] % 4; cnt["pt"] += 1
                        pt = PT[pi]; ptk = f"PT{pi}"
                        P.I("act", "activation", r=[f"ST{si}"], w=[ptk], out=pt[:], in_=ST[si][:], func=AF.Exp)
                        if kc == QB:
                            P.I("dve", "tensor_tensor", r=[ptk, "tril"], w=[ptk], out=pt[:].rearrange("p (r q) -> p r q", r=4),
                                in0=pt[:].rearrange("p (r q) -> p r q", r=4), in1=tril[:, 0, :].unsqueeze(1).to_broadcast([128, 4, 128]), op=ALU.mult)
                        if br == 2 and kc == QB - 4:
                            P.I("dve", "tensor_tensor", r=[ptk, "tril"], w=[ptk], out=pt[:].rearrange("p (r q) -> p r q", r=4),
                                in0=pt[:].rearrange("p (r q) -> p r q", r=4), in1=tril[:, 1, :].unsqueeze(1).to_broadcast([128, 4, 128]), op=ALU.mult)
                        vv = Vs[:, kc, :] if br == 1 else Vw[:, kc % 8, :]
                        P.MM(ot[:], vv, pt[:], start=(n == 0), stop=(n == len(kcs) - 1), r=["Vs" if br == 1 else "Vw", ptk], w=[otk])
                    combine(br, ot, otk, True)
                ai = cnt["acc"] % 2; cnt["acc"] += 1
                P.D("sp", out=oT_dst(t0, 128).rearrange("(r x) q -> x r q", x=64), in_=acc[ai][:].rearrange("p (r q) -> p r q", r=4),
                    r=[f"acc{ai}"], w=[f"oT{QB}"])
        P.wait_all("sp", [f"oT{q}" for q in range(NBLK * 4)])
        P.barrier()
        P.emit()


EPS = 1e-6
NEG = -1.0e30


def declare_b(nc, T, pfx=""):
    d = {}

    def inp(name, shape, dt=F32):
        d[name] = nc.dram_tensor(pfx + name, shape, dt, kind="ExternalInput").ap()

    def outp(name, shape, dt=F32):
        d[name] = nc.dram_tensor(pfx + name, shape, dt, kind="ExternalOutput").ap()

    def scr(name, shape, dt=F32):
        d[name] = nc.dram_tensor(pfx + name, shape, dt, kind="Internal").ap()

    inp("hT", [1024, T]); inp("oT", [1024, T]); inp("wout", [1024, 1024]); inp("gffn", [1024])
    inp("wq", [1024, 2048]); inp("keysT", [16, 128, 128]); inp("uT", [1024, 16384]); inp("v", [16384, 1024])
    inp("gnext", [1024])
    outp("hT_out", [1024, T]); outp("nT_out", [1024, T])
    scr("h2T", [1024, T]); scr("hnbf", [1024, T], BF16); scr("qTd", [128, T // 128, 16, 128])
    scr("GT", [T // 128, 128, 16384], BF16); scr("uTbf", [1024, 16384], BF16); scr("vbf", [16384, 1024], BF16)
    return d


def emit_b(nc, P, T, d, cast_weights=True, pfx="", oT_blk=None, nT_dst=None):
    NB = T // 512
    NT = T // 128
    NB2 = T // 256
    fm = lambda ap: ap.rearrange("(m p) t -> p m t", p=128)

    if cast_weights:
        for i in range(8):
            P.D("pool", out=d["uTbf"][i * 128:(i + 1) * 128, :], in_=d["uT"][i * 128:(i + 1) * 128, :], w=[f"uTbf{i}"])
        for i in range(8):
            P.D("pool", out=d["vbf"][i * 2048:(i + 1) * 2048, :], in_=d["v"][i * 2048:(i + 1) * 2048, :], w=[f"vbf{i}"])

    with contextlib.ExitStack() as st:
        sb = lambda n, s, dt=F32: st.enter_context(nc.sbuf_tensor(pfx + "p0_" + n, s, dt))
        ps = lambda n, s, dt=F32: st.enter_context(nc.psum_tensor(pfx + "p0_" + n, s, dt))
        wout = sb("wout", [128, 8, 1024], BF16)
        g = sb("g", [128, 8]); ones = sb("ones", [128, 128])
        hTt = [sb(f"hTt{i}", [128, 8, 512]) for i in range(2)]
        oTt = [sb(f"oTt{i}", [128, 8, 512], BF16) for i in range(2)]
        h2 = sb("h2", [128, 8, 512]); hnb = sb("hnb", [128, 8, 512], BF16)
        rstd = sb("rstd", [128, 512])
        wqt = [sb(f"wqt{i}", [128, 8, 512]) for i in range(2)]
        qs = [sb(f"qs{i}", [128, 4, 512]) for i in range(2)]
        pp = [ps(f"pp{i}", [128, 512]) for i in range(2)]
        ss = ps("ss", [128, 512])

        P.D("pool", out=wout[:], in_=d["wout"].rearrange("(k p) c -> p k c", p=128), w=["wout"])
        P.D("sp", out=g[:], in_=d["gffn"].rearrange("(m p) -> p m", p=128), w=["g"], allow_slow_non_contiguous=True)
        P.I("pool", "memset", w=["ones"], ap=ones[:], constant=1.0)
        nmm = 0
        nwq = 0
        if oT_blk is not None:
            P.dma("sp", lambda e: e.dma_start(out=d["oTq"].rearrange("r (c t) -> c r t", t=min(1024, T)), in_=oT_blk()), writes=["oTq"])
        for b in range(NB):
            tsl = slice(b * 512, (b + 1) * 512)
            ht, ot = hTt[b % 2], oTt[b % 2]
            hk, ok = f"hTt{b % 2}", f"oTt{b % 2}"
            P.D("sp", out=ht[:], in_=fm(d["hT"])[:, :, tsl], w=[hk])
            if oT_blk is None:
                P.D("pool", out=ot[:], in_=fm(d["oT"])[:, :, tsl], w=[ok])
            else:
                P.D("pool", out=ot[:], in_=fm(d["oTq"])[:, :, tsl], r=["oTq"], w=[ok])
            for m in range(8):
                p_ = pp[nmm % 2]; pk = f"pp{nmm % 2}"; nmm += 1
                for k in range(8):
                    P.MM(p_[:], wout[:, k, m * 128:(m + 1) * 128], ot[:, k, :], start=(k == 0), stop=(k == 7),
                         r=["wout", ok], w=[pk])
                P.I("dve", "tensor_tensor", r=[pk, hk], w=["h2"], out=h2[:, m, :], in0=p_[:], in1=ht[:, m, :], op=ALU.add)
            P.D("sp", out=fm(d["h2T"])[:, :, tsl], in_=h2[:], r=["h2"], w=[f"h2T{b}"])
            P.I("act", "activation", r=["h2"], w=[hk], out=ht[:], in_=h2[:], func=AF.Square)
            for m in range(8):
                P.MM(ss[:], ones[:], ht[:, m, :], start=(m == 0), stop=(m == 7), r=["ones", hk], w=["ss"])
            P.I("act", "activation", r=["ss"], w=["rstd"], out=rstd[:], in_=ss[:], func=AF.Sqrt, scale=1.0 / 1024, bias=EPS)
            P.I("dve", "reciprocal", r=["rstd"], w=["rstd"], out=rstd[:], in_=rstd[:])
            for m in range(8):
                P.I("dve", "scalar_tensor_tensor", r=["h2", "g", "rstd"], w=["h2"], out=h2[:, m, :], in0=h2[:, m, :],
                    scalar=g[:, m:m + 1], in1=rstd[:], op0=ALU.mult, op1=ALU.mult)
            P.I("pool", "tensor_copy", r=["h2"], w=["hnb"], out=hnb[:], in_=h2[:])
            P.D("sp", out=fm(d["hnbf"])[:, :, tsl], in_=hnb[:], r=["hnb"], w=[f"hnbf{b}"])
            for jg in range(4):
                wt = wqt[nwq % 2]; wk = f"wqt{nwq % 2}"; q_ = qs[nwq % 2]; qk = f"qs{nwq % 2}"; nwq += 1
                P.D("sp", out=wt[:], in_=d["wq"].rearrange("(m p) c -> p m c", p=128)[:, :, jg * 512:(jg + 1) * 512], w=[wk])
                for jj in range(4):
                    p_ = pp[nmm % 2]; pk = f"pp{nmm % 2}"; nmm += 1
                    for m in range(8):
                        P.MM(p_[:], wt[:, m, jj * 128:(jj + 1) * 128], h2[:, m, :], start=(m == 0), stop=(m == 7),
                             r=[wk, "h2"], w=[pk])
                    P.I("act", "copy", r=[pk], w=[qk], out=q_[:, jj, :], in_=p_[:])
                for t4 in range(4):
                    P.D("sp", out=d["qTd"][:, b * 4 + t4, jg * 4:(jg + 1) * 4, :], in_=q_[:, :, t4 * 128:(t4 + 1) * 128],
                        r=[qk], w=[f"qTd{b}_{jg}_{t4}"])
        P.barrier()
        P.emit()

    with contextlib.ExitStack() as st:
        sb = lambda n, s, dt=F32: st.enter_context(nc.sbuf_tensor(pfx + "p1_" + n, s, dt))
        ps = lambda n, s, dt=F32: st.enter_context(nc.psum_tensor(pfx + "p1_" + n, s, dt))
        keys = sb("keys", [128, 16, 128]); ident = sb("ident", [128, 128], BF16); identf = sb("identf", [128, 128])
        qt = [sb(f"qt{i}", [128, 16, 128]) for i in range(2)]
        S12 = [sb(f"S12{i}", [128, 16, 128]) for i in range(2)]
        scr_ = sb("scr", [128, 256]); TS = sb("TS", [128, 16, 16]); cand = sb("cand", [128, 8, 256]); BS = sb("BS", [128, 8, 16])
        ex = sb("ex", [128, 8, 16]); Z = sb("Z", [128, 8]); lnZ = sb("lnZ", [128, 8]); bias = sb("bias", [128, 8])
        SUM = [sb(f"SUM{i}", [128, 8, 128]) for i in range(6)]
        E = [sb(f"E{i}", [128, 1024], BF16) for i in range(3)]
        GH = [[sb(f"GH{i}_{h}", [128, 1024], BF16) for h in range(8)] for i in range(2)]
        GTp = [sb(f"GTp{i}", [128, 8, 128], BF16) for i in range(4)]
        sc = ps("sc", [128, 16, 128])
        acc = [ps(f"acc{i}", [128, 4, 128]) for i in range(2)]

        P.D("sp", out=keys[:], in_=d["keysT"].rearrange("j c k -> c j k"), w=["keys"])
        P.I("pool", "memset", w=["identf"], ap=identf[:], constant=1.0)
        P.I("pool", "affine_select", r=["identf"], w=["identf"], out=identf[:], in_=identf[:], pattern=[[-1, 128]],
            compare_op=ALU.is_equal, fill=0.0, base=0, channel_multiplier=1)
        P.I("pool", "tensor_copy", r=["identf"], w=["ident"], out=ident[:], in_=identf[:])
        nsum = 0; nacc = 0; ngtp = 0; ngh = 0
        for tt in range(NT):
            q_ = qt[tt % 2]; qk = f"qt{tt % 2}"; S = S12[tt % 2]; Sk = f"S12{tt % 2}"
            P.D("sp", out=q_[:], in_=d["qTd"][:, tt, :, :], w=[qk])
            for j in range(16):
                P.MM(sc[:, j, :], q_[:, j, :], keys[:, j, :], r=[qk, "keys"], w=["sc"])
            P.I("act", "copy", r=["sc"], w=[Sk + "a"], out=S[:, 0:8, :], in_=sc[:, 0:8, :])
            P.I("dve", "tensor_copy", r=["sc"], w=[Sk + "b"], out=S[:, 8:16, :], in_=sc[:, 8:16, :])
            Sr = [Sk + "a", Sk + "b"]
            for j in range(16):
                P.I("dve", "max", r=Sr, w=["TS"], out=TS[:, j, 0:8], in_=S[:, j, :])
                P.I("dve", "match_replace", r=Sr + ["TS"], w=["scr"], out=scr_[:, 0:128], in_to_replace=TS[:, j, 0:8],
                    in_values=S[:, j, :], imm_value=NEG)
                P.I("dve", "max", r=["scr"], w=["TS"], out=TS[:, j, 8:16], in_=scr_[:, 0:128])
            TS4 = TS[:].rearrange("p (h two) a -> p h two a", two=2)
            P.I("dve", "tensor_tensor", r=["TS"], w=["cand"], out=cand[:].rearrange("p h (a b) -> p h a b", b=16),
                in0=TS4[:, :, 0, :].unsqueeze(3).to_broadcast([128, 8, 16, 16]),
                in1=TS4[:, :, 1, :].unsqueeze(2).to_broadcast([128, 8, 16, 16]), op=ALU.add)
            for h in range(8):
                P.I("dve", "max", r=["cand"], w=["BS"], out=BS[:, h, 0:8], in_=cand[:, h, :])
                P.I("dve", "match_replace", r=["cand", "BS"], w=["scr"], out=scr_[:, 0:256], in_to_replace=BS[:, h, 0:8],
                    in_values=cand[:, h, :], imm_value=NEG)
                P.I("dve", "max", r=["scr"], w=["BS"], out=BS[:, h, 8:16], in_=scr_[:, 0:256])
            P.I("dve", "tensor_tensor", r=["BS"], w=["ex"], out=ex[:], in0=BS[:], in1=BS[:, :, 0:1].to_broadcast([128, 8, 16]),
                op=ALU.subtract)
            P.I("act", "activation", r=["ex"], w=["ex"], out=ex[:], in_=ex[:], func=AF.Exp)
            P.I("dve", "reduce_sum", r=["ex"], w=["Z"], out=Z[:], in_=ex[:], axis=AX.X)
            P.I("act", "activation", r=["Z"], w=["lnZ"], out=lnZ[:], in_=Z[:], func=AF.Ln)
            P.I("dve", "scalar_tensor_tensor", r=["BS", "lnZ"], w=["bias"], out=bias[:], in0=BS[:, :, 0], scalar=-1.0,
                in1=lnZ[:], op0=ALU.mult, op1=ALU.subtract)
            def add_op(it):
                sx_, h_ = it // 8, it % 8
                su = SUM[it % 6]; sk = f"SUM{it % 6}"
                if False:
                    for a in range(8):
                        P.I("act", "activation", r=Sr, w=[sk], out=su[:, a, :], in_=S[:, 2 * h_ + 1, :], func=AF.Identity,
                            bias=S[:, 2 * h_, sx_ * 8 + a:sx_ * 8 + a + 1], scale=1.0)
                else:
                    P.I("dve", "tensor_tensor", r=Sr, w=[sk], out=su[:],
                        in0=S[:, 2 * h_, sx_ * 8:(sx_ + 1) * 8].unsqueeze(2).to_broadcast([128, 8, 128]),
                        in1=S[:, 2 * h_ + 1, :].unsqueeze(1).to_broadcast([128, 8, 128]), op=ALU.add)

            LOOK = 4
            deferred = []
            for it0 in range(LOOK):
                add_op(it0)
            for sx in range(16):
                ghs = GH[ngh % 2]; gk = f"GH{ngh % 2}_"; ngh += 1
                for h in range(8):
                    it = sx * 8 + h
                    if it + LOOK < 128:
                        add_op(it + LOOK)
                    if h == 4 and deferred:
                        deferred.pop(0)()
                    su = SUM[it % 6]; sk = f"SUM{it % 6}"; e_ = E[it % 3]; ek = f"E{it % 3}"
                    P.I("act", "activation", r=[sk, "bias"], w=[ek], out=e_[:], in_=su[:].rearrange("p a b -> p (a b)"),
                        func=AF.Exp, bias=bias[:, h:h + 1])
                    P.I("dve", "scalar_tensor_tensor", r=[sk, ek, "BS"], w=[gk + str(h)], out=ghs[h][:],
                        in0=su[:].rearrange("p a b -> p (a b)"), scalar=BS[:, h, 15:16], in1=e_[:], op0=ALU.is_ge, op1=ALU.mult)
                def flush(sx=sx, tt=tt, ghs=ghs, gk=gk):
                    nonlocal nacc, ngtp
                    gp = GTp[ngtp % 4]; gpk = f"GTp{ngtp % 4}"; ngtp += 1
                    for c4 in range(2):
                        a_ = acc[nacc % 2]; ak = f"acc{nacc % 2}"; nacc += 1
                        for ci in range(4):
                            c = c4 * 4 + ci
                            for h in range(8):
                                P.MM(a_[:, ci, :], ghs[h][:, c * 128:(c + 1) * 128], ident[:], start=(h == 0), stop=(h == 7),
                                     r=[gk + str(h), "ident"], w=[ak])
                        P.I("act", "copy", r=[ak], w=[gpk], out=gp[:, c4 * 4:(c4 + 1) * 4, :], in_=a_[:])
                    P.D("sp", out=d["GT"][tt, :, sx * 1024:(sx + 1) * 1024], in_=gp[:].rearrange("p c t -> p (c t)"), r=[gpk],
                        w=[f"GT{tt}_{sx}"])
                deferred.append(flush)
            while deferred:
                deferred.pop(0)()
        P.barrier()
        P.emit()

    NB5 = T // 512
    with contextlib.ExitStack() as st:
        sb = lambda n, s, dt=F32: st.enter_context(nc.sbuf_tensor(pfx + "p2_" + n, s, dt))
        ps = lambda n, s, dt=F32: st.enter_context(nc.psum_tensor(pfx + "p2_" + n, s, dt))
        U = [sb(f"U{i}", [128, 8, 1024], BF16) for i in range(2)]
        V = [sb(f"V{i}", [128, 8, 1024], BF16) for i in range(2)]
        Gg = [sb(f"Gg{i}", [128, 4, 8, 128], BF16) for i in range(2)]
        hn2 = [sb(f"hn2{i}", [128, 8, 512], BF16) for i in range(2)]
        ge = [sb(f"ge{i}", [128, 512], BF16) for i in range(2)]
        gh = [sb(f"gh{i}", [128, 8, 512], BF16) for i in range(2)]
        Yacc = sb("Yacc", [128, 8, 512]); h2b = sb("h2b", [128, 8, 512]); sq2 = sb("sq2", [128, 8, 512])
        rstd2 = sb("rstd2", [128, 512]); nrm = sb("nrm", [128, 8, 512], d["nT_out"].dtype)
        gn = sb("gn", [128, 8]); ones2 = sb("ones2", [128, 128])
        Hp = [ps(f"Hp{i}", [128, 512]) for i in range(2)]
        Yp = ps("Yp", [128, 4, 512])
        ss2 = ps("ss2", [128, 512])
        P.D("sp", out=gn[:], in_=d["gnext"].rearrange("(m p) -> p m", p=128), w=["gn"], allow_slow_non_contiguous=True)
        P.I("pool", "memset", w=["ones2"], ap=ones2[:], constant=1.0)
        nw = 0; nh = 0
        for blk in range(NB5):
            tsl = slice(blk * 512, (blk + 1) * 512)
            hb = hn2[blk % 2]; hbk = f"hn2{blk % 2}"
            P.D("sp", out=hb[:], in_=fm(d["hnbf"])[:, :, tsl], w=[hbk])
            P.D("sp", out=h2b[:], in_=fm(d["h2T"])[:, :, tsl], w=["h2b"])
            for eg in range(16):
                u_ = U[nw % 2]; uk = f"U{nw % 2}"; v_ = V[nw % 2]; vk = f"V{nw % 2}"; g_ = Gg[nw % 2]; gk = f"Gg{nw % 2}"
                gh_ = gh[nw % 2]; ghk = f"gh{nw % 2}"; nw += 1
                P.D("sp", out=u_[:], in_=d["uTbf"].rearrange("(m p) e -> p m e", p=128)[:, :, eg * 1024:(eg + 1) * 1024], w=[uk])
                P.D("act", out=v_[:], in_=d["vbf"].rearrange("(c p) x -> p c x", p=128)[:, eg * 8:(eg + 1) * 8, :], w=[vk])
                for t4 in range(4):
                    P.D("sp", out=g_[:, t4, :, :], in_=d["GT"][blk * 4 + t4, :, eg * 1024:(eg + 1) * 1024].rearrange("p (c t) -> p c t", t=128),
                        w=[gk + str(t4)])
                gks = [gk + str(t4) for t4 in range(4)]
                for c in range(8):
                    hp = Hp[nh % 2]; hpk = f"Hp{nh % 2}"; ge_ = ge[nh % 2]; gek = f"ge{nh % 2}"; nh += 1
                    for m in range(8):
                        P.MM(hp[:], u_[:, m, c * 128:(c + 1) * 128], hb[:, m, :], start=(m == 0), stop=(m == 7), r=[uk, hbk], w=[hpk])
                    P.I("act", "activation", r=[hpk], w=[gek], out=ge_[:], in_=hp[:], func=AF.Gelu_apprx_tanh)
                    P.I("dve", "tensor_tensor", r=[gek] + gks, w=[ghk + str(c)], out=gh_[:, c, :].rearrange("p (tt t) -> p tt t", t=128),
                        in0=ge_[:].rearrange("p (tt t) -> p tt t", t=128), in1=g_[:, :, c, :], op=ALU.mult)
                ghs = [ghk + str(c) for c in range(8)]
                for half in range(2):
                    for mi in range(4):
                        m = half * 4 + mi
                        for c in range(8):
                            P.MM(Yp[:, mi, :], v_[:, c, m * 128:(m + 1) * 128], gh_[:, c, :], start=(c == 0), stop=(c == 7), r=[vk] + ghs, w=["Yp"])
                    if eg == 0:
                        P.I("dve", "tensor_copy", r=["Yp"], w=[f"Yacc{half}"], out=Yacc[:, half * 4:(half + 1) * 4, :], in_=Yp[:])
                    else:
                        P.I("dve", "tensor_tensor", r=["Yp", f"Yacc{half}"], w=[f"Yacc{half}"], out=Yacc[:, half * 4:(half + 1) * 4, :],
                            in0=Yp[:], in1=Yacc[:, half * 4:(half + 1) * 4, :], op=ALU.add)
            P.I("dve", "tensor_tensor", r=["Yacc0", "Yacc1", "h2b"], w=["h2b"], out=h2b[:], in0=Yacc[:], in1=h2b[:], op=ALU.add)
            P.D("sp", out=fm(d["hT_out"])[:, :, tsl], in_=h2b[:], r=["h2b"], w=[f"hT_out{blk}"])
            P.I("act", "activation", r=["h2b"], w=["sq2"], out=sq2[:], in_=h2b[:], func=AF.Square)
            for m in range(8):
                P.MM(ss2[:], ones2[:], sq2[:, m, :], start=(m == 0), stop=(m == 7), r=["ones2", "sq2"], w=["ss2"])
            P.I("act", "activation", r=["ss2"], w=["rstd2"], out=rstd2[:], in_=ss2[:], func=AF.Sqrt, scale=1.0 / 1024, bias=EPS)
            P.I("dve", "reciprocal", r=["rstd2"], w=["rstd2"], out=rstd2[:], in_=rstd2[:])
            for m in range(8):
                P.I("dve", "scalar_tensor_tensor", r=["h2b", "gn", "rstd2"], w=["nrm"], out=nrm[:, m, :], in0=h2b[:, m, :],
                    scalar=gn[:, m:m + 1], in1=rstd2[:], op0=ALU.mult, op1=ALU.mult)
            if nT_dst is None:
                P.D("sp", out=fm(d["nT_out"])[:, :, tsl], in_=nrm[:], r=["nrm"], w=[f"nT_out{blk}"])
            else:
                for hf in range(2):
                    P.D("sp", out=nT_dst(2 * blk + hf), in_=nrm[:, :, hf * 256:(hf + 1) * 256], r=["nrm"], w=[f"nT_out{blk}_{hf}"])
        outs = [f"hT_out{b}" for b in range(NB5)]
        outs += [f"nT_out{b}" for b in range(NB5)] if nT_dst is None else [f"nT_out{b}_{hf}" for b in range(NB5) for hf in range(2)]
        P.wait_all("sp", outs)
        P.barrier()
        P.emit()


def declare_c(nc, S, pfx="", fused=False):
    d = {}

    def t(name, shape, kind, dt=F32):
        d[name] = nc.dram_tensor(pfx + name, shape, dt, kind=kind).ap()

    if not fused:
        t("nT", [1024, S], "ExternalInput")
    t("wq", [1024, 256], "ExternalInput"); t("wk", [1024, 256], "ExternalInput"); t("wv", [1024, 256], "ExternalInput")
    t("wf", [1024, 4], "ExternalInput"); t("fb", [4], "ExternalInput")
    t("maskd", [128, 4, 512], "ExternalInput")
    t("oT", [256, S], "Internal" if fused else "ExternalOutput")
    return d


def emit_c(nc, P, S, d, nT_loads=None, oT_dst=None):
    if oT_dst is None:
        oT_dst = lambda t0, n: d["oT"][:, t0:t0 + n]
    NBLK = S // 512
    NCH = S // 128
    with contextlib.ExitStack() as st:
        sb = lambda n, s, dt=F32: st.enter_context(nc.sbuf_tensor("c_" + n, s, dt))
        ps = lambda n, s, dt=F32: st.enter_context(nc.psum_tensor("c_" + n, s, dt))
        KT = sb("KT", [128, 2, S], BF16)
        Vr = sb("Vr", [128, NCH, 4 * 65 + 63], BF16)
        Cr = sb("Cr", [128, NCH, 4]); nbias = sb("nbias", [128, 4, NCH])
        nb = [sb(f"nb{i}", [128, 8, 512], BF16) for i in range(2)]
        wq = sb("wq", [128, 8, 256], BF16); wk = sb("wk", [128, 8, 256], BF16); wv = sb("wv", [128, 8, 256], BF16)
        wf = sb("wf", [128, 8, 4], BF16); fb = sb("fb", [128, 4])
        QT = [sb(f"QT{i}", [128, 512], BF16) for i in range(4)]
        PT = [sb(f"PT{i}", [128, 512], BF16) for i in range(4)]
        maskd = sb("maskd", [128, 4, 512], BF16)
        tri = sb("tri", [128, 128]); ones = sb("ones", [128, 128])
        lf = sb("lf", [128, 4]); tot = sb("tot", [128, 4]); totmid = sb("totmid", [128, 4])
        zrow = sb("zrow", [65, 512]); ocp = sb("ocp", [64, 512]); osb = [sb(f"osb{i}", [64, 512]) for i in range(2)]
        pp = [ps(f"pp{i}", [128, 512]) for i in range(2)]
        ST = [ps(f"ST{i}", [128, 512]) for i in range(3)]
        OT = [ps(f"OT{i}", [128, 512]) for i in range(2)]
        MISC = ps("MISC", [128, 512]); ZB = MISC[0:64, :]; cs = MISC[:, 0:8]

        for nm, tl in (("wq", wq), ("wk", wk), ("wv", wv)):
            P.D("pool", out=tl[:], in_=d[nm].rearrange("(m p) c -> p m c", p=128), w=[nm])
        P.D("pool", out=wf[:], in_=d["wf"].rearrange("(m p) c -> p m c", p=128), w=["wf"])
        P.D("sp", out=fb[:], in_=d["fb"].partition_broadcast(128), w=["fb"])
        P.D("pool", out=maskd[:], in_=d["maskd"], w=["maskd"])
        P.I("pool", "memset", w=["ones"], ap=ones[:], constant=1.0)
        P.I("pool", "memset", w=["tri"], ap=tri[:], constant=1.0)
        P.I("pool", "affine_select", r=["tri"], w=["tri"], out=tri[:], in_=tri[:], pattern=[[1, 128]],
            compare_op=ALU.is_ge, fill=0.0, base=0, channel_multiplier=-1)
        P.I("pool", "memset", w=["tot"], ap=tot[:], constant=0.0)
        P.I("pool", "memset", w=["Vr"], ap=Vr[:], constant=0.0)
        P.I("pool", "memset", r=["Vr"], w=["Vr"], ap=Vr[:, :, 0:260].rearrange("p c (h x) -> p c h x", x=65)[:, :, :, 64:65], constant=1.0)
        for i in range(4):
            P.I("pool", "memset", w=[f"QT{i}"], ap=QT[i][:], constant=0.0)
        npp = 0; nst = 0; npt = 0; nhead = 0
        for blk in range(NBLK):
            tsl = slice(blk * 512, (blk + 1) * 512)
            n_ = nb[blk % 2]; nk = f"nb{blk % 2}"
            if nT_loads is None:
                P.D("pool", out=n_[:], in_=d["nT"].rearrange("(m p) t -> p m t", p=128)[:, :, tsl], w=[nk])
            else:
                for csl, src in nT_loads(blk):
                    P.D("pool", out=n_[:, :, csl], in_=src, w=[nk])
            for pair in range(2):
                p_ = pp[npp % 2]; pk = f"pp{npp % 2}"; npp += 1
                for m in range(8):
                    P.MM(p_[:], wq[:, m, pair * 128:(pair + 1) * 128], n_[:, m, :], start=(m == 0), stop=(m == 7), r=["wq", nk], w=[pk])
                for hh in range(2):
                    rs = slice(hh * 64, hh * 64 + 64)
                    P.I("act", "mul", r=[pk], w=[f"QT{2 * pair + hh}"], out=QT[2 * pair + hh][rs, :], in_=p_[rs, :], mul=0.125)
                p_ = pp[npp % 2]; pk = f"pp{npp % 2}"; npp += 1
                for m in range(8):
                    P.MM(p_[:], wk[:, m, pair * 128:(pair + 1) * 128], n_[:, m, :], start=(m == 0), stop=(m == 7), r=["wk", nk], w=[pk])
                P.I("dve", "tensor_copy", r=[pk], w=["KT"], out=KT[:, pair, tsl], in_=p_[:])
            for t4 in range(4):
                ch = blk * 4 + t4
                p_ = pp[npp % 2]; pk = f"pp{npp % 2}"; npp += 1
                for m in range(8):
                    P.MM(p_[:, 0:256], n_[:, m, t4 * 128:(t4 + 1) * 128], wv[:, m, :], start=(m == 0), stop=(m == 7), r=["wv", nk], w=[pk])
                P.I("act", "copy", r=[pk], w=["Vr"], out=Vr[:, ch, 0:260].rearrange("p (h x) -> p h x", x=65)[:, :, 0:64], in_=p_[:, 0:256].rearrange("p (h x) -> p h x", x=64))
                for m in range(8):
                    P.MM(cs[:, 0:4], n_[:, m, t4 * 128:(t4 + 1) * 128], wf[:, m, :], start=(m == 0), stop=(m == 7), r=["wf", nk], w=["MISC"])
                P.I("dve", "tensor_tensor", r=["MISC", "fb"], w=["lf"], out=lf[:], in0=cs[:, 0:4], in1=fb[:], op=ALU.add)
                P.I("act", "activation", r=["lf"], w=["lf"], out=lf[:], in_=lf[:], func=AF.Exp, scale=-1.0)
                P.I("act", "activation", r=["lf"], w=["lf"], out=lf[:], in_=lf[:], func=AF.Ln, bias=1.0)
                P.I("dve", "tensor_scalar", r=["lf"], w=["lf"], out=lf[:], in0=lf[:], scalar1=-1.0, scalar2=None, op0=ALU.mult)
                P.MM(cs[:, 0:4], tri[:], lf[:], r=["tri", "lf"], w=["MISC"])
                P.MM(cs[:, 4:8], ones[:], lf[:], r=["ones", "lf"], w=["MISC"])
                P.I("dve", "tensor_tensor", r=["MISC", "tot"], w=["Cr"], out=Cr[:, ch, :], in0=cs[:, 0:4], in1=tot[:], op=ALU.add)
                P.I("dve", "tensor_tensor", r=["MISC", "tot"], w=["tot"], out=tot[:], in0=cs[:, 4:8], in1=tot[:], op=ALU.add)
                if t4 == 1:
                    P.I("dve", "tensor_copy", r=["tot"], w=["totmid"], out=totmid[:], in_=tot[:])
            nch = blk * 4 + 4
            for hl in range(4):
                P.I("dve", "tensor_scalar", r=["Cr", "totmid"], w=["nbias"], out=nbias[:, hl, 0:nch], in0=Cr[:, 0:nch, hl],
                    scalar1=totmid[:, hl:hl + 1], scalar2=-1.0, op0=ALU.subtract, op1=ALU.mult)
            pairs = [(hl, kc) for hl in range(4) for kc in range(nch)]

            def qk(i):
                nonlocal nst
                hl, kc = pairs[i]
                s_ = ST[i % 3]
                P.MM(s_[:], KT[:, hl // 2, kc * 128:(kc + 1) * 128], QT[hl][:], r=["KT", f"QT{hl}"], w=[f"ST{i % 3}"])

            qk(0)
            if len(pairs) > 1:
                qk(1)
            for i, (hl, kc) in enumerate(pairs):
                if i + 2 < len(pairs):
                    qk(i + 2)
                s_ = ST[i % 3]; sk = f"ST{i % 3}"
                pt = PT[npt % 4]; ptk = f"PT{npt % 4}"; npt += 1
                P.I("act", "activation", r=[sk, "nbias"], w=[ptk], out=pt[:], in_=s_[:], func=AF.Exp, bias=nbias[:, hl, kc:kc + 1])
                if kc >= blk * 4:
                    P.I("dve", "tensor_tensor", r=[ptk, "maskd"], w=[ptk], out=pt[:], in0=pt[:], in1=maskd[:, kc - blk * 4, :], op=ALU.mult)
                if kc == 0:
                    ot = OT[nhead % 2]; otk = f"OT{nhead % 2}"; ob = osb[nhead % 2]; obk = f"osb{nhead % 2}"; nhead += 1
                P.MM(ot[:], Vr[:, kc, hl * 65:hl * 65 + 128], pt[:], start=(kc == 0), stop=(kc == nch - 1), r=["Vr", ptk], w=[otk])
                if kc == nch - 1:
                    P.I("dve", "tensor_scalar", r=[otk], w=["zrow"], out=zrow[64:65, :], in0=ot[64:65, :], scalar1=1e-30, scalar2=None, op0=ALU.max)
                    P.I("dve", "reciprocal", r=["zrow"], w=["zrow"], out=zrow[64:65, :], in_=zrow[64:65, :])
                    P.MM(ZB, ones[64:65, 0:64], zrow[64:65, :], r=["ones", "zrow"], w=["MISC"])
                    P.I("act", "copy", r=[otk], w=["ocp"], out=ocp[:], in_=ot[0:64, :])
                    P.I("dve", "tensor_tensor", r=["ocp", "MISC"], w=[obk], out=ob[:], in0=ocp[:], in1=ZB, op=ALU.mult)
                    P.D("sp", out=oT_dst(blk * 512, 512)[hl * 64:(hl + 1) * 64, :], in_=ob[:], r=[obk], w=[f"oT{blk}_{hl}"])
        P.wait_all("sp", [f"oT{b}_{h}" for b in range(NBLK) for h in range(4)])
        P.barrier()
        P.emit()


S_FULL = 16384
T_CORE = 4096
G4 = [[0, 1, 2, 3], [4, 5, 6, 7]]
_PROG = {}
_B_IN = (("wout", [1024, 1024]), ("gffn", [1024]), ("wq", [1024, 2048]), ("keysT", [16, 128, 128]), ("uT", [1024, 16384]),
         ("v", [16384, 1024]), ("gnext", [1024]))


def _declare_b_fused(nc, T, pfx, hT_ap, out_kind):
    d = {}
    for name, shape in _B_IN:
        d[name] = nc.dram_tensor(pfx + name, shape, F32, kind="ExternalInput").ap()
    d["hT"] = hT_ap
    d["hT_out"] = nc.dram_tensor(pfx + "hT_out", [1024, T], F32, kind="Internal").ap()
    d["nT_out"] = nc.dram_tensor(pfx + "nT_out", [1024, T], F32, kind=out_kind).ap()
    scr = lambda name, shape, dt=F32: nc.dram_tensor(pfx + name, shape, dt, kind="Internal").ap()
    d["h2T"] = scr("h2T", [1024, T]); d["hnbf"] = scr("hnbf", [1024, T], BF16); d["qTd"] = scr("qTd", [128, T // 128, 16, 128])
    d["oTq"] = scr("oTq", [1024, T])
    d["GT"] = scr("GT", [T // 128, 128, 16384], BF16); d["uTbf"] = scr("uTbf", [1024, 16384], BF16); d["vbf"] = scr("vbf", [16384, 1024], BF16)
    return d


def _build_fused(S=S_FULL, T=T_CORE):
    if (S, T) in _PROG:
        return _PROG[(S, T)]
    nc = bass.Bass("TRN2", target_bir_lowering=False)
    with contextlib.ExitStack() as st:
        P = Prog(nc)
        P.alloc_sems(st)
        da = declare_a(nc, S, pfx="A_", fused=True)
        xTs = nc.dram_tensor("B0_xTs", [1024, T], F32, kind="ExternalInput").ap()
        db0 = _declare_b_fused(nc, T, "B0_", xTs, "Internal")
        dc = declare_c(nc, S, pfx="C_", fused=True)
        db1 = _declare_b_fused(nc, T, "B1_", db0["hT_out"], "ExternalOutput")
        CW = min(1024, T)
        NC1 = S // CW
        CW2 = 256
        NC2 = T // CW2
        dt_ = lambda name, shape: nc.dram_tensor(name, shape, F32, kind="Internal").ap()
        x1_in = dt_("x1_in", [NC1, 256, CW]); x1_out = dt_("x1_out", [NC1, 1024, CW])
        x2_in = dt_("x2_in", [NC2, 1024, CW2]); x2_out = dt_("x2_out", [NC2, 4096, CW2])
        x3_in = dt_("x3_in", [NC1, 256, CW]); x3_out = dt_("x3_out", [NC1, 1024, CW])

        def gather(src, dst, n, name):
            for j in range(n):
                P.cc((lambda j: (lambda e: e.collective_compute("AllGather", ALU.bypass, replica_groups=G4, ins=[src[j]], outs=[dst[j]])))(j),
                     writes=[name])
            P.barrier()

        def chunked_dst(buf):
            return lambda t0, n: buf[t0 // CW, :, (t0 % CW):(t0 % CW) + n]

        def quarter_src(buf):
            def f():
                q = nc.partition_id() % 4
                return buf[bass.ds(q * (T // CW), T // CW), :, :]
            return f

        bpr = T // 512

        def nT_loads(blk):
            rank, lb = blk // bpr, blk % bpr
            return [(slice(h * CW2, (h + 1) * CW2),
                     x2_out[lb * 2 + h, rank * 1024:(rank + 1) * 1024, :].rearrange("(m p) t -> p m t", p=128)) for h in range(2)]

        emit_a(nc, P, S, da, oT_dst=chunked_dst(x1_in))
        gather(x1_in, x1_out, NC1, "x1")
        emit_b(nc, P, T, db0, pfx="B0_", oT_blk=quarter_src(x1_out),
               nT_dst=lambda blk: x2_in[blk].rearrange("(m p) t -> p m t", p=128))
        gather(x2_in, x2_out, NC2, "x2")
        emit_c(nc, P, S, dc, nT_loads=nT_loads, oT_dst=chunked_dst(x3_in))
        gather(x3_in, x3_out, NC1, "x3")
        emit_b(nc, P, T, db1, pfx="B1_", oT_blk=quarter_src(x3_out))
    _PROG[(S, T)] = nc
    return nc


def _peer_inputs(pfx, wout, gffn, wq, keys, u, v, gnext):
    f32 = lambda a: np.ascontiguousarray(np.asarray(a, dtype=np.float32))
    return {pfx + "wout": f32(wout), pfx + "gffn": f32(gffn), pfx + "wq": f32(wq),
            pfx + "keysT": np.ascontiguousarray(f32(keys).transpose(0, 1, 3, 2).reshape(16, 128, 128)),
            pfx + "uT": np.ascontiguousarray(f32(u).T), pfx + "v": f32(v), pfx + "gnext": f32(gnext)}


def kernel(x, l0_attn_norm, l0_w_in, l0_cmp_pe_k, l0_cmp_w1_k, l0_cmp_w2_k, l0_cmp_pe_v, l0_cmp_w1_v, l0_cmp_w2_v, l0_w_out,
           l0_ffn_norm, l0_peer_wq, l0_peer_keys, l0_peer_u, l0_peer_v,
           l1_attn_norm, l1_w_in, l1_f_bias, l1_w_out,
           l1_ffn_norm, l1_peer_wq, l1_peer_keys, l1_peer_u, l1_peer_v,
           final_norm):
    f32 = lambda a: np.ascontiguousarray(np.asarray(a, dtype=np.float32))
    x = f32(x)
    B, S, D = x.shape
    T_CORE = S // 4
    nc = _build_fused(S, T_CORE)
    consts = consts_a(); ropeq, ropek = rope_tables(S)
    args0 = [f32(a) for a in (l0_attn_norm, l0_w_in, l0_cmp_pe_k, l0_cmp_w1_k, l0_cmp_w2_k, l0_cmp_pe_v, l0_cmp_w1_v, l0_cmp_w2_v)]
    pb0 = _peer_inputs("B0_", l0_w_out, l0_ffn_norm, l0_peer_wq, l0_peer_keys, l0_peer_u, l0_peer_v, l1_attn_norm)
    pb1 = _peer_inputs("B1_", l1_w_out, l1_ffn_norm, l1_peer_wq, l1_peer_keys, l1_peer_u, l1_peer_v, final_norm)
    w1 = f32(l1_w_in); fbias = f32(l1_f_bias)
    kk = np.arange(128)[:, None, None]; ii = np.arange(4)[None, :, None]; qq = np.arange(512)[None, None, :]
    maskd = (kk <= qq - 128 * ii).astype(np.float32)
    xT = [np.ascontiguousarray(x[b].T) for b in range(B)]
    maps = []
    for c in range(8):
        b, q = c // 4, c % 4
        m = {"A_" + k: v for k, v in host_inputs_a(x[b], *args0, q, consts, ropeq, ropek).items()}
        m["A_xT"] = xT[b]
        m["B0_xTs"] = np.ascontiguousarray(xT[b][:, q * T_CORE:(q + 1) * T_CORE])
        m.update(pb0); m.update(pb1)
        h0 = 4 * q
        m.update({"C_wq": np.ascontiguousarray(w1[:, h0 * 64:(h0 + 4) * 64]),
                  "C_wk": np.ascontiguousarray(w1[:, 1024 + h0 * 64:1024 + (h0 + 4) * 64]),
                  "C_wv": np.ascontiguousarray(w1[:, 2048 + h0 * 64:2048 + (h0 + 4) * 64]),
                  "C_wf": np.ascontiguousarray(w1[:, 3072 + h0:3072 + h0 + 4]), "C_fb": fbias[h0:h0 + 4].copy(), "C_maskd": maskd})
        maps.append(m)
    res = run_bass_kernel_spmd(nc, maps, core_ids=list(range(8))).results
    out = np.empty((B, S, D), np.float32)
    for c in range(8):
        b, q = c // 4, c % 4
        out[b, q * T_CORE:(q + 1) * T_CORE, :] = res[c]["B1_nT_out"].T
    return out
```

```python
import contextlib
import numpy as np
import concourse.bass as bass
import concourse.mybir as mybir
from concourse.bass_utils import run_bass_kernel_spmd

F32 = mybir.dt.float32
BF16 = mybir.dt.bfloat16
AF = mybir.ActivationFunctionType
ALU = mybir.AluOpType
AX = mybir.AxisListType

N_DMA_SEMS = 8


class Prog:
    COMPUTE = ("pe", "dve", "act", "pool")
    QUEUES = ("sp", "act", "pool")

    def __init__(self, nc):
        self.nc = nc
        self.ops = {e: [] for e in ("pe", "dve", "act", "pool", "sp")}
        self.cnt = {}
        self.last_w = {}
        self.readers = {}
        self.waited = {e: {} for e in self.ops}
        self.dma_n = {q: 0 for q in self.QUEUES}
        self.semkeys = []
        for e in self.COMPUTE:
            self._mk(("c", e))
        for q in self.QUEUES:
            for i in range(N_DMA_SEMS):
                self._mk(("d", q, i))
        self._mk(("cc",))
        self.sems = {}
        self.pending = {e: False for e in self.ops}
        self.lazy_pe_inc = False

    def _mk(self, k):
        self.cnt[k] = 0
        self.semkeys.append(k)

    def _need(self, eng, tok, waits):
        if tok is None:
            return
        k, v = tok
        if k == ("c", eng) and eng == "pe":
            return
        if self.waited[eng].get(k, 0) >= v:
            return
        self.waited[eng][k] = v
        waits.append((k, v))

    def _deps(self, eng, reads, writes):
        waits = []
        for b in reads:
            self._need(eng, self.last_w.get(b), waits)
        for b in writes:
            self._need(eng, self.last_w.get(b), waits)
            for t in self.readers.get(b, {}).items():
                if t[0] == ("c", eng):
                    continue
                self._need(eng, t, waits)
        return waits

    def _commit(self, tok, reads, writes):
        for b in reads:
            self.readers.setdefault(b, {})[tok[0]] = tok[1]
        for b in writes:
            self.last_w[b] = tok
            self.readers[b] = {}

    def op(self, eng, fn, reads=(), writes=(), inc=True):
        waits = self._deps(eng, reads, writes)
        k = ("c", eng)
        if inc:
            self.cnt[k] += 1
            tok = (k, self.cnt[k])
            self.pending[eng] = False
        else:
            tok = (k, self.cnt[k] + 1)
            self.pending[eng] = True
        self._commit(tok, reads, writes)
        self.ops[eng].append((waits, fn, (k, 1) if inc else None))
        return tok

    def dma(self, q, fn, reads=(), writes=()):
        waits = self._deps(q, reads, writes)
        n = self.dma_n[q]
        self.dma_n[q] += 1
        k = ("d", q, n % N_DMA_SEMS)
        self._need(q, (k, self.cnt[k]) if self.cnt[k] else None, waits)
        self.cnt[k] += 16
        tok = (k, self.cnt[k])
        self._commit(tok, reads, writes)
        self.ops[q].append((waits, fn, (k, 16)))
        return tok

    def cc(self, fn, reads=(), writes=()):
        waits = self._deps("pool", reads, writes)
        k = ("cc",)
        self._need("pool", (k, self.cnt[k]) if self.cnt[k] else None, waits)
        self.cnt[k] += 1
        tok = (k, self.cnt[k])
        self._commit(tok, reads, writes)
        self.ops["pool"].append((waits, fn, (k, 1)))
        return tok

    def I(self, eng, method, r=(), w=(), **kw):
        return self.op(eng, lambda e: getattr(e, method)(**kw), reads=r, writes=w)

    def MM(self, out, lhsT, rhs, start=True, stop=True, r=(), w=()):
        return self.op("pe", lambda e: e.matmul(out, lhsT=lhsT, rhs=rhs, start=start, stop=stop), reads=r, writes=w,
                       inc=(stop or not self.lazy_pe_inc))

    def D(self, q, out, in_, r=(), w=(), **kw):
        return self.dma(q, lambda e: e.dma_start(out=out, in_=in_, **kw), reads=r, writes=w)

    def barrier(self):
        assert not any(self.pending.values()), self.pending
        for eng in self.ops:
            waits = []
            for k in self.semkeys:
                if self.cnt[k]:
                    self._need(eng, (k, self.cnt[k]), waits)
            self.ops[eng].append((waits, None, None))
        self.last_w = {}
        self.readers = {}

    def wait_all(self, eng, bufs):
        waits = []
        for b in bufs:
            self._need(eng, self.last_w.get(b), waits)
        self.ops[eng].append((waits, None, None))

    def alloc_sems(self, st):
        for k in self.semkeys:
            self.sems[k] = st.enter_context(self.nc.semaphore("s_" + "_".join(map(str, k))))

    def emit(self):
        nc = self.nc
        import contextlib
        with contextlib.ExitStack() as st:
            if not self.sems:
                self.alloc_sems(st)
            block = st.enter_context(nc.Block())
            engobj = {"pe": "tensor", "dve": "vector", "act": "scalar", "pool": "gpsimd", "sp": "sync"}

            def mk(ename):
                ops = self.ops[ename]

                def body(eng):
                    for waits, fn, inc in ops:
                        for (k, v) in waits:
                            eng.wait_ge(self.sems[k], v)
                        if fn is not None:
                            ins = fn(eng)
                            if inc is not None:
                                ins.then_inc(self.sems[inc[0]], inc[1])
                return body

            for ename, attr in engobj.items():
                if self.ops[ename]:
                    getattr(block, attr)(mk(ename))
        self.ops = {e: [] for e in self.ops}


EPS = 1e-6


def consts_a():
    c = {}
    q = np.arange(128)[:, None]; rel = np.arange(512)[None, :] - 256
    cur = (q >= 64).astype(np.int64)
    M = (rel <= cur - 2).astype(np.float32)
    A = np.where(rel == cur, 10000.0, np.where(rel == cur - 1, 10001.0, np.where(rel > cur, -1.0, 0.0))).astype(np.float32)
    c["pats"] = np.stack([M, A], 1)
    ci = np.arange(128)[:, None]; qi = np.arange(128)[None, :]
    D = (16 * ci - qi).astype(np.float32)
    Dz = D.copy(); Dz[0, :] = 1e9
    c["D16"] = np.stack([D, Dz], 1)
    c["tril"] = np.stack([(ci <= qi), (ci > qi)], 1).astype(np.float32)
    c["Sel"] = np.broadcast_to(np.eye(12, dtype=np.float32)[:, :, None], (12, 12, 64)).copy()
    cc = np.arange(1024)[:, None] - 1; jj = np.arange(256)[None, :]
    lo = np.maximum(cc * 16, jj * 64); hi = np.minimum(cc * 16 + 32, (jj + 1) * 64)
    m = np.maximum(hi - lo, 0).astype(np.float32) / 32.0
    m[0, :] = 0.0
    c["slcm"] = np.ascontiguousarray(m.reshape(8, 128, 256).transpose(1, 0, 2))
    return c


def epat_table(S):
    n = np.arange(S)[None, :]; r = np.arange(64)[:, None]
    return (30000.0 * (((n // 64) % 64) == r)).astype(np.float32)


def rope_tables(S):
    half = 32
    inv = (10000.0 ** (-np.arange(half, dtype=np.float32) / half)).astype(np.float32)
    ang = (np.arange(S, dtype=np.float32)[None, :] * inv[:, None]).astype(np.float32)
    cos = np.cos(ang).astype(np.float32); sin = np.sin(ang).astype(np.float32)
    cosf = np.concatenate([cos, cos], 0); sinf = np.concatenate([-sin, sin], 0)
    rk = np.stack([cosf, sinf], 1)
    return np.ascontiguousarray(rk * 0.125), np.ascontiguousarray(rk)


def host_inputs_a(xb, gattn, w_in, pe_k, w1_k, w2_k, pe_v, w1_v, w2_v, g, consts, ropeq, ropek):
    def sw(w):
        w = w.reshape(w.shape[0], -1, 64)
        return np.concatenate([w[..., 32:], w[..., :32]], -1).reshape(w.shape[0], -1)
    kv = lambda i: w_in[:, 1024 + i * 256 + g * 64:1024 + i * 256 + (g + 1) * 64]
    wq = w_in[:, g * 256:(g + 1) * 256]
    d = dict(consts)
    d["xT"] = np.ascontiguousarray(xb.T)
    d["gattn"] = gattn
    d["wqa"] = np.ascontiguousarray(np.concatenate([wq, sw(wq)], 1))
    d["wka"] = np.ascontiguousarray(np.concatenate([kv(0), kv(1), sw(kv(0)), kv(2), sw(kv(2)), kv(4), sw(kv(4))], 1))
    d["wtok"] = np.ascontiguousarray(np.concatenate([kv(3), kv(5)], 1))
    gc = [2560 + br * 16 + g * 4 + r for br in range(3) for r in range(4)]
    d["wg"] = np.ascontiguousarray(w_in[:, gc])
    d["w1s"] = np.ascontiguousarray(np.concatenate([w1_k[g].transpose(1, 0, 2), w1_v[g].transpose(1, 0, 2)], 0))
    d["peT"] = np.ascontiguousarray(np.concatenate([pe_k[g].T, pe_v[g].T], 0))
    d["w2s"] = np.ascontiguousarray(np.stack([w2_k[g], w2_v[g]], 1))
    d["ropeq"] = ropeq; d["ropek"] = ropek
    d["Epat"] = epat_table(xb.shape[0])
    return d


def declare_a(nc, S, pfx="", fused=False):
    d = {}

    def t(name, shape, kind="ExternalInput", dt=F32):
        d[name] = nc.dram_tensor(pfx + name, shape, dt, kind=kind).ap()

    t("xT", [1024, S]); t("gattn", [1024]); t("wqa", [1024, 512]); t("wka", [1024, 448]); t("wtok", [1024, 128]); t("wg", [1024, 12])
    t("w1s", [128, 32, 128]); t("peT", [128, 32]); t("w2s", [128, 2, 64]); t("ropeq", [64, 2, S]); t("ropek", [64, 2, S])
    t("pats", [128, 2, 512]); t("D16", [128, 2, 128]); t("tril", [128, 2, 128]); t("Epat", [64, S]); t("Sel", [12, 12, 64])
    t("slcm", [128, 8, 256])
    t("oT", [256, S], kind="Internal" if fused else "ExternalOutput")
    return d


def emit_a(nc, P, S, d, oT_dst=None, block_hook=None):
    if oT_dst is None:
        oT_dst = lambda t0, n: d["oT"][:, t0:t0 + n]
    NBLK = S // 512
    NCH = S // 128
    with contextlib.ExitStack() as st:
        sb = lambda n, s, dt=F32: st.enter_context(nc.sbuf_tensor("a_" + n, s, dt))
        ps = lambda n, s, dt=F32: st.enter_context(nc.psum_tensor("a_" + n, s, dt))
        KsT = sb("KsT", [128, S], BF16); Vs = sb("Vs", [128, NCH, 128], BF16)
        KwT = sb("KwT", [128, 8, 128], BF16); Vw = sb("Vw", [128, 8, 128], BF16)
        KcT = sb("KcT", [128, 1024], BF16); Vc = sb("Vc", [128, 8, 128], BF16)
        slcm = sb("slcm", [128, 8, 256], BF16)
        Qaug = [sb(f"Qaug{i}", [128, 4, 512], BF16) for i in range(2)]
        pats = sb("pats", [128, 2, 512]); D16 = sb("D16", [128, 2, 128]); tril = sb("tril", [128, 2, 128], BF16)
        Sel = sb("Sel", [12, 12, 64])
        wqa = sb("wqa", [128, 8, 512], BF16); wka = sb("wka", [128, 8, 448], BF16); wtok = sb("wtok", [128, 8, 128], BF16)
        wg = sb("wg", [128, 8, 12], BF16)
        w1s = sb("w1s", [128, 32, 128], BF16); peT = sb("peT", [128, 32], BF16); w2s = sb("w2s", [128, 2, 64], BF16)
        hb = sb("hb", [128, 2]); g = sb("g", [128, 8])
        ones_b = sb("ones_b", [128, 128], BF16); ones_f = sb("ones_f", [128, 128]); identf = sb("identf", [128, 128])
        xT = sb("xT", [128, 8, 512]); sq = sb("sq", [128, 8, 512], BF16); rstd = sb("rstd", [128, 512]); xnb = sb("xnb", [128, 8, 512], BF16)
        rq = sb("rq", [64, 2, 512]); rk = sb("rk", [64, 2, 512])
        t1 = [sb(f"t1_{i}", [64, 512]) for i in range(2)]; t2 = [sb(f"t2_{i}", [64, 512]) for i in range(2)]
        Qd = sb("Qd", [64, 4, 4, 128], BF16)
        CV = sb("CV", [128, 528], BF16); hidk = sb("hidk", [128, 32], BF16); hvp = sb("hvp", [128, 128], BF16)
        gT = sb("gT", [12, 512])
        PTc = sb("PTc", [128, 8, 512], BF16); PT = [sb(f"PT{i}", [128, 512], BF16) for i in range(4)]
        zc = sb("zc", [1, 512]); impS = sb("impS", [128, 256]); scr = sb("scr", [128, 256]); m8 = sb("m8", [128, 16])
        NT = sb("NT", [128, 256])
        zrow = sb("zrow", [65, 512]); gbs = sb("gbs", [64, 512]); acc = [sb(f"acc{i}", [64, 512]) for i in range(2)]
        tmp = sb("tmp", [64, 512])
        pp0 = ps("pp0", [128, 512])
        ST = [ps(f"ST{i}", [128, 512]) for i in range(3)]
        OA = [ps(f"OA{i}", [128, 512]) for i in range(2)]
        IMP = ps("IMP", [128, 512]); AUX = ps("AUX", [128, 512])
        pp = [pp0, IMP]; ppk = ["pp0", "IMP"]

        for nm, tl in (("wqa", wqa), ("wka", wka), ("wtok", wtok), ("wg", wg)):
            P.D("pool", out=tl[:], in_=d[nm].rearrange("(m p) c -> p m c", p=128), w=[nm])
        for nm, tl in (("w1s", w1s), ("peT", peT), ("w2s", w2s), ("slcm", slcm), ("tril", tril)):
            P.D("pool", out=tl[:], in_=d[nm], w=[nm])
        for nm, tl in (("pats", pats), ("D16", D16), ("Sel", Sel)):
            P.D("sp", out=tl[:], in_=d[nm], w=[nm])
        P.D("pool", out=KsT[64:128, :], in_=d["Epat"], w=["KsE"])
        P.D("sp", out=g[:], in_=d["gattn"].rearrange("(m p) -> p m", p=128), w=["g"], allow_slow_non_contiguous=True)
        P.I("pool", "memset", w=["ones_b"], ap=ones_b[:], constant=1.0)
        P.I("pool", "memset", w=["ones_f"], ap=ones_f[:], constant=1.0)
        P.I("pool", "memset", w=["identf"], ap=identf[:], constant=1.0)
        P.I("pool", "affine_select", r=["identf"], w=["identf"], out=identf[:], in_=identf[:], pattern=[[-1, 128]],
            compare_op=ALU.is_equal, fill=0.0, base=0, channel_multiplier=1)
        P.I("pool", "memset", w=["Vs"], ap=Vs[:], constant=0.0)
        P.I("pool", "memset", r=["Vs"], w=["Vs"], ap=Vs[:, :, 64:65], constant=1.0)
        P.I("pool", "memset", w=["Vw"], ap=Vw[:], constant=0.0)
        P.I("pool", "memset", r=["Vw"], w=["Vw"], ap=Vw[:, :, 64:65], constant=1.0)
        P.I("pool", "memset", w=["KwT"], ap=KwT[:], constant=0.0)
        for i in range(2):
            P.I("pool", "memset", w=[f"Qaug{i}q", f"Qaug{i}m"], ap=Qaug[i][:], constant=0.0)
        P.I("pool", "memset", w=["KcT"], ap=KcT[:], constant=0.0)
        P.I("pool", "memset", w=["Vc"], ap=Vc[:], constant=0.0)
        P.I("pool", "memset", w=["CVk", "CVv"], ap=CV[:], constant=0.0)
        P.I("pool", "memset", w=["hvp"], ap=hvp[:], constant=0.0)
        for kvi in range(2):
            rows = slice(kvi * 64, kvi * 64 + 64)
            for l in range(32):
                P.MM(pp[0][:, kvi:kvi + 1], w1s[rows, l, :], peT[rows, l:l + 1], start=(l == 0), stop=(l == 31), r=["w1s", "peT"], w=["pp0"])
        P.I("dve", "tensor_copy", r=["pp0"], w=["hb"], out=hb[:], in_=pp[0][:, 0:2])

        cnt = {"pp": 0, "t": 0, "pt": 0, "oa": 0, "acc": 0}

        def proj(c0, M):
            i = cnt["pp"] % 2; cnt["pp"] += 1
            return pp[i], ppk[i]

        for blk in range(NBLK):
            tsl = slice(blk * 512, (blk + 1) * 512)
            if block_hook is not None:
                block_hook(blk)
            P.D("sp", out=xT[:], in_=d["xT"].rearrange("(m p) t -> p m t", p=128)[:, :, tsl], w=["xT"])
            P.D("sp", out=rq[:], in_=d["ropeq"][:, :, tsl], w=["rq"])
            P.D("sp", out=rk[:], in_=d["ropek"][:, :, tsl], w=["rk"])
            P.I("act", "activation", r=["xT"], w=["sq"], out=sq[:], in_=xT[:], func=AF.Square)
            for m in range(8):
                P.MM(AUX[:], ones_b[:], sq[:, m, :], start=(m == 0), stop=(m == 7), r=["ones_b", "sq"], w=["AUX"])
            P.I("act", "activation", r=["AUX"], w=["rstd"], out=rstd[:], in_=AUX[:], func=AF.Sqrt, scale=1.0 / 1024, bias=EPS)
            P.I("dve", "reciprocal", r=["rstd"], w=["rstd"], out=rstd[:], in_=rstd[:])
            for m in range(8):
                P.I("dve", "scalar_tensor_tensor", r=["xT", "g", "rstd"], w=["xnb"], out=xnb[:, m, :], in0=xT[:, m, :],
                    scalar=g[:, m:m + 1], in1=rstd[:], op0=ALU.mult, op1=ALU.mult)

            def fmproj(wt, wk_, c0, M):
                p_, pk = proj(c0, M)
                for m in range(8):
                    P.MM(p_[0:M, :], wt[:, m, c0:c0 + M], xnb[:, m, :], start=(m == 0), stop=(m == 7), r=[wk_, "xnb"], w=[pk])
                return p_, pk

            def rope(wt, wk_, ca, cb, tab, tabk, out_ap, outk):
                pa, pak = fmproj(wt, wk_, ca, 64)
                i = cnt["t"] % 2; cnt["t"] += 1
                P.I("dve", "tensor_tensor", r=[pak, tabk], w=[f"t1_{i}"], out=t1[i][:], in0=pa[0:64, :], in1=tab[:, 0, :], op=ALU.mult)
                pb, pbk = fmproj(wt, wk_, cb, 64)
                P.I("dve", "tensor_tensor", r=[pbk, tabk], w=[f"t2_{i}"], out=t2[i][:], in0=pb[0:64, :], in1=tab[:, 1, :], op=ALU.mult)
                a_, b_ = t1[i][:], t2[i][:]
                if len(out_ap.shape) == 3:
                    a_ = a_.rearrange("p (a q) -> p a q", a=4); b_ = b_.rearrange("p (a q) -> p a q", a=4)
                P.I("pool", "tensor_tensor", r=[f"t1_{i}", f"t2_{i}"], w=[outk], out=out_ap, in0=a_, in1=b_, op=ALU.add)

            for r in range(4):
                rope(wqa, "wqa", r * 64, 256 + r * 64, rq, "rq", Qd[:, :, r, :], "Qd")
            rope(wka, "wka", 192, 256, rk, "rk", KsT[0:64, tsl], "KsT")
            rope(wka, "wka", 320, 384, rk, "rk", KwT[0:64, (blk % 2) * 4:(blk % 2) * 4 + 4, :], "KwT")
            pa, pak = fmproj(wka, "wka", 0, 128)
            i = cnt["t"] % 2; cnt["t"] += 1
            P.I("dve", "tensor_tensor", r=[pak, "rk"], w=[f"t1_{i}"], out=t1[i][:], in0=pa[0:64, :], in1=rk[:, 0, :], op=ALU.mult)
            P.I("act", "copy", r=[pak], w=["CVv"], out=CV[64:128, 16:528], in_=pa[64:128, :])
            pb, pbk = fmproj(wka, "wka", 128, 64)
            P.I("dve", "tensor_tensor", r=[pbk, "rk"], w=[f"t2_{i}"], out=t2[i][:], in0=pb[0:64, :], in1=rk[:, 1, :], op=ALU.mult)
            P.I("pool", "tensor_tensor", r=[f"t1_{i}", f"t2_{i}"], w=["CVk"], out=CV[0:64, 16:528], in0=t1[i][:], in1=t2[i][:], op=ALU.add)
            pgt, pgk = fmproj(wg, "wg", 0, 12)
            P.I("act", "activation", r=[pgk], w=["gT"], out=gT[:], in_=pgt[0:12, :], func=AF.Sigmoid)
            for t4 in range(4):
                ch = blk * 4 + t4
                i = cnt["pp"] % 2; cnt["pp"] += 1
                for m in range(8):
                    P.MM(pp[i][:, 0:128], xnb[:, m, t4 * 128:(t4 + 1) * 128], wtok[:, m, :], start=(m == 0), stop=(m == 7),
                         r=["wtok", "xnb"], w=[ppk[i]])
                P.I("act", "copy", r=[ppk[i]], w=["Vs"], out=Vs[:, ch, 0:64], in_=pp[i][:, 0:64])
                P.I("act", "copy", r=[ppk[i]], w=["Vw"], out=Vw[:, ch % 8, 0:64], in_=pp[i][:, 64:128])
            CVv = CV[:].rearrange("p (c s) -> p c s", s=16)
            i = cnt["pp"] % 2; cnt["pp"] += 1
            for l in range(32):
                P.MM(pp[i][:, 0:32], w1s[0:64, l, :], CVv[0:64, l // 16:l // 16 + 32, l % 16], start=(l == 0), stop=(l == 31),
                     r=["w1s", "CVk"], w=[ppk[i]])
            P.I("act", "activation", r=[ppk[i], "hb"], w=["hidk"], out=hidk[:], in_=pp[i][:, 0:32], func=AF.Gelu_apprx_tanh, bias=hb[:, 0:1])
            i2 = cnt["pp"] % 2; cnt["pp"] += 1
            P.MM(pp[i2][0:64, 0:32], w2s[:, 0, :], hidk[:], r=["w2s", "hidk"], w=[ppk[i2]])
            P.I("dve", "tensor_copy", r=[ppk[i2]], w=["KcT"], out=KcT[0:64, 32 * blk:32 * blk + 32], in_=pp[i2][0:64, 0:32])
            i = cnt["pp"] % 2; cnt["pp"] += 1
            for l in range(32):
                P.MM(pp[i][:, 0:32], w1s[64:128, l, :], CVv[64:128, l // 16:l // 16 + 32, l % 16], start=(l == 0), stop=(l == 31),
                     r=["w1s", "CVv"], w=[ppk[i]])
            off = (32 * blk) % 128
            P.I("act", "activation", r=[ppk[i], "hb"], w=["hvp"], out=hvp[:, off:off + 32], in_=pp[i][:, 0:32], func=AF.Gelu_apprx_tanh, bias=hb[:, 1:2])
            i2 = cnt["pp"] % 2; cnt["pp"] += 1
            P.MM(pp[i2][:, 0:64], hvp[:], w2s[:, 1, :], r=["w2s", "hvp"], w=[ppk[i2]])
            P.I("dve", "tensor_copy", r=[ppk[i2]], w=["Vc"], out=Vc[off:off + 32, (32 * blk) // 128, 0:64], in_=pp[i2][off:off + 32, 0:64])
            P.I("act", "copy", r=["CVk", "CVv"], w=["CVk", "CVv"], out=CV[:, 0:16], in_=CV[:, 512:528])

            for qi in range(4):
                QB = blk * 4 + qi
                t0 = 128 * QB
                Qb = Qd[:, qi, :, :].rearrange("p r q -> p (r q)")

                def combine(br, ot, otk, normalize):
                    ai = cnt["acc"] % 2
                    for r in range(4):
                        P.MM(AUX[0:64, r * 128:(r + 1) * 128], Sel[:, br * 4 + r, :], gT[:, qi * 128:(qi + 1) * 128], r=["Sel", "gT"], w=["AUX"])
                    P.I("act", "copy", r=["AUX"], w=["gbs"], out=gbs[:], in_=AUX[0:64, :])
                    if normalize:
                        P.I("dve", "tensor_scalar", r=[otk], w=["zrow"], out=zrow[64:65, :], in0=ot[64:65, :], scalar1=1e-30, scalar2=None, op0=ALU.max)
                        P.I("dve", "reciprocal", r=["zrow"], w=["zrow"], out=zrow[64:65, :], in_=zrow[64:65, :])
                        P.MM(AUX[0:64, :], ones_f[64:65, 0:64], zrow[64:65, :], r=["ones_f", "zrow"], w=["AUX"])
                        P.I("dve", "tensor_tensor", r=["gbs", "AUX"], w=["gbs"], out=gbs[:], in0=gbs[:], in1=AUX[0:64, :], op=ALU.mult)
                    if br == 0:
                        P.I("dve", "tensor_tensor", r=[otk, "gbs"], w=[f"acc{ai}"], out=acc[ai][:], in0=ot[0:64, :], in1=gbs[:], op=ALU.mult)
                    else:
                        P.I("dve", "tensor_tensor", r=[otk, "gbs"], w=["tmp"], out=tmp[:], in0=ot[0:64, :], in1=gbs[:], op=ALU.mult)
                        P.I("pool", "tensor_tensor", r=["tmp", f"acc{ai}"], w=[f"acc{ai}"], out=acc[ai][:], in0=acc[ai][:], in1=tmp[:], op=ALU.add)

                qa = QB % 2
                ng = QB // 32 + 1
                P.I("pool", "tensor_copy", r=["Qd"], w=[f"Qaug{qa}q"], out=Qaug[qa][0:64, 0:ng, :],
                    in_=Qb.unsqueeze(1).to_broadcast([64, ng, 512]))
                Qfull = Qaug[qa][:, 0, :]
                Qr = [f"Qaug{qa}q", f"Qaug{qa}m"]
                jmax = (t0 + 112) // 2048
                nj = jmax + 1
                for j in range(nj):
                    si = cnt["pt"] % 3; cnt["pt"] += 1
                    P.MM(ST[si][:], KcT[:, j * 128:(j + 1) * 128], Qfull, r=["KcT"] + Qr, w=[f"ST{si}"])
                    P.I("act", "activation", r=[f"ST{si}"], w=[f"PTc{j}"], out=PTc[:, j, :], in_=ST[si][:], func=AF.Exp)
                    delta = t0 - 2048 * j - 15
                    if j == 0 or delta < 2032:
                        P.I("dve", "scalar_tensor_tensor", r=["D16", f"PTc{j}"], w=[f"PTc{j}"], out=PTc[:, j, :].rearrange("p (r q) -> p r q", r=4),
                            in0=D16[:, 1 if j == 0 else 0, :].unsqueeze(1).to_broadcast([128, 4, 128]), scalar=float(delta),
                            in1=PTc[:, j, :].rearrange("p (r q) -> p r q", r=4), op0=ALU.is_le, op1=ALU.mult)
                    P.MM(AUX[0:1, :], ones_b[:, 0:1], PTc[:, j, :], start=(j == 0), stop=(j == jmax), r=["ones_b", f"PTc{j}"], w=["AUX"])
                P.I("dve", "tensor_scalar", r=["AUX"], w=["zc"], out=zc[:], in0=AUX[0:1, :], scalar1=1e-30, scalar2=None, op0=ALU.max)
                P.I("dve", "reciprocal", r=["zc"], w=["zc"], out=zc[:], in_=zc[:])
                P.MM(AUX[:], ones_f[0:1, :], zc[0:1, :], r=["ones_f", "zc"], w=["AUX"])
                pk_all = [f"PTc{j}" for j in range(nj)]
                P.I("dve", "tensor_tensor", r=pk_all + ["AUX"], w=pk_all, out=PTc[:, 0:nj, :], in0=PTc[:, 0:nj, :],
                    in1=AUX[:].unsqueeze(1).to_broadcast([128, nj, 512]), op=ALU.mult)
                oi = cnt["oa"] % 2; cnt["oa"] += 1
                for j in range(nj):
                    P.MM(OA[oi][:], Vc[:, j, :], PTc[:, j, :], start=(j == 0), stop=(j == jmax), r=["Vc", f"PTc{j}"], w=[f"OA{oi}"])
                for j in range(nj):
                    for r in range(4):
                        P.MM(IMP[:, 0:256], PTc[:, j, r * 128:(r + 1) * 128], slcm[:, j, :], start=(j == 0 and r == 0),
                             stop=(j == jmax and r == 3), r=["slcm", f"PTc{j}"], w=["IMP"])
                combine(0, OA[oi], f"OA{oi}", False)
                jb = 2 * QB
                P.I("dve", "tensor_tensor", r=["IMP", "pats"], w=["impS"], out=impS[:], in0=IMP[:, 0:256], in1=pats[:, 0, 256 - jb:512 - jb], op=ALU.mult)
                P.I("dve", "tensor_tensor", r=["impS", "pats"], w=["impS"], out=impS[:], in0=impS[:], in1=pats[:, 1, 256 - jb:512 - jb], op=ALU.add)
                P.I("dve", "memset", r=["impS"], w=["impS"], ap=impS[:, 0:1], constant=10002.0)
                P.I("dve", "max", r=["impS"], w=["m8"], out=m8[:, 0:8], in_=impS[:])
                P.I("dve", "match_replace", r=["impS", "m8"], w=["scr"], out=scr[:], in_to_replace=m8[:, 0:8], in_values=impS[:], imm_value=-2.0)
                P.I("dve", "max", r=["scr"], w=["m8"], out=m8[:, 8:16], in_=scr[:])
                P.I("dve", "tensor_scalar", r=["impS", "m8"], w=["NT"], out=NT[:], in0=impS[:], scalar1=m8[:, 15:16], scalar2=1.0,
                    op0=ALU.is_ge, op1=ALU.subtract)
                for jt in range(2):
                    P.op("pe", (lambda jt: (lambda e: e.transpose(out=IMP[:, jt * 128:(jt + 1) * 128], in_=NT[:, jt * 128:(jt + 1) * 128], identity=identf[:])))(jt),
                         reads=["NT", "identf"], writes=["IMP"])
                for g_ in range(ng):
                    half = g_ % 2
                    P.I("act", "copy", r=["IMP"], w=[f"Qaug{qa}m"], out=Qaug[qa][64:128, g_, :].rearrange("p (r q) -> p r q", r=4),
                        in_=IMP[64 * half:64 * half + 64, (g_ // 2) * 128:(g_ // 2 + 1) * 128].unsqueeze(1).to_broadcast([64, 4, 128]))

                for br in (1, 2):
                    kcs = list(range(0, QB + 1)) if br == 1 else list(range(max(0, QB - 4), QB + 1))
                    oi = cnt["oa"] % 2; cnt["oa"] += 1
                    ot = OA[oi]; otk = f"OA{oi}"
                    base = cnt["pt"]

                    def qk(n, br=br, kcs=kcs, base=base, qa=qa, Qfull=Qfull, Qr=Qr):
                        kc = kcs[n]; si = (base + n) % 3
                        if br == 1:
                            P.MM(ST[si][:], KsT[:, kc * 128:(kc + 1) * 128], Qaug[qa][:, kc // 32, :],
                                 r=["KsT", "KsE", f"Qaug{qa}q", f"Qaug{qa}m"], w=[f"ST{si}"])
                        else:
                            P.MM(ST[si][:], KwT[:, kc % 8, :], Qfull, r=["KwT"] + Qr, w=[f"ST{si}"])

                    qk(0)
                    if len(kcs) > 1:
                        qk(1)
                    for n, kc in enumerate(kcs):
                        if n + 2 < len(kcs):
                            qk(n + 2)
                        si = (base + n) % 3
                        pi = cnt["pt"] % 4; cnt["pt"] += 1
                        pt = PT[pi]; ptk = f"PT{pi}"
                        P.I("act", "activation", r=[f"ST{si}"], w=[ptk], out=pt[:], in_=ST[si][:], func=AF.Exp)
                        if kc == QB:
                            P.I("dve", "tensor_tensor", r=[ptk, "tril"], w=[ptk], out=pt[:].rearrange("p (r q) -> p r q", r=4),
                                in0=pt[:].rearrange("p (r q) -> p r q", r=4), in1=tril[:, 0, :].unsqueeze(1).to_broadcast([128, 4, 128]), op=ALU.mult)
                        if br == 2 and kc == QB - 4:
                            P.I("dve", "tensor_tensor", r=[ptk, "tril"], w=[ptk], out=pt[:].rearrange("p (r q) -> p r q", r=4),
                                in0=pt[:].rearrange("p (r q) -> p r q", r=4), in1=tril[:, 1, :].unsqueeze(1).to_broadcast([128, 4, 128]), op=ALU.mult)
                        vv = Vs[:, kc, :] if br == 1 else Vw[:, kc % 8, :]
                        P.MM(ot[:], vv, pt[:], start=(n == 0), stop=(n == len(kcs) - 1), r=["Vs" if br == 1 else "Vw", ptk], w=[otk])
                    combine(br, ot, otk, True)
                ai = cnt["acc"] % 2; cnt["acc"] += 1
                P.D("sp", out=oT_dst(t0, 128).rearrange("(r x) q -> x r q", x=64), in_=acc[ai][:].rearrange("p (r q) -> p r q", r=4),
                    r=[f"acc{ai}"], w=[f"oT{QB}"])
        P.wait_all("sp", [f"oT{q}" for q in range(NBLK * 4)])
        P.barrier()
        P.emit()


EPS = 1e-6
NEG = -1.0e30


def declare_b(nc, T, pfx=""):
    d = {}

    def inp(name, shape, dt=F32):
        d[name] = nc.dram_tensor(pfx + name, shape, dt, kind="ExternalInput").ap()

    def outp(name, shape, dt=F32):
        d[name] = nc.dram_tensor(pfx + name, shape, dt, kind="ExternalOutput").ap()

    def scr(name, shape, dt=F32):
        d[name] = nc.dram_tensor(pfx + name, shape, dt, kind="Internal").ap()

    inp("hT", [1024, T]); inp("oT", [1024, T]); inp("wout", [1024, 1024]); inp("gffn", [1024])
    inp("wq", [1024, 2048]); inp("keysT", [16, 128, 128]); inp("uT", [1024, 16384]); inp("v", [16384, 1024])
    inp("gnext", [1024])
    outp("hT_out", [1024, T]); outp("nT_out", [1024, T])
    scr("h2T", [1024, T]); scr("hnbf", [1024, T], BF16); scr("qTd", [128, T // 128, 16, 128])
    scr("GT", [T // 128, 128, 16384], BF16); scr("uTbf", [1024, 16384], BF16); scr("vbf", [16384, 1024], BF16)
    return d


def b_cast_ops(P, d, pfx=""):
    ops = []
    for i in range(8):
        ops.append((lambda i: (lambda: P.D("pool", out=d["uTbf"][i * 128:(i + 1) * 128, :], in_=d["uT"][i * 128:(i + 1) * 128, :], w=[pfx + f"uTbf{i}"])))(i))
    for i in range(8):
        ops.append((lambda i: (lambda: P.D("pool", out=d["vbf"][i * 2048:(i + 1) * 2048, :], in_=d["v"][i * 2048:(i + 1) * 2048, :], w=[pfx + f"vbf{i}"])))(i))
    return ops


def emit_b_casts(P, d, pfx=""):
    for f in b_cast_ops(P, d, pfx):
        f()


def emit_b(nc, P, T, d, cast_weights=True, pfx="", oT_blk=None, nT_dst=None):
    NB = T // 512
    NT = T // 128
    NB2 = T // 256
    fm = lambda ap: ap.rearrange("(m p) t -> p m t", p=128)

    if cast_weights:
        emit_b_casts(P, d)

    with contextlib.ExitStack() as st:
        sb = lambda n, s, dt=F32: st.enter_context(nc.sbuf_tensor(pfx + "p0_" + n, s, dt))
        ps = lambda n, s, dt=F32: st.enter_context(nc.psum_tensor(pfx + "p0_" + n, s, dt))
        wout = sb("wout", [128, 8, 1024], BF16)
        g = sb("g", [128, 8]); ones = sb("ones", [128, 128])
        hTt = [sb(f"hTt{i}", [128, 8, 512]) for i in range(2)]
        oTt = [sb(f"oTt{i}", [128, 8, 512], BF16) for i in range(2)]
        h2 = sb("h2", [128, 8, 512]); hnb = sb("hnb", [128, 8, 512], BF16)
        rstd = sb("rstd", [128, 512])
        wqt = [sb(f"wqt{i}", [128, 8, 512]) for i in range(2)]
        qs = [sb(f"qs{i}", [128, 4, 512]) for i in range(2)]
        pp = [ps(f"pp{i}", [128, 512]) for i in range(2)]
        ss = ps("ss", [128, 512])

        P.D("pool", out=wout[:], in_=d["wout"].rearrange("(k p) c -> p k c", p=128), w=["wout"])
        P.D("sp", out=g[:], in_=d["gffn"].rearrange("(m p) -> p m", p=128), w=["g"], allow_slow_non_contiguous=True)
        P.I("pool", "memset", w=["ones"], ap=ones[:], constant=1.0)
        nmm = 0
        nwq = 0
        if oT_blk is not None:
            P.dma("sp", lambda e: e.dma_start(out=d["oTq"].rearrange("r (c t) -> c r t", t=min(1024, T)), in_=oT_blk()), writes=["oTq"])
        for b in range(NB):
            tsl = slice(b * 512, (b + 1) * 512)
            ht, ot = hTt[b % 2], oTt[b % 2]
            hk, ok = f"hTt{b % 2}", f"oTt{b % 2}"
            P.D("sp", out=ht[:], in_=fm(d["hT"])[:, :, tsl], w=[hk])
            if oT_blk is None:
                P.D("pool", out=ot[:], in_=fm(d["oT"])[:, :, tsl], w=[ok])
            else:
                P.D("pool", out=ot[:], in_=fm(d["oTq"])[:, :, tsl], r=["oTq"], w=[ok])
            for m in range(8):
                p_ = pp[nmm % 2]; pk = f"pp{nmm % 2}"; nmm += 1
                for k in range(8):
                    P.MM(p_[:], wout[:, k, m * 128:(m + 1) * 128], ot[:, k, :], start=(k == 0), stop=(k == 7),
                         r=["wout", ok], w=[pk])
                P.I("dve", "tensor_tensor", r=[pk, hk], w=["h2"], out=h2[:, m, :], in0=p_[:], in1=ht[:, m, :], op=ALU.add)
            P.D("sp", out=fm(d["h2T"])[:, :, tsl], in_=h2[:], r=["h2"], w=[f"h2T{b}"])
            P.I("act", "activation", r=["h2"], w=[hk], out=ht[:], in_=h2[:], func=AF.Square)
            for m in range(8):
                P.MM(ss[:], ones[:], ht[:, m, :], start=(m == 0), stop=(m == 7), r=["ones", hk], w=["ss"])
            P.I("act", "activation", r=["ss"], w=["rstd"], out=rstd[:], in_=ss[:], func=AF.Sqrt, scale=1.0 / 1024, bias=EPS)
            P.I("dve", "reciprocal", r=["rstd"], w=["rstd"], out=rstd[:], in_=rstd[:])
            for m in range(8):
                P.I("dve", "scalar_tensor_tensor", r=["h2", "g", "rstd"], w=["h2"], out=h2[:, m, :], in0=h2[:, m, :],
                    scalar=g[:, m:m + 1], in1=rstd[:], op0=ALU.mult, op1=ALU.mult)
            P.I("pool", "tensor_copy", r=["h2"], w=["hnb"], out=hnb[:], in_=h2[:])
            P.D("sp", out=fm(d["hnbf"])[:, :, tsl], in_=hnb[:], r=["hnb"], w=[f"hnbf{b}"])
            for jg in range(4):
                wt = wqt[nwq % 2]; wk = f"wqt{nwq % 2}"; q_ = qs[nwq % 2]; qk = f"qs{nwq % 2}"; nwq += 1
                P.D("sp", out=wt[:], in_=d["wq"].rearrange("(m p) c -> p m c", p=128)[:, :, jg * 512:(jg + 1) * 512], w=[wk])
                for jj in range(4):
                    p_ = pp[nmm % 2]; pk = f"pp{nmm % 2}"; nmm += 1
                    for m in range(8):
                        P.MM(p_[:], wt[:, m, jj * 128:(jj + 1) * 128], h2[:, m, :], start=(m == 0), stop=(m == 7),
                             r=[wk, "h2"], w=[pk])
                    P.I("act", "copy", r=[pk], w=[qk], out=q_[:, jj, :], in_=p_[:])
                for t4 in range(4):
                    P.D("sp", out=d["qTd"][:, b * 4 + t4, jg * 4:(jg + 1) * 4, :], in_=q_[:, :, t4 * 128:(t4 + 1) * 128],
                        r=[qk], w=[f"qTd{b}_{jg}_{t4}"])
        P.barrier()
        P.emit()

    with contextlib.ExitStack() as st:
        sb = lambda n, s, dt=F32: st.enter_context(nc.sbuf_tensor(pfx + "p1_" + n, s, dt))
        ps = lambda n, s, dt=F32: st.enter_context(nc.psum_tensor(pfx + "p1_" + n, s, dt))
        keys = sb("keys", [128, 16, 128]); ident = sb("ident", [128, 128], BF16); identf = sb("identf", [128, 128])
        qt = [sb(f"qt{i}", [128, 16, 128]) for i in range(2)]
        S12 = [sb(f"S12{i}", [128, 16, 128]) for i in range(2)]
        scr_ = sb("scr", [128, 256]); TS = sb("TS", [128, 16, 16]); cand = sb("cand", [128, 8, 256]); BS = sb("BS", [128, 8, 16])
        ex = sb("ex", [128, 8, 16]); Z = sb("Z", [128, 8]); lnZ = sb("lnZ", [128, 8]); bias = sb("bias", [128, 8])
        SUM = [sb(f"SUM{i}", [128, 8, 128]) for i in range(6)]
        E = [sb(f"E{i}", [128, 1024], BF16) for i in range(3)]
        GH = [[sb(f"GH{i}_{h}", [128, 1024], BF16) for h in range(8)] for i in range(2)]
        GTp = [sb(f"GTp{i}", [128, 8, 128], BF16) for i in range(4)]
        sc = ps("sc", [128, 16, 128])
        acc = [ps(f"acc{i}", [128, 4, 128]) for i in range(2)]

        P.D("sp", out=keys[:], in_=d["keysT"].rearrange("j c k -> c j k"), w=["keys"])
        P.I("pool", "memset", w=["identf"], ap=identf[:], constant=1.0)
        P.I("pool", "affine_select", r=["identf"], w=["identf"], out=identf[:], in_=identf[:], pattern=[[-1, 128]],
            compare_op=ALU.is_equal, fill=0.0, base=0, channel_multiplier=1)
        P.I("pool", "tensor_copy", r=["identf"], w=["ident"], out=ident[:], in_=identf[:])
        nsum = 0; nacc = 0; ngtp = 0; ngh = 0
        for tt in range(NT):
            q_ = qt[tt % 2]; qk = f"qt{tt % 2}"; S = S12[tt % 2]; Sk = f"S12{tt % 2}"
            P.D("sp", out=q_[:], in_=d["qTd"][:, tt, :, :], w=[qk])
            for j in range(16):
                P.MM(sc[:, j, :], q_[:, j, :], keys[:, j, :], r=[qk, "keys"], w=["sc"])
            P.I("act", "copy", r=["sc"], w=[Sk + "a"], out=S[:, 0:8, :], in_=sc[:, 0:8, :])
            P.I("dve", "tensor_copy", r=["sc"], w=[Sk + "b"], out=S[:, 8:16, :], in_=sc[:, 8:16, :])
            Sr = [Sk + "a", Sk + "b"]
            for j in range(16):
                P.I("dve", "max", r=Sr, w=["TS"], out=TS[:, j, 0:8], in_=S[:, j, :])
                P.I("dve", "match_replace", r=Sr + ["TS"], w=["scr"], out=scr_[:, 0:128], in_to_replace=TS[:, j, 0:8],
                    in_values=S[:, j, :], imm_value=NEG)
                P.I("dve", "max", r=["scr"], w=["TS"], out=TS[:, j, 8:16], in_=scr_[:, 0:128])
            TS4 = TS[:].rearrange("p (h two) a -> p h two a", two=2)
            P.I("dve", "tensor_tensor", r=["TS"], w=["cand"], out=cand[:].rearrange("p h (a b) -> p h a b", b=16),
                in0=TS4[:, :, 0, :].unsqueeze(3).to_broadcast([128, 8, 16, 16]),
                in1=TS4[:, :, 1, :].unsqueeze(2).to_broadcast([128, 8, 16, 16]), op=ALU.add)
            for h in range(8):
                P.I("dve", "max", r=["cand"], w=["BS"], out=BS[:, h, 0:8], in_=cand[:, h, :])
                P.I("dve", "match_replace", r=["cand", "BS"], w=["scr"], out=scr_[:, 0:256], in_to_replace=BS[:, h, 0:8],
                    in_values=cand[:, h, :], imm_value=NEG)
                P.I("dve", "max", r=["scr"], w=["BS"], out=BS[:, h, 8:16], in_=scr_[:, 0:256])
            P.I("dve", "tensor_tensor", r=["BS"], w=["ex"], out=ex[:], in0=BS[:], in1=BS[:, :, 0:1].to_broadcast([128, 8, 16]),
                op=ALU.subtract)
            P.I("act", "activation", r=["ex"], w=["ex"], out=ex[:], in_=ex[:], func=AF.Exp)
            P.I("dve", "reduce_sum", r=["ex"], w=["Z"], out=Z[:], in_=ex[:], axis=AX.X)
            P.I("act", "activation", r=["Z"], w=["lnZ"], out=lnZ[:], in_=Z[:], func=AF.Ln)
            P.I("dve", "scalar_tensor_tensor", r=["BS", "lnZ"], w=["bias"], out=bias[:], in0=BS[:, :, 0], scalar=-1.0,
                in1=lnZ[:], op0=ALU.mult, op1=ALU.subtract)
            def add_op(it):
                sx_, h_ = it // 8, it % 8
                su = SUM[it % 6]; sk = f"SUM{it % 6}"
                if False:
                    for a in range(8):
                        P.I("act", "activation", r=Sr, w=[sk], out=su[:, a, :], in_=S[:, 2 * h_ + 1, :], func=AF.Identity,
                            bias=S[:, 2 * h_, sx_ * 8 + a:sx_ * 8 + a + 1], scale=1.0)
                else:
                    P.I("dve", "tensor_tensor", r=Sr, w=[sk], out=su[:],
                        in0=S[:, 2 * h_, sx_ * 8:(sx_ + 1) * 8].unsqueeze(2).to_broadcast([128, 8, 128]),
                        in1=S[:, 2 * h_ + 1, :].unsqueeze(1).to_broadcast([128, 8, 128]), op=ALU.add)

            LOOK = 4
            deferred = []
            for it0 in range(LOOK):
                add_op(it0)
            for sx in range(16):
                ghs = GH[ngh % 2]; gk = f"GH{ngh % 2}_"; ngh += 1
                for h in range(8):
                    it = sx * 8 + h
                    if it + LOOK < 128:
                        add_op(it + LOOK)
                    if h == 4 and deferred:
                        deferred.pop(0)()
                    su = SUM[it % 6]; sk = f"SUM{it % 6}"; e_ = E[it % 3]; ek = f"E{it % 3}"
                    P.I("act", "activation", r=[sk, "bias"], w=[ek], out=e_[:], in_=su[:].rearrange("p a b -> p (a b)"),
                        func=AF.Exp, bias=bias[:, h:h + 1])
                    P.I("dve", "scalar_tensor_tensor", r=[sk, ek, "BS"], w=[gk + str(h)], out=ghs[h][:],
                        in0=su[:].rearrange("p a b -> p (a b)"), scalar=BS[:, h, 15:16], in1=e_[:], op0=ALU.is_ge, op1=ALU.mult)
                def flush(sx=sx, tt=tt, ghs=ghs, gk=gk):
                    nonlocal nacc, ngtp
                    gp = GTp[ngtp % 4]; gpk = f"GTp{ngtp % 4}"; ngtp += 1
                    for c4 in range(2):
                        a_ = acc[nacc % 2]; ak = f"acc{nacc % 2}"; nacc += 1
                        for ci in range(4):
                            c = c4 * 4 + ci
                            for h in range(8):
                                P.MM(a_[:, ci, :], ghs[h][:, c * 128:(c + 1) * 128], ident[:], start=(h == 0), stop=(h == 7),
                                     r=[gk + str(h), "ident"], w=[ak])
                        P.I("act", "copy", r=[ak], w=[gpk], out=gp[:, c4 * 4:(c4 + 1) * 4, :], in_=a_[:])
                    P.D("sp", out=d["GT"][tt, :, sx * 1024:(sx + 1) * 1024], in_=gp[:].rearrange("p c t -> p (c t)"), r=[gpk],
                        w=[f"GT{tt}_{sx}"])
                deferred.append(flush)
            while deferred:
                deferred.pop(0)()
        P.barrier()
        P.emit()

    NB5 = T // 512
    with contextlib.ExitStack() as st:
        sb = lambda n, s, dt=F32: st.enter_context(nc.sbuf_tensor(pfx + "p2_" + n, s, dt))
        ps = lambda n, s, dt=F32: st.enter_context(nc.psum_tensor(pfx + "p2_" + n, s, dt))
        U = [sb(f"U{i}", [128, 8, 1024], BF16) for i in range(2)]
        V = [sb(f"V{i}", [128, 8, 1024], BF16) for i in range(2)]
        Gg = [sb(f"Gg{i}", [128, 4, 8, 128], BF16) for i in range(2)]
        hn2 = [sb(f"hn2{i}", [128, 8, 512], BF16) for i in range(2)]
        ge = [sb(f"ge{i}", [128, 512], BF16) for i in range(2)]
        gh = [sb(f"gh{i}", [128, 8, 512], BF16) for i in range(2)]
        Yacc = sb("Yacc", [128, 8, 512]); h2b = sb("h2b", [128, 8, 512]); sq2 = sb("sq2", [128, 8, 512])
        rstd2 = sb("rstd2", [128, 512]); nrm = sb("nrm", [128, 8, 512], d["nT_out"].dtype)
        gn = sb("gn", [128, 8]); ones2 = sb("ones2", [128, 128])
        Hp = [ps(f"Hp{i}", [128, 512]) for i in range(2)]
        Yp = ps("Yp", [128, 4, 512])
        ss2 = ps("ss2", [128, 512])
        P.D("sp", out=gn[:], in_=d["gnext"].rearrange("(m p) -> p m", p=128), w=["gn"], allow_slow_non_contiguous=True)
        P.I("pool", "memset", w=["ones2"], ap=ones2[:], constant=1.0)
        nw = 0; nh = 0
        for blk in range(NB5):
            tsl = slice(blk * 512, (blk + 1) * 512)
            hb = hn2[blk % 2]; hbk = f"hn2{blk % 2}"
            P.D("sp", out=hb[:], in_=fm(d["hnbf"])[:, :, tsl], w=[hbk])
            P.D("sp", out=h2b[:], in_=fm(d["h2T"])[:, :, tsl], w=["h2b"])
            for eg in range(16):
                u_ = U[nw % 2]; uk = f"U{nw % 2}"; v_ = V[nw % 2]; vk = f"V{nw % 2}"; g_ = Gg[nw % 2]; gk = f"Gg{nw % 2}"
                gh_ = gh[nw % 2]; ghk = f"gh{nw % 2}"; nw += 1
                P.D("sp", out=u_[:], in_=d["uTbf"].rearrange("(m p) e -> p m e", p=128)[:, :, eg * 1024:(eg + 1) * 1024], w=[uk])
                P.D("act", out=v_[:], in_=d["vbf"].rearrange("(c p) x -> p c x", p=128)[:, eg * 8:(eg + 1) * 8, :], w=[vk])
                for t4 in range(4):
                    P.D("sp", out=g_[:, t4, :, :], in_=d["GT"][blk * 4 + t4, :, eg * 1024:(eg + 1) * 1024].rearrange("p (c t) -> p c t", t=128),
                        w=[gk + str(t4)])
                gks = [gk + str(t4) for t4 in range(4)]
                for c in range(8):
                    hp = Hp[nh % 2]; hpk = f"Hp{nh % 2}"; ge_ = ge[nh % 2]; gek = f"ge{nh % 2}"; nh += 1
                    for m in range(8):
                        P.MM(hp[:], u_[:, m, c * 128:(c + 1) * 128], hb[:, m, :], start=(m == 0), stop=(m == 7), r=[uk, hbk], w=[hpk])
                    P.I("act", "activation", r=[hpk], w=[gek], out=ge_[:], in_=hp[:], func=AF.Gelu_apprx_tanh)
                    P.I("dve", "tensor_tensor", r=[gek] + gks, w=[ghk + str(c)], out=gh_[:, c, :].rearrange("p (tt t) -> p tt t", t=128),
                        in0=ge_[:].rearrange("p (tt t) -> p tt t", t=128), in1=g_[:, :, c, :], op=ALU.mult)
                ghs = [ghk + str(c) for c in range(8)]
                for half in range(2):
                    for mi in range(4):
                        m = half * 4 + mi
                        for c in range(8):
                            P.MM(Yp[:, mi, :], v_[:, c, m * 128:(m + 1) * 128], gh_[:, c, :], start=(c == 0), stop=(c == 7), r=[vk] + ghs, w=["Yp"])
                    if eg == 0:
                        P.I("dve", "tensor_copy", r=["Yp"], w=[f"Yacc{half}"], out=Yacc[:, half * 4:(half + 1) * 4, :], in_=Yp[:])
                    else:
                        P.I("dve", "tensor_tensor", r=["Yp", f"Yacc{half}"], w=[f"Yacc{half}"], out=Yacc[:, half * 4:(half + 1) * 4, :],
                            in0=Yp[:], in1=Yacc[:, half * 4:(half + 1) * 4, :], op=ALU.add)
            P.I("dve", "tensor_tensor", r=["Yacc0", "Yacc1", "h2b"], w=["h2b"], out=h2b[:], in0=Yacc[:], in1=h2b[:], op=ALU.add)
            P.D("sp", out=fm(d["hT_out"])[:, :, tsl], in_=h2b[:], r=["h2b"], w=[f"hT_out{blk}"])
            P.I("act", "activation", r=["h2b"], w=["sq2"], out=sq2[:], in_=h2b[:], func=AF.Square)
            for m in range(8):
                P.MM(ss2[:], ones2[:], sq2[:, m, :], start=(m == 0), stop=(m == 7), r=["ones2", "sq2"], w=["ss2"])
            P.I("act", "activation", r=["ss2"], w=["rstd2"], out=rstd2[:], in_=ss2[:], func=AF.Sqrt, scale=1.0 / 1024, bias=EPS)
            P.I("dve", "reciprocal", r=["rstd2"], w=["rstd2"], out=rstd2[:], in_=rstd2[:])
            for m in range(8):
                P.I("dve", "scalar_tensor_tensor", r=["h2b", "gn", "rstd2"], w=["nrm"], out=nrm[:, m, :], in0=h2b[:, m, :],
                    scalar=gn[:, m:m + 1], in1=rstd2[:], op0=ALU.mult, op1=ALU.mult)
            if nT_dst is None:
                P.D("sp", out=fm(d["nT_out"])[:, :, tsl], in_=nrm[:], r=["nrm"], w=[f"nT_out{blk}"])
            else:
                for hf in range(2):
                    P.D("sp", out=nT_dst(2 * blk + hf), in_=nrm[:, :, hf * 256:(hf + 1) * 256], r=["nrm"], w=[f"nT_out{blk}_{hf}"])
        outs = [f"hT_out{b}" for b in range(NB5)]
        outs += [f"nT_out{b}" for b in range(NB5)] if nT_dst is None else [f"nT_out{b}_{hf}" for b in range(NB5) for hf in range(2)]
        P.wait_all("sp", outs)
        P.barrier()
        P.emit()


def declare_c(nc, S, pfx="", fused=False):
    d = {}

    def t(name, shape, kind, dt=F32):
        d[name] = nc.dram_tensor(pfx + name, shape, dt, kind=kind).ap()

    if not fused:
        t("nT", [1024, S], "ExternalInput")
    t("wq", [1024, 256], "ExternalInput"); t("wk", [1024, 256], "ExternalInput"); t("wv", [1024, 256], "ExternalInput")
    t("wf", [1024, 4], "ExternalInput"); t("fb", [4], "ExternalInput")
    t("maskd", [128, 4, 512], "ExternalInput")
    t("oT", [256, S], "Internal" if fused else "ExternalOutput")
    return d


def emit_c(nc, P, S, d, nT_loads=None, oT_dst=None):
    if oT_dst is None:
        oT_dst = lambda t0, n: d["oT"][:, t0:t0 + n]
    NBLK = S // 512
    NCH = S // 128
    with contextlib.ExitStack() as st:
        sb = lambda n, s, dt=F32: st.enter_context(nc.sbuf_tensor("c_" + n, s, dt))
        ps = lambda n, s, dt=F32: st.enter_context(nc.psum_tensor("c_" + n, s, dt))
        KT = sb("KT", [128, 2, S], BF16)
        Vr = sb("Vr", [128, NCH, 4 * 65 + 63], BF16)
        Cr = sb("Cr", [128, NCH, 4]); nbias = sb("nbias", [128, 4, NCH])
        nb = [sb(f"nb{i}", [128, 8, 512], BF16) for i in range(2)]
        wq = sb("wq", [128, 8, 256], BF16); wk = sb("wk", [128, 8, 256], BF16); wv = sb("wv", [128, 8, 256], BF16)
        wf = sb("wf", [128, 8, 4], BF16); fb = sb("fb", [128, 4])
        QT = [sb(f"QT{i}", [128, 512], BF16) for i in range(4)]
        PT = [sb(f"PT{i}", [128, 512], BF16) for i in range(4)]
        maskd = sb("maskd", [128, 4, 512], BF16)
        tri = sb("tri", [128, 128]); ones = sb("ones", [128, 128])
        lf = sb("lf", [128, 4]); tot = sb("tot", [128, 4]); totmid = sb("totmid", [128, 4])
        zrow = sb("zrow", [65, 512]); ocp = sb("ocp", [64, 512]); osb = [sb(f"osb{i}", [64, 512]) for i in range(2)]
        pp = [ps(f"pp{i}", [128, 512]) for i in range(2)]
        ST = [ps(f"ST{i}", [128, 512]) for i in range(3)]
        OT = [ps(f"OT{i}", [128, 512]) for i in range(2)]
        MISC = ps("MISC", [128, 512]); ZB = MISC[0:64, :]; cs = MISC[:, 0:8]

        for nm, tl in (("wq", wq), ("wk", wk), ("wv", wv)):
            P.D("pool", out=tl[:], in_=d[nm].rearrange("(m p) c -> p m c", p=128), w=[nm])
        P.D("pool", out=wf[:], in_=d["wf"].rearrange("(m p) c -> p m c", p=128), w=["wf"])
        P.D("sp", out=fb[:], in_=d["fb"].partition_broadcast(128), w=["fb"])
        P.D("pool", out=maskd[:], in_=d["maskd"], w=["maskd"])
        P.I("pool", "memset", w=["ones"], ap=ones[:], constant=1.0)
        P.I("pool", "memset", w=["tri"], ap=tri[:], constant=1.0)
        P.I("pool", "affine_select", r=["tri"], w=["tri"], out=tri[:], in_=tri[:], pattern=[[1, 128]],
            compare_op=ALU.is_ge, fill=0.0, base=0, channel_multiplier=-1)
        P.I("pool", "memset", w=["tot"], ap=tot[:], constant=0.0)
        P.I("pool", "memset", w=["Vr"], ap=Vr[:], constant=0.0)
        P.I("pool", "memset", r=["Vr"], w=["Vr"], ap=Vr[:, :, 0:260].rearrange("p c (h x) -> p c h x", x=65)[:, :, :, 64:65], constant=1.0)
        for i in range(4):
            P.I("pool", "memset", w=[f"QT{i}"], ap=QT[i][:], constant=0.0)
        npp = 0; nst = 0; npt = 0; nhead = 0
        for blk in range(NBLK):
            tsl = slice(blk * 512, (blk + 1) * 512)
            n_ = nb[blk % 2]; nk = f"nb{blk % 2}"
            if nT_loads is None:
                P.D("pool", out=n_[:], in_=d["nT"].rearrange("(m p) t -> p m t", p=128)[:, :, tsl], w=[nk])
            else:
                for csl, src in nT_loads(blk):
                    P.D("pool", out=n_[:, :, csl], in_=src, w=[nk])
            for pair in range(2):
                p_ = pp[npp % 2]; pk = f"pp{npp % 2}"; npp += 1
                for m in range(8):
                    P.MM(p_[:], wq[:, m, pair * 128:(pair + 1) * 128], n_[:, m, :], start=(m == 0), stop=(m == 7), r=["wq", nk], w=[pk])
                for hh in range(2):
                    rs = slice(hh * 64, hh * 64 + 64)
                    P.I("act", "mul", r=[pk], w=[f"QT{2 * pair + hh}"], out=QT[2 * pair + hh][rs, :], in_=p_[rs, :], mul=0.125)
                p_ = pp[npp % 2]; pk = f"pp{npp % 2}"; npp += 1
                for m in range(8):
                    P.MM(p_[:], wk[:, m, pair * 128:(pair + 1) * 128], n_[:, m, :], start=(m == 0), stop=(m == 7), r=["wk", nk], w=[pk])
                P.I("dve", "tensor_copy", r=[pk], w=["KT"], out=KT[:, pair, tsl], in_=p_[:])
            for t4 in range(4):
                ch = blk * 4 + t4
                p_ = pp[npp % 2]; pk = f"pp{npp % 2}"; npp += 1
                for m in range(8):
                    P.MM(p_[:, 0:256], n_[:, m, t4 * 128:(t4 + 1) * 128], wv[:, m, :], start=(m == 0), stop=(m == 7), r=["wv", nk], w=[pk])
                P.I("act", "copy", r=[pk], w=["Vr"], out=Vr[:, ch, 0:260].rearrange("p (h x) -> p h x", x=65)[:, :, 0:64], in_=p_[:, 0:256].rearrange("p (h x) -> p h x", x=64))
                for m in range(8):
                    P.MM(cs[:, 0:4], n_[:, m, t4 * 128:(t4 + 1) * 128], wf[:, m, :], start=(m == 0), stop=(m == 7), r=["wf", nk], w=["MISC"])
                P.I("dve", "tensor_tensor", r=["MISC", "fb"], w=["lf"], out=lf[:], in0=cs[:, 0:4], in1=fb[:], op=ALU.add)
                P.I("act", "activation", r=["lf"], w=["lf"], out=lf[:], in_=lf[:], func=AF.Exp, scale=-1.0)
                P.I("act", "activation", r=["lf"], w=["lf"], out=lf[:], in_=lf[:], func=AF.Ln, bias=1.0)
                P.I("dve", "tensor_scalar", r=["lf"], w=["lf"], out=lf[:], in0=lf[:], scalar1=-1.0, scalar2=None, op0=ALU.mult)
                P.MM(cs[:, 0:4], tri[:], lf[:], r=["tri", "lf"], w=["MISC"])
                P.MM(cs[:, 4:8], ones[:], lf[:], r=["ones", "lf"], w=["MISC"])
                P.I("dve", "tensor_tensor", r=["MISC", "tot"], w=["Cr"], out=Cr[:, ch, :], in0=cs[:, 0:4], in1=tot[:], op=ALU.add)
                P.I("dve", "tensor_tensor", r=["MISC", "tot"], w=["tot"], out=tot[:], in0=cs[:, 4:8], in1=tot[:], op=ALU.add)
                if t4 == 1:
                    P.I("dve", "tensor_copy", r=["tot"], w=["totmid"], out=totmid[:], in_=tot[:])
            nch = blk * 4 + 4
            for hl in range(4):
                P.I("dve", "tensor_scalar", r=["Cr", "totmid"], w=["nbias"], out=nbias[:, hl, 0:nch], in0=Cr[:, 0:nch, hl],
                    scalar1=totmid[:, hl:hl + 1], scalar2=-1.0, op0=ALU.subtract, op1=ALU.mult)
            pairs = [(hl, kc) for hl in range(4) for kc in range(nch)]

            def qk(i):
                nonlocal nst
                hl, kc = pairs[i]
                s_ = ST[i % 3]
                P.MM(s_[:], KT[:, hl // 2, kc * 128:(kc + 1) * 128], QT[hl][:], r=["KT", f"QT{hl}"], w=[f"ST{i % 3}"])

            qk(0)
            if len(pairs) > 1:
                qk(1)
            for i, (hl, kc) in enumerate(pairs):
                if i + 2 < len(pairs):
                    qk(i + 2)
                s_ = ST[i % 3]; sk = f"ST{i % 3}"
                pt = PT[npt % 4]; ptk = f"PT{npt % 4}"; npt += 1
                P.I("act", "activation", r=[sk, "nbias"], w=[ptk], out=pt[:], in_=s_[:], func=AF.Exp, bias=nbias[:, hl, kc:kc + 1])
                if kc >= blk * 4:
                    P.I("dve", "tensor_tensor", r=[ptk, "maskd"], w=[ptk], out=pt[:], in0=pt[:], in1=maskd[:, kc - blk * 4, :], op=ALU.mult)
                if kc == 0:
                    ot = OT[nhead % 2]; otk = f"OT{nhead % 2}"; ob = osb[nhead % 2]; obk = f"osb{nhead % 2}"; nhead += 1
                P.MM(ot[:], Vr[:, kc, hl * 65:hl * 65 + 128], pt[:], start=(kc == 0), stop=(kc == nch - 1), r=["Vr", ptk], w=[otk])
                if kc == nch - 1:
                    P.I("dve", "tensor_scalar", r=[otk], w=["zrow"], out=zrow[64:65, :], in0=ot[64:65, :], scalar1=1e-30, scalar2=None, op0=ALU.max)
                    P.I("dve", "reciprocal", r=["zrow"], w=["zrow"], out=zrow[64:65, :], in_=zrow[64:65, :])
                    P.MM(ZB, ones[64:65, 0:64], zrow[64:65, :], r=["ones", "zrow"], w=["MISC"])
                    P.I("act", "copy", r=[otk], w=["ocp"], out=ocp[:], in_=ot[0:64, :])
                    P.I("dve", "tensor_tensor", r=["ocp", "MISC"], w=[obk], out=ob[:], in0=ocp[:], in1=ZB, op=ALU.mult)
                    P.D("sp", out=oT_dst(blk * 512, 512)[hl * 64:(hl + 1) * 64, :], in_=ob[:], r=[obk], w=[f"oT{blk}_{hl}"])
        P.wait_all("sp", [f"oT{b}_{h}" for b in range(NBLK) for h in range(4)])
        P.barrier()
        P.emit()


S_FULL = 16384
T_CORE = 4096
G4 = [[0, 1, 2, 3], [4, 5, 6, 7]]
_PROG = {}
_B_IN = (("wout", [1024, 1024]), ("gffn", [1024]), ("wq", [1024, 2048]), ("keysT", [16, 128, 128]), ("uT", [1024, 16384]),
         ("v", [16384, 1024]), ("gnext", [1024]))


def _declare_b_fused(nc, T, pfx, hT_ap, out_kind):
    d = {}
    for name, shape in _B_IN:
        d[name] = nc.dram_tensor(pfx + name, shape, F32, kind="ExternalInput").ap()
    d["hT"] = hT_ap
    d["hT_out"] = nc.dram_tensor(pfx + "hT_out", [1024, T], F32, kind="Internal").ap()
    d["nT_out"] = nc.dram_tensor(pfx + "nT_out", [1024, T], F32, kind=out_kind).ap()
    scr = lambda name, shape, dt=F32: nc.dram_tensor(pfx + name, shape, dt, kind="Internal").ap()
    d["h2T"] = scr("h2T", [1024, T]); d["hnbf"] = scr("hnbf", [1024, T], BF16); d["qTd"] = scr("qTd", [128, T // 128, 16, 128])
    d["oTq"] = scr("oTq", [1024, T])
    d["GT"] = scr("GT", [T // 128, 128, 16384], BF16); d["uTbf"] = scr("uTbf", [1024, 16384], BF16); d["vbf"] = scr("vbf", [16384, 1024], BF16)
    return d


def _build_fused(S=S_FULL, T=T_CORE):
    if (S, T) in _PROG:
        return _PROG[(S, T)]
    nc = bass.Bass("TRN2", target_bir_lowering=False)
    with contextlib.ExitStack() as st:
        P = Prog(nc)
        P.alloc_sems(st)
        da = declare_a(nc, S, pfx="A_", fused=True)
        xTs = nc.dram_tensor("B0_xTs", [1024, T], F32, kind="ExternalInput").ap()
        db0 = _declare_b_fused(nc, T, "B0_", xTs, "Internal")
        dc = declare_c(nc, S, pfx="C_", fused=True)
        db1 = _declare_b_fused(nc, T, "B1_", db0["hT_out"], "ExternalOutput")
        CW = min(1024, T)
        NC1 = S // CW
        CW2 = 256
        NC2 = T // CW2
        dt_ = lambda name, shape: nc.dram_tensor(name, shape, F32, kind="Internal").ap()
        x1_in = dt_("x1_in", [NC1, 256, CW]); x1_out = dt_("x1_out", [NC1, 1024, CW])
        x2_in = dt_("x2_in", [NC2, 1024, CW2]); x2_out = dt_("x2_out", [NC2, 4096, CW2])
        x3_in = dt_("x3_in", [NC1, 256, CW]); x3_out = dt_("x3_out", [NC1, 1024, CW])

        def gather(src, dst, n, name):
            for j in range(n):
                P.cc((lambda j: (lambda e: e.collective_compute("AllGather", ALU.bypass, replica_groups=G4, ins=[src[j]], outs=[dst[j]])))(j),
                     writes=[name])
            P.barrier()

        def chunked_dst(buf):
            return lambda t0, n: buf[t0 // CW, :, (t0 % CW):(t0 % CW) + n]

        def quarter_src(buf):
            def f():
                q = nc.partition_id() % 4
                return buf[bass.ds(q * (T // CW), T // CW), :, :]
            return f

        bpr = T // 512

        def nT_loads(blk):
            rank, lb = blk // bpr, blk % bpr
            return [(slice(h * CW2, (h + 1) * CW2),
                     x2_out[lb * 2 + h, rank * 1024:(rank + 1) * 1024, :].rearrange("(m p) t -> p m t", p=128)) for h in range(2)]

        casts = b_cast_ops(P, db0, "B0_") + b_cast_ops(P, db1, "B1_")
        nblk_a = S // 512

        def cast_hook(blk):
            per = -(-len(casts) // nblk_a)
            for f in casts[blk * per:(blk + 1) * per]:
                f()

        emit_a(nc, P, S, da, oT_dst=chunked_dst(x1_in), block_hook=cast_hook)
        gather(x1_in, x1_out, NC1, "x1")
        emit_b(nc, P, T, db0, cast_weights=False, pfx="B0_", oT_blk=quarter_src(x1_out),
               nT_dst=lambda blk: x2_in[blk].rearrange("(m p) t -> p m t", p=128))
        gather(x2_in, x2_out, NC2, "x2")
        emit_c(nc, P, S, dc, nT_loads=nT_loads, oT_dst=chunked_dst(x3_in))
        gather(x3_in, x3_out, NC1, "x3")
        emit_b(nc, P, T, db1, cast_weights=False, pfx="B1_", oT_blk=quarter_src(x3_out))
    _PROG[(S, T)] = nc
    return nc


def _peer_inputs(pfx, wout, gffn, wq, keys, u, v, gnext):
    f32 = lambda a: np.ascontiguousarray(np.asarray(a, dtype=np.float32))
    return {pfx + "wout": f32(wout), pfx + "gffn": f32(gffn), pfx + "wq": f32(wq),
            pfx + "keysT": np.ascontiguousarray(f32(keys).transpose(0, 1, 3, 2).reshape(16, 128, 128)),
            pfx + "uT": np.ascontiguousarray(f32(u).T), pfx + "v": f32(v), pfx + "gnext": f32(gnext)}


def kernel(x, l0_attn_norm, l0_w_in, l0_cmp_pe_k, l0_cmp_w1_k, l0_cmp_w2_k, l0_cmp_pe_v, l0_cmp_w1_v, l0_cmp_w2_v, l0_w_out,
           l0_ffn_norm, l0_peer_wq, l0_peer_keys, l0_peer_u, l0_peer_v,
           l1_attn_norm, l1_w_in, l1_f_bias, l1_w_out,
           l1_ffn_norm, l1_peer_wq, l1_peer_keys, l1_peer_u, l1_peer_v,
           final_norm):
    f32 = lambda a: np.ascontiguousarray(np.asarray(a, dtype=np.float32))
    x = f32(x)
    B, S, D = x.shape
    T_CORE = S // 4
    nc = _build_fused(S, T_CORE)
    consts = consts_a(); ropeq, ropek = rope_tables(S)
    args0 = [f32(a) for a in (l0_attn_norm, l0_w_in, l0_cmp_pe_k, l0_cmp_w1_k, l0_cmp_w2_k, l0_cmp_pe_v, l0_cmp_w1_v, l0_cmp_w2_v)]
    pb0 = _peer_inputs("B0_", l0_w_out, l0_ffn_norm, l0_peer_wq, l0_peer_keys, l0_peer_u, l0_peer_v, l1_attn_norm)
    pb1 = _peer_inputs("B1_", l1_w_out, l1_ffn_norm, l1_peer_wq, l1_peer_keys, l1_peer_u, l1_peer_v, final_norm)
    w1 = f32(l1_w_in); fbias = f32(l1_f_bias)
    kk = np.arange(128)[:, None, None]; ii = np.arange(4)[None, :, None]; qq = np.arange(512)[None, None, :]
    maskd = (kk <= qq - 128 * ii).astype(np.float32)
    xT = [np.ascontiguousarray(x[b].T) for b in range(B)]
    maps = []
    for c in range(8):
        b, q = c // 4, c % 4
        m = {"A_" + k: v for k, v in host_inputs_a(x[b], *args0, q, consts, ropeq, ropek).items()}
        m["A_xT"] = xT[b]
        m["B0_xTs"] = np.ascontiguousarray(xT[b][:, q * T_CORE:(q + 1) * T_CORE])
        m.update(pb0); m.update(pb1)
        h0 = 4 * q
        m.update({"C_wq": np.ascontiguousarray(w1[:, h0 * 64:(h0 + 4) * 64]),
                  "C_wk": np.ascontiguousarray(w1[:, 1024 + h0 * 64:1024 + (h0 + 4) * 64]),
                  "C_wv": np.ascontiguousarray(w1[:, 2048 + h0 * 64:2048 + (h0 + 4) * 64]),
                  "C_wf": np.ascontiguousarray(w1[:, 3072 + h0:3072 + h0 + 4]), "C_fb": fbias[h0:h0 + 4].copy(), "C_maskd": maskd})
        maps.append(m)
    res = run_bass_kernel_spmd(nc, maps, core_ids=list(range(8))).results
    out = np.empty((B, S, D), np.float32)
    for c in range(8):
        b, q = c // 4, c % 4
        out[b, q * T_CORE:(q + 1) * T_CORE, :] = res[c]["B1_nT_out"].T
    return out
```

```python
import contextlib
import numpy as np
import concourse.bass as bass
import concourse.mybir as mybir
from concourse.bass_utils import run_bass_kernel_spmd

F32 = mybir.dt.float32
BF16 = mybir.dt.bfloat16
AF = mybir.ActivationFunctionType
ALU = mybir.AluOpType
AX = mybir.AxisListType

N_DMA_SEMS = 8


class Prog:
    COMPUTE = ("pe", "dve", "act", "pool")
    QUEUES = ("sp", "act", "pool")

    def __init__(self, nc):
        self.nc = nc
        self.ops = {e: [] for e in ("pe", "dve", "act", "pool", "sp")}
        self.cnt = {}
        self.last_w = {}
        self.readers = {}
        self.waited = {e: {} for e in self.ops}
        self.dma_n = {q: 0 for q in self.QUEUES}
        self.semkeys = []
        for e in self.COMPUTE:
            self._mk(("c", e))
        for q in self.QUEUES:
            for i in range(N_DMA_SEMS):
                self._mk(("d", q, i))
        self._mk(("cc",))
        self.sems = {}
        self.pending = {e: False for e in self.ops}
        self.lazy_pe_inc = False

    def _mk(self, k):
        self.cnt[k] = 0
        self.semkeys.append(k)

    def _need(self, eng, tok, waits):
        if tok is None:
            return
        k, v = tok
        if k == ("c", eng) and eng == "pe":
            return
        if self.waited[eng].get(k, 0) >= v:
            return
        self.waited[eng][k] = v
        waits.append((k, v))

    def _deps(self, eng, reads, writes):
        waits = []
        for b in reads:
            self._need(eng, self.last_w.get(b), waits)
        for b in writes:
            self._need(eng, self.last_w.get(b), waits)
            for t in self.readers.get(b, {}).items():
                if t[0] == ("c", eng):
                    continue
                self._need(eng, t, waits)
        return waits

    def _commit(self, tok, reads, writes):
        for b in reads:
            self.readers.setdefault(b, {})[tok[0]] = tok[1]
        for b in writes:
            self.last_w[b] = tok
            self.readers[b] = {}

    def op(self, eng, fn, reads=(), writes=(), inc=True):
        waits = self._deps(eng, reads, writes)
        k = ("c", eng)
        if inc:
            self.cnt[k] += 1
            tok = (k, self.cnt[k])
            self.pending[eng] = False
        else:
            tok = (k, self.cnt[k] + 1)
            self.pending[eng] = True
        self._commit(tok, reads, writes)
        self.ops[eng].append((waits, fn, (k, 1) if inc else None))
        return tok

    def dma(self, q, fn, reads=(), writes=()):
        waits = self._deps(q, reads, writes)
        n = self.dma_n[q]
        self.dma_n[q] += 1
        k = ("d", q, n % N_DMA_SEMS)
        self._need(q, (k, self.cnt[k]) if self.cnt[k] else None, waits)
        self.cnt[k] += 16
        tok = (k, self.cnt[k])
        self._commit(tok, reads, writes)
        self.ops[q].append((waits, fn, (k, 16)))
        return tok

    def cc(self, fn, reads=(), writes=()):
        waits = self._deps("pool", reads, writes)
        k = ("cc",)
        self._need("pool", (k, self.cnt[k]) if self.cnt[k] else None, waits)
        self.cnt[k] += 1
        tok = (k, self.cnt[k])
        self._commit(tok, reads, writes)
        self.ops["pool"].append((waits, fn, (k, 1)))
        return tok

    def I(self, eng, method, r=(), w=(), **kw):
        return self.op(eng, lambda e: getattr(e, method)(**kw), reads=r, writes=w)

    def MM(self, out, lhsT, rhs, start=True, stop=True, r=(), w=()):
        return self.op("pe", lambda e: e.matmul(out, lhsT=lhsT, rhs=rhs, start=start, stop=stop), reads=r, writes=w,
                       inc=(stop or not self.lazy_pe_inc))

    def D(self, q, out, in_, r=(), w=(), **kw):
        return self.dma(q, lambda e: e.dma_start(out=out, in_=in_, **kw), reads=r, writes=w)

    def barrier(self):
        assert not any(self.pending.values()), self.pending
        for eng in self.ops:
            waits = []
            for k in self.semkeys:
                if self.cnt[k]:
                    self._need(eng, (k, self.cnt[k]), waits)
            self.ops[eng].append((waits, None, None))
        self.last_w = {}
        self.readers = {}

    def wait_all(self, eng, bufs):
        waits = []
        for b in bufs:
            self._need(eng, self.last_w.get(b), waits)
        self.ops[eng].append((waits, None, None))

    def alloc_sems(self, st):
        for k in self.semkeys:
            self.sems[k] = st.enter_context(self.nc.semaphore("s_" + "_".join(map(str, k))))

    def emit(self):
        nc = self.nc
        import contextlib
        with contextlib.ExitStack() as st:
            if not self.sems:
                self.alloc_sems(st)
            block = st.enter_context(nc.Block())
            engobj = {"pe": "tensor", "dve": "vector", "act": "scalar", "pool": "gpsimd", "sp": "sync"}

            def mk(ename):
                ops = self.ops[ename]

                def body(eng):
                    for waits, fn, inc in ops:
                        for (k, v) in waits:
                            eng.wait_ge(self.sems[k], v)
                        if fn is not None:
                            ins = fn(eng)
                            if inc is not None:
                                ins.then_inc(self.sems[inc[0]], inc[1])
                return body

            for ename, attr in engobj.items():
                if self.ops[ename]:
                    getattr(block, attr)(mk(ename))
        self.ops = {e: [] for e in self.ops}


EPS = 1e-6


def consts_a():
    c = {}
    q = np.arange(128)[:, None]; rel = np.arange(512)[None, :] - 256
    cur = (q >= 64).astype(np.int64)
    M = (rel <= cur - 2).astype(np.float32)
    A = np.where(rel == cur, 10000.0, np.where(rel == cur - 1, 10001.0, np.where(rel > cur, -1.0, 0.0))).astype(np.float32)
    c["pats"] = np.stack([M, A], 1)
    ci = np.arange(128)[:, None]; qi = np.arange(128)[None, :]
    D = (16 * ci - qi).astype(np.float32)
    Dz = D.copy(); Dz[0, :] = 1e9
    c["D16"] = np.stack([D, Dz], 1)
    c["tril"] = np.stack([(ci <= qi), (ci > qi)], 1).astype(np.float32)
    c["Sel"] = np.broadcast_to(np.eye(12, dtype=np.float32)[:, :, None], (12, 12, 64)).copy()
    cc = np.arange(1024)[:, None] - 1; jj = np.arange(256)[None, :]
    lo = np.maximum(cc * 16, jj * 64); hi = np.minimum(cc * 16 + 32, (jj + 1) * 64)
    m = np.maximum(hi - lo, 0).astype(np.float32) / 32.0
    m[0, :] = 0.0
    c["slcm"] = np.ascontiguousarray(m.reshape(8, 128, 256).transpose(1, 0, 2))
    return c


def epat_table(S):
    n = np.arange(S)[None, :]; r = np.arange(64)[:, None]
    return (30000.0 * (((n // 64) % 64) == r)).astype(np.float32)


def rope_tables(S):
    half = 32
    inv = (10000.0 ** (-np.arange(half, dtype=np.float32) / half)).astype(np.float32)
    ang = (np.arange(S, dtype=np.float32)[None, :] * inv[:, None]).astype(np.float32)
    cos = np.cos(ang).astype(np.float32); sin = np.sin(ang).astype(np.float32)
    cosf = np.concatenate([cos, cos], 0); sinf = np.concatenate([-sin, sin], 0)
    rk = np.stack([cosf, sinf], 1)
    return np.ascontiguousarray(rk * 0.125), np.ascontiguousarray(rk)


def host_inputs_a(xb, gattn, w_in, pe_k, w1_k, w2_k, pe_v, w1_v, w2_v, g, consts, ropeq, ropek):
    def sw(w):
        w = w.reshape(w.shape[0], -1, 64)
        return np.concatenate([w[..., 32:], w[..., :32]], -1).reshape(w.shape[0], -1)
    kv = lambda i: w_in[:, 1024 + i * 256 + g * 64:1024 + i * 256 + (g + 1) * 64]
    wq = w_in[:, g * 256:(g + 1) * 256]
    d = dict(consts)
    d["xT"] = np.ascontiguousarray(xb.T)
    d["gattn"] = gattn
    d["wqa"] = np.ascontiguousarray(np.concatenate([wq, sw(wq)], 1))
    d["wka"] = np.ascontiguousarray(np.concatenate([kv(0), kv(1), sw(kv(0)), kv(2), sw(kv(2)), kv(4), sw(kv(4))], 1))
    d["wtok"] = np.ascontiguousarray(np.concatenate([kv(3), kv(5)], 1))
    gc = [2560 + br * 16 + g * 4 + r for br in range(3) for r in range(4)]
    d["wg"] = np.ascontiguousarray(w_in[:, gc])
    d["w1s"] = np.ascontiguousarray(np.concatenate([w1_k[g].transpose(1, 0, 2), w1_v[g].transpose(1, 0, 2)], 0))
    d["peT"] = np.ascontiguousarray(np.concatenate([pe_k[g].T, pe_v[g].T], 0))
    d["w2s"] = np.ascontiguousarray(np.stack([w2_k[g], w2_v[g]], 1))
    d["ropeq"] = ropeq; d["ropek"] = ropek
    d["Epat"] = epat_table(xb.shape[0])
    return d


def declare_a(nc, S, pfx="", fused=False):
    d = {}

    def t(name, shape, kind="ExternalInput", dt=F32):
        d[name] = nc.dram_tensor(pfx + name, shape, dt, kind=kind).ap()

    t("xT", [1024, S]); t("gattn", [1024]); t("wqa", [1024, 512]); t("wka", [1024, 448]); t("wtok", [1024, 128]); t("wg", [1024, 12])
    t("w1s", [128, 32, 128]); t("peT", [128, 32]); t("w2s", [128, 2, 64]); t("ropeq", [64, 2, S]); t("ropek", [64, 2, S])
    t("pats", [128, 2, 512]); t("D16", [128, 2, 128]); t("tril", [128, 2, 128]); t("Epat", [64, S]); t("Sel", [12, 12, 64])
    t("slcm", [128, 8, 256])
    t("oT", [256, S], kind="Internal" if fused else "ExternalOutput")
    return d


def emit_a(nc, P, S, d, oT_dst=None, block_hook=None):
    if oT_dst is None:
        oT_dst = lambda t0, n: d["oT"][:, t0:t0 + n]
    NBLK = S // 512
    NCH = S // 128
    with contextlib.ExitStack() as st:
        sb = lambda n, s, dt=F32: st.enter_context(nc.sbuf_tensor("a_" + n, s, dt))
        ps = lambda n, s, dt=F32: st.enter_context(nc.psum_tensor("a_" + n, s, dt))
        KsT = sb("KsT", [128, S], BF16); Vs = sb("Vs", [128, NCH, 128], BF16)
        KwT = sb("KwT", [128, 8, 128], BF16); Vw = sb("Vw", [128, 8, 128], BF16)
        KcT = sb("KcT", [128, 1024], BF16); Vc = sb("Vc", [128, 8, 128], BF16)
        slcm = sb("slcm", [128, 8, 256], BF16)
        Qaug = [sb(f"Qaug{i}", [128, 4, 512], BF16) for i in range(2)]
        pats = sb("pats", [128, 2, 512]); D16 = sb("D16", [128, 2, 128]); tril = sb("tril", [128, 2, 128], BF16)
        Sel = sb("Sel", [12, 12, 64])
        wqa = sb("wqa", [128, 8, 512], BF16); wka = sb("wka", [128, 8, 448], BF16); wtok = sb("wtok", [128, 8, 128], BF16)
        wg = sb("wg", [128, 8, 12], BF16)
        w1s = sb("w1s", [128, 32, 128], BF16); peT = sb("peT", [128, 32], BF16); w2s = sb("w2s", [128, 2, 64], BF16)
        hb = sb("hb", [128, 2]); g = sb("g", [128, 8])
        ones_b = sb("ones_b", [128, 128], BF16); ones_f = sb("ones_f", [128, 128]); identf = sb("identf", [128, 128])
        xT = sb("xT", [128, 8, 512]); sq = sb("sq", [128, 8, 512], BF16); rstd = sb("rstd", [128, 512]); xnb = sb("xnb", [128, 8, 512], BF16)
        rq = sb("rq", [64, 2, 512]); rk = sb("rk", [64, 2, 512])
        t1 = [sb(f"t1_{i}", [64, 512]) for i in range(2)]; t2 = [sb(f"t2_{i}", [64, 512]) for i in range(2)]
        Qd = sb("Qd", [64, 4, 4, 128], BF16)
        CV = sb("CV", [128, 528], BF16); hidk = sb("hidk", [128, 32], BF16); hvp = sb("hvp", [128, 128], BF16)
        gT = sb("gT", [12, 512])
        PTc = sb("PTc", [128, 8, 512], BF16); PT = [sb(f"PT{i}", [128, 512], BF16) for i in range(4)]
        zc = sb("zc", [1, 512]); impS = sb("impS", [128, 256]); scr = sb("scr", [128, 256]); m8 = sb("m8", [128, 16])
        NT = sb("NT", [128, 256])
        zrow = sb("zrow", [65, 512]); gbs = sb("gbs", [64, 512]); acc = [sb(f"acc{i}", [64, 512]) for i in range(2)]
        tmp = sb("tmp", [64, 512])
        pp0 = ps("pp0", [128, 512])
        ST = [ps(f"ST{i}", [128, 512]) for i in range(3)]
        OA = [ps(f"OA{i}", [128, 512]) for i in range(2)]
        IMP = ps("IMP", [128, 512]); AUX = ps("AUX", [128, 512])
        pp = [pp0, IMP]; ppk = ["pp0", "IMP"]

        for nm, tl in (("wqa", wqa), ("wka", wka), ("wtok", wtok), ("wg", wg)):
            P.D("pool", out=tl[:], in_=d[nm].rearrange("(m p) c -> p m c", p=128), w=[nm])
        for nm, tl in (("w1s", w1s), ("peT", peT), ("w2s", w2s), ("slcm", slcm), ("tril", tril)):
            P.D("pool", out=tl[:], in_=d[nm], w=[nm])
        for nm, tl in (("pats", pats), ("D16", D16), ("Sel", Sel)):
            P.D("sp", out=tl[:], in_=d[nm], w=[nm])
        P.D("pool", out=KsT[64:128, :], in_=d["Epat"], w=["KsE"])
        P.D("sp", out=g[:], in_=d["gattn"].rearrange("(m p) -> p m", p=128), w=["g"], allow_slow_non_contiguous=True)
        P.I("pool", "memset", w=["ones_b"], ap=ones_b[:], constant=1.0)
        P.I("pool", "memset", w=["ones_f"], ap=ones_f[:], constant=1.0)
        P.I("pool", "memset", w=["identf"], ap=identf[:], constant=1.0)
        P.I("pool", "affine_select", r=["identf"], w=["identf"], out=identf[:], in_=identf[:], pattern=[[-1, 128]],
            compare_op=ALU.is_equal, fill=0.0, base=0, channel_multiplier=1)
        P.I("pool", "memset", w=["Vs"], ap=Vs[:], constant=0.0)
        P.I("pool", "memset", r=["Vs"], w=["Vs"], ap=Vs[:, :, 64:65], constant=1.0)
        P.I("pool", "memset", w=["Vw"], ap=Vw[:], constant=0.0)
        P.I("pool", "memset", r=["Vw"], w=["Vw"], ap=Vw[:, :, 64:65], constant=1.0)
        P.I("pool", "memset", w=["KwT"], ap=KwT[:], constant=0.0)
        for i in range(2):
            P.I("pool", "memset", w=[f"Qaug{i}q", f"Qaug{i}m"], ap=Qaug[i][:], constant=0.0)
        P.I("pool", "memset", w=["KcT"], ap=KcT[:], constant=0.0)
        P.I("pool", "memset", w=["Vc"], ap=Vc[:], constant=0.0)
        P.I("pool", "memset", w=["CVk", "CVv"], ap=CV[:], constant=0.0)
        P.I("pool", "memset", w=["hvp"], ap=hvp[:], constant=0.0)
        for kvi in range(2):
            rows = slice(kvi * 64, kvi * 64 + 64)
            for l in range(32):
                P.MM(pp[0][:, kvi:kvi + 1], w1s[rows, l, :], peT[rows, l:l + 1], start=(l == 0), stop=(l == 31), r=["w1s", "peT"], w=["pp0"])
        P.I("dve", "tensor_copy", r=["pp0"], w=["hb"], out=hb[:], in_=pp[0][:, 0:2])

        cnt = {"pp": 0, "t": 0, "pt": 0, "oa": 0, "acc": 0}

        def proj(c0, M):
            i = cnt["pp"] % 2; cnt["pp"] += 1
            return pp[i], ppk[i]

        for blk in range(NBLK):
            tsl = slice(blk * 512, (blk + 1) * 512)
            if block_hook is not None:
                block_hook(blk)
            P.D("sp", out=xT[:], in_=d["xT"].rearrange("(m p) t -> p m t", p=128)[:, :, tsl], w=["xT"])
            P.D("sp", out=rq[:], in_=d["ropeq"][:, :, tsl], w=["rq"])
            P.D("sp", out=rk[:], in_=d["ropek"][:, :, tsl], w=["rk"])
            P.I("act", "activation", r=["xT"], w=["sq"], out=sq[:], in_=xT[:], func=AF.Square)
            for m in range(8):
                P.MM(AUX[:], ones_b[:], sq[:, m, :], start=(m == 0), stop=(m == 7), r=["ones_b", "sq"], w=["AUX"])
            P.I("act", "activation", r=["AUX"], w=["rstd"], out=rstd[:], in_=AUX[:], func=AF.Sqrt, scale=1.0 / 1024, bias=EPS)
            P.I("dve", "reciprocal", r=["rstd"], w=["rstd"], out=rstd[:], in_=rstd[:])
            for m in range(8):
                P.I("dve", "scalar_tensor_tensor", r=["xT", "g", "rstd"], w=["xnb"], out=xnb[:, m, :], in0=xT[:, m, :],
                    scalar=g[:, m:m + 1], in1=rstd[:], op0=ALU.mult, op1=ALU.mult)

            def fmproj(wt, wk_, c0, M):
                p_, pk = proj(c0, M)
                for m in range(8):
                    P.MM(p_[0:M, :], wt[:, m, c0:c0 + M], xnb[:, m, :], start=(m == 0), stop=(m == 7), r=[wk_, "xnb"], w=[pk])
                return p_, pk

            def rope(wt, wk_, ca, cb, tab, tabk, out_ap, outk):
                pa, pak = fmproj(wt, wk_, ca, 64)
                i = cnt["t"] % 2; cnt["t"] += 1
                P.I("dve", "tensor_tensor", r=[pak, tabk], w=[f"t1_{i}"], out=t1[i][:], in0=pa[0:64, :], in1=tab[:, 0, :], op=ALU.mult)
                pb, pbk = fmproj(wt, wk_, cb, 64)
                P.I("dve", "tensor_tensor", r=[pbk, tabk], w=[f"t2_{i}"], out=t2[i][:], in0=pb[0:64, :], in1=tab[:, 1, :], op=ALU.mult)
                a_, b_ = t1[i][:], t2[i][:]
                if len(out_ap.shape) == 3:
                    a_ = a_.rearrange("p (a q) -> p a q", a=4); b_ = b_.rearrange("p (a q) -> p a q", a=4)
                P.I("pool", "tensor_tensor", r=[f"t1_{i}", f"t2_{i}"], w=[outk], out=out_ap, in0=a_, in1=b_, op=ALU.add)

            for r in range(4):
                rope(wqa, "wqa", r * 64, 256 + r * 64, rq, "rq", Qd[:, :, r, :], "Qd")
            rope(wka, "wka", 192, 256, rk, "rk", KsT[0:64, tsl], "KsT")
            rope(wka, "wka", 320, 384, rk, "rk", KwT[0:64, (blk % 2) * 4:(blk % 2) * 4 + 4, :], "KwT")
            pa, pak = fmproj(wka, "wka", 0, 128)
            i = cnt["t"] % 2; cnt["t"] += 1
            P.I("dve", "tensor_tensor", r=[pak, "rk"], w=[f"t1_{i}"], out=t1[i][:], in0=pa[0:64, :], in1=rk[:, 0, :], op=ALU.mult)
            P.I("act", "copy", r=[pak], w=["CVv"], out=CV[64:128, 16:528], in_=pa[64:128, :])
            pb, pbk = fmproj(wka, "wka", 128, 64)
            P.I("dve", "tensor_tensor", r=[pbk, "rk"], w=[f"t2_{i}"], out=t2[i][:], in0=pb[0:64, :], in1=rk[:, 1, :], op=ALU.mult)
            P.I("pool", "tensor_tensor", r=[f"t1_{i}", f"t2_{i}"], w=["CVk"], out=CV[0:64, 16:528], in0=t1[i][:], in1=t2[i][:], op=ALU.add)
            pgt, pgk = fmproj(wg, "wg", 0, 12)
            P.I("act", "activation", r=[pgk], w=["gT"], out=gT[:], in_=pgt[0:12, :], func=AF.Sigmoid)
            for t4 in range(4):
                ch = blk * 4 + t4
                i = cnt["pp"] % 2; cnt["pp"] += 1
                for m in range(8):
                    P.MM(pp[i][:, 0:128], xnb[:, m, t4 * 128:(t4 + 1) * 128], wtok[:, m, :], start=(m == 0), stop=(m == 7),
                         r=["wtok", "xnb"], w=[ppk[i]])
                P.I("act", "copy", r=[ppk[i]], w=["Vs"], out=Vs[:, ch, 0:64], in_=pp[i][:, 0:64])
                P.I("act", "copy", r=[ppk[i]], w=["Vw"], out=Vw[:, ch % 8, 0:64], in_=pp[i][:, 64:128])
            CVv = CV[:].rearrange("p (c s) -> p c s", s=16)
            i = cnt["pp"] % 2; cnt["pp"] += 1
            for l in range(32):
                P.MM(pp[i][:, 0:32], w1s[0:64, l, :], CVv[0:64, l // 16:l // 16 + 32, l % 16], start=(l == 0), stop=(l == 31),
                     r=["w1s", "CVk"], w=[ppk[i]])
            P.I("act", "activation", r=[ppk[i], "hb"], w=["hidk"], out=hidk[:], in_=pp[i][:, 0:32], func=AF.Gelu_apprx_tanh, bias=hb[:, 0:1])
            i2 = cnt["pp"] % 2; cnt["pp"] += 1
            P.MM(pp[i2][0:64, 0:32], w2s[:, 0, :], hidk[:], r=["w2s", "hidk"], w=[ppk[i2]])
            P.I("dve", "tensor_copy", r=[ppk[i2]], w=["KcT"], out=KcT[0:64, 32 * blk:32 * blk + 32], in_=pp[i2][0:64, 0:32])
            i = cnt["pp"] % 2; cnt["pp"] += 1
            for l in range(32):
                P.MM(pp[i][:, 0:32], w1s[64:128, l, :], CVv[64:128, l // 16:l // 16 + 32, l % 16], start=(l == 0), stop=(l == 31),
                     r=["w1s", "CVv"], w=[ppk[i]])
            off = (32 * blk) % 128
            P.I("act", "activation", r=[ppk[i], "hb"], w=["hvp"], out=hvp[:, off:off + 32], in_=pp[i][:, 0:32], func=AF.Gelu_apprx_tanh, bias=hb[:, 1:2])
            i2 = cnt["pp"] % 2; cnt["pp"] += 1
            P.MM(pp[i2][:, 0:64], hvp[:], w2s[:, 1, :], r=["w2s", "hvp"], w=[ppk[i2]])
            P.I("dve", "tensor_copy", r=[ppk[i2]], w=["Vc"], out=Vc[off:off + 32, (32 * blk) // 128, 0:64], in_=pp[i2][off:off + 32, 0:64])
            P.I("act", "copy", r=["CVk", "CVv"], w=["CVk", "CVv"], out=CV[:, 0:16], in_=CV[:, 512:528])

            for qi in range(4):
                QB = blk * 4 + qi
                t0 = 128 * QB
                Qb = Qd[:, qi, :, :].rearrange("p r q -> p (r q)")

                def combine(br, ot, otk, normalize):
                    ai = cnt["acc"] % 2
                    for r in range(4):
                        P.MM(AUX[0:64, r * 128:(r + 1) * 128], Sel[:, br * 4 + r, :], gT[:, qi * 128:(qi + 1) * 128], r=["Sel", "gT"], w=["AUX"])
                    P.I("act", "copy", r=["AUX"], w=["gbs"], out=gbs[:], in_=AUX[0:64, :])
                    if normalize:
                        P.I("dve", "tensor_scalar", r=[otk], w=["zrow"], out=zrow[64:65, :], in0=ot[64:65, :], scalar1=1e-30, scalar2=None, op0=ALU.max)
                        P.I("dve", "reciprocal", r=["zrow"], w=["zrow"], out=zrow[64:65, :], in_=zrow[64:65, :])
                        P.MM(AUX[0:64, :], ones_f[64:65, 0:64], zrow[64:65, :], r=["ones_f", "zrow"], w=["AUX"])
                        P.I("dve", "tensor_tensor", r=["gbs", "AUX"], w=["gbs"], out=gbs[:], in0=gbs[:], in1=AUX[0:64, :], op=ALU.mult)
                    if br == 0:
                        P.I("dve", "tensor_tensor", r=[otk, "gbs"], w=[f"acc{ai}"], out=acc[ai][:], in0=ot[0:64, :], in1=gbs[:], op=ALU.mult)
                    else:
                        P.I("dve", "tensor_tensor", r=[otk, "gbs"], w=["tmp"], out=tmp[:], in0=ot[0:64, :], in1=gbs[:], op=ALU.mult)
                        P.I("pool", "tensor_tensor", r=["tmp", f"acc{ai}"], w=[f"acc{ai}"], out=acc[ai][:], in0=acc[ai][:], in1=tmp[:], op=ALU.add)

                qa = QB % 2
                ng = QB // 32 + 1
                P.I("pool", "tensor_copy", r=["Qd"], w=[f"Qaug{qa}q"], out=Qaug[qa][0:64, 0:ng, :],
                    in_=Qb.unsqueeze(1).to_broadcast([64, ng, 512]))
                Qfull = Qaug[qa][:, 0, :]
                Qr = [f"Qaug{qa}q", f"Qaug{qa}m"]
                jmax = (t0 + 112) // 2048
                nj = jmax + 1
                for j in range(nj):
                    si = cnt["pt"] % 3; cnt["pt"] += 1
                    P.MM(ST[si][:], KcT[:, j * 128:(j + 1) * 128], Qfull, r=["KcT"] + Qr, w=[f"ST{si}"])
                    P.I("act", "activation", r=[f"ST{si}"], w=[f"PTc{j}"], out=PTc[:, j, :], in_=ST[si][:], func=AF.Exp)
                    delta = t0 - 2048 * j - 15
                    if j == 0 or delta < 2032:
                        P.I("dve", "scalar_tensor_tensor", r=["D16", f"PTc{j}"], w=[f"PTc{j}"], out=PTc[:, j, :].rearrange("p (r q) -> p r q", r=4),
                            in0=D16[:, 1 if j == 0 else 0, :].unsqueeze(1).to_broadcast([128, 4, 128]), scalar=float(delta),
                            in1=PTc[:, j, :].rearrange("p (r q) -> p r q", r=4), op0=ALU.is_le, op1=ALU.mult)
                    P.MM(AUX[0:1, :], ones_b[:, 0:1], PTc[:, j, :], start=(j == 0), stop=(j == jmax), r=["ones_b", f"PTc{j}"], w=["AUX"])
                P.I("dve", "tensor_scalar", r=["AUX"], w=["zc"], out=zc[:], in0=AUX[0:1, :], scalar1=1e-30, scalar2=None, op0=ALU.max)
                P.I("dve", "reciprocal", r=["zc"], w=["zc"], out=zc[:], in_=zc[:])
                P.MM(AUX[:], ones_f[0:1, :], zc[0:1, :], r=["ones_f", "zc"], w=["AUX"])
                pk_all = [f"PTc{j}" for j in range(nj)]
                P.I("dve", "tensor_tensor", r=pk_all + ["AUX"], w=pk_all, out=PTc[:, 0:nj, :], in0=PTc[:, 0:nj, :],
                    in1=AUX[:].unsqueeze(1).to_broadcast([128, nj, 512]), op=ALU.mult)
                oi = cnt["oa"] % 2; cnt["oa"] += 1
                for j in range(nj):
                    P.MM(OA[oi][:], Vc[:, j, :], PTc[:, j, :], start=(j == 0), stop=(j == jmax), r=["Vc", f"PTc{j}"], w=[f"OA{oi}"])
                for j in range(nj):
                    for r in range(4):
                        P.MM(IMP[:, 0:256], PTc[:, j, r * 128:(r + 1) * 128], slcm[:, j, :], start=(j == 0 and r == 0),
                             stop=(j == jmax and r == 3), r=["slcm", f"PTc{j}"], w=["IMP"])
                combine(0, OA[oi], f"OA{oi}", False)
                jb = 2 * QB
                P.I("dve", "tensor_tensor", r=["IMP", "pats"], w=["impS"], out=impS[:], in0=IMP[:, 0:256], in1=pats[:, 0, 256 - jb:512 - jb], op=ALU.mult)
                P.I("dve", "tensor_tensor", r=["impS", "pats"], w=["impS"], out=impS[:], in0=impS[:], in1=pats[:, 1, 256 - jb:512 - jb], op=ALU.add)
                P.I("dve", "memset", r=["impS"], w=["impS"], ap=impS[:, 0:1], constant=10002.0)
                P.I("dve", "max", r=["impS"], w=["m8"], out=m8[:, 0:8], in_=impS[:])
                P.I("dve", "match_replace", r=["impS", "m8"], w=["scr"], out=scr[:], in_to_replace=m8[:, 0:8], in_values=impS[:], imm_value=-2.0)
                P.I("dve", "max", r=["scr"], w=["m8"], out=m8[:, 8:16], in_=scr[:])
                P.I("dve", "tensor_scalar", r=["impS", "m8"], w=["NT"], out=NT[:], in0=impS[:], scalar1=m8[:, 15:16], scalar2=1.0,
                    op0=ALU.is_ge, op1=ALU.subtract)
                for jt in range(2):
                    P.op("pe", (lambda jt: (lambda e: e.transpose(out=IMP[:, jt * 128:(jt + 1) * 128], in_=NT[:, jt * 128:(jt + 1) * 128], identity=identf[:])))(jt),
                         reads=["NT", "identf"], writes=["IMP"])
                for g_ in range(ng):
                    half = g_ % 2
                    P.I("act", "copy", r=["IMP"], w=[f"Qaug{qa}m"], out=Qaug[qa][64:128, g_, :].rearrange("p (r q) -> p r q", r=4),
                        in_=IMP[64 * half:64 * half + 64, (g_ // 2) * 128:(g_ // 2 + 1) * 128].unsqueeze(1).to_broadcast([64, 4, 128]))

                for br in (1, 2):
                    kcs = list(range(0, QB + 1)) if br == 1 else list(range(max(0, QB - 4), QB + 1))
                    oi = cnt["oa"] % 2; cnt["oa"] += 1
                    ot = OA[oi]; otk = f"OA{oi}"
                    base = cnt["pt"]

                    def qk(n, br=br, kcs=kcs, base=base, qa=qa, Qfull=Qfull, Qr=Qr):
                        kc = kcs[n]; si = (base + n) % 3
                        if br == 1:
                            P.MM(ST[si][:], KsT[:, kc * 128:(kc + 1) * 128], Qaug[qa][:, kc // 32, :],
                                 r=["KsT", "KsE", f"Qaug{qa}q", f"Qaug{qa}m"], w=[f"ST{si}"])
                        else:
                            P.MM(ST[si][:], KwT[:, kc % 8, :], Qfull, r=["KwT"] + Qr, w=[f"ST{si}"])

                    qk(0)
                    if len(kcs) > 1:
                        qk(1)
                    for n, kc in enumerate(kcs):
                        if n + 2 < len(kcs):
                            qk(n + 2)
                        si = (base + n) % 3
                        pi = cnt["pt"] % 4; cnt["pt"] += 1
                        pt = PT[pi]; ptk = f"PT{pi}"
                        P.I("act", "activation", r=[f"ST{si}"], w=[ptk], out=pt[:], in_=ST[si][:], func=AF.Exp)
                        if kc == QB:
                            P.I("dve", "tensor_tensor", r=[ptk, "tril"], w=[ptk], out=pt[:].rearrange("p (r q) -> p r q", r=4),
                                in0=pt[:].rearrange("p (r q) -> p r q", r=4), in1=tril[:, 0, :].unsqueeze(1).to_broadcast([128, 4, 128]), op=ALU.mult)
                        if br == 2 and kc == QB - 4:
                            P.I("dve", "tensor_tensor", r=[ptk, "tril"], w=[ptk], out=pt[:].rearrange("p (r q) -> p r q", r=4),
                                in0=pt[:].rearrange("p (r q) -> p r q", r=4), in1=tril[:, 1, :].unsqueeze(1).to_broadcast([128, 4, 128]), op=ALU.mult)
                        vv = Vs[:, kc, :] if br == 1 else Vw[:, kc % 8, :]
                        P.MM(ot[:], vv, pt[:], start=(n == 0), stop=(n == len(kcs) - 1), r=["Vs" if br == 1 else "Vw", ptk], w=[otk])
                    combine(br, ot, otk, True)
                ai = cnt["acc"] % 2; cnt["acc"] += 1
                P.D("sp", out=oT_dst(t0, 128).rearrange("(r x) q -> x r q", x=64), in_=acc[ai][:].rearrange("p (r q) -> p r q", r=4),
                    r=[f"acc{ai}"], w=[f"oT{QB}"])
        P.wait_all("sp", [f"oT{q}" for q in range(NBLK * 4)])
        P.barrier()
        P.emit()


EPS = 1e-6
NEG = -1.0e30


def declare_b(nc, T, pfx=""):
    d = {}

    def inp(name, shape, dt=F32):
        d[name] = nc.dram_tensor(pfx + name, shape, dt, kind="ExternalInput").ap()

    def outp(name, shape, dt=F32):
        d[name] = nc.dram_tensor(pfx + name, shape, dt, kind="ExternalOutput").ap()

    def scr(name, shape, dt=F32):
        d[name] = nc.dram_tensor(pfx + name, shape, dt, kind="Internal").ap()

    inp("hT", [1024, T]); inp("oT", [1024, T]); inp("wout", [1024, 1024]); inp("gffn", [1024])
    inp("wq", [1024, 2048]); inp("keysT", [16, 128, 128]); inp("uT", [1024, 16384]); inp("v", [16384, 1024])
    inp("gnext", [1024])
    outp("hT_out", [1024, T]); outp("nT_out", [1024, T])
    scr("h2T", [1024, T]); scr("hnbf", [1024, T], BF16); scr("qTd", [128, T // 128, 16, 128])
    scr("GT", [T // 128, 128, 16384], BF16); scr("uTbf", [1024, 16384], BF16); scr("vbf", [16384, 1024], BF16)
    return d


def b_cast_ops(P, d, pfx=""):
    ops = []
    for i in range(8):
        ops.append((lambda i: (lambda: P.D("pool", out=d["uTbf"][i * 128:(i + 1) * 128, :], in_=d["uT"][i * 128:(i + 1) * 128, :], w=[pfx + f"uTbf{i}"])))(i))
    for i in range(8):
        ops.append((lambda i: (lambda: P.D("pool", out=d["vbf"][i * 2048:(i + 1) * 2048, :], in_=d["v"][i * 2048:(i + 1) * 2048, :], w=[pfx + f"vbf{i}"])))(i))
    return ops


def emit_b_casts(P, d, pfx=""):
    for f in b_cast_ops(P, d, pfx):
        f()


def emit_b(nc, P, T, d, cast_weights=True, pfx="", oT_blk=None, nT_dst=None):
    NB = T // 512
    NT = T // 128
    NB2 = T // 256
    fm = lambda ap: ap.rearrange("(m p) t -> p m t", p=128)

    if cast_weights:
        emit_b_casts(P, d)

    with contextlib.ExitStack() as st:
        sb = lambda n, s, dt=F32: st.enter_context(nc.sbuf_tensor(pfx + "p0_" + n, s, dt))
        ps = lambda n, s, dt=F32: st.enter_context(nc.psum_tensor(pfx + "p0_" + n, s, dt))
        wout = sb("wout", [128, 8, 1024], BF16)
        g = sb("g", [128, 8]); ones = sb("ones", [128, 128])
        hTt = [sb(f"hTt{i}", [128, 8, 512]) for i in range(2)]
        oTt = [sb(f"oTt{i}", [128, 8, 512], BF16) for i in range(2)]
        h2 = sb("h2", [128, 8, 512]); hnb = sb("hnb", [128, 8, 512], BF16)
        rstd = sb("rstd", [128, 512])
        wqt = [sb(f"wqt{i}", [128, 8, 512]) for i in range(2)]
        qs = [sb(f"qs{i}", [128, 4, 512]) for i in range(2)]
        pp = [ps(f"pp{i}", [128, 512]) for i in range(2)]
        ss = ps("ss", [128, 512])

        P.D("pool", out=wout[:], in_=d["wout"].rearrange("(k p) c -> p k c", p=128), w=["wout"])
        P.D("sp", out=g[:], in_=d["gffn"].rearrange("(m p) -> p m", p=128), w=["g"], allow_slow_non_contiguous=True)
        P.I("pool", "memset", w=["ones"], ap=ones[:], constant=1.0)
        nmm = 0
        nwq = 0
        if oT_blk is not None:
            P.dma("sp", lambda e: e.dma_start(out=d["oTq"].rearrange("r (c t) -> c r t", t=min(1024, T)), in_=oT_blk()), writes=["oTq"])
        def load_wq(idx):
            jg_ = idx % 4
            P.D("act", out=wqt[idx % 2][:], in_=d["wq"].rearrange("(m p) c -> p m c", p=128)[:, :, jg_ * 512:(jg_ + 1) * 512],
                w=[f"wqt{idx % 2}"])

        load_wq(0)
        for b in range(NB):
            tsl = slice(b * 512, (b + 1) * 512)
            ht, ot = hTt[b % 2], oTt[b % 2]
            hk, ok = f"hTt{b % 2}", f"oTt{b % 2}"
            P.D("sp", out=ht[:], in_=fm(d["hT"])[:, :, tsl], w=[hk])
            if oT_blk is None:
                P.D("pool", out=ot[:], in_=fm(d["oT"])[:, :, tsl], w=[ok])
            else:
                P.D("pool", out=ot[:], in_=fm(d["oTq"])[:, :, tsl], r=["oTq"], w=[ok])
            for m in range(8):
                p_ = pp[nmm % 2]; pk = f"pp{nmm % 2}"; nmm += 1
                for k in range(8):
                    P.MM(p_[:], wout[:, k, m * 128:(m + 1) * 128], ot[:, k, :], start=(k == 0), stop=(k == 7),
                         r=["wout", ok], w=[pk])
                P.I("dve", "tensor_tensor", r=[pk, hk], w=["h2"], out=h2[:, m, :], in0=p_[:], in1=ht[:, m, :], op=ALU.add)
            P.D("sp", out=fm(d["h2T"])[:, :, tsl], in_=h2[:], r=["h2"], w=[f"h2T{b}"])
            P.I("act", "activation", r=["h2"], w=[hk], out=ht[:], in_=h2[:], func=AF.Square)
            for m in range(8):
                P.MM(ss[:], ones[:], ht[:, m, :], start=(m == 0), stop=(m == 7), r=["ones", hk], w=["ss"])
            P.I("act", "activation", r=["ss"], w=["rstd"], out=rstd[:], in_=ss[:], func=AF.Sqrt, scale=1.0 / 1024, bias=EPS)
            P.I("dve", "reciprocal", r=["rstd"], w=["rstd"], out=rstd[:], in_=rstd[:])
            for m in range(8):
                P.I("dve", "scalar_tensor_tensor", r=["h2", "g", "rstd"], w=["h2"], out=h2[:, m, :], in0=h2[:, m, :],
                    scalar=g[:, m:m + 1], in1=rstd[:], op0=ALU.mult, op1=ALU.mult)
            P.I("pool", "tensor_copy", r=["h2"], w=["hnb"], out=hnb[:], in_=h2[:])
            P.D("sp", out=fm(d["hnbf"])[:, :, tsl], in_=hnb[:], r=["hnb"], w=[f"hnbf{b}"])
            for jg in range(4):
                wt = wqt[nwq % 2]; wk = f"wqt{nwq % 2}"; q_ = qs[nwq % 2]; qk = f"qs{nwq % 2}"; nwq += 1
                if nwq < NB * 4:
                    load_wq(nwq)
                for jj in range(4):
                    p_ = pp[nmm % 2]; pk = f"pp{nmm % 2}"; nmm += 1
                    for m in range(8):
                        P.MM(p_[:], wt[:, m, jj * 128:(jj + 1) * 128], h2[:, m, :], start=(m == 0), stop=(m == 7),
                             r=[wk, "h2"], w=[pk])
                    P.I("act", "copy", r=[pk], w=[qk], out=q_[:, jj, :], in_=p_[:])
                for t4 in range(4):
                    P.D("sp", out=d["qTd"][:, b * 4 + t4, jg * 4:(jg + 1) * 4, :], in_=q_[:, :, t4 * 128:(t4 + 1) * 128],
                        r=[qk], w=[f"qTd{b}_{jg}_{t4}"])
        P.barrier()
        P.emit()

    with contextlib.ExitStack() as st:
        sb = lambda n, s, dt=F32: st.enter_context(nc.sbuf_tensor(pfx + "p1_" + n, s, dt))
        ps = lambda n, s, dt=F32: st.enter_context(nc.psum_tensor(pfx + "p1_" + n, s, dt))
        keys = sb("keys", [128, 16, 128]); ident = sb("ident", [128, 128], BF16); identf = sb("identf", [128, 128])
        qt = [sb(f"qt{i}", [128, 16, 128]) for i in range(2)]
        S12 = [sb(f"S12{i}", [128, 16, 128]) for i in range(2)]
        scr_ = sb("scr", [128, 256]); TS = sb("TS", [128, 16, 16]); cand = sb("cand", [128, 8, 256]); BS = sb("BS", [128, 8, 16])
        ex = sb("ex", [128, 8, 16]); Z = sb("Z", [128, 8]); lnZ = sb("lnZ", [128, 8]); bias = sb("bias", [128, 8])
        SUM = [sb(f"SUM{i}", [128, 8, 128]) for i in range(6)]
        E = [sb(f"E{i}", [128, 1024], BF16) for i in range(3)]
        GH = [[sb(f"GH{i}_{h}", [128, 1024], BF16) for h in range(8)] for i in range(2)]
        GTp = [sb(f"GTp{i}", [128, 8, 128], BF16) for i in range(4)]
        sc = ps("sc", [128, 16, 128])
        acc = [ps(f"acc{i}", [128, 4, 128]) for i in range(2)]

        P.D("sp", out=keys[:], in_=d["keysT"].rearrange("j c k -> c j k"), w=["keys"])
        P.I("pool", "memset", w=["identf"], ap=identf[:], constant=1.0)
        P.I("pool", "affine_select", r=["identf"], w=["identf"], out=identf[:], in_=identf[:], pattern=[[-1, 128]],
            compare_op=ALU.is_equal, fill=0.0, base=0, channel_multiplier=1)
        P.I("pool", "tensor_copy", r=["identf"], w=["ident"], out=ident[:], in_=identf[:])
        nsum = 0; nacc = 0; ngtp = 0; ngh = 0
        for tt in range(NT):
            q_ = qt[tt % 2]; qk = f"qt{tt % 2}"; S = S12[tt % 2]; Sk = f"S12{tt % 2}"
            P.D("sp", out=q_[:], in_=d["qTd"][:, tt, :, :], w=[qk])
            for j in range(16):
                P.MM(sc[:, j, :], q_[:, j, :], keys[:, j, :], r=[qk, "keys"], w=["sc"])
            P.I("act", "copy", r=["sc"], w=[Sk + "a"], out=S[:, 0:8, :], in_=sc[:, 0:8, :])
            P.I("dve", "tensor_copy", r=["sc"], w=[Sk + "b"], out=S[:, 8:16, :], in_=sc[:, 8:16, :])
            Sr = [Sk + "a", Sk + "b"]
            for j in range(16):
                P.I("dve", "max", r=Sr, w=["TS"], out=TS[:, j, 0:8], in_=S[:, j, :])
                P.I("dve", "match_replace", r=Sr + ["TS"], w=["scr"], out=scr_[:, 0:128], in_to_replace=TS[:, j, 0:8],
                    in_values=S[:, j, :], imm_value=NEG)
                P.I("dve", "max", r=["scr"], w=["TS"], out=TS[:, j, 8:16], in_=scr_[:, 0:128])
            TS4 = TS[:].rearrange("p (h two) a -> p h two a", two=2)
            P.I("dve", "tensor_tensor", r=["TS"], w=["cand"], out=cand[:].rearrange("p h (a b) -> p h a b", b=16),
                in0=TS4[:, :, 0, :].unsqueeze(3).to_broadcast([128, 8, 16, 16]),
                in1=TS4[:, :, 1, :].unsqueeze(2).to_broadcast([128, 8, 16, 16]), op=ALU.add)
            for h in range(8):
                P.I("dve", "max", r=["cand"], w=["BS"], out=BS[:, h, 0:8], in_=cand[:, h, :])
                P.I("dve", "match_replace", r=["cand", "BS"], w=["scr"], out=scr_[:, 0:256], in_to_replace=BS[:, h, 0:8],
                    in_values=cand[:, h, :], imm_value=NEG)
                P.I("dve", "max", r=["scr"], w=["BS"], out=BS[:, h, 8:16], in_=scr_[:, 0:256])
            P.I("dve", "tensor_tensor", r=["BS"], w=["ex"], out=ex[:], in0=BS[:], in1=BS[:, :, 0:1].to_broadcast([128, 8, 16]),
                op=ALU.subtract)
            P.I("act", "activation", r=["ex"], w=["ex"], out=ex[:], in_=ex[:], func=AF.Exp)
            P.I("dve", "reduce_sum", r=["ex"], w=["Z"], out=Z[:], in_=ex[:], axis=AX.X)
            P.I("act", "activation", r=["Z"], w=["lnZ"], out=lnZ[:], in_=Z[:], func=AF.Ln)
            P.I("dve", "scalar_tensor_tensor", r=["BS", "lnZ"], w=["bias"], out=bias[:], in0=BS[:, :, 0], scalar=-1.0,
                in1=lnZ[:], op0=ALU.mult, op1=ALU.subtract)
            def add_op(it):
                sx_, h_ = it // 8, it % 8
                su = SUM[it % 6]; sk = f"SUM{it % 6}"
                if False:
                    for a in range(8):
                        P.I("act", "activation", r=Sr, w=[sk], out=su[:, a, :], in_=S[:, 2 * h_ + 1, :], func=AF.Identity,
                            bias=S[:, 2 * h_, sx_ * 8 + a:sx_ * 8 + a + 1], scale=1.0)
                else:
                    P.I("dve", "tensor_tensor", r=Sr, w=[sk], out=su[:],
                        in0=S[:, 2 * h_, sx_ * 8:(sx_ + 1) * 8].unsqueeze(2).to_broadcast([128, 8, 128]),
                        in1=S[:, 2 * h_ + 1, :].unsqueeze(1).to_broadcast([128, 8, 128]), op=ALU.add)

            LOOK = 4
            deferred = []
            for it0 in range(LOOK):
                add_op(it0)
            for sx in range(16):
                ghs = GH[ngh % 2]; gk = f"GH{ngh % 2}_"; ngh += 1
                for h in range(8):
                    it = sx * 8 + h
                    if it + LOOK < 128:
                        add_op(it + LOOK)
                    if h == 4 and deferred:
                        deferred.pop(0)()
                    su = SUM[it % 6]; sk = f"SUM{it % 6}"; e_ = E[it % 3]; ek = f"E{it % 3}"
                    P.I("act", "activation", r=[sk, "bias"], w=[ek], out=e_[:], in_=su[:].rearrange("p a b -> p (a b)"),
                        func=AF.Exp, bias=bias[:, h:h + 1])
                    P.I("dve", "scalar_tensor_tensor", r=[sk, ek, "BS"], w=[gk + str(h)], out=ghs[h][:],
                        in0=su[:].rearrange("p a b -> p (a b)"), scalar=BS[:, h, 15:16], in1=e_[:], op0=ALU.is_ge, op1=ALU.mult)
                def flush(sx=sx, tt=tt, ghs=ghs, gk=gk):
                    nonlocal nacc, ngtp
                    gp = GTp[ngtp % 4]; gpk = f"GTp{ngtp % 4}"; ngtp += 1
                    for c4 in range(2):
                        a_ = acc[nacc % 2]; ak = f"acc{nacc % 2}"; nacc += 1
                        for ci in range(4):
                            c = c4 * 4 + ci
                            for h in range(8):
                                P.MM(a_[:, ci, :], ghs[h][:, c * 128:(c + 1) * 128], ident[:], start=(h == 0), stop=(h == 7),
                                     r=[gk + str(h), "ident"], w=[ak])
                        P.I("act", "copy", r=[ak], w=[gpk], out=gp[:, c4 * 4:(c4 + 1) * 4, :], in_=a_[:])
                    P.D("sp", out=d["GT"][tt, :, sx * 1024:(sx + 1) * 1024], in_=gp[:].rearrange("p c t -> p (c t)"), r=[gpk],
                        w=[f"GT{tt}_{sx}"])
                deferred.append(flush)
            while deferred:
                deferred.pop(0)()
        P.barrier()
        P.emit()

    NB5 = T // 512
    with contextlib.ExitStack() as st:
        sb = lambda n, s, dt=F32: st.enter_context(nc.sbuf_tensor(pfx + "p2_" + n, s, dt))
        ps = lambda n, s, dt=F32: st.enter_context(nc.psum_tensor(pfx + "p2_" + n, s, dt))
        U = [sb(f"U{i}", [128, 8, 1024], BF16) for i in range(2)]
        V = [sb(f"V{i}", [128, 8, 1024], BF16) for i in range(2)]
        Gg = [sb(f"Gg{i}", [128, 4, 8, 128], BF16) for i in range(2)]
        hn2 = [sb(f"hn2{i}", [128, 8, 512], BF16) for i in range(2)]
        ge = [sb(f"ge{i}", [128, 512], BF16) for i in range(2)]
        gh = [sb(f"gh{i}", [128, 8, 512], BF16) for i in range(2)]
        Yacc = sb("Yacc", [128, 8, 512]); h2b = sb("h2b", [128, 8, 512]); sq2 = sb("sq2", [128, 8, 512])
        rstd2 = sb("rstd2", [128, 512]); nrm = sb("nrm", [128, 8, 512], d["nT_out"].dtype)
        gn = sb("gn", [128, 8]); ones2 = sb("ones2", [128, 128])
        Hp = [ps(f"Hp{i}", [128, 512]) for i in range(2)]
        Yp = ps("Yp", [128, 4, 512])
        ss2 = ps("ss2", [128, 512])
        P.D("sp", out=gn[:], in_=d["gnext"].rearrange("(m p) -> p m", p=128), w=["gn"], allow_slow_non_contiguous=True)
        P.I("pool", "memset", w=["ones2"], ap=ones2[:], constant=1.0)
        nw = 0; nh = 0
        for blk in range(NB5):
            tsl = slice(blk * 512, (blk + 1) * 512)
            hb = hn2[blk % 2]; hbk = f"hn2{blk % 2}"
            P.D("sp", out=hb[:], in_=fm(d["hnbf"])[:, :, tsl], w=[hbk])
            P.D("sp", out=h2b[:], in_=fm(d["h2T"])[:, :, tsl], w=["h2b"])
            for eg in range(16):
                u_ = U[nw % 2]; uk = f"U{nw % 2}"; v_ = V[nw % 2]; vk = f"V{nw % 2}"; g_ = Gg[nw % 2]; gk = f"Gg{nw % 2}"
                gh_ = gh[nw % 2]; ghk = f"gh{nw % 2}"; nw += 1
                P.D("sp", out=u_[:], in_=d["uTbf"].rearrange("(m p) e -> p m e", p=128)[:, :, eg * 1024:(eg + 1) * 1024], w=[uk])
                P.D("act", out=v_[:], in_=d["vbf"].rearrange("(c p) x -> p c x", p=128)[:, eg * 8:(eg + 1) * 8, :], w=[vk])
                for t4 in range(4):
                    P.D("sp", out=g_[:, t4, :, :], in_=d["GT"][blk * 4 + t4, :, eg * 1024:(eg + 1) * 1024].rearrange("p (c t) -> p c t", t=128),
                        w=[gk + str(t4)])
                gks = [gk + str(t4) for t4 in range(4)]
                for c in range(8):
                    hp = Hp[nh % 2]; hpk = f"Hp{nh % 2}"; ge_ = ge[nh % 2]; gek = f"ge{nh % 2}"; nh += 1
                    for m in range(8):
                        P.MM(hp[:], u_[:, m, c * 128:(c + 1) * 128], hb[:, m, :], start=(m == 0), stop=(m == 7), r=[uk, hbk], w=[hpk])
                    P.I("act", "activation", r=[hpk], w=[gek], out=ge_[:], in_=hp[:], func=AF.Gelu_apprx_tanh)
                    P.I("dve", "tensor_tensor", r=[gek] + gks, w=[ghk + str(c)], out=gh_[:, c, :].rearrange("p (tt t) -> p tt t", t=128),
                        in0=ge_[:].rearrange("p (tt t) -> p tt t", t=128), in1=g_[:, :, c, :], op=ALU.mult)
                ghs = [ghk + str(c) for c in range(8)]
                for half in range(2):
                    for mi in range(4):
                        m = half * 4 + mi
                        for c in range(8):
                            P.MM(Yp[:, mi, :], v_[:, c, m * 128:(m + 1) * 128], gh_[:, c, :], start=(c == 0), stop=(c == 7), r=[vk] + ghs, w=["Yp"])
                    if eg == 0:
                        P.I("dve", "tensor_copy", r=["Yp"], w=[f"Yacc{half}"], out=Yacc[:, half * 4:(half + 1) * 4, :], in_=Yp[:])
                    else:
                        P.I("dve", "tensor_tensor", r=["Yp", f"Yacc{half}"], w=[f"Yacc{half}"], out=Yacc[:, half * 4:(half + 1) * 4, :],
                            in0=Yp[:], in1=Yacc[:, half * 4:(half + 1) * 4, :], op=ALU.add)
            P.I("dve", "tensor_tensor", r=["Yacc0", "Yacc1", "h2b"], w=["h2b"], out=h2b[:], in0=Yacc[:], in1=h2b[:], op=ALU.add)
            P.D("sp", out=fm(d["hT_out"])[:, :, tsl], in_=h2b[:], r=["h2b"], w=[f"hT_out{blk}"])
            P.I("act", "activation", r=["h2b"], w=["sq2"], out=sq2[:], in_=h2b[:], func=AF.Square)
            for m in range(8):
                P.MM(ss2[:], ones2[:], sq2[:, m, :], start=(m == 0), stop=(m == 7), r=["ones2", "sq2"], w=["ss2"])
            P.I("act", "activation", r=["ss2"], w=["rstd2"], out=rstd2[:], in_=ss2[:], func=AF.Sqrt, scale=1.0 / 1024, bias=EPS)
            P.I("dve", "reciprocal", r=["rstd2"], w=["rstd2"], out=rstd2[:], in_=rstd2[:])
            for m in range(8):
                P.I("dve", "scalar_tensor_tensor", r=["h2b", "gn", "rstd2"], w=["nrm"], out=nrm[:, m, :], in0=h2b[:, m, :],
                    scalar=gn[:, m:m + 1], in1=rstd2[:], op0=ALU.mult, op1=ALU.mult)
            if nT_dst is None:
                P.D("sp", out=fm(d["nT_out"])[:, :, tsl], in_=nrm[:], r=["nrm"], w=[f"nT_out{blk}"])
            else:
                for hf in range(2):
                    P.D("sp", out=nT_dst(2 * blk + hf), in_=nrm[:, :, hf * 256:(hf + 1) * 256], r=["nrm"], w=[f"nT_out{blk}_{hf}"])
        outs = [f"hT_out{b}" for b in range(NB5)]
        outs += [f"nT_out{b}" for b in range(NB5)] if nT_dst is None else [f"nT_out{b}_{hf}" for b in range(NB5) for hf in range(2)]
        P.wait_all("sp", outs)
        P.barrier()
        P.emit()


def declare_c(nc, S, pfx="", fused=False):
    d = {}

    def t(name, shape, kind, dt=F32):
        d[name] = nc.dram_tensor(pfx + name, shape, dt, kind=kind).ap()

    if not fused:
        t("nT", [1024, S], "ExternalInput")
    t("wq", [1024, 256], "ExternalInput"); t("wk", [1024, 256], "ExternalInput"); t("wv", [1024, 256], "ExternalInput")
    t("wf", [1024, 4], "ExternalInput"); t("fb", [4], "ExternalInput")
    t("maskd", [128, 4, 512], "ExternalInput")
    t("oT", [256, S], "Internal" if fused else "ExternalOutput")
    return d


def emit_c(nc, P, S, d, nT_loads=None, oT_dst=None):
    if oT_dst is None:
        oT_dst = lambda t0, n: d["oT"][:, t0:t0 + n]
    NBLK = S // 512
    NCH = S // 128
    with contextlib.ExitStack() as st:
        sb = lambda n, s, dt=F32: st.enter_context(nc.sbuf_tensor("c_" + n, s, dt))
        ps = lambda n, s, dt=F32: st.enter_context(nc.psum_tensor("c_" + n, s, dt))
        KT = sb("KT", [128, 2, S], BF16)
        Vr = sb("Vr", [128, NCH, 4 * 65 + 63], BF16)
        Cr = sb("Cr", [128, NCH, 4]); nbias = sb("nbias", [128, 4, NCH])
        nb = [sb(f"nb{i}", [128, 8, 512], BF16) for i in range(2)]
        wq = sb("wq", [128, 8, 256], BF16); wk = sb("wk", [128, 8, 256], BF16); wv = sb("wv", [128, 8, 256], BF16)
        wf = sb("wf", [128, 8, 4], BF16); fb = sb("fb", [128, 4])
        QT = [sb(f"QT{i}", [128, 512], BF16) for i in range(4)]
        PT = [sb(f"PT{i}", [128, 512], BF16) for i in range(4)]
        maskd = sb("maskd", [128, 4, 512], BF16)
        tri = sb("tri", [128, 128]); ones = sb("ones", [128, 128])
        lf = sb("lf", [128, 4]); tot = sb("tot", [128, 4]); totmid = sb("totmid", [128, 4])
        zrow = sb("zrow", [65, 512]); ocp = sb("ocp", [64, 512]); osb = [sb(f"osb{i}", [64, 512]) for i in range(2)]
        pp = [ps(f"pp{i}", [128, 512]) for i in range(2)]
        ST = [ps(f"ST{i}", [128, 512]) for i in range(3)]
        OT = [ps(f"OT{i}", [128, 512]) for i in range(2)]
        MISC = ps("MISC", [128, 512]); ZB = MISC[0:64, :]; cs = MISC[:, 0:8]

        for nm, tl in (("wq", wq), ("wk", wk), ("wv", wv)):
            P.D("pool", out=tl[:], in_=d[nm].rearrange("(m p) c -> p m c", p=128), w=[nm])
        P.D("pool", out=wf[:], in_=d["wf"].rearrange("(m p) c -> p m c", p=128), w=["wf"])
        P.D("sp", out=fb[:], in_=d["fb"].partition_broadcast(128), w=["fb"])
        P.D("pool", out=maskd[:], in_=d["maskd"], w=["maskd"])
        P.I("pool", "memset", w=["ones"], ap=ones[:], constant=1.0)
        P.I("pool", "memset", w=["tri"], ap=tri[:], constant=1.0)
        P.I("pool", "affine_select", r=["tri"], w=["tri"], out=tri[:], in_=tri[:], pattern=[[1, 128]],
            compare_op=ALU.is_ge, fill=0.0, base=0, channel_multiplier=-1)
        P.I("pool", "memset", w=["tot"], ap=tot[:], constant=0.0)
        P.I("pool", "memset", w=["Vr"], ap=Vr[:], constant=0.0)
        P.I("pool", "memset", r=["Vr"], w=["Vr"], ap=Vr[:, :, 0:260].rearrange("p c (h x) -> p c h x", x=65)[:, :, :, 64:65], constant=1.0)
        for i in range(4):
            P.I("pool", "memset", w=[f"QT{i}"], ap=QT[i][:], constant=0.0)
        npp = 0; nst = 0; npt = 0; nhead = 0
        for blk in range(NBLK):
            tsl = slice(blk * 512, (blk + 1) * 512)
            n_ = nb[blk % 2]; nk = f"nb{blk % 2}"
            if nT_loads is None:
                P.D("pool", out=n_[:], in_=d["nT"].rearrange("(m p) t -> p m t", p=128)[:, :, tsl], w=[nk])
            else:
                for csl, src in nT_loads(blk):
                    P.D("pool", out=n_[:, :, csl], in_=src, w=[nk])
            for pair in range(2):
                p_ = pp[npp % 2]; pk = f"pp{npp % 2}"; npp += 1
                for m in range(8):
                    P.MM(p_[:], wq[:, m, pair * 128:(pair + 1) * 128], n_[:, m, :], start=(m == 0), stop=(m == 7), r=["wq", nk], w=[pk])
                for hh in range(2):
                    rs = slice(hh * 64, hh * 64 + 64)
                    P.I("act", "mul", r=[pk], w=[f"QT{2 * pair + hh}"], out=QT[2 * pair + hh][rs, :], in_=p_[rs, :], mul=0.125)
                p_ = pp[npp % 2]; pk = f"pp{npp % 2}"; npp += 1
                for m in range(8):
                    P.MM(p_[:], wk[:, m, pair * 128:(pair + 1) * 128], n_[:, m, :], start=(m == 0), stop=(m == 7), r=["wk", nk], w=[pk])
                P.I("dve", "tensor_copy", r=[pk], w=["KT"], out=KT[:, pair, tsl], in_=p_[:])
            for t4 in range(4):
                ch = blk * 4 + t4
                p_ = pp[npp % 2]; pk = f"pp{npp % 2}"; npp += 1
                for m in range(8):
                    P.MM(p_[:, 0:256], n_[:, m, t4 * 128:(t4 + 1) * 128], wv[:, m, :], start=(m == 0), stop=(m == 7), r=["wv", nk], w=[pk])
                P.I("act", "copy", r=[pk], w=["Vr"], out=Vr[:, ch, 0:260].rearrange("p (h x) -> p h x", x=65)[:, :, 0:64], in_=p_[:, 0:256].rearrange("p (h x) -> p h x", x=64))
                for m in range(8):
                    P.MM(cs[:, 0:4], n_[:, m, t4 * 128:(t4 + 1) * 128], wf[:, m, :], start=(m == 0), stop=(m == 7), r=["wf", nk], w=["MISC"])
                P.I("dve", "tensor_tensor", r=["MISC", "fb"], w=["lf"], out=lf[:], in0=cs[:, 0:4], in1=fb[:], op=ALU.add)
                P.I("act", "activation", r=["lf"], w=["lf"], out=lf[:], in_=lf[:], func=AF.Exp, scale=-1.0)
                P.I("act", "activation", r=["lf"], w=["lf"], out=lf[:], in_=lf[:], func=AF.Ln, bias=1.0)
                P.I("dve", "tensor_scalar", r=["lf"], w=["lf"], out=lf[:], in0=lf[:], scalar1=-1.0, scalar2=None, op0=ALU.mult)
                P.MM(cs[:, 0:4], tri[:], lf[:], r=["tri", "lf"], w=["MISC"])
                P.MM(cs[:, 4:8], ones[:], lf[:], r=["ones", "lf"], w=["MISC"])
                P.I("dve", "tensor_tensor", r=["MISC", "tot"], w=["Cr"], out=Cr[:, ch, :], in0=cs[:, 0:4], in1=tot[:], op=ALU.add)
                P.I("dve", "tensor_tensor", r=["MISC", "tot"], w=["tot"], out=tot[:], in0=cs[:, 4:8], in1=tot[:], op=ALU.add)
                if t4 == 1:
                    P.I("dve", "tensor_copy", r=["tot"], w=["totmid"], out=totmid[:], in_=tot[:])
            nch = blk * 4 + 4
            for hl in range(4):
                P.I("dve", "tensor_scalar", r=["Cr", "totmid"], w=["nbias"], out=nbias[:, hl, 0:nch], in0=Cr[:, 0:nch, hl],
                    scalar1=totmid[:, hl:hl + 1], scalar2=-1.0, op0=ALU.subtract, op1=ALU.mult)
            pairs = [(hl, kc) for hl in range(4) for kc in range(nch)]

            def qk(i):
                nonlocal nst
                hl, kc = pairs[i]
                s_ = ST[i % 3]
                P.MM(s_[:], KT[:, hl // 2, kc * 128:(kc + 1) * 128], QT[hl][:], r=["KT", f"QT{hl}"], w=[f"ST{i % 3}"])

            qk(0)
            if len(pairs) > 1:
                qk(1)
            for i, (hl, kc) in enumerate(pairs):
                if i + 2 < len(pairs):
                    qk(i + 2)
                s_ = ST[i % 3]; sk = f"ST{i % 3}"
                pt = PT[npt % 4]; ptk = f"PT{npt % 4}"; npt += 1
                P.I("act", "activation", r=[sk, "nbias"], w=[ptk], out=pt[:], in_=s_[:], func=AF.Exp, bias=nbias[:, hl, kc:kc + 1])
                if kc >= blk * 4:
                    P.I("dve", "tensor_tensor", r=[ptk, "maskd"], w=[ptk], out=pt[:], in0=pt[:], in1=maskd[:, kc - blk * 4, :], op=ALU.mult)
                if kc == 0:
                    ot = OT[nhead % 2]; otk = f"OT{nhead % 2}"; ob = osb[nhead % 2]; obk = f"osb{nhead % 2}"; nhead += 1
                P.MM(ot[:], Vr[:, kc, hl * 65:hl * 65 + 128], pt[:], start=(kc == 0), stop=(kc == nch - 1), r=["Vr", ptk], w=[otk])
                if kc == nch - 1:
                    P.I("dve", "tensor_scalar", r=[otk], w=["zrow"], out=zrow[64:65, :], in0=ot[64:65, :], scalar1=1e-30, scalar2=None, op0=ALU.max)
                    P.I("dve", "reciprocal", r=["zrow"], w=["zrow"], out=zrow[64:65, :], in_=zrow[64:65, :])
                    P.MM(ZB, ones[64:65, 0:64], zrow[64:65, :], r=["ones", "zrow"], w=["MISC"])
                    P.I("act", "copy", r=[otk], w=["ocp"], out=ocp[:], in_=ot[0:64, :])
                    P.I("dve", "tensor_tensor", r=["ocp", "MISC"], w=[obk], out=ob[:], in0=ocp[:], in1=ZB, op=ALU.mult)
                    P.D("sp", out=oT_dst(blk * 512, 512)[hl * 64:(hl + 1) * 64, :], in_=ob[:], r=[obk], w=[f"oT{blk}_{hl}"])
        P.wait_all("sp", [f"oT{b}_{h}" for b in range(NBLK) for h in range(4)])
        P.barrier()
        P.emit()


S_FULL = 16384
T_CORE = 4096
G4 = [[0, 1, 2, 3], [4, 5, 6, 7]]
_PROG = {}
_B_IN = (("wout", [1024, 1024]), ("gffn", [1024]), ("wq", [1024, 2048]), ("keysT", [16, 128, 128]), ("uT", [1024, 16384]),
         ("v", [16384, 1024]), ("gnext", [1024]))


def _declare_b_fused(nc, T, pfx, hT_ap, out_kind):
    d = {}
    for name, shape in _B_IN:
        d[name] = nc.dram_tensor(pfx + name, shape, F32, kind="ExternalInput").ap()
    d["hT"] = hT_ap
    d["hT_out"] = nc.dram_tensor(pfx + "hT_out", [1024, T], F32, kind="Internal").ap()
    d["nT_out"] = nc.dram_tensor(pfx + "nT_out", [1024, T], F32, kind=out_kind).ap()
    scr = lambda name, shape, dt=F32: nc.dram_tensor(pfx + name, shape, dt, kind="Internal").ap()
    d["h2T"] = scr("h2T", [1024, T]); d["hnbf"] = scr("hnbf", [1024, T], BF16); d["qTd"] = scr("qTd", [128, T // 128, 16, 128])
    d["oTq"] = scr("oTq", [1024, T])
    d["GT"] = scr("GT", [T // 128, 128, 16384], BF16); d["uTbf"] = scr("uTbf", [1024, 16384], BF16); d["vbf"] = scr("vbf", [16384, 1024], BF16)
    return d


def _build_fused(S=S_FULL, T=T_CORE):
    if (S, T) in _PROG:
        return _PROG[(S, T)]
    nc = bass.Bass("TRN2", target_bir_lowering=False)
    with contextlib.ExitStack() as st:
        P = Prog(nc)
        P.alloc_sems(st)
        da = declare_a(nc, S, pfx="A_", fused=True)
        xTs = nc.dram_tensor("B0_xTs", [1024, T], F32, kind="ExternalInput").ap()
        db0 = _declare_b_fused(nc, T, "B0_", xTs, "Internal")
        dc = declare_c(nc, S, pfx="C_", fused=True)
        db1 = _declare_b_fused(nc, T, "B1_", db0["hT_out"], "ExternalOutput")
        CW = min(1024, T)
        NC1 = S // CW
        CW2 = 256
        NC2 = T // CW2
        dt_ = lambda name, shape: nc.dram_tensor(name, shape, F32, kind="Internal").ap()
        x1_in = dt_("x1_in", [NC1, 256, CW]); x1_out = dt_("x1_out", [NC1, 1024, CW])
        x2_in = dt_("x2_in", [NC2, 1024, CW2]); x2_out = dt_("x2_out", [NC2, 4096, CW2])
        x3_in = dt_("x3_in", [NC1, 256, CW]); x3_out = dt_("x3_out", [NC1, 1024, CW])

        def gather(src, dst, n, name):
            for j in range(n):
                P.cc((lambda j: (lambda e: e.collective_compute("AllGather", ALU.bypass, replica_groups=G4, ins=[src[j]], outs=[dst[j]])))(j),
                     writes=[name])
            P.barrier()

        def chunked_dst(buf):
            return lambda t0, n: buf[t0 // CW, :, (t0 % CW):(t0 % CW) + n]

        def quarter_src(buf):
            def f():
                q = nc.partition_id() % 4
                return buf[bass.ds(q * (T // CW), T // CW), :, :]
            return f

        bpr = T // 512

        def nT_loads(blk):
            rank, lb = blk // bpr, blk % bpr
            return [(slice(h * CW2, (h + 1) * CW2),
                     x2_out[lb * 2 + h, rank * 1024:(rank + 1) * 1024, :].rearrange("(m p) t -> p m t", p=128)) for h in range(2)]

        casts = b_cast_ops(P, db0, "B0_") + b_cast_ops(P, db1, "B1_")
        nblk_a = S // 512

        def cast_hook(blk):
            per = -(-len(casts) // nblk_a)
            for f in casts[blk * per:(blk + 1) * per]:
                f()

        emit_a(nc, P, S, da, oT_dst=chunked_dst(x1_in), block_hook=cast_hook)
        gather(x1_in, x1_out, NC1, "x1")
        emit_b(nc, P, T, db0, cast_weights=False, pfx="B0_", oT_blk=quarter_src(x1_out),
               nT_dst=lambda blk: x2_in[blk].rearrange("(m p) t -> p m t", p=128))
        gather(x2_in, x2_out, NC2, "x2")
        emit_c(nc, P, S, dc, nT_loads=nT_loads, oT_dst=chunked_dst(x3_in))
        gather(x3_in, x3_out, NC1, "x3")
        emit_b(nc, P, T, db1, cast_weights=False, pfx="B1_", oT_blk=quarter_src(x3_out))
    _PROG[(S, T)] = nc
    return nc


def _peer_inputs(pfx, wout, gffn, wq, keys, u, v, gnext):
    f32 = lambda a: np.ascontiguousarray(np.asarray(a, dtype=np.float32))
    return {pfx + "wout": f32(wout), pfx + "gffn": f32(gffn), pfx + "wq": f32(wq),
            pfx + "keysT": np.ascontiguousarray(f32(keys).transpose(0, 1, 3, 2).reshape(16, 128, 128)),
            pfx + "uT": np.ascontiguousarray(f32(u).T), pfx + "v": f32(v), pfx + "gnext": f32(gnext)}


def kernel(x, l0_attn_norm, l0_w_in, l0_cmp_pe_k, l0_cmp_w1_k, l0_cmp_w2_k, l0_cmp_pe_v, l0_cmp_w1_v, l0_cmp_w2_v, l0_w_out,
           l0_ffn_norm, l0_peer_wq, l0_peer_keys, l0_peer_u, l0_peer_v,
           l1_attn_norm, l1_w_in, l1_f_bias, l1_w_out,
           l1_ffn_norm, l1_peer_wq, l1_peer_keys, l1_peer_u, l1_peer_v,
           final_norm):
    f32 = lambda a: np.ascontiguousarray(np.asarray(a, dtype=np.float32))
    x = f32(x)
    B, S, D = x.shape
    T_CORE = S // 4
    nc = _build_fused(S, T_CORE)
    consts = consts_a(); ropeq, ropek = rope_tables(S)
    args0 = [f32(a) for a in (l0_attn_norm, l0_w_in, l0_cmp_pe_k, l0_cmp_w1_k, l0_cmp_w2_k, l0_cmp_pe_v, l0_cmp_w1_v, l0_cmp_w2_v)]
    pb0 = _peer_inputs("B0_", l0_w_out, l0_ffn_norm, l0_peer_wq, l0_peer_keys, l0_peer_u, l0_peer_v, l1_attn_norm)
    pb1 = _peer_inputs("B1_", l1_w_out, l1_ffn_norm, l1_peer_wq, l1_peer_keys, l1_peer_u, l1_peer_v, final_norm)
    w1 = f32(l1_w_in); fbias = f32(l1_f_bias)
    kk = np.arange(128)[:, None, None]; ii = np.arange(4)[None, :, None]; qq = np.arange(512)[None, None, :]
    maskd = (kk <= qq - 128 * ii).astype(np.float32)
    xT = [np.ascontiguousarray(x[b].T) for b in range(B)]
    maps = []
    for c in range(8):
        b, q = c // 4, c % 4
        m = {"A_" + k: v for k, v in host_inputs_a(x[b], *args0, q, consts, ropeq, ropek).items()}
        m["A_xT"] = xT[b]
        m["B0_xTs"] = np.ascontiguousarray(xT[b][:, q * T_CORE:(q + 1) * T_CORE])
        m.update(pb0); m.update(pb1)
        h0 = 4 * q
        m.update({"C_wq": np.ascontiguousarray(w1[:, h0 * 64:(h0 + 4) * 64]),
                  "C_wk": np.ascontiguousarray(w1[:, 1024 + h0 * 64:1024 + (h0 + 4) * 64]),
                  "C_wv": np.ascontiguousarray(w1[:, 2048 + h0 * 64:2048 + (h0 + 4) * 64]),
                  "C_wf": np.ascontiguousarray(w1[:, 3072 + h0:3072 + h0 + 4]), "C_fb": fbias[h0:h0 + 4].copy(), "C_maskd": maskd})
        maps.append(m)
    res = run_bass_kernel_spmd(nc, maps, core_ids=list(range(8))).results
    out = np.empty((B, S, D), np.float32)
    for c in range(8):
        b, q = c // 4, c % 4
        out[b, q * T_CORE:(q + 1) * T_CORE, :] = res[c]["B1_nT_out"].T
    return out
```

```python
import contextlib
import numpy as np
import concourse.bass as bass
import concourse.mybir as mybir
from concourse.bass_utils import run_bass_kernel_spmd

F32 = mybir.dt.float32
BF16 = mybir.dt.bfloat16
AF = mybir.ActivationFunctionType
ALU = mybir.AluOpType
AX = mybir.AxisListType

N_DMA_SEMS = 8


class Prog:
    COMPUTE = ("pe", "dve", "act", "pool")
    QUEUES = ("sp", "act", "pool")

    def __init__(self, nc):
        self.nc = nc
        self.ops = {e: [] for e in ("pe", "dve", "act", "pool", "sp")}
        self.cnt = {}
        self.last_w = {}
        self.readers = {}
        self.waited = {e: {} for e in self.ops}
        self.dma_n = {q: 0 for q in self.QUEUES}
        self.semkeys = []
        for e in self.COMPUTE:
            self._mk(("c", e))
        for q in self.QUEUES:
            for i in range(N_DMA_SEMS):
                self._mk(("d", q, i))
        self._mk(("cc",))
        self.sems = {}
        self.pending = {e: False for e in self.ops}
        self.lazy_pe_inc = False

    def _mk(self, k):
        self.cnt[k] = 0
        self.semkeys.append(k)

    def _need(self, eng, tok, waits):
        if tok is None:
            return
        k, v = tok
        if k == ("c", eng) and eng == "pe":
            return
        if self.waited[eng].get(k, 0) >= v:
            return
        self.waited[eng][k] = v
        waits.append((k, v))

    def _deps(self, eng, reads, writes):
        waits = []
        for b in reads:
            self._need(eng, self.last_w.get(b), waits)
        for b in writes:
            self._need(eng, self.last_w.get(b), waits)
            for t in self.readers.get(b, {}).items():
                if t[0] == ("c", eng):
                    continue
                self._need(eng, t, waits)
        return waits

    def _commit(self, tok, reads, writes):
        for b in reads:
            self.readers.setdefault(b, {})[tok[0]] = tok[1]
        for b in writes:
            self.last_w[b] = tok
            self.readers[b] = {}

    def op(self, eng, fn, reads=(), writes=(), inc=True):
        waits = self._deps(eng, reads, writes)
        k = ("c", eng)
        if inc:
            self.cnt[k] += 1
            tok = (k, self.cnt[k])
            self.pending[eng] = False
        else:
            tok = (k, self.cnt[k] + 1)
            self.pending[eng] = True
        self._commit(tok, reads, writes)
        self.ops[eng].append((waits, fn, (k, 1) if inc else None))
        return tok

    def dma(self, q, fn, reads=(), writes=()):
        waits = self._deps(q, reads, writes)
        n = self.dma_n[q]
        self.dma_n[q] += 1
        k = ("d", q, n % N_DMA_SEMS)
        self._need(q, (k, self.cnt[k]) if self.cnt[k] else None, waits)
        self.cnt[k] += 16
        tok = (k, self.cnt[k])
        self._commit(tok, reads, writes)
        self.ops[q].append((waits, fn, (k, 16)))
        return tok

    def cc(self, fn, reads=(), writes=()):
        waits = self._deps("pool", reads, writes)
        k = ("cc",)
        self._need("pool", (k, self.cnt[k]) if self.cnt[k] else None, waits)
        self.cnt[k] += 1
        tok = (k, self.cnt[k])
        self._commit(tok, reads, writes)
        self.ops["pool"].append((waits, fn, (k, 1)))
        return tok

    def I(self, eng, method, r=(), w=(), **kw):
        return self.op(eng, lambda e: getattr(e, method)(**kw), reads=r, writes=w)

    def MM(self, out, lhsT, rhs, start=True, stop=True, r=(), w=()):
        return self.op("pe", lambda e: e.matmul(out, lhsT=lhsT, rhs=rhs, start=start, stop=stop), reads=r, writes=w,
                       inc=(stop or not self.lazy_pe_inc))

    def D(self, q, out, in_, r=(), w=(), **kw):
        return self.dma(q, lambda e: e.dma_start(out=out, in_=in_, **kw), reads=r, writes=w)

    def barrier(self):
        assert not any(self.pending.values()), self.pending
        for eng in self.ops:
            waits = []
            for k in self.semkeys:
                if self.cnt[k]:
                    self._need(eng, (k, self.cnt[k]), waits)
            self.ops[eng].append((waits, None, None))
        self.last_w = {}
        self.readers = {}

    def wait_all(self, eng, bufs):
        waits = []
        for b in bufs:
            self._need(eng, self.last_w.get(b), waits)
        self.ops[eng].append((waits, None, None))

    def alloc_sems(self, st):
        for k in self.semkeys:
            self.sems[k] = st.enter_context(self.nc.semaphore("s_" + "_".join(map(str, k))))

    def emit(self):
        nc = self.nc
        import contextlib
        with contextlib.ExitStack() as st:
            if not self.sems:
                self.alloc_sems(st)
            block = st.enter_context(nc.Block())
            engobj = {"pe": "tensor", "dve": "vector", "act": "scalar", "pool": "gpsimd", "sp": "sync"}

            def mk(ename):
                ops = self.ops[ename]

                def body(eng):
                    for waits, fn, inc in ops:
                        for (k, v) in waits:
                            eng.wait_ge(self.sems[k], v)
                        if fn is not None:
                            ins = fn(eng)
                            if inc is not None:
                                ins.then_inc(self.sems[inc[0]], inc[1])
                return body

            for ename, attr in engobj.items():
                if self.ops[ename]:
                    getattr(block, attr)(mk(ename))
        self.ops = {e: [] for e in self.ops}


EPS = 1e-6


def consts_a():
    c = {}
    q = np.arange(128)[:, None]; rel = np.arange(512)[None, :] - 256
    cur = (q >= 64).astype(np.int64)
    M = (rel <= cur - 2).astype(np.float32)
    A = np.where(rel == cur, 10000.0, np.where(rel == cur - 1, 10001.0, np.where(rel > cur, -1.0, 0.0))).astype(np.float32)
    c["pats"] = np.stack([M, A], 1)
    ci = np.arange(128)[:, None]; qi = np.arange(128)[None, :]
    D = (16 * ci - qi).astype(np.float32)
    Dz = D.copy(); Dz[0, :] = 1e9
    c["D16"] = np.stack([D, Dz], 1)
    c["tril"] = np.stack([(ci <= qi), (ci > qi)], 1).astype(np.float32)
    c["Sel"] = np.broadcast_to(np.eye(12, dtype=np.float32)[:, :, None], (12, 12, 64)).copy()
    cc = np.arange(1024)[:, None] - 1; jj = np.arange(256)[None, :]
    lo = np.maximum(cc * 16, jj * 64); hi = np.minimum(cc * 16 + 32, (jj + 1) * 64)
    m = np.maximum(hi - lo, 0).astype(np.float32) / 32.0
    m[0, :] = 0.0
    c["slcm"] = np.ascontiguousarray(m.reshape(8, 128, 256).transpose(1, 0, 2))
    return c


def epat_table(S):
    n = np.arange(S)[None, :]; r = np.arange(64)[:, None]
    return (30000.0 * (((n // 64) % 64) == r)).astype(np.float32)


def rope_tables(S):
    half = 32
    inv = (10000.0 ** (-np.arange(half, dtype=np.float32) / half)).astype(np.float32)
    ang = (np.arange(S, dtype=np.float32)[None, :] * inv[:, None]).astype(np.float32)
    cos = np.cos(ang).astype(np.float32); sin = np.sin(ang).astype(np.float32)
    cosf = np.concatenate([cos, cos], 0); sinf = np.concatenate([-sin, sin], 0)
    rk = np.stack([cosf, sinf], 1)
    return np.ascontiguousarray(rk * 0.125), np.ascontiguousarray(rk)


def host_inputs_a(xb, gattn, w_in, pe_k, w1_k, w2_k, pe_v, w1_v, w2_v, g, consts, ropeq, ropek):
    def sw(w):
        w = w.reshape(w.shape[0], -1, 64)
        return np.concatenate([w[..., 32:], w[..., :32]], -1).reshape(w.shape[0], -1)
    kv = lambda i: w_in[:, 1024 + i * 256 + g * 64:1024 + i * 256 + (g + 1) * 64]
    wq = w_in[:, g * 256:(g + 1) * 256]
    d = dict(consts)
    d["xT"] = np.ascontiguousarray(xb.T)
    d["gattn"] = gattn
    d["wqa"] = np.ascontiguousarray(np.concatenate([wq, sw(wq)], 1))
    d["wka"] = np.ascontiguousarray(np.concatenate([kv(0), kv(1), sw(kv(0)), kv(2), sw(kv(2)), kv(4), sw(kv(4))], 1))
    d["wtok"] = np.ascontiguousarray(np.concatenate([kv(3), kv(5)], 1))
    gc = [2560 + br * 16 + g * 4 + r for br in range(3) for r in range(4)]
    d["wg"] = np.ascontiguousarray(w_in[:, gc])
    d["w1s"] = np.ascontiguousarray(np.concatenate([w1_k[g].transpose(1, 0, 2), w1_v[g].transpose(1, 0, 2)], 0))
    d["peT"] = np.ascontiguousarray(np.concatenate([pe_k[g].T, pe_v[g].T], 0))
    d["w2s"] = np.ascontiguousarray(np.stack([w2_k[g], w2_v[g]], 1))
    d["ropeq"] = ropeq; d["ropek"] = ropek
    d["Epat"] = epat_table(xb.shape[0])
    return d


def declare_a(nc, S, pfx="", fused=False):
    d = {}

    def t(name, shape, kind="ExternalInput", dt=F32):
        d[name] = nc.dram_tensor(pfx + name, shape, dt, kind=kind).ap()

    t("xT", [1024, S]); t("gattn", [1024]); t("wqa", [1024, 512]); t("wka", [1024, 448]); t("wtok", [1024, 128]); t("wg", [1024, 12])
    t("w1s", [128, 32, 128]); t("peT", [128, 32]); t("w2s", [128, 2, 64]); t("ropeq", [64, 2, S]); t("ropek", [64, 2, S])
    t("pats", [128, 2, 512]); t("D16", [128, 2, 128]); t("tril", [128, 2, 128]); t("Epat", [64, S]); t("Sel", [12, 12, 64])
    t("slcm", [128, 8, 256])
    t("oT", [256, S], kind="Internal" if fused else "ExternalOutput")
    return d


def emit_a(nc, P, S, d, oT_dst=None, block_hook=None):
    if oT_dst is None:
        oT_dst = lambda t0, n: d["oT"][:, t0:t0 + n]
    NBLK = S // 512
    NCH = S // 128
    with contextlib.ExitStack() as st:
        sb = lambda n, s, dt=F32: st.enter_context(nc.sbuf_tensor("a_" + n, s, dt))
        ps = lambda n, s, dt=F32: st.enter_context(nc.psum_tensor("a_" + n, s, dt))
        KsT = sb("KsT", [128, S], BF16); Vs = sb("Vs", [128, NCH, 128], BF16)
        KwT = sb("KwT", [128, 8, 128], BF16); Vw = sb("Vw", [128, 8, 128], BF16)
        KcT = sb("KcT", [128, 1024], BF16); Vc = sb("Vc", [128, 8, 128], BF16)
        slcm = sb("slcm", [128, 8, 256], BF16)
        Qaug = [sb(f"Qaug{i}", [128, 4, 512], BF16) for i in range(2)]
        pats = sb("pats", [128, 2, 512]); D16 = sb("D16", [128, 2, 128]); tril = sb("tril", [128, 2, 128], BF16)
        Sel = sb("Sel", [12, 12, 64])
        wqa = sb("wqa", [128, 8, 512], BF16); wka = sb("wka", [128, 8, 448], BF16); wtok = sb("wtok", [128, 8, 128], BF16)
        wg = sb("wg", [128, 8, 12], BF16)
        w1s = sb("w1s", [128, 32, 128], BF16); peT = sb("peT", [128, 32], BF16); w2s = sb("w2s", [128, 2, 64], BF16)
        hb = sb("hb", [128, 2]); g = sb("g", [128, 8])
        ones_b = sb("ones_b", [128, 128], BF16); ones_f = sb("ones_f", [128, 128]); identf = sb("identf", [128, 128])
        xT = sb("xT", [128, 8, 512]); sq = sb("sq", [128, 8, 512], BF16); rstd = sb("rstd", [128, 512]); xnb = sb("xnb", [128, 8, 512], BF16)
        rq = sb("rq", [64, 2, 512]); rk = sb("rk", [64, 2, 512])
        t1 = [sb(f"t1_{i}", [64, 512]) for i in range(2)]; t2 = [sb(f"t2_{i}", [64, 512]) for i in range(2)]
        Qd = sb("Qd", [64, 4, 4, 128], BF16)
        CV = sb("CV", [128, 528], BF16); hidk = sb("hidk", [128, 32], BF16); hvp = sb("hvp", [128, 128], BF16)
        gT = sb("gT", [12, 512])
        PTc = sb("PTc", [128, 8, 512], BF16); PT = [sb(f"PT{i}", [128, 512], BF16) for i in range(4)]
        zc = sb("zc", [1, 512]); impS = sb("impS", [128, 256]); scr = sb("scr", [128, 256]); m8 = sb("m8", [128, 16])
        NT = sb("NT", [128, 256])
        zrow = sb("zrow", [65, 512]); gbs = sb("gbs", [64, 512]); acc = [sb(f"acc{i}", [64, 512]) for i in range(2)]
        tmp = sb("tmp", [64, 512])
        pp0 = ps("pp0", [128, 512])
        ST = [ps(f"ST{i}", [128, 512]) for i in range(3)]
        OA = [ps(f"OA{i}", [128, 512]) for i in range(2)]
        IMP = ps("IMP", [128, 512]); AUX = ps("AUX", [128, 512])
        pp = [pp0, IMP]; ppk = ["pp0", "IMP"]

        for nm, tl in (("wqa", wqa), ("wka", wka), ("wtok", wtok), ("wg", wg)):
            P.D("pool", out=tl[:], in_=d[nm].rearrange("(m p) c -> p m c", p=128), w=[nm])
        for nm, tl in (("w1s", w1s), ("peT", peT), ("w2s", w2s), ("slcm", slcm), ("tril", tril)):
            P.D("pool", out=tl[:], in_=d[nm], w=[nm])
        for nm, tl in (("pats", pats), ("D16", D16), ("Sel", Sel)):
            P.D("sp", out=tl[:], in_=d[nm], w=[nm])
        P.D("pool", out=KsT[64:128, :], in_=d["Epat"], w=["KsE"])
        P.D("sp", out=g[:], in_=d["gattn"].rearrange("(m p) -> p m", p=128), w=["g"], allow_slow_non_contiguous=True)
        P.I("pool", "memset", w=["ones_b"], ap=ones_b[:], constant=1.0)
        P.I("pool", "memset", w=["ones_f"], ap=ones_f[:], constant=1.0)
        P.I("pool", "memset", w=["identf"], ap=identf[:], constant=1.0)
        P.I("pool", "affine_select", r=["identf"], w=["identf"], out=identf[:], in_=identf[:], pattern=[[-1, 128]],
            compare_op=ALU.is_equal, fill=0.0, base=0, channel_multiplier=1)
        P.I("pool", "memset", w=["Vs"], ap=Vs[:], constant=0.0)
        P.I("pool", "memset", r=["Vs"], w=["Vs"], ap=Vs[:, :, 64:65], constant=1.0)
        P.I("pool", "memset", w=["Vw"], ap=Vw[:], constant=0.0)
        P.I("pool", "memset", r=["Vw"], w=["Vw"], ap=Vw[:, :, 64:65], constant=1.0)
        P.I("pool", "memset", w=["KwT"], ap=KwT[:], constant=0.0)
        for i in range(2):
            P.I("pool", "memset", w=[f"Qaug{i}q", f"Qaug{i}m"], ap=Qaug[i][:], constant=0.0)
        P.I("pool", "memset", w=["KcT"], ap=KcT[:], constant=0.0)
        P.I("pool", "memset", w=["Vc"], ap=Vc[:], constant=0.0)
        P.I("pool", "memset", w=["CVk", "CVv"], ap=CV[:], constant=0.0)
        P.I("pool", "memset", w=["hvp"], ap=hvp[:], constant=0.0)
        for kvi in range(2):
            rows = slice(kvi * 64, kvi * 64 + 64)
            for l in range(32):
                P.MM(pp[0][:, kvi:kvi + 1], w1s[rows, l, :], peT[rows, l:l + 1], start=(l == 0), stop=(l == 31), r=["w1s", "peT"], w=["pp0"])
        P.I("dve", "tensor_copy", r=["pp0"], w=["hb"], out=hb[:], in_=pp[0][:, 0:2])

        cnt = {"pp": 0, "t": 0, "pt": 0, "oa": 0, "acc": 0}

        def proj(c0, M):
            i = cnt["pp"] % 2; cnt["pp"] += 1
            return pp[i], ppk[i]

        for blk in range(NBLK):
            tsl = slice(blk * 512, (blk + 1) * 512)
            if block_hook is not None:
                block_hook(blk)
            P.D("sp", out=xT[:], in_=d["xT"].rearrange("(m p) t -> p m t", p=128)[:, :, tsl], w=["xT"])
            P.D("sp", out=rq[:], in_=d["ropeq"][:, :, tsl], w=["rq"])
            P.D("sp", out=rk[:], in_=d["ropek"][:, :, tsl], w=["rk"])
            P.I("act", "activation", r=["xT"], w=["sq"], out=sq[:], in_=xT[:], func=AF.Square)
            for m in range(8):
                P.MM(AUX[:], ones_b[:], sq[:, m, :], start=(m == 0), stop=(m == 7), r=["ones_b", "sq"], w=["AUX"])
            P.I("act", "activation", r=["AUX"], w=["rstd"], out=rstd[:], in_=AUX[:], func=AF.Sqrt, scale=1.0 / 1024, bias=EPS)
            P.I("dve", "reciprocal", r=["rstd"], w=["rstd"], out=rstd[:], in_=rstd[:])
            for m in range(8):
                P.I("dve", "scalar_tensor_tensor", r=["xT", "g", "rstd"], w=["xnb"], out=xnb[:, m, :], in0=xT[:, m, :],
                    scalar=g[:, m:m + 1], in1=rstd[:], op0=ALU.mult, op1=ALU.mult)

            def fmproj(wt, wk_, c0, M):
                p_, pk = proj(c0, M)
                for m in range(8):
                    P.MM(p_[0:M, :], wt[:, m, c0:c0 + M], xnb[:, m, :], start=(m == 0), stop=(m == 7), r=[wk_, "xnb"], w=[pk])
                return p_, pk

            def rope(wt, wk_, ca, cb, tab, tabk, out_ap, outk):
                pa, pak = fmproj(wt, wk_, ca, 64)
                i = cnt["t"] % 2; cnt["t"] += 1
                P.I("dve", "tensor_tensor", r=[pak, tabk], w=[f"t1_{i}"], out=t1[i][:], in0=pa[0:64, :], in1=tab[:, 0, :], op=ALU.mult)
                pb, pbk = fmproj(wt, wk_, cb, 64)
                P.I("dve", "tensor_tensor", r=[pbk, tabk], w=[f"t2_{i}"], out=t2[i][:], in0=pb[0:64, :], in1=tab[:, 1, :], op=ALU.mult)
                a_, b_ = t1[i][:], t2[i][:]
                if len(out_ap.shape) == 3:
                    a_ = a_.rearrange("p (a q) -> p a q", a=4); b_ = b_.rearrange("p (a q) -> p a q", a=4)
                P.I("pool", "tensor_tensor", r=[f"t1_{i}", f"t2_{i}"], w=[outk], out=out_ap, in0=a_, in1=b_, op=ALU.add)

            for r in range(4):
                rope(wqa, "wqa", r * 64, 256 + r * 64, rq, "rq", Qd[:, :, r, :], "Qd")
            rope(wka, "wka", 192, 256, rk, "rk", KsT[0:64, tsl], "KsT")
            rope(wka, "wka", 320, 384, rk, "rk", KwT[0:64, (blk % 2) * 4:(blk % 2) * 4 + 4, :], "KwT")
            pa, pak = fmproj(wka, "wka", 0, 128)
            i = cnt["t"] % 2; cnt["t"] += 1
            P.I("dve", "tensor_tensor", r=[pak, "rk"], w=[f"t1_{i}"], out=t1[i][:], in0=pa[0:64, :], in1=rk[:, 0, :], op=ALU.mult)
            P.I("act", "copy", r=[pak], w=["CVv"], out=CV[64:128, 16:528], in_=pa[64:128, :])
            pb, pbk = fmproj(wka, "wka", 128, 64)
            P.I("dve", "tensor_tensor", r=[pbk, "rk"], w=[f"t2_{i}"], out=t2[i][:], in0=pb[0:64, :], in1=rk[:, 1, :], op=ALU.mult)
            P.I("pool", "tensor_tensor", r=[f"t1_{i}", f"t2_{i}"], w=["CVk"], out=CV[0:64, 16:528], in0=t1[i][:], in1=t2[i][:], op=ALU.add)
            pgt, pgk = fmproj(wg, "wg", 0, 12)
            P.I("act", "activation", r=[pgk], w=["gT"], out=gT[:], in_=pgt[0:12, :], func=AF.Sigmoid)
            for t4 in range(4):
                ch = blk * 4 + t4
                i = cnt["pp"] % 2; cnt["pp"] += 1
                for m in range(8):
                    P.MM(pp[i][:, 0:128], xnb[:, m, t4 * 128:(t4 + 1) * 128], wtok[:, m, :], start=(m == 0), stop=(m == 7),
                         r=["wtok", "xnb"], w=[ppk[i]])
                P.I("act", "copy", r=[ppk[i]], w=["Vs"], out=Vs[:, ch, 0:64], in_=pp[i][:, 0:64])
                P.I("act", "copy", r=[ppk[i]], w=["Vw"], out=Vw[:, ch % 8, 0:64], in_=pp[i][:, 64:128])
            CVv = CV[:].rearrange("p (c s) -> p c s", s=16)
            i = cnt["pp"] % 2; cnt["pp"] += 1
            for l in range(32):
                P.MM(pp[i][:, 0:32], w1s[0:64, l, :], CVv[0:64, l // 16:l // 16 + 32, l % 16], start=(l == 0), stop=(l == 31),
                     r=["w1s", "CVk"], w=[ppk[i]])
            P.I("act", "activation", r=[ppk[i], "hb"], w=["hidk"], out=hidk[:], in_=pp[i][:, 0:32], func=AF.Gelu_apprx_tanh, bias=hb[:, 0:1])
            i2 = cnt["pp"] % 2; cnt["pp"] += 1
            P.MM(pp[i2][0:64, 0:32], w2s[:, 0, :], hidk[:], r=["w2s", "hidk"], w=[ppk[i2]])
            P.I("dve", "tensor_copy", r=[ppk[i2]], w=["KcT"], out=KcT[0:64, 32 * blk:32 * blk + 32], in_=pp[i2][0:64, 0:32])
            i = cnt["pp"] % 2; cnt["pp"] += 1
            for l in range(32):
                P.MM(pp[i][:, 0:32], w1s[64:128, l, :], CVv[64:128, l // 16:l // 16 + 32, l % 16], start=(l == 0), stop=(l == 31),
                     r=["w1s", "CVv"], w=[ppk[i]])
            off = (32 * blk) % 128
            P.I("act", "activation", r=[ppk[i], "hb"], w=["hvp"], out=hvp[:, off:off + 32], in_=pp[i][:, 0:32], func=AF.Gelu_apprx_tanh, bias=hb[:, 1:2])
            i2 = cnt["pp"] % 2; cnt["pp"] += 1
            P.MM(pp[i2][:, 0:64], hvp[:], w2s[:, 1, :], r=["w2s", "hvp"], w=[ppk[i2]])
            P.I("dve", "tensor_copy", r=[ppk[i2]], w=["Vc"], out=Vc[off:off + 32, (32 * blk) // 128, 0:64], in_=pp[i2][off:off + 32, 0:64])
            P.I("act", "copy", r=["CVk", "CVv"], w=["CVk", "CVv"], out=CV[:, 0:16], in_=CV[:, 512:528])

            for qi in range(4):
                QB = blk * 4 + qi
                t0 = 128 * QB
                Qb = Qd[:, qi, :, :].rearrange("p r q -> p (r q)")

                def combine(br, ot, otk, normalize):
                    ai = cnt["acc"] % 2
                    for r in range(4):
                        P.MM(AUX[0:64, r * 128:(r + 1) * 128], Sel[:, br * 4 + r, :], gT[:, qi * 128:(qi + 1) * 128], r=["Sel", "gT"], w=["AUX"])
                    P.I("act", "copy", r=["AUX"], w=["gbs"], out=gbs[:], in_=AUX[0:64, :])
                    if normalize:
                        P.I("dve", "tensor_scalar", r=[otk], w=["zrow"], out=zrow[64:65, :], in0=ot[64:65, :], scalar1=1e-30, scalar2=None, op0=ALU.max)
                        P.I("dve", "reciprocal", r=["zrow"], w=["zrow"], out=zrow[64:65, :], in_=zrow[64:65, :])
                        P.MM(AUX[0:64, :], ones_f[64:65, 0:64], zrow[64:65, :], r=["ones_f", "zrow"], w=["AUX"])
                        P.I("dve", "tensor_tensor", r=["gbs", "AUX"], w=["gbs"], out=gbs[:], in0=gbs[:], in1=AUX[0:64, :], op=ALU.mult)
                    if br == 0:
                        P.I("dve", "tensor_tensor", r=[otk, "gbs"], w=[f"acc{ai}"], out=acc[ai][:], in0=ot[0:64, :], in1=gbs[:], op=ALU.mult)
                    else:
                        P.I("dve", "tensor_tensor", r=[otk, "gbs"], w=["tmp"], out=tmp[:], in0=ot[0:64, :], in1=gbs[:], op=ALU.mult)
                        P.I("pool", "tensor_tensor", r=["tmp", f"acc{ai}"], w=[f"acc{ai}"], out=acc[ai][:], in0=acc[ai][:], in1=tmp[:], op=ALU.add)

                qa = QB % 2
                ng = QB // 32 + 1
                P.I("pool", "tensor_copy", r=["Qd"], w=[f"Qaug{qa}q"], out=Qaug[qa][0:64, 0:ng, :],
                    in_=Qb.unsqueeze(1).to_broadcast([64, ng, 512]))
                Qfull = Qaug[qa][:, 0, :]
                Qr = [f"Qaug{qa}q", f"Qaug{qa}m"]
                jmax = (t0 + 112) // 2048
                nj = jmax + 1
                for j in range(nj):
                    si = cnt["pt"] % 3; cnt["pt"] += 1
                    P.MM(ST[si][:], KcT[:, j * 128:(j + 1) * 128], Qfull, r=["KcT"] + Qr, w=[f"ST{si}"])
                    P.I("act", "activation", r=[f"ST{si}"], w=[f"PTc{j}"], out=PTc[:, j, :], in_=ST[si][:], func=AF.Exp)
                    delta = t0 - 2048 * j - 15
                    if j == 0 or delta < 2032:
                        P.I("dve", "scalar_tensor_tensor", r=["D16", f"PTc{j}"], w=[f"PTc{j}"], out=PTc[:, j, :].rearrange("p (r q) -> p r q", r=4),
                            in0=D16[:, 1 if j == 0 else 0, :].unsqueeze(1).to_broadcast([128, 4, 128]), scalar=float(delta),
                            in1=PTc[:, j, :].rearrange("p (r q) -> p r q", r=4), op0=ALU.is_le, op1=ALU.mult)
                    P.MM(AUX[0:1, :], ones_b[:, 0:1], PTc[:, j, :], start=(j == 0), stop=(j == jmax), r=["ones_b", f"PTc{j}"], w=["AUX"])
                P.I("dve", "tensor_scalar", r=["AUX"], w=["zc"], out=zc[:], in0=AUX[0:1, :], scalar1=1e-30, scalar2=None, op0=ALU.max)
                P.I("dve", "reciprocal", r=["zc"], w=["zc"], out=zc[:], in_=zc[:])
                P.MM(AUX[:], ones_f[0:1, :], zc[0:1, :], r=["ones_f", "zc"], w=["AUX"])
                pk_all = [f"PTc{j}" for j in range(nj)]
                P.I("dve", "tensor_tensor", r=pk_all + ["AUX"], w=pk_all, out=PTc[:, 0:nj, :], in0=PTc[:, 0:nj, :],
                    in1=AUX[:].unsqueeze(1).to_broadcast([128, nj, 512]), op=ALU.mult)
                oi = cnt["oa"] % 2; cnt["oa"] += 1
                for j in range(nj):
                    P.MM(OA[oi][:], Vc[:, j, :], PTc[:, j, :], start=(j == 0), stop=(j == jmax), r=["Vc", f"PTc{j}"], w=[f"OA{oi}"])
                for j in range(nj):
                    for r in range(4):
                        P.MM(IMP[:, 0:256], PTc[:, j, r * 128:(r + 1) * 128], slcm[:, j, :], start=(j == 0 and r == 0),
                             stop=(j == jmax and r == 3), r=["slcm", f"PTc{j}"], w=["IMP"])
                combine(0, OA[oi], f"OA{oi}", False)
                jb = 2 * QB
                P.I("dve", "tensor_tensor", r=["IMP", "pats"], w=["impS"], out=impS[:], in0=IMP[:, 0:256], in1=pats[:, 0, 256 - jb:512 - jb], op=ALU.mult)
                P.I("dve", "tensor_tensor", r=["impS", "pats"], w=["impS"], out=impS[:], in0=impS[:], in1=pats[:, 1, 256 - jb:512 - jb], op=ALU.add)
                P.I("dve", "memset", r=["impS"], w=["impS"], ap=impS[:, 0:1], constant=10002.0)
                P.I("dve", "max", r=["impS"], w=["m8"], out=m8[:, 0:8], in_=impS[:])
                P.I("dve", "match_replace", r=["impS", "m8"], w=["scr"], out=scr[:], in_to_replace=m8[:, 0:8], in_values=impS[:], imm_value=-2.0)
                P.I("dve", "max", r=["scr"], w=["m8"], out=m8[:, 8:16], in_=scr[:])
                P.I("dve", "tensor_scalar", r=["impS", "m8"], w=["NT"], out=NT[:], in0=impS[:], scalar1=m8[:, 15:16], scalar2=1.0,
                    op0=ALU.is_ge, op1=ALU.subtract)
                for jt in range(2):
                    P.op("pe", (lambda jt: (lambda e: e.transpose(out=IMP[:, jt * 128:(jt + 1) * 128], in_=NT[:, jt * 128:(jt + 1) * 128], identity=identf[:])))(jt),
                         reads=["NT", "identf"], writes=["IMP"])
                for g_ in range(ng):
                    half = g_ % 2
                    P.I("act", "copy", r=["IMP"], w=[f"Qaug{qa}m"], out=Qaug[qa][64:128, g_, :].rearrange("p (r q) -> p r q", r=4),
                        in_=IMP[64 * half:64 * half + 64, (g_ // 2) * 128:(g_ // 2 + 1) * 128].unsqueeze(1).to_broadcast([64, 4, 128]))

                for br in (1, 2):
                    kcs = list(range(0, QB + 1)) if br == 1 else list(range(max(0, QB - 4), QB + 1))
                    oi = cnt["oa"] % 2; cnt["oa"] += 1
                    ot = OA[oi]; otk = f"OA{oi}"
                    base = cnt["pt"]

                    def qk(n, br=br, kcs=kcs, base=base, qa=qa, Qfull=Qfull, Qr=Qr):
                        kc = kcs[n]; si = (base + n) % 3
                        if br == 1:
                            P.MM(ST[si][:], KsT[:, kc * 128:(kc + 1) * 128], Qaug[qa][:, kc // 32, :],
                                 r=["KsT", "KsE", f"Qaug{qa}q", f"Qaug{qa}m"], w=[f"ST{si}"])
                        else:
                            P.MM(ST[si][:], KwT[:, kc % 8, :], Qfull, r=["KwT"] + Qr, w=[f"ST{si}"])

                    qk(0)
                    if len(kcs) > 1:
                        qk(1)
                    for n, kc in enumerate(kcs):
                        if n + 2 < len(kcs):
                            qk(n + 2)
                        si = (base + n) % 3
                        pi = cnt["pt"] % 4; cnt["pt"] += 1
                        pt = PT[pi]; ptk = f"PT{pi}"
                        P.I("act", "activation", r=[f"ST{si}"], w=[ptk], out=pt[:], in_=ST[si][:], func=AF.Exp)
                        if kc == QB:
                            P.I("dve", "tensor_tensor", r=[ptk, "tril"], w=[ptk], out=pt[:].rearrange("p (r q) -> p r q", r=4),
                                in0=pt[:].rearrange("p (r q) -> p r q", r=4), in1=tril[:, 0, :].unsqueeze(1).to_broadcast([128, 4, 128]), op=ALU.mult)
                        if br == 2 and kc == QB - 4:
                            P.I("dve", "tensor_tensor", r=[ptk, "tril"], w=[ptk], out=pt[:].rearrange("p (r q) -> p r q", r=4),
                                in0=pt[:].rearrange("p (r q) -> p r q", r=4), in1=tril[:, 1, :].unsqueeze(1).to_broadcast([128, 4, 128]), op=ALU.mult)
                        vv = Vs[:, kc, :] if br == 1 else Vw[:, kc % 8, :]
                        P.MM(ot[:], vv, pt[:], start=(n == 0), stop=(n == len(kcs) - 1), r=["Vs" if br == 1 else "Vw", ptk], w=[otk])
                    combine(br, ot, otk, True)
                ai = cnt["acc"] % 2; cnt["acc"] += 1
                P.D("sp", out=oT_dst(t0, 128).rearrange("(r x) q -> x r q", x=64), in_=acc[ai][:].rearrange("p (r q) -> p r q", r=4),
                    r=[f"acc{ai}"], w=[f"oT{QB}"])
        P.wait_all("sp", [f"oT{q}" for q in range(NBLK * 4)])
        P.barrier()
        P.emit()


EPS = 1e-6
NEG = -1.0e30


def declare_b(nc, T, pfx=""):
    d = {}

    def inp(name, shape, dt=F32):
        d[name] = nc.dram_tensor(pfx + name, shape, dt, kind="ExternalInput").ap()

    def outp(name, shape, dt=F32):
        d[name] = nc.dram_tensor(pfx + name, shape, dt, kind="ExternalOutput").ap()

    def scr(name, shape, dt=F32):
        d[name] = nc.dram_tensor(pfx + name, shape, dt, kind="Internal").ap()

    inp("hT", [1024, T]); inp("oT", [1024, T]); inp("wout", [1024, 1024]); inp("gffn", [1024])
    inp("wq", [1024, 2048]); inp("keysT", [16, 128, 128]); inp("uT", [1024, 16384]); inp("v", [16384, 1024])
    inp("gnext", [1024])
    outp("hT_out", [1024, T]); outp("nT_out", [1024, T])
    scr("h2T", [1024, T]); scr("hnbf", [1024, T], BF16); scr("qTd", [128, T // 128, 16, 128])
    scr("GT", [T // 128, 128, 16384], BF16); scr("uTbf", [1024, 16384], BF16); scr("vbf", [16384, 1024], BF16)
    return d


def b_cast_ops(P, d, pfx=""):
    ops = []
    for i in range(8):
        ops.append((lambda i: (lambda: P.D("pool", out=d["uTbf"][i * 128:(i + 1) * 128, :], in_=d["uT"][i * 128:(i + 1) * 128, :], w=[pfx + f"uTbf{i}"])))(i))
    for i in range(8):
        ops.append((lambda i: (lambda: P.D("pool", out=d["vbf"][i * 2048:(i + 1) * 2048, :], in_=d["v"][i * 2048:(i + 1) * 2048, :], w=[pfx + f"vbf{i}"])))(i))
    return ops


def emit_b_casts(P, d, pfx=""):
    for f in b_cast_ops(P, d, pfx):
        f()


def emit_b(nc, P, T, d, cast_weights=True, pfx="", oT_blk=None, nT_dst=None):
    NB = T // 512
    NT = T // 128
    NB2 = T // 256
    fm = lambda ap: ap.rearrange("(m p) t -> p m t", p=128)

    if cast_weights:
        emit_b_casts(P, d)

    with contextlib.ExitStack() as st:
        sb = lambda n, s, dt=F32: st.enter_context(nc.sbuf_tensor(pfx + "p0_" + n, s, dt))
        ps = lambda n, s, dt=F32: st.enter_context(nc.psum_tensor(pfx + "p0_" + n, s, dt))
        wout = sb("wout", [128, 8, 1024], BF16)
        g = sb("g", [128, 8]); ones = sb("ones", [128, 128])
        hTt = [sb(f"hTt{i}", [128, 8, 512]) for i in range(2)]
        oTt = [sb(f"oTt{i}", [128, 8, 512], BF16) for i in range(2)]
        h2 = sb("h2", [128, 8, 512]); hnb = sb("hnb", [128, 8, 512], BF16)
        rstd = sb("rstd", [128, 512])
        wqt = [sb(f"wqt{i}", [128, 8, 512]) for i in range(2)]
        qs = [sb(f"qs{i}", [128, 4, 512]) for i in range(2)]
        pp = [ps(f"pp{i}", [128, 512]) for i in range(2)]
        ss = ps("ss", [128, 512])

        P.D("pool", out=wout[:], in_=d["wout"].rearrange("(k p) c -> p k c", p=128), w=["wout"])
        P.D("sp", out=g[:], in_=d["gffn"].rearrange("(m p) -> p m", p=128), w=["g"], allow_slow_non_contiguous=True)
        P.I("pool", "memset", w=["ones"], ap=ones[:], constant=1.0)
        nmm = 0
        nwq = 0
        if oT_blk is not None:
            P.dma("sp", lambda e: e.dma_start(out=d["oTq"].rearrange("r (c t) -> c r t", t=min(1024, T)), in_=oT_blk()), writes=["oTq"])
        def load_wq(idx):
            jg_ = idx % 4
            P.D("act", out=wqt[idx % 2][:], in_=d["wq"].rearrange("(m p) c -> p m c", p=128)[:, :, jg_ * 512:(jg_ + 1) * 512],
                w=[f"wqt{idx % 2}"])

        def load_in(b_):
            tsl_ = slice(b_ * 512, (b_ + 1) * 512)
            P.D("sp", out=hTt[b_ % 2][:], in_=fm(d["hT"])[:, :, tsl_], w=[f"hTt{b_ % 2}"])
            if oT_blk is None:
                P.D("pool", out=oTt[b_ % 2][:], in_=fm(d["oT"])[:, :, tsl_], w=[f"oTt{b_ % 2}"])
            else:
                P.D("pool", out=oTt[b_ % 2][:], in_=fm(d["oTq"])[:, :, tsl_], r=["oTq"], w=[f"oTt{b_ % 2}"])

        load_wq(0)
        load_in(0)
        for b in range(NB):
            tsl = slice(b * 512, (b + 1) * 512)
            ht, ot = hTt[b % 2], oTt[b % 2]
            hk, ok = f"hTt{b % 2}", f"oTt{b % 2}"
            if b + 1 < NB:
                load_in(b + 1)
            for m in range(8):
                p_ = pp[nmm % 2]; pk = f"pp{nmm % 2}"; nmm += 1
                for k in range(8):
                    P.MM(p_[:], wout[:, k, m * 128:(m + 1) * 128], ot[:, k, :], start=(k == 0), stop=(k == 7),
                         r=["wout", ok], w=[pk])
                P.I("dve", "tensor_tensor", r=[pk, hk], w=["h2"], out=h2[:, m, :], in0=p_[:], in1=ht[:, m, :], op=ALU.add)
            P.D("sp", out=fm(d["h2T"])[:, :, tsl], in_=h2[:], r=["h2"], w=[f"h2T{b}"])
            P.I("act", "activation", r=["h2"], w=[hk], out=ht[:], in_=h2[:], func=AF.Square)
            for m in range(8):
                P.MM(ss[:], ones[:], ht[:, m, :], start=(m == 0), stop=(m == 7), r=["ones", hk], w=["ss"])
            P.I("act", "activation", r=["ss"], w=["rstd"], out=rstd[:], in_=ss[:], func=AF.Sqrt, scale=1.0 / 1024, bias=EPS)
            P.I("dve", "reciprocal", r=["rstd"], w=["rstd"], out=rstd[:], in_=rstd[:])
            for m in range(8):
                P.I("dve", "scalar_tensor_tensor", r=["h2", "g", "rstd"], w=["h2"], out=h2[:, m, :], in0=h2[:, m, :],
                    scalar=g[:, m:m + 1], in1=rstd[:], op0=ALU.mult, op1=ALU.mult)
            P.I("pool", "tensor_copy", r=["h2"], w=["hnb"], out=hnb[:], in_=h2[:])
            P.D("sp", out=fm(d["hnbf"])[:, :, tsl], in_=hnb[:], r=["hnb"], w=[f"hnbf{b}"])
            for jg in range(4):
                wt = wqt[nwq % 2]; wk = f"wqt{nwq % 2}"; q_ = qs[nwq % 2]; qk = f"qs{nwq % 2}"; nwq += 1
                if nwq < NB * 4:
                    load_wq(nwq)
                for jj in range(4):
                    p_ = pp[nmm % 2]; pk = f"pp{nmm % 2}"; nmm += 1
                    for m in range(8):
                        P.MM(p_[:], wt[:, m, jj * 128:(jj + 1) * 128], h2[:, m, :], start=(m == 0), stop=(m == 7),
                             r=[wk, "h2"], w=[pk])
                    P.I("act", "copy", r=[pk], w=[qk], out=q_[:, jj, :], in_=p_[:])
                for t4 in range(4):
                    P.D("sp", out=d["qTd"][:, b * 4 + t4, jg * 4:(jg + 1) * 4, :], in_=q_[:, :, t4 * 128:(t4 + 1) * 128],
                        r=[qk], w=[f"qTd{b}_{jg}_{t4}"])
        P.barrier()
        P.emit()

    with contextlib.ExitStack() as st:
        sb = lambda n, s, dt=F32: st.enter_context(nc.sbuf_tensor(pfx + "p1_" + n, s, dt))
        ps = lambda n, s, dt=F32: st.enter_context(nc.psum_tensor(pfx + "p1_" + n, s, dt))
        keys = sb("keys", [128, 16, 128]); ident = sb("ident", [128, 128], BF16); identf = sb("identf", [128, 128])
        qt = [sb(f"qt{i}", [128, 16, 128]) for i in range(2)]
        S12 = [sb(f"S12{i}", [128, 16, 128]) for i in range(2)]
        scr_ = sb("scr", [128, 256]); TS = sb("TS", [128, 16, 16]); cand = sb("cand", [128, 8, 256]); BS = sb("BS", [128, 8, 16])
        ex = sb("ex", [128, 8, 16]); Z = sb("Z", [128, 8]); lnZ = sb("lnZ", [128, 8]); bias = sb("bias", [128, 8])
        SUM = [sb(f"SUM{i}", [128, 8, 128]) for i in range(6)]
        E = [sb(f"E{i}", [128, 1024], BF16) for i in range(3)]
        GH = [[sb(f"GH{i}_{h}", [128, 1024], BF16) for h in range(8)] for i in range(2)]
        GTp = [sb(f"GTp{i}", [128, 8, 128], BF16) for i in range(4)]
        sc = ps("sc", [128, 16, 128])
        acc = [ps(f"acc{i}", [128, 4, 128]) for i in range(2)]

        P.D("sp", out=keys[:], in_=d["keysT"].rearrange("j c k -> c j k"), w=["keys"])
        P.I("pool", "memset", w=["identf"], ap=identf[:], constant=1.0)
        P.I("pool", "affine_select", r=["identf"], w=["identf"], out=identf[:], in_=identf[:], pattern=[[-1, 128]],
            compare_op=ALU.is_equal, fill=0.0, base=0, channel_multiplier=1)
        P.I("pool", "tensor_copy", r=["identf"], w=["ident"], out=ident[:], in_=identf[:])
        nsum = 0; nacc = 0; ngtp = 0; ngh = 0
        for tt in range(NT):
            q_ = qt[tt % 2]; qk = f"qt{tt % 2}"; S = S12[tt % 2]; Sk = f"S12{tt % 2}"
            P.D("sp", out=q_[:], in_=d["qTd"][:, tt, :, :], w=[qk])
            for j in range(16):
                P.MM(sc[:, j, :], q_[:, j, :], keys[:, j, :], r=[qk, "keys"], w=["sc"])
            P.I("act", "copy", r=["sc"], w=[Sk + "a"], out=S[:, 0:8, :], in_=sc[:, 0:8, :])
            P.I("dve", "tensor_copy", r=["sc"], w=[Sk + "b"], out=S[:, 8:16, :], in_=sc[:, 8:16, :])
            Sr = [Sk + "a", Sk + "b"]
            for j in range(16):
                P.I("dve", "max", r=Sr, w=["TS"], out=TS[:, j, 0:8], in_=S[:, j, :])
                P.I("dve", "match_replace", r=Sr + ["TS"], w=["scr"], out=scr_[:, 0:128], in_to_replace=TS[:, j, 0:8],
                    in_values=S[:, j, :], imm_value=NEG)
                P.I("dve", "max", r=["scr"], w=["TS"], out=TS[:, j, 8:16], in_=scr_[:, 0:128])
            TS4 = TS[:].rearrange("p (h two) a -> p h two a", two=2)
            P.I("dve", "tensor_tensor", r=["TS"], w=["cand"], out=cand[:].rearrange("p h (a b) -> p h a b", b=16),
                in0=TS4[:, :, 0, :].unsqueeze(3).to_broadcast([128, 8, 16, 16]),
                in1=TS4[:, :, 1, :].unsqueeze(2).to_broadcast([128, 8, 16, 16]), op=ALU.add)
            for h in range(8):
                P.I("dve", "max", r=["cand"], w=["BS"], out=BS[:, h, 0:8], in_=cand[:, h, :])
                P.I("dve", "match_replace", r=["cand", "BS"], w=["scr"], out=scr_[:, 0:256], in_to_replace=BS[:, h, 0:8],
                    in_values=cand[:, h, :], imm_value=NEG)
                P.I("dve", "max", r=["scr"], w=["BS"], out=BS[:, h, 8:16], in_=scr_[:, 0:256])
            P.I("dve", "tensor_tensor", r=["BS"], w=["ex"], out=ex[:], in0=BS[:], in1=BS[:, :, 0:1].to_broadcast([128, 8, 16]),
                op=ALU.subtract)
            P.I("act", "activation", r=["ex"], w=["ex"], out=ex[:], in_=ex[:], func=AF.Exp)
            P.I("dve", "reduce_sum", r=["ex"], w=["Z"], out=Z[:], in_=ex[:], axis=AX.X)
            P.I("act", "activation", r=["Z"], w=["lnZ"], out=lnZ[:], in_=Z[:], func=AF.Ln)
            P.I("dve", "scalar_tensor_tensor", r=["BS", "lnZ"], w=["bias"], out=bias[:], in0=BS[:, :, 0], scalar=-1.0,
                in1=lnZ[:], op0=ALU.mult, op1=ALU.subtract)
            def add_op(it):
                sx_, h_ = it // 8, it % 8
                su = SUM[it % 6]; sk = f"SUM{it % 6}"
                if False:
                    for a in range(8):
                        P.I("act", "activation", r=Sr, w=[sk], out=su[:, a, :], in_=S[:, 2 * h_ + 1, :], func=AF.Identity,
                            bias=S[:, 2 * h_, sx_ * 8 + a:sx_ * 8 + a + 1], scale=1.0)
                else:
                    P.I("dve", "tensor_tensor", r=Sr, w=[sk], out=su[:],
                        in0=S[:, 2 * h_, sx_ * 8:(sx_ + 1) * 8].unsqueeze(2).to_broadcast([128, 8, 128]),
                        in1=S[:, 2 * h_ + 1, :].unsqueeze(1).to_broadcast([128, 8, 128]), op=ALU.add)

            LOOK = 4
            deferred = []
            for it0 in range(LOOK):
                add_op(it0)
            for sx in range(16):
                ghs = GH[ngh % 2]; gk = f"GH{ngh % 2}_"; ngh += 1
                for h in range(8):
                    it = sx * 8 + h
                    if it + LOOK < 128:
                        add_op(it + LOOK)
                    if h == 4 and deferred:
                        deferred.pop(0)()
                    su = SUM[it % 6]; sk = f"SUM{it % 6}"; e_ = E[it % 3]; ek = f"E{it % 3}"
                    P.I("act", "activation", r=[sk, "bias"], w=[ek], out=e_[:], in_=su[:].rearrange("p a b -> p (a b)"),
                        func=AF.Exp, bias=bias[:, h:h + 1])
                    P.I("dve", "scalar_tensor_tensor", r=[sk, ek, "BS"], w=[gk + str(h)], out=ghs[h][:],
                        in0=su[:].rearrange("p a b -> p (a b)"), scalar=BS[:, h, 15:16], in1=e_[:], op0=ALU.is_ge, op1=ALU.mult)
                def flush(sx=sx, tt=tt, ghs=ghs, gk=gk):
                    nonlocal nacc, ngtp
                    gp = GTp[ngtp % 4]; gpk = f"GTp{ngtp % 4}"; ngtp += 1
                    for c4 in range(2):
                        a_ = acc[nacc % 2]; ak = f"acc{nacc % 2}"; nacc += 1
                        for ci in range(4):
                            c = c4 * 4 + ci
                            for h in range(8):
                                P.MM(a_[:, ci, :], ghs[h][:, c * 128:(c + 1) * 128], ident[:], start=(h == 0), stop=(h == 7),
                                     r=[gk + str(h), "ident"], w=[ak])
                        P.I("act", "copy", r=[ak], w=[gpk], out=gp[:, c4 * 4:(c4 + 1) * 4, :], in_=a_[:])
                    P.D("sp", out=d["GT"][tt, :, sx * 1024:(sx + 1) * 1024], in_=gp[:].rearrange("p c t -> p (c t)"), r=[gpk],
                        w=[f"GT{tt}_{sx}"])
                deferred.append(flush)
            while deferred:
                deferred.pop(0)()
        P.barrier()
        P.emit()

    NB5 = T // 512
    with contextlib.ExitStack() as st:
        sb = lambda n, s, dt=F32: st.enter_context(nc.sbuf_tensor(pfx + "p2_" + n, s, dt))
        ps = lambda n, s, dt=F32: st.enter_context(nc.psum_tensor(pfx + "p2_" + n, s, dt))
        U = [sb(f"U{i}", [128, 8, 1024], BF16) for i in range(2)]
        V = [sb(f"V{i}", [128, 8, 1024], BF16) for i in range(2)]
        Gg = [sb(f"Gg{i}", [128, 4, 8, 128], BF16) for i in range(2)]
        hn2 = [sb(f"hn2{i}", [128, 8, 512], BF16) for i in range(2)]
        ge = [sb(f"ge{i}", [128, 512], BF16) for i in range(2)]
        gh = [sb(f"gh{i}", [128, 8, 512], BF16) for i in range(2)]
        Yacc = sb("Yacc", [128, 8, 512]); h2b = sb("h2b", [128, 8, 512]); sq2 = sb("sq2", [128, 8, 512])
        rstd2 = sb("rstd2", [128, 512]); nrm = sb("nrm", [128, 8, 512], d["nT_out"].dtype)
        gn = sb("gn", [128, 8]); ones2 = sb("ones2", [128, 128])
        Hp = [ps(f"Hp{i}", [128, 512]) for i in range(2)]
        Yp = ps("Yp", [128, 4, 512])
        ss2 = ps("ss2", [128, 512])
        P.D("sp", out=gn[:], in_=d["gnext"].rearrange("(m p) -> p m", p=128), w=["gn"], allow_slow_non_contiguous=True)
        P.I("pool", "memset", w=["ones2"], ap=ones2[:], constant=1.0)
        nw = 0; nh = 0
        for blk in range(NB5):
            tsl = slice(blk * 512, (blk + 1) * 512)
            hb = hn2[blk % 2]; hbk = f"hn2{blk % 2}"
            P.D("sp", out=hb[:], in_=fm(d["hnbf"])[:, :, tsl], w=[hbk])
            P.D("sp", out=h2b[:], in_=fm(d["h2T"])[:, :, tsl], w=["h2b"])
            for eg in range(16):
                u_ = U[nw % 2]; uk = f"U{nw % 2}"; v_ = V[nw % 2]; vk = f"V{nw % 2}"; g_ = Gg[nw % 2]; gk = f"Gg{nw % 2}"
                gh_ = gh[nw % 2]; ghk = f"gh{nw % 2}"; nw += 1
                P.D("sp", out=u_[:], in_=d["uTbf"].rearrange("(m p) e -> p m e", p=128)[:, :, eg * 1024:(eg + 1) * 1024], w=[uk])
                P.D("act", out=v_[:], in_=d["vbf"].rearrange("(c p) x -> p c x", p=128)[:, eg * 8:(eg + 1) * 8, :], w=[vk])
                for t4 in range(4):
                    P.D("sp", out=g_[:, t4, :, :], in_=d["GT"][blk * 4 + t4, :, eg * 1024:(eg + 1) * 1024].rearrange("p (c t) -> p c t", t=128),
                        w=[gk + str(t4)])
                gks = [gk + str(t4) for t4 in range(4)]
                for c in range(8):
                    hp = Hp[nh % 2]; hpk = f"Hp{nh % 2}"; ge_ = ge[nh % 2]; gek = f"ge{nh % 2}"; nh += 1
                    for m in range(8):
                        P.MM(hp[:], u_[:, m, c * 128:(c + 1) * 128], hb[:, m, :], start=(m == 0), stop=(m == 7), r=[uk, hbk], w=[hpk])
                    P.I("act", "activation", r=[hpk], w=[gek], out=ge_[:], in_=hp[:], func=AF.Gelu_apprx_tanh)
                    P.I("dve", "tensor_tensor", r=[gek] + gks, w=[ghk + str(c)], out=gh_[:, c, :].rearrange("p (tt t) -> p tt t", t=128),
                        in0=ge_[:].rearrange("p (tt t) -> p tt t", t=128), in1=g_[:, :, c, :], op=ALU.mult)
                ghs = [ghk + str(c) for c in range(8)]
                for half in range(2):
                    for mi in range(4):
                        m = half * 4 + mi
                        for c in range(8):
                            P.MM(Yp[:, mi, :], v_[:, c, m * 128:(m + 1) * 128], gh_[:, c, :], start=(c == 0), stop=(c == 7), r=[vk] + ghs, w=["Yp"])
                    if eg == 0:
                        P.I("dve", "tensor_copy", r=["Yp"], w=[f"Yacc{half}"], out=Yacc[:, half * 4:(half + 1) * 4, :], in_=Yp[:])
                    else:
                        P.I("dve", "tensor_tensor", r=["Yp", f"Yacc{half}"], w=[f"Yacc{half}"], out=Yacc[:, half * 4:(half + 1) * 4, :],
                            in0=Yp[:], in1=Yacc[:, half * 4:(half + 1) * 4, :], op=ALU.add)
            P.I("dve", "tensor_tensor", r=["Yacc0", "Yacc1", "h2b"], w=["h2b"], out=h2b[:], in0=Yacc[:], in1=h2b[:], op=ALU.add)
            P.D("sp", out=fm(d["hT_out"])[:, :, tsl], in_=h2b[:], r=["h2b"], w=[f"hT_out{blk}"])
            P.I("act", "activation", r=["h2b"], w=["sq2"], out=sq2[:], in_=h2b[:], func=AF.Square)
            for m in range(8):
                P.MM(ss2[:], ones2[:], sq2[:, m, :], start=(m == 0), stop=(m == 7), r=["ones2", "sq2"], w=["ss2"])
            P.I("act", "activation", r=["ss2"], w=["rstd2"], out=rstd2[:], in_=ss2[:], func=AF.Sqrt, scale=1.0 / 1024, bias=EPS)
            P.I("dve", "reciprocal", r=["rstd2"], w=["rstd2"], out=rstd2[:], in_=rstd2[:])
            for m in range(8):
                P.I("dve", "scalar_tensor_tensor", r=["h2b", "gn", "rstd2"], w=["nrm"], out=nrm[:, m, :], in0=h2b[:, m, :],
                    scalar=gn[:, m:m + 1], in1=rstd2[:], op0=ALU.mult, op1=ALU.mult)
            if nT_dst is None:
                P.D("sp", out=fm(d["nT_out"])[:, :, tsl], in_=nrm[:], r=["nrm"], w=[f"nT_out{blk}"])
            else:
                for hf in range(2):
                    P.D("sp", out=nT_dst(2 * blk + hf), in_=nrm[:, :, hf * 256:(hf + 1) * 256], r=["nrm"], w=[f"nT_out{blk}_{hf}"])
        outs = [f"hT_out{b}" for b in range(NB5)]
        outs += [f"nT_out{b}" for b in range(NB5)] if nT_dst is None else [f"nT_out{b}_{hf}" for b in range(NB5) for hf in range(2)]
        P.wait_all("sp", outs)
        P.barrier()
        P.emit()


def declare_c(nc, S, pfx="", fused=False):
    d = {}

    def t(name, shape, kind, dt=F32):
        d[name] = nc.dram_tensor(pfx + name, shape, dt, kind=kind).ap()

    if not fused:
        t("nT", [1024, S], "ExternalInput")
    t("wq", [1024, 256], "ExternalInput"); t("wk", [1024, 256], "ExternalInput"); t("wv", [1024, 256], "ExternalInput")
    t("wf", [1024, 4], "ExternalInput"); t("fb", [4], "ExternalInput")
    t("maskd", [128, 4, 512], "ExternalInput")
    t("oT", [256, S], "Internal" if fused else "ExternalOutput")
    return d


def emit_c(nc, P, S, d, nT_loads=None, oT_dst=None):
    if oT_dst is None:
        oT_dst = lambda t0, n: d["oT"][:, t0:t0 + n]
    NBLK = S // 512
    NCH = S // 128
    with contextlib.ExitStack() as st:
        sb = lambda n, s, dt=F32: st.enter_context(nc.sbuf_tensor("c_" + n, s, dt))
        ps = lambda n, s, dt=F32: st.enter_context(nc.psum_tensor("c_" + n, s, dt))
        KT = sb("KT", [128, 2, S], BF16)
        Vr = sb("Vr", [128, NCH, 4 * 65 + 63], BF16)
        Cr = sb("Cr", [128, NCH, 4]); nbias = sb("nbias", [128, 4, NCH])
        nb = [sb(f"nb{i}", [128, 8, 512], BF16) for i in range(2)]
        wq = sb("wq", [128, 8, 256], BF16); wk = sb("wk", [128, 8, 256], BF16); wv = sb("wv", [128, 8, 256], BF16)
        wf = sb("wf", [128, 8, 4], BF16); fb = sb("fb", [128, 4])
        QT = [sb(f"QT{i}", [128, 512], BF16) for i in range(4)]
        PT = [sb(f"PT{i}", [128, 512], BF16) for i in range(4)]
        maskd = sb("maskd", [128, 4, 512], BF16)
        tri = sb("tri", [128, 128]); ones = sb("ones", [128, 128])
        lf = sb("lf", [128, 4]); tot = sb("tot", [128, 4]); totmid = sb("totmid", [128, 4])
        zrow = sb("zrow", [65, 512]); ocp = sb("ocp", [64, 512]); osb = [sb(f"osb{i}", [64, 512]) for i in range(2)]
        pp = [ps(f"pp{i}", [128, 512]) for i in range(2)]
        ST = [ps(f"ST{i}", [128, 512]) for i in range(3)]
        OT = [ps(f"OT{i}", [128, 512]) for i in range(2)]
        MISC = ps("MISC", [128, 512]); ZB = MISC[0:64, :]; cs = MISC[:, 0:8]

        for nm, tl in (("wq", wq), ("wk", wk), ("wv", wv)):
            P.D("pool", out=tl[:], in_=d[nm].rearrange("(m p) c -> p m c", p=128), w=[nm])
        P.D("pool", out=wf[:], in_=d["wf"].rearrange("(m p) c -> p m c", p=128), w=["wf"])
        P.D("sp", out=fb[:], in_=d["fb"].partition_broadcast(128), w=["fb"])
        P.D("pool", out=maskd[:], in_=d["maskd"], w=["maskd"])
        P.I("pool", "memset", w=["ones"], ap=ones[:], constant=1.0)
        P.I("pool", "memset", w=["tri"], ap=tri[:], constant=1.0)
        P.I("pool", "affine_select", r=["tri"], w=["tri"], out=tri[:], in_=tri[:], pattern=[[1, 128]],
            compare_op=ALU.is_ge, fill=0.0, base=0, channel_multiplier=-1)
        P.I("pool", "memset", w=["tot"], ap=tot[:], constant=0.0)
        P.I("pool", "memset", w=["Vr"], ap=Vr[:], constant=0.0)
        P.I("pool", "memset", r=["Vr"], w=["Vr"], ap=Vr[:, :, 0:260].rearrange("p c (h x) -> p c h x", x=65)[:, :, :, 64:65], constant=1.0)
        for i in range(4):
            P.I("pool", "memset", w=[f"QT{i}"], ap=QT[i][:], constant=0.0)
        npp = 0; nst = 0; npt = 0; nhead = 0
        for blk in range(NBLK):
            tsl = slice(blk * 512, (blk + 1) * 512)
            n_ = nb[blk % 2]; nk = f"nb{blk % 2}"
            if nT_loads is None:
                P.D("pool", out=n_[:], in_=d["nT"].rearrange("(m p) t -> p m t", p=128)[:, :, tsl], w=[nk])
            else:
                for csl, src in nT_loads(blk):
                    P.D("pool", out=n_[:, :, csl], in_=src, w=[nk])
            for pair in range(2):
                p_ = pp[npp % 2]; pk = f"pp{npp % 2}"; npp += 1
                for m in range(8):
                    P.MM(p_[:], wq[:, m, pair * 128:(pair + 1) * 128], n_[:, m, :], start=(m == 0), stop=(m == 7), r=["wq", nk], w=[pk])
                for hh in range(2):
                    rs = slice(hh * 64, hh * 64 + 64)
                    P.I("act", "mul", r=[pk], w=[f"QT{2 * pair + hh}"], out=QT[2 * pair + hh][rs, :], in_=p_[rs, :], mul=0.125)
                p_ = pp[npp % 2]; pk = f"pp{npp % 2}"; npp += 1
                for m in range(8):
                    P.MM(p_[:], wk[:, m, pair * 128:(pair + 1) * 128], n_[:, m, :], start=(m == 0), stop=(m == 7), r=["wk", nk], w=[pk])
                P.I("dve", "tensor_copy", r=[pk], w=["KT"], out=KT[:, pair, tsl], in_=p_[:])
            for t4 in range(4):
                ch = blk * 4 + t4
                p_ = pp[npp % 2]; pk = f"pp{npp % 2}"; npp += 1
                for m in range(8):
                    P.MM(p_[:, 0:256], n_[:, m, t4 * 128:(t4 + 1) * 128], wv[:, m, :], start=(m == 0), stop=(m == 7), r=["wv", nk], w=[pk])
                P.I("act", "copy", r=[pk], w=["Vr"], out=Vr[:, ch, 0:260].rearrange("p (h x) -> p h x", x=65)[:, :, 0:64], in_=p_[:, 0:256].rearrange("p (h x) -> p h x", x=64))
                for m in range(8):
                    P.MM(cs[:, 0:4], n_[:, m, t4 * 128:(t4 + 1) * 128], wf[:, m, :], start=(m == 0), stop=(m == 7), r=["wf", nk], w=["MISC"])
                P.I("dve", "tensor_tensor", r=["MISC", "fb"], w=["lf"], out=lf[:], in0=cs[:, 0:4], in1=fb[:], op=ALU.add)
                P.I("act", "activation", r=["lf"], w=["lf"], out=lf[:], in_=lf[:], func=AF.Exp, scale=-1.0)
                P.I("act", "activation", r=["lf"], w=["lf"], out=lf[:], in_=lf[:], func=AF.Ln, bias=1.0)
                P.I("dve", "tensor_scalar", r=["lf"], w=["lf"], out=lf[:], in0=lf[:], scalar1=-1.0, scalar2=None, op0=ALU.mult)
                P.MM(cs[:, 0:4], tri[:], lf[:], r=["tri", "lf"], w=["MISC"])
                P.MM(cs[:, 4:8], ones[:], lf[:], r=["ones", "lf"], w=["MISC"])
                P.I("dve", "tensor_tensor", r=["MISC", "tot"], w=["Cr"], out=Cr[:, ch, :], in0=cs[:, 0:4], in1=tot[:], op=ALU.add)
                P.I("dve", "tensor_tensor", r=["MISC", "tot"], w=["tot"], out=tot[:], in0=cs[:, 4:8], in1=tot[:], op=ALU.add)
                if t4 == 1:
                    P.I("dve", "tensor_copy", r=["tot"], w=["totmid"], out=totmid[:], in_=tot[:])
            nch = blk * 4 + 4
            for hl in range(4):
                P.I("dve", "tensor_scalar", r=["Cr", "totmid"], w=["nbias"], out=nbias[:, hl, 0:nch], in0=Cr[:, 0:nch, hl],
                    scalar1=totmid[:, hl:hl + 1], scalar2=-1.0, op0=ALU.subtract, op1=ALU.mult)
            pairs = [(hl, kc) for hl in range(4) for kc in range(nch)]

            def qk(i):
                nonlocal nst
                hl, kc = pairs[i]
                s_ = ST[i % 3]
                P.MM(s_[:], KT[:, hl // 2, kc * 128:(kc + 1) * 128], QT[hl][:], r=["KT", f"QT{hl}"], w=[f"ST{i % 3}"])

            qk(0)
            if len(pairs) > 1:
                qk(1)
            for i, (hl, kc) in enumerate(pairs):
                if i + 2 < len(pairs):
                    qk(i + 2)
                s_ = ST[i % 3]; sk = f"ST{i % 3}"
                pt = PT[npt % 4]; ptk = f"PT{npt % 4}"; npt += 1
                P.I("act", "activation", r=[sk, "nbias"], w=[ptk], out=pt[:], in_=s_[:], func=AF.Exp, bias=nbias[:, hl, kc:kc + 1])
                if kc >= blk * 4:
                    P.I("dve", "tensor_tensor", r=[ptk, "maskd"], w=[ptk], out=pt[:], in0=pt[:], in1=maskd[:, kc - blk * 4, :], op=ALU.mult)
                if kc == 0:
                    ot = OT[nhead % 2]; otk = f"OT{nhead % 2}"; ob = osb[nhead % 2]; obk = f"osb{nhead % 2}"; nhead += 1
                P.MM(ot[:], Vr[:, kc, hl * 65:hl * 65 + 128], pt[:], start=(kc == 0), stop=(kc == nch - 1), r=["Vr", ptk], w=[otk])
                if kc == nch - 1:
                    P.I("dve", "tensor_scalar", r=[otk], w=["zrow"], out=zrow[64:65, :], in0=ot[64:65, :], scalar1=1e-30, scalar2=None, op0=ALU.max)
                    P.I("dve", "reciprocal", r=["zrow"], w=["zrow"], out=zrow[64:65, :], in_=zrow[64:65, :])
                    P.MM(ZB, ones[64:65, 0:64], zrow[64:65, :], r=["ones", "zrow"], w=["MISC"])
                    P.I("act", "copy", r=[otk], w=["ocp"], out=ocp[:], in_=ot[0:64, :])
                    P.I("dve", "tensor_tensor", r=["ocp", "MISC"], w=[obk], out=ob[:], in0=ocp[:], in1=ZB, op=ALU.mult)
                    P.D("sp", out=oT_dst(blk * 512, 512)[hl * 64:(hl + 1) * 64, :], in_=ob[:], r=[obk], w=[f"oT{blk}_{hl}"])
        P.wait_all("sp", [f"oT{b}_{h}" for b in range(NBLK) for h in range(4)])
        P.barrier()
        P.emit()


S_FULL = 16384
T_CORE = 4096
G4 = [[0, 1, 2, 3], [4, 5, 6, 7]]
_PROG = {}
_B_IN = (("wout", [1024, 1024]), ("gffn", [1024]), ("wq", [1024, 2048]), ("keysT", [16, 128, 128]), ("uT", [1024, 16384]),
         ("v", [16384, 1024]), ("gnext", [1024]))


def _declare_b_fused(nc, T, pfx, hT_ap, out_kind):
    d = {}
    for name, shape in _B_IN:
        d[name] = nc.dram_tensor(pfx + name, shape, F32, kind="ExternalInput").ap()
    d["hT"] = hT_ap
    d["hT_out"] = nc.dram_tensor(pfx + "hT_out", [1024, T], F32, kind="Internal").ap()
    d["nT_out"] = nc.dram_tensor(pfx + "nT_out", [1024, T], F32, kind=out_kind).ap()
    scr = lambda name, shape, dt=F32: nc.dram_tensor(pfx + name, shape, dt, kind="Internal").ap()
    d["h2T"] = scr("h2T", [1024, T]); d["hnbf"] = scr("hnbf", [1024, T], BF16); d["qTd"] = scr("qTd", [128, T // 128, 16, 128])
    d["oTq"] = scr("oTq", [1024, T])
    d["GT"] = scr("GT", [T // 128, 128, 16384], BF16); d["uTbf"] = scr("uTbf", [1024, 16384], BF16); d["vbf"] = scr("vbf", [16384, 1024], BF16)
    return d


def _build_fused(S=S_FULL, T=T_CORE):
    if (S, T) in _PROG:
        return _PROG[(S, T)]
    nc = bass.Bass("TRN2", target_bir_lowering=False)
    with contextlib.ExitStack() as st:
        P = Prog(nc)
        P.alloc_sems(st)
        da = declare_a(nc, S, pfx="A_", fused=True)
        xTs = nc.dram_tensor("B0_xTs", [1024, T], F32, kind="ExternalInput").ap()
        db0 = _declare_b_fused(nc, T, "B0_", xTs, "Internal")
        dc = declare_c(nc, S, pfx="C_", fused=True)
        db1 = _declare_b_fused(nc, T, "B1_", db0["hT_out"], "ExternalOutput")
        CW = min(1024, T)
        NC1 = S // CW
        CW2 = 256
        NC2 = T // CW2
        dt_ = lambda name, shape: nc.dram_tensor(name, shape, F32, kind="Internal").ap()
        x1_in = dt_("x1_in", [NC1, 256, CW]); x1_out = dt_("x1_out", [NC1, 1024, CW])
        x2_in = dt_("x2_in", [NC2, 1024, CW2]); x2_out = dt_("x2_out", [NC2, 4096, CW2])
        x3_in = dt_("x3_in", [NC1, 256, CW]); x3_out = dt_("x3_out", [NC1, 1024, CW])

        def gather(src, dst, n, name):
            for j in range(n):
                P.cc((lambda j: (lambda e: e.collective_compute("AllGather", ALU.bypass, replica_groups=G4, ins=[src[j]], outs=[dst[j]])))(j),
                     writes=[name])
            P.barrier()

        def chunked_dst(buf):
            return lambda t0, n: buf[t0 // CW, :, (t0 % CW):(t0 % CW) + n]

        def quarter_src(buf):
            def f():
                q = nc.partition_id() % 4
                return buf[bass.ds(q * (T // CW), T // CW), :, :]
            return f

        bpr = T // 512

        def nT_loads(blk):
            rank, lb = blk // bpr, blk % bpr
            return [(slice(h * CW2, (h + 1) * CW2),
                     x2_out[lb * 2 + h, rank * 1024:(rank + 1) * 1024, :].rearrange("(m p) t -> p m t", p=128)) for h in range(2)]

        casts = b_cast_ops(P, db0, "B0_") + b_cast_ops(P, db1, "B1_")
        nblk_a = S // 512

        def cast_hook(blk):
            per = -(-len(casts) // nblk_a)
            for f in casts[blk * per:(blk + 1) * per]:
                f()

        emit_a(nc, P, S, da, oT_dst=chunked_dst(x1_in), block_hook=cast_hook)
        gather(x1_in, x1_out, NC1, "x1")
        emit_b(nc, P, T, db0, cast_weights=False, pfx="B0_", oT_blk=quarter_src(x1_out),
               nT_dst=lambda blk: x2_in[blk].rearrange("(m p) t -> p m t", p=128))
        gather(x2_in, x2_out, NC2, "x2")
        emit_c(nc, P, S, dc, nT_loads=nT_loads, oT_dst=chunked_dst(x3_in))
        gather(x3_in, x3_out, NC1, "x3")
        emit_b(nc, P, T, db1, cast_weights=False, pfx="B1_", oT_blk=quarter_src(x3_out))
    _PROG[(S, T)] = nc
    return nc


def _peer_inputs(pfx, wout, gffn, wq, keys, u, v, gnext):
    f32 = lambda a: np.ascontiguousarray(np.asarray(a, dtype=np.float32))
    return {pfx + "wout": f32(wout), pfx + "gffn": f32(gffn), pfx + "wq": f32(wq),
            pfx + "keysT": np.ascontiguousarray(f32(keys).transpose(0, 1, 3, 2).reshape(16, 128, 128)),
            pfx + "uT": np.ascontiguousarray(f32(u).T), pfx + "v": f32(v), pfx + "gnext": f32(gnext)}


def kernel(x, l0_attn_norm, l0_w_in, l0_cmp_pe_k, l0_cmp_w1_k, l0_cmp_w2_k, l0_cmp_pe_v, l0_cmp_w1_v, l0_cmp_w2_v, l0_w_out,
           l0_ffn_norm, l0_peer_wq, l0_peer_keys, l0_peer_u, l0_peer_v,
           l1_attn_norm, l1_w_in, l1_f_bias, l1_w_out,
           l1_ffn_norm, l1_peer_wq, l1_peer_keys, l1_peer_u, l1_peer_v,
           final_norm):
    f32 = lambda a: np.ascontiguousarray(np.asarray(a, dtype=np.float32))
    x = f32(x)
    B, S, D = x.shape
    T_CORE = S // 4
    nc = _build_fused(S, T_CORE)
    consts = consts_a(); ropeq, ropek = rope_tables(S)
    args0 = [f32(a) for a in (l0_attn_norm, l0_w_in, l0_cmp_pe_k, l0_cmp_w1_k, l0_cmp_w2_k, l0_cmp_pe_v, l0_cmp_w1_v, l0_cmp_w2_v)]
    pb0 = _peer_inputs("B0_", l0_w_out, l0_ffn_norm, l0_peer_wq, l0_peer_keys, l0_peer_u, l0_peer_v, l1_attn_norm)
    pb1 = _peer_inputs("B1_", l1_w_out, l1_ffn_norm, l1_peer_wq, l1_peer_keys, l1_peer_u, l1_peer_v, final_norm)
    w1 = f32(l1_w_in); fbias = f32(l1_f_bias)
    kk = np.arange(128)[:, None, None]; ii = np.arange(4)[None, :, None]; qq = np.arange(512)[None, None, :]
    maskd = (kk <= qq - 128 * ii).astype(np.float32)
    xT = [np.ascontiguousarray(x[b].T) for b in range(B)]
    maps = []
    for c in range(8):
        b, q = c // 4, c % 4
        m = {"A_" + k: v for k, v in host_inputs_a(x[b], *args0, q, consts, ropeq, ropek).items()}
        m["A_xT"] = xT[b]
        m["B0_xTs"] = np.ascontiguousarray(xT[b][:, q * T_CORE:(q + 1) * T_CORE])
        m.update(pb0); m.update(pb1)
        h0 = 4 * q
        m.update({"C_wq": np.ascontiguousarray(w1[:, h0 * 64:(h0 + 4) * 64]),
                  "C_wk": np.ascontiguousarray(w1[:, 1024 + h0 * 64:1024 + (h0 + 4) * 64]),
                  "C_wv": np.ascontiguousarray(w1[:, 2048 + h0 * 64:2048 + (h0 + 4) * 64]),
                  "C_wf": np.ascontiguousarray(w1[:, 3072 + h0:3072 + h0 + 4]), "C_fb": fbias[h0:h0 + 4].copy(), "C_maskd": maskd})
        maps.append(m)
    res = run_bass_kernel_spmd(nc, maps, core_ids=list(range(8))).results
    out = np.empty((B, S, D), np.float32)
    for c in range(8):
        b, q = c // 4, c % 4
        out[b, q * T_CORE:(q + 1) * T_CORE, :] = res[c]["B1_nT_out"].T
    return out
```
